# Optimizing a Trainium2 kernel written in Bass

```python
import math
import jax, jax.numpy as jnp
from jax import lax
import numpy as np

D_MODEL = 1024
BATCH = 4
SEQ = 4096
DEPTH = 4

GRID_W = 64
CTX_LEN = 256
HEAD_DIM = 128
FNET_GROUPS = 4
FNET_GROUP_DIM = 64
FNET_WIDTH = FNET_GROUPS * FNET_GROUP_DIM
GQA_Q_HEADS = 6
GQA_KV_HEADS = 2
GQA_GROUP = GQA_Q_HEADS // GQA_KV_HEADS
AB_IN_WIDTH = FNET_WIDTH + (GQA_Q_HEADS + 2 * GQA_KV_HEADS) * HEAD_DIM
AB_OUT_WIDTH = FNET_WIDTH + GQA_Q_HEADS * HEAD_DIM
ROPE_THETA = 10000.0
Q_BLOCK = 128
NA_HEADS = 8
NA_WIDTH = NA_HEADS * HEAD_DIM
NA_KH = 8
NA_KW = 16
NA_ROWS_PER_BLOCK = Q_BLOCK // GRID_W
NEG_INF = -1e30
N_EXPERTS = 64
TOP_K = 8
EXPERT_DIM = 256
SHARED_DIM = 256
ROUTE_SCALE = 2.5
MOE_BLOCK = 128
DN_ALPHA = (2 * DEPTH) ** 0.25
DN_BETA = (8 * DEPTH) ** -0.25
N_EVEN = (DEPTH + 1) // 2
N_ODD = DEPTH // 2
LN_EPS = 1e-6
RMS_EPS = 1e-6

kernel_name = "hybrid_fourier_gqa_natten_moe_deepnorm"


def layer_norm(x, g, b):
    xf = x.astype(jnp.float32)
    mu = xf.mean(-1, keepdims=True)
    xc = xf - mu
    var = (xc * xc).mean(-1, keepdims=True)
    return (xc * lax.rsqrt(var + LN_EPS) * g + b).astype(x.dtype)


def rms_norm(x, g):
    xf = x.astype(jnp.float32)
    return (xf * lax.rsqrt((xf * xf).mean(-1, keepdims=True) + RMS_EPS) * g).astype(x.dtype)


def axial_rope(x):
    n_tok = x.shape[1]
    t = jnp.arange(n_tok)
    half = HEAD_DIM // 2
    nf = half // 2
    inv = ROPE_THETA ** (-(2.0 / half) * jnp.arange(nf, dtype=jnp.float32))

    def rot(v, pos):
        ang = pos.astype(jnp.float32)[:, None] * inv
        cos = jnp.cos(ang)[:, None, :]
        sin = jnp.sin(ang)[:, None, :]
        v1, v2 = v[..., :nf], v[..., nf:]
        return jnp.concatenate([v1 * cos - v2 * sin, v1 * sin + v2 * cos], axis=-1)

    xf = x.astype(jnp.float32)
    out = jnp.concatenate([rot(xf[..., :half], t // GRID_W), rot(xf[..., half:], t % GRID_W)], axis=-1)
    return out.astype(x.dtype)


def gqa_attend(q, k, v):
    s = jnp.einsum("bqkgd,bnkd->bkgqn", q, k).astype(jnp.float32) * HEAD_DIM ** -0.5
    p = jax.nn.softmax(s, axis=-1).astype(v.dtype)
    return jnp.einsum("bkgqn,bnkd->bqkgd", p, v)


def fourier_mix(f, w_fnet):
    bsz, n_tok, _ = f.shape
    fg = f.reshape(bsz, n_tok, FNET_GROUPS, FNET_GROUP_DIM).astype(jnp.float32)
    fr = jnp.fft.fft2(fg, axes=(1, 3), norm="ortho").real.astype(f.dtype)
    return jnp.einsum("blgc,gcd->blgd", fr, w_fnet).reshape(bsz, n_tok, FNET_WIDTH)


def mixer_ab(h_lat, h_ctx, w_in, w_fnet, q_gain, k_gain, w_out, want_ctx):
    bsz, n_lat, _ = h_lat.shape
    n_ctx = h_ctx.shape[1]
    nq = GQA_Q_HEADS * HEAD_DIM
    nkv = GQA_KV_HEADS * HEAD_DIM
    cuts = [FNET_WIDTH, FNET_WIDTH + nq, FNET_WIDTH + nq + nkv]
    f, q, k, v = jnp.split(h_lat @ w_in, cuts, axis=-1)
    q = axial_rope(rms_norm(q.reshape(bsz, n_lat, GQA_Q_HEADS, HEAD_DIM), q_gain))
    k = axial_rope(rms_norm(k.reshape(bsz, n_lat, GQA_KV_HEADS, HEAD_DIM), k_gain))
    v = v.reshape(bsz, n_lat, GQA_KV_HEADS, HEAD_DIM)
    if want_ctx:
        fc, qc, kc, vc = jnp.split(h_ctx @ w_in, cuts, axis=-1)
    else:
        kc, vc = jnp.split(h_ctx @ w_in[:, cuts[1]:], [nkv], axis=-1)
    kc = rms_norm(kc.reshape(bsz, n_ctx, GQA_KV_HEADS, HEAD_DIM), k_gain)
    vc = vc.reshape(bsz, n_ctx, GQA_KV_HEADS, HEAD_DIM)
    k_all = jnp.concatenate([k, kc], axis=1)
    v_all = jnp.concatenate([v, vc], axis=1)
    nb = n_lat // Q_BLOCK
    qb = q.reshape(bsz, nb, Q_BLOCK, GQA_KV_HEADS, GQA_GROUP, HEAD_DIM).swapaxes(0, 1)
    o = lax.map(lambda qi: gqa_attend(qi, k_all, v_all), qb)
    o = o.swapaxes(0, 1).reshape(bsz, n_lat, nq)
    y_lat = jnp.concatenate([fourier_mix(f, w_fnet), o], axis=-1) @ w_out
    if not want_ctx:
        return y_lat, None
    qc = rms_norm(qc.reshape(bsz, n_ctx, GQA_Q_HEADS, HEAD_DIM), q_gain)
    qc = qc.reshape(bsz, n_ctx, GQA_KV_HEADS, GQA_GROUP, HEAD_DIM)
    oc = gqa_attend(qc, kc, vc).reshape(bsz, n_ctx, nq)
    y_ctx = jnp.concatenate([fourier_mix(fc, w_fnet), oc], axis=-1) @ w_out
    return y_lat, y_ctx


def mixer_na(h_lat, h_ctx, w_in, rpb, w_out, want_ctx):
    bsz, n_lat, _ = h_lat.shape
    n_ctx = h_ctx.shape[1]
    rows = n_lat // GRID_W
    kh = min(NA_KH, rows)
    kw = min(NA_KW, GRID_W)
    qr = NA_ROWS_PER_BLOCK
    nbr = min(qr + kh - 1, rows)
    nblk = rows // qr
    scale = HEAD_DIM ** -0.5
    q, k, v = jnp.split(h_lat @ w_in, 3, axis=-1)
    if want_ctx:
        qc, kc, vc = jnp.split(h_ctx @ w_in, 3, axis=-1)
    else:
        kc, vc = jnp.split(h_ctx @ w_in[:, NA_WIDTH:], 2, axis=-1)
    kc = kc.reshape(bsz, n_ctx, NA_HEADS, HEAD_DIM)
    vc = vc.reshape(bsz, n_ctx, NA_HEADS, HEAD_DIM)
    k_grid = k.reshape(bsz, rows, GRID_W, NA_HEADS, HEAD_DIM)
    v_grid = v.reshape(bsz, rows, GRID_W, NA_HEADS, HEAD_DIM)
    q_blocks = q.reshape(bsz, nblk, qr * GRID_W, NA_HEADS, HEAD_DIM).swapaxes(0, 1)
    col = jnp.arange(GRID_W)
    col_start = jnp.clip(col - kw // 2, 0, GRID_W - kw)
    in_col = (col[None, :] >= col_start[:, None]) & (col[None, :] < col_start[:, None] + kw)
    dc = jnp.clip(col[None, :] - col[:, None] + NA_KW - 1, 0, 2 * NA_KW - 2)

    def block(args):
        i, qi = args
        qrow = i * qr + jnp.arange(qr)
        rstart = jnp.clip(qrow - kh // 2, 0, rows - kh)
        bs = jnp.minimum(rstart[0], rows - nbr)
        krow = bs + jnp.arange(nbr)
        kb = lax.dynamic_slice_in_dim(k_grid, bs, nbr, axis=1).reshape(bsz, nbr * GRID_W, NA_HEADS, HEAD_DIM)
        vb = lax.dynamic_slice_in_dim(v_grid, bs, nbr, axis=1).reshape(bsz, nbr * GRID_W, NA_HEADS, HEAD_DIM)
        in_row = (krow[None, :] >= rstart[:, None]) & (krow[None, :] < rstart[:, None] + kh)
        dr = jnp.clip(krow[None, :] - qrow[:, None] + NA_KH - 1, 0, 2 * NA_KH - 2)
        mask = (in_row[:, None, :, None] & in_col[None, :, None, :]).reshape(qr * GRID_W, nbr * GRID_W)
        bias = rpb[:, dr[:, None, :, None], dc[None, :, None, :]].reshape(NA_HEADS, qr * GRID_W, nbr * GRID_W)
        s_loc = jnp.einsum("bqhd,bnhd->bhqn", qi, kb).astype(jnp.float32) * scale + bias.astype(jnp.float32)
        s_loc = jnp.where(mask, s_loc, NEG_INF)
        s_ctx = jnp.einsum("bqhd,bnhd->bhqn", qi, kc).astype(jnp.float32) * scale
        p = jax.nn.softmax(jnp.concatenate([s_loc, s_ctx], axis=-1), axis=-1).astype(vb.dtype)
        n_loc = nbr * GRID_W
        return (jnp.einsum("bhqn,bnhd->bqhd", p[..., :n_loc], vb)
                + jnp.einsum("bhqn,bnhd->bqhd", p[..., n_loc:], vc))

    o = lax.map(block, (jnp.arange(nblk), q_blocks))
    y_lat = o.swapaxes(0, 1).reshape(bsz, n_lat, NA_WIDTH) @ w_out
    if not want_ctx:
        return y_lat, None
    qc = qc.reshape(bsz, n_ctx, NA_HEADS, 1, HEAD_DIM)
    oc = gqa_attend(qc, kc, vc).reshape(bsz, n_ctx, NA_WIDTH)
    return y_lat, oc @ w_out


def moe_ffn(h, w_router, e_bias, w_gate, w_up, w_down, s_gate, s_up, s_down):
    n_tok, d = h.shape
    scores = jax.nn.sigmoid((h @ w_router).astype(jnp.float32))
    _, idx = lax.top_k(scores + e_bias.astype(jnp.float32), TOP_K)
    wts = jnp.take_along_axis(scores, idx, axis=-1)
    wts = wts / wts.sum(-1, keepdims=True) * ROUTE_SCALE
    n_pairs = n_tok * TOP_K
    flat_e = idx.reshape(-1)
    order = jnp.argsort(flat_e)
    sorted_e = flat_e[order]
    sorted_tok = (order // TOP_K).astype(jnp.int32)
    sorted_w = wts.reshape(-1)[order]
    counts = jnp.bincount(flat_e, length=N_EXPERTS)
    padded = (counts + MOE_BLOCK - 1) // MOE_BLOCK * MOE_BLOCK
    pad_end = jnp.cumsum(padded)
    pad_start = pad_end - padded
    grp_start = jnp.cumsum(counts) - counts
    dest = pad_start[sorted_e] + jnp.arange(n_pairs) - grp_start[sorted_e]
    n_blocks = (n_pairs + N_EXPERTS * (MOE_BLOCK - 1)) // MOE_BLOCK + 1
    n_slots = n_blocks * MOE_BLOCK
    slot_tok = jnp.full((n_slots,), n_tok, jnp.int32).at[dest].set(sorted_tok)
    slot_w = jnp.zeros((n_slots,), jnp.float32).at[dest].set(sorted_w)
    block_e = jnp.minimum(jnp.searchsorted(pad_end, jnp.arange(n_blocks) * MOE_BLOCK, side="right"),
                          N_EXPERTS - 1)
    h_pad = jnp.concatenate([h, jnp.zeros((1, d), h.dtype)], axis=0)

    def expert_block(args):
        tok, e = args
        xb = h_pad[tok]
        a = jax.nn.silu(xb @ w_gate[e]) * (xb @ w_up[e])
        return a @ w_down[e]

    yb = lax.map(expert_block, (slot_tok.reshape(n_blocks, MOE_BLOCK), block_e))
    y = yb.reshape(n_slots, d) * slot_w[:, None].astype(h.dtype)
    routed = jnp.zeros((n_tok + 1, d), h.dtype).at[slot_tok].add(y)[:n_tok]
    shared = (jax.nn.silu(h @ s_gate) * (h @ s_up)) @ s_down
    return routed + shared


def setup_inputs(seed: int = 0) -> dict:
    key = jax.random.key(seed)
    ks = iter(jax.random.split(key, 32))
    D = D_MODEL

    def nrm(shape, scale):
        return jax.random.normal(next(ks), shape, jnp.float32) * scale

    return {
        "x": nrm((BATCH, SEQ, D), 1.0),
        "c": nrm((BATCH, D), 1.0),
        "ctx": nrm((BATCH, CTX_LEN, D), 1.0),
        "c_ctx": nrm((D,), 1.0),
        "w_mod": nrm((DEPTH, D, 6 * D), 0.5 * D ** -0.5),
        "b_mod": nrm((DEPTH, 6 * D), 0.02),
        "ln1_g": 1.0 + nrm((DEPTH, D), 0.05),
        "ln1_b": nrm((DEPTH, D), 0.02),
        "ln2_g": 1.0 + nrm((DEPTH, D), 0.05),
        "ln2_b": nrm((DEPTH, D), 0.02),
        "ab_w_in": nrm((N_EVEN, D, AB_IN_WIDTH), D ** -0.5),
        "ab_w_fnet": nrm((N_EVEN, FNET_GROUPS, FNET_GROUP_DIM, FNET_GROUP_DIM), FNET_GROUP_DIM ** -0.5),
        "ab_q_norm": 1.0 + nrm((N_EVEN, HEAD_DIM), 0.05),
        "ab_k_norm": 1.0 + nrm((N_EVEN, HEAD_DIM), 0.05),
        "ab_w_out": nrm((N_EVEN, AB_OUT_WIDTH, D), AB_OUT_WIDTH ** -0.5 * DN_BETA),
        "na_w_in": nrm((N_ODD, D, 3 * NA_WIDTH), D ** -0.5),
        "na_rpb": nrm((N_ODD, NA_HEADS, 2 * NA_KH - 1, 2 * NA_KW - 1), 0.5),
        "na_w_out": nrm((N_ODD, NA_WIDTH, D), NA_WIDTH ** -0.5 * DN_BETA),
        "moe_w_router": nrm((DEPTH, D, N_EXPERTS), D ** -0.5),
        "moe_bias": nrm((DEPTH, N_EXPERTS), 0.01),
        "moe_w_gate": nrm((DEPTH, N_EXPERTS, D, EXPERT_DIM), D ** -0.5),
        "moe_w_up": nrm((DEPTH, N_EXPERTS, D, EXPERT_DIM), D ** -0.5),
        "moe_w_down": nrm((DEPTH, N_EXPERTS, EXPERT_DIM, D), EXPERT_DIM ** -0.5 * DN_BETA),
        "sh_w_gate": nrm((DEPTH, D, SHARED_DIM), D ** -0.5),
        "sh_w_up": nrm((DEPTH, D, SHARED_DIM), D ** -0.5),
        "sh_w_down": nrm((DEPTH, SHARED_DIM, D), SHARED_DIM ** -0.5 * DN_BETA),
    }


def reference(x, c, ctx, c_ctx, w_mod, b_mod, ln1_g, ln1_b, ln2_g, ln2_b,
              ab_w_in, ab_w_fnet, ab_q_norm, ab_k_norm, ab_w_out,
              na_w_in, na_rpb, na_w_out,
              moe_w_router, moe_bias, moe_w_gate, moe_w_up, moe_w_down,
              sh_w_gate, sh_w_up, sh_w_down):
    bsz, n_lat, d = x.shape
    n_ctx = ctx.shape[1]
    for l in range(DEPTH):
        want_ctx = l < DEPTH - 1
        j = l // 2
        mod = jax.nn.silu(c) @ w_mod[l] + b_mod[l]
        mod_c = jax.nn.silu(c_ctx) @ w_mod[l] + b_mod[l]
        sh1, sc1, g1, sh2, sc2, g2 = jnp.split(mod[:, None, :], 6, axis=-1)
        csh1, csc1, cg1, csh2, csc2, cg2 = jnp.split(mod_c, 6, axis=-1)
        h = x * (1.0 + sc1) + sh1
        hc = ctx * (1.0 + csc1) + csh1
        if l % 2 == 0:
            y, yc = mixer_ab(h, hc, ab_w_in[j], ab_w_fnet[j], ab_q_norm[j], ab_k_norm[j], ab_w_out[j], want_ctx)
        else:
            y, yc = mixer_na(h, hc, na_w_in[j], na_rpb[j], na_w_out[j], want_ctx)
        x = layer_norm(DN_ALPHA * x + g1 * y, ln1_g[l], ln1_b[l])
        h2 = (x * (1.0 + sc2) + sh2).reshape(bsz * n_lat, d)
        moe_args = (moe_w_router[l], moe_bias[l], moe_w_gate[l], moe_w_up[l], moe_w_down[l],
                    sh_w_gate[l], sh_w_up[l], sh_w_down[l])
        if want_ctx:
            ctx = layer_norm(DN_ALPHA * ctx + cg1 * yc, ln1_g[l], ln1_b[l])
            hc2 = (ctx * (1.0 + csc2) + csh2).reshape(bsz * n_ctx, d)
            ff = moe_ffn(jnp.concatenate([h2, hc2], axis=0), *moe_args)
            ff_lat = ff[:bsz * n_lat].reshape(bsz, n_lat, d)
            ff_ctx = ff[bsz * n_lat:].reshape(bsz, n_ctx, d)
            ctx = layer_norm(DN_ALPHA * ctx + cg2 * ff_ctx, ln2_g[l], ln2_b[l])
        else:
            ff_lat = moe_ffn(h2, *moe_args).reshape(bsz, n_lat, d)
        x = layer_norm(DN_ALPHA * x + g2 * ff_lat, ln2_g[l], ln2_b[l])
    return x
```

```python
import math
import numpy as np
import ml_dtypes
import concourse.bass as bass
import concourse.mybir as mybir
from concourse.bass_utils import run_bass_kernel_spmd
from contextlib import ExitStack

F32 = mybir.dt.float32
BF16 = mybir.dt.bfloat16
AF = mybir.ActivationFunctionType
ALU = mybir.AluOpType
AX = mybir.AxisListType

P = 128
D = 1024
KC = 8
NLAT = 2048
NCTX = 128
NT = NLAT + NCTX
DEPTH = 4
ALPHA = (2 * DEPTH) ** 0.25
LN_EPS = 1e-6
RMS_EPS = 1e-6
HD = 128
SCALE = HD ** -0.5
NEG = -1e30
NEXP = 65
ROUTE_SCALE = 2.5

ENGS = ("pe", "act", "dve", "pool", "sp")
DTSIZE = {F32: 4, BF16: 2}


class V:
    __slots__ = ("t", "ap")

    def __init__(self, t, ap):
        self.t = t
        self.ap = ap

    def __getitem__(self, idx):
        return V(self.t, self.ap[idx])

    def r(self, pat, **kw):
        return V(self.t, self.ap.rearrange(pat, **kw))

    def bc(self, shape):
        return V(self.t, self.ap.broadcast_to(shape))

    def bitcast(self, dt):
        return V(self.t, self.ap.bitcast(dt))


class T:
    def __init__(self, h, name, space, rng=None):
        self.h = h
        self.name = name
        self.space = space
        self.w = None
        self.r_ = []
        self.dsem = None
        self.dcnt = 0
        self.rng = rng

    def __getitem__(self, idx):
        return V(self, self.h[idx])

    def r(self, pat, **kw):
        return V(self, self.h.rearrange(pat, **kw))

    def v(self):
        return V(self, self.h)


class Ctx:
    def __init__(self, nc, arena_bytes=211968):
        self.nc = nc
        self.es = ExitStack()
        self.E = {"pe": nc.tensor, "act": nc.scalar, "dve": nc.vector, "pool": nc.gpsimd, "sp": nc.sync}
        self.sem = {}
        self.cnt = {}
        for e in ENGS:
            self.sem[e] = self.es.enter_context(nc.semaphore("s_" + e))
            self.cnt[e] = 0
        self.seen = {e: {} for e in ENGS}
        self.nbuf = 0
        self.sem_pool = []
        self.arena = self.es.enter_context(nc.sbuf_tensor("arena", [P, arena_bytes // 2], BF16))
        self.arena_bytes = arena_bytes
        self.free_list = [(0, arena_bytes)]
        self.grave = []
        self.live = {}
        self.banks = []
        for i in range(8):
            h = self.es.enter_context(nc.psum_tensor(f"bank{i}", [P, 512], F32))
            self.banks.append(T(h[:], f"bank{i}", "ps"))

    def alloc(self, name, nelem, dt, parts=P):
        nbytes = (nelem * DTSIZE[dt] + 63) // 64 * 64
        for i, (s, e) in enumerate(self.free_list):
            if e - s >= nbytes:
                self.free_list[i] = (s + nbytes, e)
                if self.free_list[i][0] == self.free_list[i][1]:
                    del self.free_list[i]
                break
        else:
            raise RuntimeError(f"arena OOM allocating {name} {nbytes}B; free={self.free_list}")
        ap = self.arena[0:parts, s // 2:(s + nbytes) // 2]
        if dt != BF16:
            ap = ap.bitcast(dt)
        ap = ap[:, 0:nelem]
        self.nbuf += 1
        t = T(ap, f"{name}_{self.nbuf}", "sb", (s, s + nbytes))
        keep = []
        for (gs, ge, toks) in self.grave:
            if gs < s + nbytes and s < ge:
                t.r_.extend(toks)
                if gs >= s and ge <= s + nbytes:
                    continue
            keep.append((gs, ge, toks))
        self.grave = keep
        return t

    def free(self, *ts):
        for t in ts:
            s, e = t.rng
            toks = [x for x in ([t.w] + t.r_) if x is not None]
            self.grave.append((s, e, self._compress(toks)))
            self.free_list.append((s, e))
            self.free_list.sort()
            merged = []
            for a, b in self.free_list:
                if merged and merged[-1][1] == a:
                    merged[-1] = (merged[-1][0], b)
                else:
                    merged.append((a, b))
            self.free_list = merged
            t.rng = None
            if t.dsem is not None:
                self.sem_pool.append((t.dsem, t.dcnt, t.dkey))
                t.dsem = None

    @staticmethod
    def _compress(toks):
        best = {}
        for tok in toks:
            key = tok[1] if tok[0] == "eng" else tok[3]
            v = tok[2]
            if key not in best or best[key][2] < v:
                best[key] = tok
        return list(best.values())

    def dram(self, name, shape, dt, kind="Internal"):
        h = self.nc.dram_tensor(name, list(shape), dt, kind=kind)
        return T(h.ap(), name, "dram")

    def _wait(self, e, tok):
        if tok is None:
            return
        if tok[0] == "eng":
            _, f, v = tok
            key = "e:" + f
            sem = self.sem[f]
        else:
            _, sem, v, key = tok
        if e == "pe" and tok[0] == "eng" and tok[1] == "pe":
            return
        if self.seen[e].get(key, 0) >= v:
            return
        self.E[e].wait_ge(sem, v)
        self.seen[e][key] = v

    def _deps(self, e, reads, writes, acc=False):
        for t in reads:
            self._wait(e, t.w)
        if not acc:
            for t in writes:
                self._wait(e, t.w)
                for tok in t.r_:
                    self._wait(e, tok)

    def op(self, e, fn, reads=(), writes=(), acc=False, inc=True):
        reads = [v.t for v in reads if v is not None]
        writes = [v.t for v in writes if v is not None]
        self._deps(e, reads, writes, acc)
        ins = fn(self.E[e])
        if inc:
            self.cnt[e] += 1
            ins.then_inc(self.sem[e], 1)
            tok = ("eng", e, self.cnt[e])
        else:
            tok = ("eng", e, self.cnt[e] + 1)
        for t in reads:
            t.r_.append(tok)
            if len(t.r_) > 24:
                t.r_ = self._compress(t.r_)
        for t in writes:
            t.w = tok
            t.r_ = []
        return ins

    def dma(self, q, out, in_, **kw):
        ot = out.t if isinstance(out, V) else None
        it = in_.t if isinstance(in_, V) else None
        oap = out.ap if isinstance(out, V) else out
        iap = in_.ap if isinstance(in_, V) else in_
        reads = [it] if it is not None else []
        writes = [ot] if ot is not None else []
        self._deps(q, reads, writes)
        owner = None
        for t in (ot, it):
            if t is not None and t.space != "dram":
                owner = t
                break
        if owner is None:
            owner = ot if ot is not None else it
        if owner.dsem is None:
            if self.sem_pool:
                sem, cnt, key = self.sem_pool.pop()
                if cnt > 0:
                    self._wait(q, ("dma", sem, cnt, key))
            else:
                self.nsem = getattr(self, "nsem", 0) + 1
                key = f"d:{self.nsem}"
                sem, cnt = self.es.enter_context(self.nc.semaphore(f"dsem{self.nsem}")), 0
            owner.dsem, owner.dcnt, owner.dkey = sem, cnt, key
        owner.dcnt += 16
        ins = self.E[q].dma_start(out=oap, in_=iap, **kw)
        ins.then_inc(owner.dsem, 16)
        tok = ("dma", owner.dsem, owner.dcnt, owner.dkey)
        for t in reads:
            t.r_.append(tok)
        for t in writes:
            t.w = tok
            t.r_ = []
        return tok

    def wait_all(self, e, ts):
        for t in ts:
            self._wait(e, t.w)
            for tok in t.r_:
                self._wait(e, tok)

    def mm(self, out, lhsT, rhs, start=True, stop=True, inc=None):
        self.op("pe", lambda e: e.matmul(out.ap, lhsT=lhsT.ap, rhs=rhs.ap, start=start, stop=stop),
                reads=[lhsT, rhs], writes=[out], acc=not start, inc=(stop if inc is None else inc))

    def transpose(self, out, in_, ident):
        self.op("pe", lambda e: e.transpose(out.ap, in_.ap, ident.ap), reads=[in_, ident], writes=[out])

    def act(self, out, in_, func, scale=None, bias=None, accum=None):
        kw = {}
        rd = [in_]
        wr = [out]
        if scale is not None:
            if isinstance(scale, V):
                kw["scale"] = scale.ap
                rd.append(scale)
            else:
                kw["scale"] = float(scale)
        if bias is not None:
            if isinstance(bias, V):
                kw["bias"] = bias.ap
                rd.append(bias)
            else:
                kw["bias"] = float(bias)
        if accum is not None:
            kw["accum_out"] = accum.ap
            wr.append(accum)
        self.op("act", lambda e: e.activation(out=out.ap, in_=in_.ap, func=func, **kw), reads=rd, writes=wr)

    def copy(self, eng, out, in_):
        if eng == "act":
            self.op("act", lambda e: e.copy(out=out.ap, in_=in_.ap), reads=[in_], writes=[out])
        else:
            self.op(eng, lambda e: e.tensor_copy(out=out.ap, in_=in_.ap), reads=[in_], writes=[out])

    def tt(self, eng, out, in0, in1, op):
        self.op(eng, lambda e: e.tensor_tensor(out=out.ap, in0=in0.ap, in1=in1.ap, op=op),
                reads=[in0, in1], writes=[out])

    def ts(self, eng, out, in0, s1, s2=None, op0=ALU.mult, op1=None):
        rd = [in0]
        a1 = s1.ap if isinstance(s1, V) else float(s1)
        if isinstance(s1, V):
            rd.append(s1)
        a2 = None
        if s2 is not None:
            a2 = s2.ap if isinstance(s2, V) else float(s2)
            if isinstance(s2, V):
                rd.append(s2)
        if op1 is None:
            self.op(eng, lambda e: e.tensor_scalar(out=out.ap, in0=in0.ap, scalar1=a1, scalar2=None, op0=op0),
                    reads=rd, writes=[out])
        else:
            self.op(eng, lambda e: e.tensor_scalar(out=out.ap, in0=in0.ap, scalar1=a1, scalar2=a2, op0=op0, op1=op1),
                    reads=rd, writes=[out])

    def stt(self, out, in0, scalar, in1, op0, op1, eng="dve"):
        rd = [in0, in1]
        a = scalar.ap if isinstance(scalar, V) else float(scalar)
        if isinstance(scalar, V):
            rd.append(scalar)
        self.op(eng, lambda e: e.scalar_tensor_tensor(out=out.ap, in0=in0.ap, scalar=a, in1=in1.ap, op0=op0, op1=op1),
                reads=rd, writes=[out])

    def memset(self, eng, out, val):
        self.op(eng, lambda e: e.memset(out.ap, val), writes=[out])

    def close(self):
        self.es.close()


class Prog:
    def __init__(self, layers, debug=None):
        self.layers = list(layers)
        self.debug = debug or {}
        self.nc = bass.Bass("TRN2", target_bir_lowering=False)
        self.c = Ctx(self.nc)
        self.inputs = {}
        self.rot = {}
        self.xg = {}

    def inp(self, name, shape, dt=F32):
        if name not in self.inputs:
            self.inputs[name] = self.nc.dram_tensor(name, list(shape), dt, kind="ExternalInput").ap()
        return self.inputs[name]

    def bank(self, i):
        return self.c.banks[i]

    def rr(self, key, n):
        v = self.rot.get(key, 0)
        self.rot[key] = v + 1
        return v % n

    def build(self):
        c = self.c
        nc = self.nc
        first = self.layers[0]
        self.XT = c.alloc("XT", KC * NT, F32)
        self.XT3 = self.XT.r("p (c t) -> p c t", c=KC)
        self.HB = c.alloc("HB", KC * NT, BF16)
        self.HB3 = self.HB.r("p (c t) -> p c t", c=KC)
        self.identb = c.alloc("identb", P, BF16)
        self.identf = c.alloc("identf", P, F32)
        c.dma("pool", self.identb.v(), self.inp("ident", [P, P]))
        c.dma("sp", self.identf.v(), self.inp("ident", [P, P]))
        self.ones_mean = c.alloc("ones_mean", P, BF16)
        self.ones_rms = c.alloc("ones_rms", P, BF16)
        self.ones1 = c.alloc("ones1", P, BF16)
        c.memset("dve", self.ones_mean.v(), 1.0 / D)
        c.memset("dve", self.ones_rms.v(), 1.0 / HD)
        c.memset("dve", self.ones1.v(), 1.0)
        self.MODV = c.alloc("MODV", 96, F32)
        self.MODV3 = self.MODV.r("p (m r) -> p m r", r=2)
        self.LNV = c.alloc("LNV", 32, F32)
        self.LNV3 = self.LNV.r("p (m c) -> p m c", c=KC)
        self.A2 = c.alloc("A2", 16, F32)
        self.A23 = self.A2.r("p (c r) -> p c r", r=2)
        self.B2 = c.alloc("B2", 16, F32)
        self.B23 = self.B2.r("p (c r) -> p c r", r=2)
        self.G = c.alloc("G", 17 * NEXP, F32)
        self.G3 = self.G.r("p (t e) -> p t e", e=NEXP)
        self.scT = c.alloc("scT", 16, F32)
        self.scT3 = self.scT.r("p (k r) -> p k r", r=2)
        c.dma("sp", self.scT.v(), self.inp("cT", [P, 16]))
        c.act(self.scT.v(), self.scT.v(), AF.Silu)
        c.dma("sp", self.XT.v(), self.inp("xT", [P, KC * NT]))
        for li, l in enumerate(self.layers):
            self.layer(l)
            if li + 1 < len(self.layers):
                self.exchange(l, self.layers[li + 1])
        xout = nc.dram_tensor("xout", [P, KC * NT], F32, kind="ExternalOutput").ap()
        tok = c.dma("sp", xout, self.XT.v())
        c._wait("sp", tok)
        c.close()
        return nc

    def exchange(self, l, lnext):
        c = self.c
        nc = self.nc
        sem = c.es.enter_context(nc.semaphore(f"ccsem{l}"))
        tok = ("dma", sem, KC, f"cc{l}")
        lst = []
        for cc in range(KC):
            xsh = nc.dram_tensor(f"xs{l}_{cc}", [P, NT], F32)
            xgh = nc.dram_tensor(f"xgi{lnext}_{cc}", [2 * P, NT], F32)
            xs = T(xsh.ap(), f"xs{l}_{cc}", "dram")
            xg = T(xgh.ap(), f"xgi{lnext}_{cc}", "dram")
            c.dma("sp", xs.v(), self.XT3[:, cc, :])
            c._deps("pool", [xs], [xg])
            ins = nc.gpsimd.collective_compute("AllGather", ALU.bypass, replica_groups=[[0, 1], [2, 3], [4, 5], [6, 7]],
                                               ins=[xsh.ap().opt()], outs=[xgh.ap().opt()])
            ins.then_inc(sem)
            xg.w = tok
            xg.r_ = []
            xs.r_.append(tok)
            lst.append((xg, xg.h))
        self.xg[lnext] = lst

    def layer(self, l):
        self.emit_mod(l)
        if self.debug.get("stop") == "mod":
            return
        self.emit_h1(l)
        if self.debug.get("stop") == "h1":
            return
        if l % 2 == 0:
            self.emit_gqa(l)
        else:
            self.emit_na(l)
        stop = self.debug.get("stop")
        if stop == "mixer":
            return
        self.emit_ln(l, 1)
        if stop == "ln1":
            return
        self.emit_moe(l)
        if stop == "moe":
            return
        self.emit_ln(l, 2)

    def emit_mod(self, l):
        c = self.c
        wmod = self.inp(f"wmod{l}", [D, 6 * D]).rearrange("(k p) n -> p k n", p=P)
        bmod = c.alloc("bmod", 48, F32)
        c.dma("sp", bmod.v(), self.inp(f"bmod{l}", [P, 48]))
        c.dma("sp", self.LNV.v(), self.inp(f"lnv{l}", [P, 32]))
        st = [c.alloc("wmst", KC * 512, F32) for _ in range(2)]
        ps = self.bank(0)
        ps3 = ps[:, 0:96].r("p (m r) -> p m r", r=2)
        for blk in range(12):
            s = st[blk % 2]
            s3 = s.r("p (k n) -> p k n", k=KC)
            c.dma("sp", s3, wmod[:, :, blk * 512:(blk + 1) * 512])
            for fc in range(4):
                for k in range(KC):
                    c.mm(ps3[:, blk * 4 + fc, :], s3[:, k, fc * 128:(fc + 1) * 128], self.scT3[:, k, :],
                         start=(k == 0), stop=(k == KC - 1))
        M3 = self.MODV3
        for r in range(2):
            c.tt("dve", M3[:, :, r], ps3[:, :, r], bmod.v(), ALU.add)
        c.ts("dve", M3[:, 8:16, :], M3[:, 8:16, :], 1.0, op0=ALU.add)
        c.ts("dve", M3[:, 32:40, :], M3[:, 32:40, :], 1.0, op0=ALU.add)
        c.ts("dve", M3[:, 16:24, :], M3[:, 16:24, :], 1.0 / ALPHA, op0=ALU.mult)
        c.ts("dve", M3[:, 40:48, :], M3[:, 40:48, :], 1.0 / ALPHA, op0=ALU.mult)
        for r in range(2):
            c.tt("dve", self.A23[:, :, r], self.LNV3[:, 0, :], M3[:, 32:40, r], ALU.mult)
            c.tt("dve", self.B23[:, :, r], self.LNV3[:, 1, :], M3[:, 32:40, r], ALU.mult)
            c.tt("dve", self.B23[:, :, r], self.B23[:, :, r], M3[:, 24:32, r], ALU.add)
        c.free(bmod, *st)

    def mod_h(self, eng, out, in_, cc, r):
        self.c.ts(eng, out, in_, self.MODV3[:, 8 + cc, r:r + 1], self.MODV3[:, cc, r:r + 1], ALU.mult, ALU.add)

    def emit_h1(self, l):
        for cc in range(KC):
            eng = ("dve", "pool")[cc % 2]
            self.mod_h(eng, self.HB3[:, cc, 0:NLAT], self.XT3[:, cc, 0:NLAT], cc, 0)
            self.mod_h(eng, self.HB3[:, cc, NLAT:NT], self.XT3[:, cc, NLAT:NT], cc, 1)

    def xg_ap(self, l):
        if l in self.xg:
            return self.xg[l]
        ap = self.inp(f"xg{l}", [KC, 2 * P, NT])
        t = T(ap, f"xg{l}", "dram")
        self.xg[l] = [(t, ap[cc]) for cc in range(KC)]
        return self.xg[l]

    def gstream(self, l, groups):
        c = self.c
        xg = self.xg_ap(l)
        st = [c.alloc("xost", KC * 256, F32) for _ in range(2)]
        ho = [c.alloc("xoh", KC * 256, BF16) for _ in range(2)]
        for i, (slot, c0, n, r) in enumerate(groups):
            s3 = st[i % 2].r("p (c t) -> p c t", c=KC)[:, :, 0:n]
            h3 = ho[i % 2].r("p (c t) -> p c t", c=KC)[:, :, 0:n]
            for cc in range(KC):
                t_, ap_ = xg[cc]
                c.dma("sp", s3[:, cc, :], V(t_, ap_[slot * P:(slot + 1) * P, c0:c0 + n]))
            for cc in range(KC):
                self.mod_h(("dve", "pool")[cc % 2], h3[:, cc, :], s3[:, cc, :], cc, r)
            yield (slot, c0, n, r, h3)
        c.free(*st, *ho)

    def load_w(self, name, dram_ap, ncols):
        c = self.c
        t = c.alloc(name, KC * ncols, BF16)
        t3 = t.r("p (k n) -> p k n", k=KC)
        c.dma("pool", t3, dram_ap.rearrange("(k p) n -> p k n", p=P))
        return t, t3

    def outproj(self, oT, wo, col0, n, r):
        c = self.c
        oTs = oT if isinstance(oT, list) else [oT]
        wos = wo if isinstance(wo, list) else [wo]
        for cc in range(KC):
            y = self.bank(6 + self.rr("yb", 2))[:, 0:n]
            for i, (o, w) in enumerate(zip(oTs, wos)):
                c.mm(y, w[:, cc * 128:(cc + 1) * 128], o, start=(i == 0), stop=(i == len(oTs) - 1))
            xs = self.XT3[:, cc, col0:col0 + n]
            c.stt(xs, y, self.MODV3[:, 16 + cc, r:r + 1], xs, ALU.mult, ALU.add)

    def rmsnorm_rope(self, ps, n, gain, out, rope_col0=None, table=None):
        c = self.c
        k = self.rr("rn", 2)
        tm = self.rn_tmp[k]
        sq = tm["sq"][:, 0:n]
        rstd = tm["rstd"][:, 0:n]
        c.act(sq, ps, AF.Square)
        ss = self.bank(2 + k)[:, 0:n]
        c.mm(ss, self.ones_rms.v(), sq)
        c.act(rstd, ss, AF.Ln, bias=RMS_EPS)
        c.act(rstd, rstd, AF.Exp, scale=-0.5)
        if rope_col0 is None:
            c.stt(out, ps, gain, rstd, ALU.mult, ALU.mult)
            return
        qn = tm["qn"][:, 0:n]
        c.stt(qn, ps, gain, rstd, ALU.mult, ALU.mult)
        rp = tm["rope"].r("p (a n) -> p a n", a=2)[:, :, 0:n]
        c.dma("sp", rp, (table if table is not None else self.rope_dram)[:, :, rope_col0:rope_col0 + n])
        pp = self.bank(4 + k)[:, 0:n]
        c.mm(pp, self.perm.v(), qn)
        t1 = tm["t1"][:, 0:n]
        t2 = tm["t2"][:, 0:n]
        c.tt("pool", t1, qn, rp[:, 0, :], ALU.mult)
        c.tt("dve", t2, pp, rp[:, 1, :], ALU.mult)
        c.tt("pool", out, t1, t2, ALU.add)

    def attn_T(self, KT, VV3, qv, n, tiles):
        c = self.c
        k = self.rr("ol", 2)
        O = self.bank(2 + 2 * k)[:, 0:n]
        L = self.bank(3 + 2 * k)[:, 0:n]
        nt = len(tiles)

        def S(i):
            s = self.bank(self.rr("sb", 2))[:, 0:n]
            kt = tiles[i]
            c.mm(s, KT[:, kt * 128:(kt + 1) * 128], qv)
            return s
        s_next = S(0)
        for i in range(nt):
            s_cur = s_next
            if i + 1 < nt:
                s_next = S(i + 1)
            pt = self.pt_tmp[self.rr("pt", 3)][:, 0:n]
            c.act(pt, s_cur, AF.Exp, scale=SCALE)
            c.mm(O, VV3[:, tiles[i], :], pt, start=(i == 0), stop=(i == nt - 1))
            c.mm(L, self.ones1.v(), pt, start=(i == 0), stop=(i == nt - 1))
        kk = self.rr("ot", 2)
        rl = self.rl_tmp[kk][:, 0:n]
        c.op("dve", lambda e: e.reciprocal(out=rl.ap, in_=L.ap), reads=[L], writes=[rl])
        oT = self.ot_tmp[kk][:, 0:n]
        c.tt("dve", oT, O, rl, ALU.mult)
        return oT

    def emit_gqa(self, l):
        c = self.c
        j = l // 2
        win = self.inp(f"win{l}", [D, 1536])
        wout = self.inp(f"wout{l}", [D, D])
        g_own = [(i * 256, 256, 0) for i in range(8)] + [(NLAT, 128, 1)]
        wf, wf3 = self.load_w("wf", win[:, 0:256], 256)
        cs2 = c.alloc("cs2", 256, BF16)
        c.dma("pool", cs2.v(), self.inp("cs2", [P, 256]))
        wfn = c.alloc("wfn", 256, BF16)
        wfn3 = wfn.r("p (j m) -> p j m", j=2)
        c.dma("pool", wfn.v(), self.inp(f"wfn{l}", [P, 256]))
        wo_f = c.alloc("wo_f", 2 * D, BF16)
        wo_f3 = wo_f.r("p (j n) -> p j n", j=2)
        c.dma("pool", wo_f3, wout[0:256, :].rearrange("(j p) n -> p j n", p=P))
        cdft = c.alloc("cdft", 512, BF16)
        cdft4 = cdft.r("p (a t k) -> p a t k", a=2, t=2)
        c.dma("pool", cdft.v(), self.inp("cdft", [P, 512]))
        FCS = c.alloc("FCS", 34 * 512, BF16)
        FCS3 = FCS.r("p (t f) -> p t f", f=512)
        fts = [c.alloc("fts", 512, BF16) for _ in range(2)]

        def stage1(buf0, n, h3):
            fps = self.bank(self.rr("fps", 2))
            fps3 = fps[:, 0:2 * n].r("p (j t) -> p j t", j=2)
            for jj in range(2):
                for k in range(KC):
                    c.mm(fps3[:, jj, :], wf3[:, k, jj * 128:(jj + 1) * 128], h3[:, k, :], start=(k == 0), stop=(k == KC - 1))
            ft = fts[self.rr("fts", 2)]
            ft3 = ft[:, 0:2 * n].r("p (j t) -> p j t", j=2)
            c.copy("act", ft3, fps3)
            for tt in range(n // 128):
                cps = self.bank(2 + self.rr("cps", 2))
                for jj in range(2):
                    c.mm(cps[:, jj * 256:(jj + 1) * 256], ft3[:, jj, tt * 128:(tt + 1) * 128], cs2.v())
                c.copy("dve", FCS3[:, (buf0 + tt * 128) // 128, :], cps.v())

        g_all = [(slot, c0, n, r) for slot in range(2) for (c0, n, r) in g_own]
        for (slot, c0, n, r, h3) in self.gstream(l, g_all):
            stage1(slot * NT + c0, n, h3)
        c.free(wf, cs2, *fts)

        dft = self.inp("dft", [4, 8, P, 4096], BF16)
        dst = [c.alloc("dftst", 4096, BF16) for _ in range(2)]
        frs = [c.alloc("frs", 512, BF16) for _ in range(2)]
        ots = [c.alloc("ofs", 512, BF16) for _ in range(2)]

        def finish(acc, n, col0, r):
            oTs = []
            for jj in range(2):
                fr = frs[jj][:, 0:n]
                c.copy("act", fr, acc[jj])
                wps = self.bank(6 + self.rr("yb", 2))[:, 0:n]
                c.mm(wps, wfn3[:, jj, :], fr)
                o = ots[jj][:, 0:n]
                c.copy("act", o, wps)
                oTs.append(o)
            self.outproj(oTs, [wo_f3[:, 0, :], wo_f3[:, 1, :]], col0, n, r)

        for kc in range(4):
            acc = [self.bank(4)[:, 0:512], self.bank(5)[:, 0:512]]
            for ng in range(8):
                dt_ = dst[self.rr("dst", 2)]
                c.dma("sp", dt_.v(), dft[kc, ng])
                dt4 = dt_.r("p (a i k) -> p a i k", a=2, i=4)
                for ni in range(4):
                    nn = ng * 4 + ni
                    ft_i = nn if nn < 16 else nn + 1
                    for jj in range(2):
                        c.mm(acc[jj], FCS3[:, ft_i, jj * 256:jj * 256 + 128], dt4[:, 0, ni, :], start=(nn == 0), stop=False)
                        c.mm(acc[jj], FCS3[:, ft_i, jj * 256 + 128:(jj + 1) * 256], dt4[:, 1, ni, :], start=False, stop=(nn == 31),
                             inc=(nn == 31 or (ni == 3 and jj == 1)))
            finish(acc, 512, kc * 512, 0)
        acc = [self.bank(4)[:, 0:128], self.bank(5)[:, 0:128]]
        for ti, ft_i in enumerate((16, 33)):
            for jj in range(2):
                c.mm(acc[jj], FCS3[:, ft_i, jj * 256:jj * 256 + 128], cdft4[:, 0, ti, :], start=(ti == 0), stop=False)
                c.mm(acc[jj], FCS3[:, ft_i, jj * 256 + 128:(jj + 1) * 256], cdft4[:, 1, ti, :], start=False, stop=(ti == 1))
        finish(acc, 128, NLAT, 1)
        c.free(FCS, wfn, wo_f, cdft, *dst, *frs, *ots)
        if self.debug.get("stop") == "fnet":
            return

        self.rope_dram = self.inp("rope", [P, 2 * 4096], BF16).rearrange("p (a n) -> p a n", a=2)
        ropeq = self.inp("ropeq", [P, 2 * NLAT], BF16).rearrange("p (a n) -> p a n", a=2)
        self.perm = c.alloc("perm", P, BF16)
        c.dma("pool", self.perm.v(), self.inp("perm", [P, P]))
        qkn = c.alloc("qkn", 2, F32)
        c.dma("sp", qkn.v(), self.inp(f"qkn{l}", [P, 2]))
        self.rn_tmp = [dict(sq=c.alloc("sq", 256, BF16), rstd=c.alloc("rstd", 256, F32), qn=c.alloc("qn", 256, BF16),
                            rope=c.alloc("rope", 512, BF16), t1=c.alloc("t1", 256, F32), t2=c.alloc("t2", 256, F32))
                       for _ in range(2)]
        self.pt_tmp = [c.alloc("pt", 512, BF16) for _ in range(3)]
        self.rl_tmp = [c.alloc("rl", 512, F32) for _ in range(2)]
        self.ot_tmp = [c.alloc("ot", 512, BF16) for _ in range(2)]
        KTt = c.alloc("KT", 2 * NT, BF16)
        KT = KTt.v()
        VVt = c.alloc("VV", 34 * 128, BF16)
        VV3 = VVt.r("p (t d) -> p t d", d=128)
        QTt = [c.alloc("QT", NT, BF16) for _ in range(2)]
        for g in range(2):
            wk, wk3 = self.load_w("wk", win[:, 1024 + g * 128:1024 + (g + 1) * 128], 128)
            wv, wv3 = self.load_w("wv", win[:, 1280 + g * 128:1280 + (g + 1) * 128], 128)

            def kv(buf0, n, r, h3, rope0):
                kps = self.bank(self.rr("kps", 2))[:, 0:n]
                for k in range(KC):
                    c.mm(kps, wk3[:, k, :], h3[:, k, :], start=(k == 0), stop=(k == KC - 1))
                self.rmsnorm_rope(kps, n, qkn[:, 1:2], KT[:, buf0:buf0 + n], rope0)
                for tt in range(n // 128):
                    vps = self.bank(6 + self.rr("yb", 2))[:, 0:128]
                    for k in range(KC):
                        c.mm(vps, h3[:, k, tt * 128:(tt + 1) * 128], wv3[:, k, :], start=(k == 0), stop=(k == KC - 1))
                    c.copy("act", VV3[:, buf0 // 128 + tt, :], vps)

            for (slot, c0, n, r, h3) in self.gstream(l, g_all):
                kv(slot * NT + c0, n, r, h3, (slot * NLAT + c0) if r == 0 else None)
            c.free(wk, wv)
            for hh in range(3):
                hg = g * 3 + hh
                wq, wq3 = self.load_w("wq", win[:, 256 + hg * 128:256 + (hg + 1) * 128], 128)
                wo = c.alloc("wo", D, BF16)
                c.dma("pool", wo.v(), wout[(2 + hg) * 128:(3 + hg) * 128, :])
                QT = QTt[self.rr("qt", 2)].v()
                for (c0, n, r) in g_own:
                    qps = self.bank(self.rr("kps", 2))[:, 0:n]
                    for k in range(KC):
                        c.mm(qps, wq3[:, k, :], self.HB3[:, k, c0:c0 + n], start=(k == 0), stop=(k == KC - 1))
                    self.rmsnorm_rope(qps, n, qkn[:, 0:1], QT[:, c0:c0 + n], c0 if r == 0 else None, table=ropeq)
                for qc in range(4):
                    oT = self.attn_T(KT, VV3, QT[:, qc * 512:(qc + 1) * 512], 512, list(range(34)))
                    self.outproj(oT, wo.v(), qc * 512, 512, 0)
                oT = self.attn_T(KT, VV3, QT[:, NLAT:NT], 128, [16, 33])
                self.outproj(oT, wo.v(), NLAT, 128, 1)
                c.free(wq, wo)
        c.free(KTt, VVt, *QTt, self.perm, qkn, *self.pt_tmp, *self.rl_tmp, *self.ot_tmp)
        for tm in self.rn_tmp:
            c.free(*tm.values())

    def emit_na(self, l):
        c = self.c
        win = self.inp(f"win{l}", [D, 3 * D])
        wout = self.inp(f"wout{l}", [D, D])
        nab = self.inp(f"nab{l}", [8, P, 5 * 768])
        HOt = c.alloc("HO", KC * 768, BF16)
        HO3 = HOt.r("p (c t) -> p c t", c=KC)
        hgroups = [(0, NLAT - 256, 256, 0), (1, 0, 256, 0), (0, NLAT, 128, 1), (1, NLAT, 128, 1)]
        hdst = [0, 256, 512, 640]
        for gi_, (slot, c0, n, r, h3) in enumerate(self.gstream(l, hgroups)):
            for cc in range(KC):
                c.copy(("dve", "pool")[cc % 2], HO3[:, cc, hdst[gi_]:hdst[gi_] + n], h3[:, cc, :])
        NK = 2816
        srcs = [(0, 256, HO3[:, :, 0:256])] + \
               [(256 + i * 512, 512, self.HB3[:, :, i * 512:(i + 1) * 512]) for i in range(4)] + \
               [(2304, 256, HO3[:, :, 256:512]), (2560, 256, HO3[:, :, 512:768])]
        KTs = [c.alloc("KT", NK, BF16) for _ in range(2)]
        VVs = [c.alloc("VV", NK, BF16) for _ in range(2)]
        QTs = [c.alloc("QT", NT, BF16) for _ in range(2)]
        nbs = [c.alloc("nb", 5 * 768, F32) for _ in range(1)]
        ss_tmp = [c.alloc("ss", 768, F32) for _ in range(2)]
        pt_tmp = [c.alloc("pt", 1024, BF16) for _ in range(3)]
        rl_tmp = [c.alloc("rl", 512, F32) for _ in range(2)]
        og_tmp = [c.alloc("og", 512, BF16) for _ in range(2)]

        for h in range(8):
            wq, wq3 = self.load_w("wq", win[:, h * 128:(h + 1) * 128], 128)
            wk, wk3 = self.load_w("wk", win[:, D + h * 128:D + (h + 1) * 128], 128)
            wv, wv3 = self.load_w("wv", win[:, 2 * D + h * 128:2 * D + (h + 1) * 128], 128)
            wo = c.alloc("wo", D, BF16)
            c.dma("pool", wo.v(), wout[h * 128:(h + 1) * 128, :])
            nb = nbs[0]
            c.dma("sp", nb.v(), nab[h])
            nb3 = nb.r("p (t w) -> p t w", w=768)
            KT = KTs[h % 2].v()
            VV3 = VVs[h % 2].r("p (t d) -> p t d", d=128)
            QT = QTs[h % 2].v()
            for (b0, n, h3) in srcs:
                kps = self.bank(6 + self.rr("yb", 2))[:, 0:n]
                for k in range(KC):
                    c.mm(kps, wk3[:, k, :], h3[:, k, :], start=(k == 0), stop=(k == KC - 1))
                c.copy("act", KT[:, b0:b0 + n], kps)
                for tt in range(n // 128):
                    vps = self.bank(6 + self.rr("yb", 2))[:, 0:128]
                    for k in range(KC):
                        c.mm(vps, h3[:, k, tt * 128:(tt + 1) * 128], wv3[:, k, :], start=(k == 0), stop=(k == KC - 1))
                    c.copy("dve", VV3[:, b0 // 128 + tt, :], vps)
            for (c0, n) in [(i * 512, 512) for i in range(4)] + [(NLAT, 128)]:
                qps = self.bank(6 + self.rr("yb", 2))[:, 0:n]
                for k in range(KC):
                    c.mm(qps, wq3[:, k, :], self.HB3[:, k, c0:c0 + n], start=(k == 0), stop=(k == KC - 1))
                c.copy("act", QT[:, c0:c0 + n], qps)
            jobs = []
            for bi in range(16):
                ty = {0: 0, 1: 1, 14: 3, 15: 4}.get(bi, 2)
                wb = 2 * bi if bi <= 13 else 28
                t0_ = wb * 64 // 128
                jobs.append((bi * 128, [t0_ + m for m in range(6)] + [20, 21], ty))
            jobs.append((NLAT, [20, 21], None))

            def S_of(job):
                q0, tiles, ty = job
                k = self.rr("nas", 2)
                SA = self.bank(2 * k)
                SB = self.bank(2 * k + 1)
                for m, kt in enumerate(tiles):
                    dst = (SA if m < 4 else SB)[:, (m % 4) * 128:(m % 4 + 1) * 128]
                    c.mm(dst, KT[:, kt * 128:(kt + 1) * 128], QT[:, q0:q0 + 128])
                return SA, SB

            def P_of(job, S):
                q0, tiles, ty = job
                SA, SB = S
                PT = pt_tmp[self.rr("napt", 3)]
                if ty is not None:
                    SS = ss_tmp[self.rr("nass", 2)]
                    c.stt(SS[:, 0:512], SA[:, 0:512], SCALE, nb3[:, ty, 0:512], ALU.mult, ALU.add)
                    c.stt(SS[:, 512:768], SB[:, 0:256], SCALE, nb3[:, ty, 512:768], ALU.mult, ALU.add)
                    c.act(PT[:, 0:768], SS[:, 0:768], AF.Exp)
                    c.act(PT[:, 768:1024], SB[:, 256:512], AF.Exp, scale=SCALE)
                else:
                    c.act(PT[:, 0:256], SA[:, 0:256], AF.Exp, scale=SCALE)
                return PT

            def PV_of(job, PT, O_dst, L_dst):
                q0, tiles, ty = job
                nt = len(tiles)
                for m, kt in enumerate(tiles):
                    c.mm(O_dst, VV3[:, kt, :], PT[:, m * 128:(m + 1) * 128], start=(m == 0), stop=(m == nt - 1))
                for m, kt in enumerate(tiles):
                    c.mm(L_dst, self.ones1.v(), PT[:, m * 128:(m + 1) * 128], start=(m == 0), stop=(m == nt - 1))

            def finish_group(gjobs):
                n = 128 * len(gjobs)
                col0 = gjobs[0][0]
                r = 0 if gjobs[0][2] is not None else 1
                kk = self.rr("narl", 2)
                rl = rl_tmp[kk][:, 0:n]
                c.act(rl, self.bank(5)[:, 0:n], AF.Ln)
                c.act(rl, rl, AF.Exp, scale=-1.0)
                og = og_tmp[kk][:, 0:n]
                c.tt("dve", og, self.bank(4)[:, 0:n], rl, ALU.mult)
                self.outproj(og, wo.v(), col0, n, r)

            S_next = S_of(jobs[0])
            gjobs = []
            for ji, job in enumerate(jobs):
                S_cur = S_next
                if ji + 1 < len(jobs):
                    S_next = S_of(jobs[ji + 1])
                PT = P_of(job, S_cur)
                slot = len(gjobs)
                PV_of(job, PT, self.bank(4)[:, slot * 128:(slot + 1) * 128], self.bank(5)[:, slot * 128:(slot + 1) * 128])
                gjobs.append(job)
                if len(gjobs) == 4 or ji == len(jobs) - 1 or jobs[ji + 1][2] is None:
                    finish_group(gjobs)
                    gjobs = []
            c.free(wq, wk, wv, wo)
        c.free(HOt, *KTs, *VVs, *QTs, *nbs, *ss_tmp, *pt_tmp, *rl_tmp, *og_tmp)

    def emit_ln(self, l, which):
        c = self.c
        gi, bi_ = (0, 1) if which == 1 else (2, 3)
        eps = LN_EPS / (ALPHA * ALPHA)
        zb = [c.alloc("zb", KC * 512, BF16) for _ in range(2)]
        zq = [c.alloc("zq", KC * 512, BF16) for _ in range(2)]
        tmp = [dict(M=c.alloc("M", 512, F32), m2=c.alloc("m2", 512, F32), rstd=c.alloc("rstdl", 512, F32)) for _ in range(2)]
        t1s = [c.alloc("t1", 512, F32) for _ in range(3)]
        if which == 1:
            h2f = [c.alloc("h2f", KC * 512, F32) for _ in range(2)]
            wr = c.alloc("wr", KC * 64, F32)
            wr3 = wr.r("p (k e) -> p k e", k=KC)
            c.dma("sp", wr.v(), self.inp(f"wr{l}", [P, KC * 64]))
            mb = c.alloc("mb", 64, F32)
            c.dma("sp", mb.v(), self.inp(f"mb{l}", [P, 64]))
            gt = [c.alloc("gt", 64 * 3 + 16, F32) for _ in range(2)]
            GTS = c.alloc("GTS", NT, F32)
            c.memset("dve", self.G3[:, :, 64:65], 1.0)
            self.GTD = c.dram(f"gtd{l}", [NEXP, NT], F32)
        groups = [(i * 512, 512, 0) for i in range(4)] + [(NLAT, 128, 1)]
        for gidx, (c0, n, r) in enumerate(groups):
            k = gidx % 2
            zb3 = zb[k].r("p (c t) -> p c t", c=KC)[:, :, 0:n]
            zq3 = zq[k].r("p (c t) -> p c t", c=KC)[:, :, 0:n]
            mean = self.bank(0)[:, 0:n]
            msq = self.bank(1)[:, 0:n]
            for cc in range(KC):
                xs = self.XT3[:, cc, c0:c0 + n]
                c.copy("pool", zb3[:, cc, :], xs)
                c.act(zq3[:, cc, :], xs, AF.Square)
            for cc in range(KC):
                c.mm(mean, self.ones_mean.v(), zb3[:, cc, :], start=(cc == 0), stop=(cc == KC - 1))
            for cc in range(KC):
                c.mm(msq, self.ones_mean.v(), zq3[:, cc, :], start=(cc == 0), stop=(cc == KC - 1))
            M = tmp[k]["M"][:, 0:n]
            m2 = tmp[k]["m2"][:, 0:n]
            rstd = tmp[k]["rstd"][:, 0:n]
            c.copy("act", M, mean)
            c.act(m2, mean, AF.Square)
            c.tt("dve", m2, msq, m2, ALU.subtract)
            c.act(rstd, m2, AF.Ln, bias=eps)
            c.act(rstd, rstd, AF.Exp, scale=-0.5)
            if which == 1:
                h2f3 = h2f[k].r("p (c t) -> p c t", c=KC)[:, :, 0:n]
            for cc in range(KC):
                xs = self.XT3[:, cc, c0:c0 + n]
                t1 = t1s[self.rr("t1", 3)][:, 0:n]
                c.tt("dve", t1, xs, M, ALU.subtract)
                c.tt("pool", t1, t1, rstd, ALU.mult)
                c.act(xs, t1, AF.Identity, scale=self.LNV3[:, gi, cc:cc + 1], bias=self.LNV3[:, bi_, cc:cc + 1])
                if which == 1:
                    c.act(h2f3[:, cc, :], t1, AF.Identity, scale=self.A23[:, cc, r:r + 1], bias=self.B23[:, cc, r:r + 1])
                    c.copy("pool", self.HB3[:, cc, c0:c0 + n], h2f3[:, cc, :])
            if which == 1:
                for tt in range(n // 128):
                    tile_i = c0 // 128 + tt
                    lg = self.bank(2 + self.rr("lg", 2))[:, 0:64]
                    for cc in range(KC):
                        c.mm(lg, h2f3[:, cc, tt * 128:(tt + 1) * 128], wr3[:, cc, :], start=(cc == 0), stop=(cc == KC - 1))
                    g_ = gt[self.rr("gt", 2)]
                    sc, sel, wsel, m8, den = g_[:, 0:64], g_[:, 64:128], g_[:, 128:192], g_[:, 192:200], g_[:, 200:201]
                    rden = g_[:, 201:202]
                    c.act(sc, lg, AF.Sigmoid)
                    c.tt("dve", sel, sc, mb.v(), ALU.add)
                    c.op("dve", lambda e: e.max(out=m8.ap, in_=sel.ap), reads=[sel], writes=[m8])
                    c.ts("dve", sel, sel, m8[:, 7:8], op0=ALU.is_ge)
                    c.tt("dve", wsel, sel, sc, ALU.mult)
                    c.op("dve", lambda e: e.reduce_sum(out=den.ap, in_=wsel.ap, axis=AX.X), reads=[wsel], writes=[den])
                    c.op("dve", lambda e: e.reciprocal(out=rden.ap, in_=den.ap), reads=[den], writes=[rden])
                    c.ts("dve", self.G3[:, tile_i, 0:64], wsel, rden, ROUTE_SCALE, ALU.mult, ALU.mult)
                    gtp = self.bank(4 + self.rr("gtp", 2))[0:NEXP, 0:128]
                    c.transpose(gtp, self.G3[:, tile_i, :], self.identf.v())
                    c.copy("act", GTS[0:NEXP, tile_i * 128:(tile_i + 1) * 128], gtp)
        if which == 1:
            c.dma("sp", self.GTD.v(), GTS[0:NEXP, :])
            c.free(*h2f, wr, mb, *gt, GTS)
        c.free(*zb, *zq, *t1s)
        for t_ in tmp:
            c.free(*t_.values())

    def emit_moe(self, l):
        c = self.c
        moew = self.inp(f"moew{l}", [NEXP, P, 6144])
        NWB = 4
        WB = [c.alloc("WB", 6144, BF16) for _ in range(NWB)]
        GB = [c.alloc("GB", NT, F32) for _ in range(NWB)]
        sT = [c.alloc("sT", 512, BF16) for _ in range(2)]
        tT = [c.alloc("tT", 512, BF16) for _ in range(2)]
        aT = [c.alloc("aT", 512, BF16) for _ in range(4)]
        groups = [(i * 256, 256, 0) for i in range(8)] + [(NLAT, 128, 1)]
        pairs = [(2 * i, 2 * i + 1) for i in range(32)] + [(64,)]

        def prefetch(e):
            w = WB[e % NWB]
            for q in range(3):
                c.dma("pool", w[:, q * 2048:(q + 1) * 2048], moew[e][:, q * 2048:(q + 1) * 2048])
            c.dma("sp", GB[e % NWB].r("p (o t) -> p o t", o=1), V(self.GTD, self.GTD.h[e:e + 1, :].partition_broadcast(P)))

        def down_mm(sts):
            e0, c0, n, r, _ = sts[0]
            Y = [self.bank(4 + cc // 2)[:, (cc % 2) * 256:(cc % 2) * 256 + n] for cc in range(KC)]
            nmm = 2 * len(sts)
            for cc in range(KC):
                i = 0
                for (e, _, _, _, a3) in sts:
                    w = WB[e % NWB].v()
                    for jj in range(2):
                        c.mm(Y[cc], w[:, 4096 + jj * 1024 + cc * 128:4096 + jj * 1024 + (cc + 1) * 128], a3[:, jj, :],
                             start=(i == 0), stop=(i == nmm - 1))
                        i += 1

        def down_acc(sts):
            e0, c0, n, r, _ = sts[0]
            Y = [self.bank(4 + cc // 2)[:, (cc % 2) * 256:(cc % 2) * 256 + n] for cc in range(KC)]
            for cc in range(KC):
                xs = self.XT3[:, cc, c0:c0 + n]
                c.stt(xs, Y[cc], self.MODV3[:, 40 + cc, r:r + 1], xs, ALU.mult, ALU.add)

        for e in pairs[0]:
            prefetch(e)
        pend = []
        ready = None
        it = 0
        for pi, pr in enumerate(pairs):
            for gidx, (c0, n, r) in enumerate(groups):
                for ei, e in enumerate(pr):
                    w = WB[e % NWB].v()
                    gb = GB[e % NWB].v()
                    k = it % 2
                    gps = self.bank(2 * k)
                    ups = self.bank(2 * k + 1)
                    g3 = gps[:, 0:2 * n].r("p (j t) -> p j t", j=2)
                    u3 = ups[:, 0:2 * n].r("p (j t) -> p j t", j=2)
                    for jj in range(2):
                        for kk in range(KC):
                            c.mm(g3[:, jj, :], w[:, kk * 256 + jj * 128:kk * 256 + (jj + 1) * 128], self.HB3[:, kk, c0:c0 + n],
                                 start=(kk == 0), stop=(kk == KC - 1))
                    for jj in range(2):
                        for kk in range(KC):
                            c.mm(u3[:, jj, :], w[:, 2048 + kk * 256 + jj * 128:2048 + kk * 256 + (jj + 1) * 128],
                                 self.HB3[:, kk, c0:c0 + n], start=(kk == 0), stop=(kk == KC - 1))
                    did = None
                    if ei == 0 and ready is not None:
                        down_mm(ready)
                        did = ready
                        ready = None
                        if gidx == 0 and pi + 1 < len(pairs):
                            for e2 in pairs[pi + 1]:
                                prefetch(e2)
                    elif ei == 0 and gidx == 0 and pi == 0:
                        for e2 in pairs[1]:
                            prefetch(e2)
                    s3 = sT[k][:, 0:2 * n].r("p (j t) -> p j t", j=2)
                    t3 = tT[k][:, 0:2 * n].r("p (j t) -> p j t", j=2)
                    a3 = aT[it % 4][:, 0:2 * n].r("p (j t) -> p j t", j=2)
                    c.act(s3, g3, AF.Silu)
                    for jj in range(2):
                        c.tt("dve", t3[:, jj, :], u3[:, jj, :], gb[:, c0:c0 + n], ALU.mult)
                    c.tt("pool", a3, s3, t3, ALU.mult)
                    if did is not None:
                        down_acc(did)
                    pend.append((e, c0, n, r, a3))
                    if ei == len(pr) - 1:
                        ready = pend
                        pend = []
                    it += 1
        down_mm(ready)
        down_acc(ready)
        c.free(*WB, *GB, *sT, *tT, *aT)


BF = ml_dtypes.bfloat16


def _fm(tok):
    T_ = tok.shape[0]
    return np.ascontiguousarray(tok.T.reshape(KC, P, T_).transpose(1, 0, 2)).reshape(P, KC * T_)


def _unfm(a, T_):
    return np.ascontiguousarray(a.reshape(P, KC, T_).transpose(1, 0, 2).reshape(D, T_).T)


_CONST_CACHE = {}


def _consts(s):
    if s in _CONST_CACHE:
        return _CONST_CACHE[s]
    out = {}
    out["ident"] = np.eye(P, dtype=np.float32)
    d = np.arange(P)
    i = d % 64
    partner = np.where(i < 32, d + 32, d - 32)
    perm = np.zeros((P, P), np.float32)
    perm[partner, d] = 1.0
    out["perm"] = perm
    pos = np.arange(4096)
    inv = 10000.0 ** (-(2.0 / 64) * np.arange(32, dtype=np.float64))
    pv = np.where((d // 64)[:, None] == 0, (pos // 64)[None, :], (pos % 64)[None, :]).astype(np.float64)
    ang = pv * inv[i % 32][:, None]
    cos = np.cos(ang)
    sin = np.sin(ang) * np.where(i < 32, -1.0, 1.0)[:, None]
    rope = np.stack([cos, sin], axis=1)
    out["rope"] = rope.reshape(P, 2 * 4096).astype(BF)
    out["ropeq"] = np.ascontiguousarray(rope[:, :, s * NLAT:(s + 1) * NLAT]).reshape(P, 2 * NLAT).astype(BF)
    cc = np.arange(64)
    c64 = np.cos(2 * np.pi * np.outer(cc, cc) / 64) / 8.0
    s64 = np.sin(2 * np.pi * np.outer(cc, cc) / 64) / 8.0
    cs2 = np.zeros((P, 256), np.float32)
    for g2 in range(2):
        cs2[g2 * 64:(g2 + 1) * 64, g2 * 64:(g2 + 1) * 64] = c64
        cs2[g2 * 64:(g2 + 1) * 64, 128 + g2 * 64:128 + (g2 + 1) * 64] = s64
    out["cs2"] = cs2
    kk = s * NLAT + np.arange(NLAT)
    ph = (np.outer(pos, kk) % 4096).astype(np.float64) * (2 * np.pi / 4096)
    tab = np.stack([np.cos(ph) / 64.0, -np.sin(ph) / 64.0], axis=0).astype(np.float32)
    tab = tab.reshape(2, 8, 4, P, 4, 512)
    out["dft"] = np.ascontiguousarray(tab.transpose(4, 1, 3, 0, 2, 5)).reshape(4, 8, P, 4096).astype(BF)
    cpos = np.stack([np.arange(128), 128 + np.arange(128)], axis=0)
    ck = s * 128 + np.arange(128)
    cph = (cpos[:, :, None] * ck[None, None, :] % 256) * (2 * np.pi / 256)
    ctab = np.stack([np.cos(cph) / 16.0, -np.sin(cph) / 16.0], axis=0)
    out["cdft"] = np.ascontiguousarray(ctab.transpose(2, 0, 1, 3)).reshape(P, 512).astype(np.float32)
    _CONST_CACHE[s] = out
    return out


def _na_bias(rpb, s):
    out = np.full((8, P, 5, 768), NEG, np.float32)
    q = np.arange(P)
    w = np.arange(768)
    qcol = q % 64
    kcol = w % 64
    cstart = np.clip(qcol - 8, 0, 48)
    okc = (kcol[None, :] >= cstart[:, None]) & (kcol[None, :] < cstart[:, None] + 16)
    dc = kcol[None, :] - qcol[:, None] + 15
    rep = {0: 0, 1: 1, 2: 2, 3: 14, 4: 15}
    for ty, bi in rep.items():
        i = 16 * s + bi
        wb = 2 * bi if bi <= 13 else 28
        qrow = 2 * i + q // 64
        rstart = np.clip(qrow - 4, 0, 56)
        krow = wb + (32 * s - 4) + w // 64
        okr = (krow[None, :] >= rstart[:, None]) & (krow[None, :] < rstart[:, None] + 8)
        dr = krow[None, :] - qrow[:, None] + 7
        ok = okr & okc
        drc = np.clip(dr, 0, 14)
        dcc = np.clip(dc, 0, 30)
        vals = rpb[:, drc, dcc]
        out[:, :, ty, :] = np.where(ok[None], vals, NEG)
    outT = out.reshape(8, P, 5, 6, P).transpose(0, 4, 2, 3, 1)
    return np.ascontiguousarray(outT).reshape(8, P, 5 * 768)


def _layer_weights(l, inp, tag):
    j = l // 2
    w = {}
    w[f"wmod{tag}"] = inp["w_mod"][l]
    w[f"bmod{tag}"] = np.ascontiguousarray(inp["b_mod"][l].reshape(48, P).T)
    w[f"lnv{tag}"] = np.ascontiguousarray(
        np.stack([inp[k][l].reshape(KC, P).T for k in ("ln1_g", "ln1_b", "ln2_g", "ln2_b")], axis=1)).reshape(P, 32)
    if l % 2 == 0:
        w[f"win{tag}"] = inp["ab_w_in"][j]
        w[f"wout{tag}"] = inp["ab_w_out"][j]
        wf = inp["ab_w_fnet"][j]
        wfn = np.zeros((P, 2, P), np.float32)
        for jj in range(2):
            wfn[0:64, jj, 0:64] = wf[2 * jj]
            wfn[64:128, jj, 64:128] = wf[2 * jj + 1]
        w[f"wfn{tag}"] = wfn.reshape(P, 256)
        w[f"qkn{tag}"] = np.ascontiguousarray(np.stack([inp["ab_q_norm"][j], inp["ab_k_norm"][j]], axis=1))
    else:
        w[f"win{tag}"] = inp["na_w_in"][j]
        w[f"wout{tag}"] = inp["na_w_out"][j]
    w[f"wr{tag}"] = np.ascontiguousarray(inp["moe_w_router"][l].reshape(KC, P, 64).transpose(1, 0, 2)).reshape(P, KC * 64)
    w[f"mb{tag}"] = np.ascontiguousarray(np.broadcast_to(inp["moe_bias"][l][None, :], (P, 64)))
    wg = np.concatenate([inp["moe_w_gate"][l], inp["sh_w_gate"][l][None]], axis=0)
    wu = np.concatenate([inp["moe_w_up"][l], inp["sh_w_up"][l][None]], axis=0)
    wd = np.concatenate([inp["moe_w_down"][l], inp["sh_w_down"][l][None]], axis=0)
    moew = np.empty((NEXP, P, 6144), np.float32)
    moew[:, :, 0:2048] = wg.reshape(NEXP, KC, P, 256).transpose(0, 2, 1, 3).reshape(NEXP, P, 2048)
    moew[:, :, 2048:4096] = wu.reshape(NEXP, KC, P, 256).transpose(0, 2, 1, 3).reshape(NEXP, P, 2048)
    moew[:, :, 4096:6144] = wd.reshape(NEXP, 2, P, D).transpose(0, 2, 1, 3).reshape(NEXP, P, 2048)
    w[f"moew{tag}"] = moew
    return w


_PROG_CACHE = {}


def _get_prog(layers, debug=None):
    key = (tuple(layers), tuple(sorted((debug or {}).items())))
    if key not in _PROG_CACHE:
        pr = Prog(list(layers), debug)
        nc = pr.build()
        _PROG_CACHE[key] = (nc, set(pr.inputs.keys()))
    return _PROG_CACHE[key]


def _init_toks(inp):
    toks = []
    for r in range(8):
        b, s = r // 2, r % 2
        toks.append(np.concatenate([inp["x"][b, s * NLAT:(s + 1) * NLAT], inp["ctx"][b, s * NCTX:(s + 1) * NCTX]], axis=0))
    return toks


def _launch(inp, layers, toks, debug=None):
    nc, names = _get_prog(layers, debug)
    lw = {}
    for l in layers:
        lw.update(_layer_weights(l, inp, l))
    fm = [_fm(t) for t in toks]
    in_maps = []
    for r in range(8):
        b, s = r // 2, r % 2
        m = dict(lw)
        cs = _consts(s)
        for k in ("ident", "perm", "rope", "ropeq", "cs2", "dft", "cdft"):
            m[k] = cs[k]
        for l in layers:
            if l % 2 == 1:
                m[f"nab{l}"] = _na_bias(inp["na_rpb"][l // 2], s)
        m["cT"] = np.ascontiguousarray(
            np.stack([inp["c"][b].reshape(KC, P).T, inp["c_ctx"].reshape(KC, P).T], axis=2)).reshape(P, 16)
        m["xT"] = fm[r]
        m[f"xg{layers[0]}"] = np.ascontiguousarray(
            np.stack([fm[r - s].reshape(P, KC, NT), fm[r - s + 1].reshape(P, KC, NT)], axis=0).transpose(2, 0, 1, 3)
        ).reshape(KC, 2 * P, NT)
        in_maps.append({k: v for k, v in m.items() if k in names})
    res = run_bass_kernel_spmd(nc, in_maps, core_ids=list(range(8)))
    return [_unfm(res.results[r]["xout"], NT) for r in range(8)]


def run_layers(inp, layers=range(DEPTH), toks=None, debug=None, fused=False):
    inp = {k: np.asarray(v) for k, v in inp.items()}
    if toks is None:
        toks = _init_toks(inp)
    if fused:
        return _launch(inp, list(layers), toks, debug)
    for l in layers:
        toks = _launch(inp, [l], toks, debug)
    return toks


def kernel(**inputs):
    toks = run_layers(inputs, fused=True)
    out = np.empty((4, 4096, D), np.float32)
    for r in range(8):
        b, s = r // 2, r % 2
        out[b, s * NLAT:(s + 1) * NLAT] = toks[r][0:NLAT]
    return out
```

```python
import math
import numpy as np
import ml_dtypes
import concourse.bass as bass
import concourse.mybir as mybir
from concourse.bass_utils import run_bass_kernel_spmd
from contextlib import ExitStack

F32 = mybir.dt.float32
BF16 = mybir.dt.bfloat16
AF = mybir.ActivationFunctionType
ALU = mybir.AluOpType
AX = mybir.AxisListType

P = 128
D = 1024
KC = 8
NLAT = 2048
NCTX = 128
NT = NLAT + NCTX
DEPTH = 4
ALPHA = (2 * DEPTH) ** 0.25
LN_EPS = 1e-6
RMS_EPS = 1e-6
HD = 128
SCALE = HD ** -0.5
NEG = -1e30
NEXP = 65
ROUTE_SCALE = 2.5

ENGS = ("pe", "act", "dve", "pool", "sp")
DTSIZE = {F32: 4, BF16: 2}


class V:
    __slots__ = ("t", "ap")

    def __init__(self, t, ap):
        self.t = t
        self.ap = ap

    def __getitem__(self, idx):
        return V(self.t, self.ap[idx])

    def r(self, pat, **kw):
        return V(self.t, self.ap.rearrange(pat, **kw))

    def bc(self, shape):
        return V(self.t, self.ap.broadcast_to(shape))

    def bitcast(self, dt):
        return V(self.t, self.ap.bitcast(dt))


class T:
    def __init__(self, h, name, space, rng=None):
        self.h = h
        self.name = name
        self.space = space
        self.w = None
        self.r_ = []
        self.dsem = None
        self.dcnt = 0
        self.rng = rng

    def __getitem__(self, idx):
        return V(self, self.h[idx])

    def r(self, pat, **kw):
        return V(self, self.h.rearrange(pat, **kw))

    def v(self):
        return V(self, self.h)


class Ctx:
    def __init__(self, nc, arena_bytes=211968):
        self.nc = nc
        self.es = ExitStack()
        self.E = {"pe": nc.tensor, "act": nc.scalar, "dve": nc.vector, "pool": nc.gpsimd, "sp": nc.sync}
        self.sem = {}
        self.cnt = {}
        for e in ENGS:
            self.sem[e] = self.es.enter_context(nc.semaphore("s_" + e))
            self.cnt[e] = 0
        self.seen = {e: {} for e in ENGS}
        self.nbuf = 0
        self.sem_pool = []
        self.arena = self.es.enter_context(nc.sbuf_tensor("arena", [P, arena_bytes // 2], BF16))
        self.arena_bytes = arena_bytes
        self.free_list = [(0, arena_bytes)]
        self.grave = []
        self.live = {}
        self.banks = []
        for i in range(8):
            h = self.es.enter_context(nc.psum_tensor(f"bank{i}", [P, 512], F32))
            self.banks.append(T(h[:], f"bank{i}", "ps"))

    def alloc(self, name, nelem, dt, parts=P):
        nbytes = (nelem * DTSIZE[dt] + 63) // 64 * 64
        for i, (s, e) in enumerate(self.free_list):
            if e - s >= nbytes:
                self.free_list[i] = (s + nbytes, e)
                if self.free_list[i][0] == self.free_list[i][1]:
                    del self.free_list[i]
                break
        else:
            raise RuntimeError(f"arena OOM allocating {name} {nbytes}B; free={self.free_list}")
        ap = self.arena[0:parts, s // 2:(s + nbytes) // 2]
        if dt != BF16:
            ap = ap.bitcast(dt)
        ap = ap[:, 0:nelem]
        self.nbuf += 1
        t = T(ap, f"{name}_{self.nbuf}", "sb", (s, s + nbytes))
        keep = []
        for (gs, ge, toks) in self.grave:
            if gs < s + nbytes and s < ge:
                t.r_.extend(toks)
                if gs >= s and ge <= s + nbytes:
                    continue
            keep.append((gs, ge, toks))
        self.grave = keep
        return t

    def free(self, *ts):
        for t in ts:
            s, e = t.rng
            toks = [x for x in ([t.w] + t.r_) if x is not None]
            self.grave.append((s, e, self._compress(toks)))
            self.free_list.append((s, e))
            self.free_list.sort()
            merged = []
            for a, b in self.free_list:
                if merged and merged[-1][1] == a:
                    merged[-1] = (merged[-1][0], b)
                else:
                    merged.append((a, b))
            self.free_list = merged
            t.rng = None
            if t.dsem is not None:
                self.sem_pool.append((t.dsem, t.dcnt, t.dkey))
                t.dsem = None

    @staticmethod
    def _compress(toks):
        best = {}
        for tok in toks:
            key = tok[1] if tok[0] == "eng" else tok[3]
            v = tok[2]
            if key not in best or best[key][2] < v:
                best[key] = tok
        return list(best.values())

    def dram(self, name, shape, dt, kind="Internal"):
        h = self.nc.dram_tensor(name, list(shape), dt, kind=kind)
        return T(h.ap(), name, "dram")

    def _wait(self, e, tok):
        if tok is None:
            return
        if tok[0] == "eng":
            _, f, v = tok
            key = "e:" + f
            sem = self.sem[f]
        else:
            _, sem, v, key = tok
        if e == "pe" and tok[0] == "eng" and tok[1] == "pe":
            return
        if self.seen[e].get(key, 0) >= v:
            return
        self.E[e].wait_ge(sem, v)
        self.seen[e][key] = v

    def _deps(self, e, reads, writes, acc=False):
        for t in reads:
            self._wait(e, t.w)
        if not acc:
            for t in writes:
                self._wait(e, t.w)
                for tok in t.r_:
                    self._wait(e, tok)

    def op(self, e, fn, reads=(), writes=(), acc=False, inc=True):
        reads = [v.t for v in reads if v is not None]
        writes = [v.t for v in writes if v is not None]
        self._deps(e, reads, writes, acc)
        ins = fn(self.E[e])
        if inc:
            self.cnt[e] += 1
            ins.then_inc(self.sem[e], 1)
            tok = ("eng", e, self.cnt[e])
        else:
            tok = ("eng", e, self.cnt[e] + 1)
        for t in reads:
            t.r_.append(tok)
            if len(t.r_) > 24:
                t.r_ = self._compress(t.r_)
        for t in writes:
            t.w = tok
            t.r_ = []
        return ins

    def dma(self, q, out, in_, **kw):
        ot = out.t if isinstance(out, V) else None
        it = in_.t if isinstance(in_, V) else None
        oap = out.ap if isinstance(out, V) else out
        iap = in_.ap if isinstance(in_, V) else in_
        reads = [it] if it is not None else []
        writes = [ot] if ot is not None else []
        self._deps(q, reads, writes)
        owner = None
        for t in (ot, it):
            if t is not None and t.space != "dram":
                owner = t
                break
        if owner is None:
            owner = ot if ot is not None else it
        if owner.dsem is None:
            if self.sem_pool:
                sem, cnt, key = self.sem_pool.pop()
                if cnt > 0:
                    self._wait(q, ("dma", sem, cnt, key))
            else:
                self.nsem = getattr(self, "nsem", 0) + 1
                key = f"d:{self.nsem}"
                sem, cnt = self.es.enter_context(self.nc.semaphore(f"dsem{self.nsem}")), 0
            owner.dsem, owner.dcnt, owner.dkey = sem, cnt, key
        owner.dcnt += 16
        ins = self.E[q].dma_start(out=oap, in_=iap, **kw)
        ins.then_inc(owner.dsem, 16)
        tok = ("dma", owner.dsem, owner.dcnt, owner.dkey)
        for t in reads:
            t.r_.append(tok)
        for t in writes:
            t.w = tok
            t.r_ = []
        return tok

    def wait_all(self, e, ts):
        for t in ts:
            self._wait(e, t.w)
            for tok in t.r_:
                self._wait(e, tok)

    def mm(self, out, lhsT, rhs, start=True, stop=True, inc=None):
        self.op("pe", lambda e: e.matmul(out.ap, lhsT=lhsT.ap, rhs=rhs.ap, start=start, stop=stop),
                reads=[lhsT, rhs], writes=[out], acc=not start, inc=(stop if inc is None else inc))

    def transpose(self, out, in_, ident):
        self.op("pe", lambda e: e.transpose(out.ap, in_.ap, ident.ap), reads=[in_, ident], writes=[out])

    def act(self, out, in_, func, scale=None, bias=None, accum=None):
        kw = {}
        rd = [in_]
        wr = [out]
        if scale is not None:
            if isinstance(scale, V):
                kw["scale"] = scale.ap
                rd.append(scale)
            else:
                kw["scale"] = float(scale)
        if bias is not None:
            if isinstance(bias, V):
                kw["bias"] = bias.ap
                rd.append(bias)
            else:
                kw["bias"] = float(bias)
        if accum is not None:
            kw["accum_out"] = accum.ap
            wr.append(accum)
        self.op("act", lambda e: e.activation(out=out.ap, in_=in_.ap, func=func, **kw), reads=rd, writes=wr)

    def copy(self, eng, out, in_):
        if eng == "act":
            self.op("act", lambda e: e.copy(out=out.ap, in_=in_.ap), reads=[in_], writes=[out])
        else:
            self.op(eng, lambda e: e.tensor_copy(out=out.ap, in_=in_.ap), reads=[in_], writes=[out])

    def tt(self, eng, out, in0, in1, op):
        self.op(eng, lambda e: e.tensor_tensor(out=out.ap, in0=in0.ap, in1=in1.ap, op=op),
                reads=[in0, in1], writes=[out])

    def ts(self, eng, out, in0, s1, s2=None, op0=ALU.mult, op1=None):
        rd = [in0]
        a1 = s1.ap if isinstance(s1, V) else float(s1)
        if isinstance(s1, V):
            rd.append(s1)
        a2 = None
        if s2 is not None:
            a2 = s2.ap if isinstance(s2, V) else float(s2)
            if isinstance(s2, V):
                rd.append(s2)
        if op1 is None:
            self.op(eng, lambda e: e.tensor_scalar(out=out.ap, in0=in0.ap, scalar1=a1, scalar2=None, op0=op0),
                    reads=rd, writes=[out])
        else:
            self.op(eng, lambda e: e.tensor_scalar(out=out.ap, in0=in0.ap, scalar1=a1, scalar2=a2, op0=op0, op1=op1),
                    reads=rd, writes=[out])

    def stt(self, out, in0, scalar, in1, op0, op1, eng="dve"):
        rd = [in0, in1]
        a = scalar.ap if isinstance(scalar, V) else float(scalar)
        if isinstance(scalar, V):
            rd.append(scalar)
        self.op(eng, lambda e: e.scalar_tensor_tensor(out=out.ap, in0=in0.ap, scalar=a, in1=in1.ap, op0=op0, op1=op1),
                reads=rd, writes=[out])

    def memset(self, eng, out, val):
        self.op(eng, lambda e: e.memset(out.ap, val), writes=[out])

    def close(self):
        self.es.close()


class Prog:
    def __init__(self, layers, debug=None):
        self.layers = list(layers)
        self.debug = debug or {}
        self.nc = bass.Bass("TRN2", target_bir_lowering=False)
        self.c = Ctx(self.nc)
        self.inputs = {}
        self.rot = {}
        self.xg = {}

    def inp(self, name, shape, dt=F32):
        if name not in self.inputs:
            self.inputs[name] = self.nc.dram_tensor(name, list(shape), dt, kind="ExternalInput").ap()
        return self.inputs[name]

    def bank(self, i):
        return self.c.banks[i]

    def rr(self, key, n):
        v = self.rot.get(key, 0)
        self.rot[key] = v + 1
        return v % n

    def build(self):
        c = self.c
        nc = self.nc
        first = self.layers[0]
        self.XT = c.alloc("XT", KC * NT, F32)
        self.XT3 = self.XT.r("p (c t) -> p c t", c=KC)
        self.HB = c.alloc("HB", KC * NT, BF16)
        self.HB3 = self.HB.r("p (c t) -> p c t", c=KC)
        self.identb = c.alloc("identb", P, BF16)
        self.identf = c.alloc("identf", P, F32)
        c.dma("pool", self.identb.v(), self.inp("ident", [P, P]))
        c.dma("sp", self.identf.v(), self.inp("ident", [P, P]))
        self.ones_mean = c.alloc("ones_mean", P, BF16)
        self.ones_rms = c.alloc("ones_rms", P, BF16)
        self.ones1 = c.alloc("ones1", P, BF16)
        c.memset("dve", self.ones_mean.v(), 1.0 / D)
        c.memset("dve", self.ones_rms.v(), 1.0 / HD)
        c.memset("dve", self.ones1.v(), 1.0)
        self.MODV = c.alloc("MODV", 96, F32)
        self.MODV3 = self.MODV.r("p (m r) -> p m r", r=2)
        self.LNV = c.alloc("LNV", 32, F32)
        self.LNV3 = self.LNV.r("p (m c) -> p m c", c=KC)
        self.A2 = c.alloc("A2", 16, F32)
        self.A23 = self.A2.r("p (c r) -> p c r", r=2)
        self.B2 = c.alloc("B2", 16, F32)
        self.B23 = self.B2.r("p (c r) -> p c r", r=2)
        self.G = c.alloc("G", 17 * NEXP, F32)
        self.G3 = self.G.r("p (t e) -> p t e", e=NEXP)
        self.scT = c.alloc("scT", 16, F32)
        self.scT3 = self.scT.r("p (k r) -> p k r", r=2)
        c.dma("sp", self.scT.v(), self.inp("cT", [P, 16]))
        c.act(self.scT.v(), self.scT.v(), AF.Silu)
        c.dma("sp", self.XT.v(), self.inp("xT", [P, KC * NT]))
        for li, l in enumerate(self.layers):
            self.layer(l)
            if li + 1 < len(self.layers):
                self.exchange(l, self.layers[li + 1])
        xout = nc.dram_tensor("xout", [P, KC * NT], F32, kind="ExternalOutput").ap()
        tok = c.dma("sp", xout, self.XT.v())
        c._wait("sp", tok)
        c.close()
        return nc

    def exchange(self, l, lnext):
        c = self.c
        nc = self.nc
        sem = c.es.enter_context(nc.semaphore(f"ccsem{l}"))
        tok = ("dma", sem, KC, f"cc{l}")
        xgh = nc.dram_tensor(f"xgi{lnext}", [KC, 2 * P, NT], F32)
        xg = T(xgh.ap(), f"xgi{lnext}", "dram")
        for cc in range(KC):
            xsh = nc.dram_tensor(f"xs{l}_{cc}", [P, NT], F32)
            xs = T(xsh.ap(), f"xs{l}_{cc}", "dram")
            c.dma("sp", xs.v(), self.XT3[:, cc, :])
            c._deps("pool", [xs], [xg] if cc == 0 else [])
            ins = nc.gpsimd.collective_compute("AllGather", ALU.bypass, replica_groups=[[0, 1], [2, 3], [4, 5], [6, 7]],
                                               ins=[xsh.ap().opt()], outs=[xgh.ap()[cc]])
            ins.then_inc(sem)
            xs.r_.append(tok)
        xg.w = tok
        xg.r_ = []
        self.xg[lnext] = xg

    def layer(self, l):
        self.emit_mod(l)
        if self.debug.get("stop") == "mod":
            return
        self.emit_h1(l)
        if self.debug.get("stop") == "h1":
            return
        if l % 2 == 0:
            self.emit_gqa(l)
        else:
            self.emit_na(l)
        stop = self.debug.get("stop")
        if stop == "mixer":
            return
        self.emit_ln(l, 1)
        if stop == "ln1":
            return
        self.emit_moe(l)
        if stop == "moe":
            return
        self.emit_ln(l, 2)

    def emit_mod(self, l):
        c = self.c
        wmod = self.inp(f"wmod{l}", [D, 6 * D]).rearrange("(k p) n -> p k n", p=P)
        bmod = c.alloc("bmod", 48, F32)
        c.dma("sp", bmod.v(), self.inp(f"bmod{l}", [P, 48]))
        c.dma("sp", self.LNV.v(), self.inp(f"lnv{l}", [P, 32]))
        st = [c.alloc("wmst", KC * 512, BF16) for _ in range(3)]
        scb = c.alloc("scb", 16, BF16)
        c.copy("dve", scb.v(), self.scT.v())
        scb3 = scb.r("p (k r) -> p k r", r=2)
        ps = self.bank(0)
        ps3 = ps[:, 0:96].r("p (m r) -> p m r", r=2)
        for blk in range(12):
            s = st[blk % 3]
            s3 = s.r("p (k n) -> p k n", k=KC)
            c.dma("pool", s3, wmod[:, :, blk * 512:(blk + 1) * 512])
            for fc in range(4):
                for k in range(KC):
                    c.mm(ps3[:, blk * 4 + fc, :], s3[:, k, fc * 128:(fc + 1) * 128], scb3[:, k, :],
                         start=(k == 0), stop=(k == KC - 1))
        M3 = self.MODV3
        for r in range(2):
            c.tt("dve", M3[:, :, r], ps3[:, :, r], bmod.v(), ALU.add)
        c.ts("dve", M3[:, 8:16, :], M3[:, 8:16, :], 1.0, op0=ALU.add)
        c.ts("dve", M3[:, 32:40, :], M3[:, 32:40, :], 1.0, op0=ALU.add)
        c.ts("dve", M3[:, 16:24, :], M3[:, 16:24, :], 1.0 / ALPHA, op0=ALU.mult)
        c.ts("dve", M3[:, 40:48, :], M3[:, 40:48, :], 1.0 / ALPHA, op0=ALU.mult)
        for r in range(2):
            c.tt("dve", self.A23[:, :, r], self.LNV3[:, 0, :], M3[:, 32:40, r], ALU.mult)
            c.tt("dve", self.B23[:, :, r], self.LNV3[:, 1, :], M3[:, 32:40, r], ALU.mult)
            c.tt("dve", self.B23[:, :, r], self.B23[:, :, r], M3[:, 24:32, r], ALU.add)
        c.free(bmod, scb, *st)

    def mod_h(self, eng, out, in_, cc, r):
        self.c.ts(eng, out, in_, self.MODV3[:, 8 + cc, r:r + 1], self.MODV3[:, cc, r:r + 1], ALU.mult, ALU.add)

    def emit_h1(self, l):
        for cc in range(KC):
            eng = ("dve", "pool")[cc % 2]
            self.mod_h(eng, self.HB3[:, cc, 0:NLAT], self.XT3[:, cc, 0:NLAT], cc, 0)
            self.mod_h(eng, self.HB3[:, cc, NLAT:NT], self.XT3[:, cc, NLAT:NT], cc, 1)

    def xg_ap(self, l):
        if l in self.xg:
            return self.xg[l]
        ap = self.inp(f"xg{l}", [KC, 2 * P, NT])
        self.xg[l] = T(ap, f"xg{l}", "dram")
        return self.xg[l]

    def gstream(self, l, groups):
        c = self.c
        xg = self.xg_ap(l)
        st = [c.alloc("xost", KC * 256, F32) for _ in range(3)]
        ho = [c.alloc("xoh", KC * 256, BF16) for _ in range(2)]
        for i, (slot, c0, n, r) in enumerate(groups):
            s3 = st[i % 3].r("p (c t) -> p c t", c=KC)[:, :, 0:n]
            h3 = ho[i % 2].r("p (c t) -> p c t", c=KC)[:, :, 0:n]
            c.dma("sp", s3, V(xg, xg.h[:, slot * P:(slot + 1) * P, c0:c0 + n].rearrange("c p t -> p c t")))
            for cc in range(KC):
                self.mod_h(("dve", "pool")[cc % 2], h3[:, cc, :], s3[:, cc, :], cc, r)
            yield (slot, c0, n, r, h3)
        c.free(*st, *ho)

    def load_w(self, name, dram_ap, ncols):
        c = self.c
        t = c.alloc(name, KC * ncols, BF16)
        t3 = t.r("p (k n) -> p k n", k=KC)
        c.dma("pool", t3, dram_ap.rearrange("(k p) n -> p k n", p=P))
        return t, t3

    def outproj(self, oT, wo, col0, n, r):
        c = self.c
        oTs = oT if isinstance(oT, list) else [oT]
        wos = wo if isinstance(wo, list) else [wo]
        for cc in range(KC):
            y = self.bank(6 + self.rr("yb", 2))[:, 0:n]
            for i, (o, w) in enumerate(zip(oTs, wos)):
                c.mm(y, w[:, cc * 128:(cc + 1) * 128], o, start=(i == 0), stop=(i == len(oTs) - 1))
            xs = self.XT3[:, cc, col0:col0 + n]
            c.stt(xs, y, self.MODV3[:, 16 + cc, r:r + 1], xs, ALU.mult, ALU.add)

    def rmsnorm_rope(self, ps, n, gain, out, rope_col0=None, table=None):
        c = self.c
        k = self.rr("rn", 2)
        tm = self.rn_tmp[k]
        sq = tm["sq"][:, 0:n]
        rstd = tm["rstd"][:, 0:n]
        c.act(sq, ps, AF.Square)
        ss = self.bank(2 + k)[:, 0:n]
        c.mm(ss, self.ones_rms.v(), sq)
        c.act(rstd, ss, AF.Ln, bias=RMS_EPS)
        c.act(rstd, rstd, AF.Exp, scale=-0.5)
        if rope_col0 is None:
            c.stt(out, ps, gain, rstd, ALU.mult, ALU.mult)
            return
        qn = tm["qn"][:, 0:n]
        c.stt(qn, ps, gain, rstd, ALU.mult, ALU.mult)
        rp = tm["rope"].r("p (a n) -> p a n", a=2)[:, :, 0:n]
        c.dma("sp", rp, (table if table is not None else self.rope_dram)[:, :, rope_col0:rope_col0 + n])
        pp = self.bank(4 + k)[:, 0:n]
        c.mm(pp, self.perm.v(), qn)
        t1 = tm["t1"][:, 0:n]
        t2 = tm["t2"][:, 0:n]
        c.tt("pool", t1, qn, rp[:, 0, :], ALU.mult)
        c.tt("dve", t2, pp, rp[:, 1, :], ALU.mult)
        c.tt("pool", out, t1, t2, ALU.add)

    def attn_T(self, KT, VV3, qv, n, tiles):
        c = self.c
        O = self.bank(2)[:, 0:n]
        L = self.bank(3)[:, 0:n]
        nt = len(tiles)
        sbanks = (0, 1, 4, 5)
        LA = 3

        def S(i):
            s = self.bank(sbanks[self.rr("sb", 4)])[:, 0:n]
            kt = tiles[i]
            c.mm(s, KT[:, kt * 128:(kt + 1) * 128], qv)
            return s
        q = [S(i) for i in range(min(LA, nt))]
        for i in range(nt):
            s_cur = q.pop(0)
            if i + LA < nt:
                q.append(S(i + LA))
            pt = self.pt_tmp[self.rr("pt", len(self.pt_tmp))][:, 0:n]
            c.act(pt, s_cur, AF.Exp, scale=SCALE)
            c.mm(O, VV3[:, tiles[i], :], pt, start=(i == 0), stop=(i == nt - 1))
            c.mm(L, self.ones1.v(), pt, start=(i == 0), stop=(i == nt - 1))
        kk = self.rr("ot", 2)
        rl = self.rl_tmp[kk][:, 0:n]
        c.act(rl, L, AF.Ln)
        c.act(rl, rl, AF.Exp, scale=-1.0)
        oT = self.ot_tmp[kk][:, 0:n]
        c.tt("dve", oT, O, rl, ALU.mult)
        return oT

    def emit_gqa(self, l):
        c = self.c
        j = l // 2
        win = self.inp(f"win{l}", [D, 1536])
        wout = self.inp(f"wout{l}", [D, D])
        g_own = [(i * 256, 256, 0) for i in range(8)] + [(NLAT, 128, 1)]
        wf, wf3 = self.load_w("wf", win[:, 0:256], 256)
        cs2 = c.alloc("cs2", 256, BF16)
        c.dma("pool", cs2.v(), self.inp("cs2", [P, 256]))
        wfn = c.alloc("wfn", 256, BF16)
        wfn3 = wfn.r("p (j m) -> p j m", j=2)
        c.dma("pool", wfn.v(), self.inp(f"wfn{l}", [P, 256]))
        wo_f = c.alloc("wo_f", 2 * D, BF16)
        wo_f3 = wo_f.r("p (j n) -> p j n", j=2)
        c.dma("pool", wo_f3, wout[0:256, :].rearrange("(j p) n -> p j n", p=P))
        cdft = c.alloc("cdft", 512, BF16)
        cdft4 = cdft.r("p (a t k) -> p a t k", a=2, t=2)
        c.dma("pool", cdft.v(), self.inp("cdft", [P, 512]))
        FCS = c.alloc("FCS", 34 * 512, BF16)
        FCS3 = FCS.r("p (t f) -> p t f", f=512)
        fts = [c.alloc("fts", 512, BF16) for _ in range(2)]

        def stage1(buf0, n, h3):
            fps = self.bank(self.rr("fps", 2))
            fps3 = fps[:, 0:2 * n].r("p (j t) -> p j t", j=2)
            for jj in range(2):
                for k in range(KC):
                    c.mm(fps3[:, jj, :], wf3[:, k, jj * 128:(jj + 1) * 128], h3[:, k, :], start=(k == 0), stop=(k == KC - 1))
            ft = fts[self.rr("fts", 2)]
            ft3 = ft[:, 0:2 * n].r("p (j t) -> p j t", j=2)
            c.copy("act", ft3, fps3)
            for tt in range(n // 128):
                cps = self.bank(2 + self.rr("cps", 2))
                for jj in range(2):
                    c.mm(cps[:, jj * 256:(jj + 1) * 256], ft3[:, jj, tt * 128:(tt + 1) * 128], cs2.v())
                c.copy("dve", FCS3[:, (buf0 + tt * 128) // 128, :], cps.v())

        g_all = [(slot, c0, n, r) for slot in range(2) for (c0, n, r) in g_own]
        for (slot, c0, n, r, h3) in self.gstream(l, g_all):
            stage1(slot * NT + c0, n, h3)
        c.free(wf, cs2, *fts)

        dft = self.inp("dft", [4, 8, P, 4096], BF16)
        dst = [c.alloc("dftst", 4096, BF16) for _ in range(2)]
        frs = [c.alloc("frs", 512, BF16) for _ in range(2)]
        ots = [c.alloc("ofs", 512, BF16) for _ in range(2)]

        def finish(acc, n, col0, r):
            oTs = []
            for jj in range(2):
                fr = frs[jj][:, 0:n]
                c.copy("act", fr, acc[jj])
                wps = self.bank(6 + self.rr("yb", 2))[:, 0:n]
                c.mm(wps, wfn3[:, jj, :], fr)
                o = ots[jj][:, 0:n]
                c.copy("act", o, wps)
                oTs.append(o)
            self.outproj(oTs, [wo_f3[:, 0, :], wo_f3[:, 1, :]], col0, n, r)

        for kc in range(4):
            acc = [self.bank(4)[:, 0:512], self.bank(5)[:, 0:512]]
            for ng in range(8):
                dt_ = dst[self.rr("dst", 2)]
                c.dma("sp", dt_.v(), dft[kc, ng])
                dt4 = dt_.r("p (a i k) -> p a i k", a=2, i=4)
                for ni in range(4):
                    nn = ng * 4 + ni
                    ft_i = nn if nn < 16 else nn + 1
                    for jj in range(2):
                        c.mm(acc[jj], FCS3[:, ft_i, jj * 256:jj * 256 + 128], dt4[:, 0, ni, :], start=(nn == 0), stop=False)
                        c.mm(acc[jj], FCS3[:, ft_i, jj * 256 + 128:(jj + 1) * 256], dt4[:, 1, ni, :], start=False, stop=(nn == 31),
                             inc=(nn == 31 or (ni == 3 and jj == 1)))
            finish(acc, 512, kc * 512, 0)
        acc = [self.bank(4)[:, 0:128], self.bank(5)[:, 0:128]]
        for ti, ft_i in enumerate((16, 33)):
            for jj in range(2):
                c.mm(acc[jj], FCS3[:, ft_i, jj * 256:jj * 256 + 128], cdft4[:, 0, ti, :], start=(ti == 0), stop=False)
                c.mm(acc[jj], FCS3[:, ft_i, jj * 256 + 128:(jj + 1) * 256], cdft4[:, 1, ti, :], start=False, stop=(ti == 1))
        finish(acc, 128, NLAT, 1)
        c.free(FCS, wfn, wo_f, cdft, *dst, *frs, *ots)
        if self.debug.get("stop") == "fnet":
            return

        self.rope_dram = self.inp("rope", [P, 2 * 4096], BF16).rearrange("p (a n) -> p a n", a=2)
        ropeq = self.inp("ropeq", [P, 2 * NLAT], BF16).rearrange("p (a n) -> p a n", a=2)
        self.perm = c.alloc("perm", P, BF16)
        c.dma("pool", self.perm.v(), self.inp("perm", [P, P]))
        qkn = c.alloc("qkn", 2, F32)
        c.dma("sp", qkn.v(), self.inp(f"qkn{l}", [P, 2]))
        self.rn_tmp = [dict(sq=c.alloc("sq", 256, BF16), rstd=c.alloc("rstd", 256, F32), qn=c.alloc("qn", 256, BF16),
                            rope=c.alloc("rope", 512, BF16), t1=c.alloc("t1", 256, F32), t2=c.alloc("t2", 256, F32))
                       for _ in range(2)]
        self.pt_tmp = [c.alloc("pt", 512, BF16) for _ in range(5)]
        self.rl_tmp = [c.alloc("rl", 512, F32) for _ in range(2)]
        self.ot_tmp = [c.alloc("ot", 512, BF16) for _ in range(2)]
        KTt = c.alloc("KT", 2 * NT, BF16)
        KT = KTt.v()
        VVt = c.alloc("VV", 34 * 128, BF16)
        VV3 = VVt.r("p (t d) -> p t d", d=128)
        QTt = [c.alloc("QT", NT, BF16) for _ in range(2)]
        for g in range(2):
            wk, wk3 = self.load_w("wk", win[:, 1024 + g * 128:1024 + (g + 1) * 128], 128)
            wv, wv3 = self.load_w("wv", win[:, 1280 + g * 128:1280 + (g + 1) * 128], 128)

            def kv(buf0, n, r, h3, rope0):
                kps = self.bank(self.rr("kps", 2))[:, 0:n]
                for k in range(KC):
                    c.mm(kps, wk3[:, k, :], h3[:, k, :], start=(k == 0), stop=(k == KC - 1))
                self.rmsnorm_rope(kps, n, qkn[:, 1:2], KT[:, buf0:buf0 + n], rope0)
                for tt in range(n // 128):
                    vps = self.bank(6 + self.rr("yb", 2))[:, 0:128]
                    for k in range(KC):
                        c.mm(vps, h3[:, k, tt * 128:(tt + 1) * 128], wv3[:, k, :], start=(k == 0), stop=(k == KC - 1))
                    c.copy("act", VV3[:, buf0 // 128 + tt, :], vps)

            for (slot, c0, n, r, h3) in self.gstream(l, g_all):
                kv(slot * NT + c0, n, r, h3, (slot * NLAT + c0) if r == 0 else None)
            c.free(wk, wv)
            for hh in range(3):
                hg = g * 3 + hh
                wq, wq3 = self.load_w("wq", win[:, 256 + hg * 128:256 + (hg + 1) * 128], 128)
                wo = c.alloc("wo", D, BF16)
                c.dma("pool", wo.v(), wout[(2 + hg) * 128:(3 + hg) * 128, :])
                QT = QTt[self.rr("qt", 2)].v()
                for (c0, n, r) in g_own:
                    qps = self.bank(self.rr("kps", 2))[:, 0:n]
                    for k in range(KC):
                        c.mm(qps, wq3[:, k, :], self.HB3[:, k, c0:c0 + n], start=(k == 0), stop=(k == KC - 1))
                    self.rmsnorm_rope(qps, n, qkn[:, 0:1], QT[:, c0:c0 + n], c0 if r == 0 else None, table=ropeq)
                for qc in range(4):
                    oT = self.attn_T(KT, VV3, QT[:, qc * 512:(qc + 1) * 512], 512, list(range(34)))
                    self.outproj(oT, wo.v(), qc * 512, 512, 0)
                oT = self.attn_T(KT, VV3, QT[:, NLAT:NT], 128, [16, 33])
                self.outproj(oT, wo.v(), NLAT, 128, 1)
                c.free(wq, wo)
        c.free(KTt, VVt, *QTt, self.perm, qkn, *self.pt_tmp, *self.rl_tmp, *self.ot_tmp)
        for tm in self.rn_tmp:
            c.free(*tm.values())

    def emit_na(self, l):
        c = self.c
        win = self.inp(f"win{l}", [D, 3 * D])
        wout = self.inp(f"wout{l}", [D, D])
        nab = self.inp(f"nab{l}", [8, P, 5 * 768])
        HOt = c.alloc("HO", KC * 768, BF16)
        HO3 = HOt.r("p (c t) -> p c t", c=KC)
        hgroups = [(0, NLAT - 256, 256, 0), (1, 0, 256, 0), (0, NLAT, 128, 1), (1, NLAT, 128, 1)]
        hdst = [0, 256, 512, 640]
        for gi_, (slot, c0, n, r, h3) in enumerate(self.gstream(l, hgroups)):
            for cc in range(KC):
                c.copy(("dve", "pool")[cc % 2], HO3[:, cc, hdst[gi_]:hdst[gi_] + n], h3[:, cc, :])
        NK = 2816
        srcs = [(0, 256, HO3[:, :, 0:256])] + \
               [(256 + i * 512, 512, self.HB3[:, :, i * 512:(i + 1) * 512]) for i in range(4)] + \
               [(2304, 256, HO3[:, :, 256:512]), (2560, 256, HO3[:, :, 512:768])]
        KTs = [c.alloc("KT", NK, BF16) for _ in range(2)]
        VVs = [c.alloc("VV", NK, BF16) for _ in range(2)]
        QTs = [c.alloc("QT", NT, BF16) for _ in range(2)]
        nbs = [c.alloc("nb", 5 * 768, F32) for _ in range(1)]
        ss_tmp = [c.alloc("ss", 768, F32) for _ in range(2)]
        pt_tmp = [c.alloc("pt", 1024, BF16) for _ in range(3)]
        rl_tmp = [c.alloc("rl", 512, F32) for _ in range(2)]
        og_tmp = [c.alloc("og", 512, BF16) for _ in range(2)]

        for h in range(8):
            wq, wq3 = self.load_w("wq", win[:, h * 128:(h + 1) * 128], 128)
            wk, wk3 = self.load_w("wk", win[:, D + h * 128:D + (h + 1) * 128], 128)
            wv, wv3 = self.load_w("wv", win[:, 2 * D + h * 128:2 * D + (h + 1) * 128], 128)
            wo = c.alloc("wo", D, BF16)
            c.dma("pool", wo.v(), wout[h * 128:(h + 1) * 128, :])
            nb = nbs[0]
            c.dma("sp", nb.v(), nab[h])
            nb3 = nb.r("p (t w) -> p t w", w=768)
            KT = KTs[h % 2].v()
            VV3 = VVs[h % 2].r("p (t d) -> p t d", d=128)
            QT = QTs[h % 2].v()
            for (b0, n, h3) in srcs:
                kps = self.bank(6 + self.rr("yb", 2))[:, 0:n]
                for k in range(KC):
                    c.mm(kps, wk3[:, k, :], h3[:, k, :], start=(k == 0), stop=(k == KC - 1))
                c.copy("act", KT[:, b0:b0 + n], kps)
                for tt in range(n // 128):
                    vps = self.bank(6 + self.rr("yb", 2))[:, 0:128]
                    for k in range(KC):
                        c.mm(vps, h3[:, k, tt * 128:(tt + 1) * 128], wv3[:, k, :], start=(k == 0), stop=(k == KC - 1))
                    c.copy("dve", VV3[:, b0 // 128 + tt, :], vps)
            for (c0, n) in [(i * 512, 512) for i in range(4)] + [(NLAT, 128)]:
                qps = self.bank(6 + self.rr("yb", 2))[:, 0:n]
                for k in range(KC):
                    c.mm(qps, wq3[:, k, :], self.HB3[:, k, c0:c0 + n], start=(k == 0), stop=(k == KC - 1))
                c.copy("act", QT[:, c0:c0 + n], qps)
            jobs = []
            for bi in range(16):
                ty = {0: 0, 1: 1, 14: 3, 15: 4}.get(bi, 2)
                wb = 2 * bi if bi <= 13 else 28
                t0_ = wb * 64 // 128
                jobs.append((bi * 128, [t0_ + m for m in range(6)] + [20, 21], ty))
            jobs.append((NLAT, [20, 21], None))

            def S_of(job):
                q0, tiles, ty = job
                k = self.rr("nas", 2)
                SA = self.bank(2 * k)
                SB = self.bank(2 * k + 1)
                for m, kt in enumerate(tiles):
                    dst = (SA if m < 4 else SB)[:, (m % 4) * 128:(m % 4 + 1) * 128]
                    c.mm(dst, KT[:, kt * 128:(kt + 1) * 128], QT[:, q0:q0 + 128])
                return SA, SB

            def P_of(job, S):
                q0, tiles, ty = job
                SA, SB = S
                PT = pt_tmp[self.rr("napt", 3)]
                if ty is not None:
                    SS = ss_tmp[self.rr("nass", 2)]
                    c.stt(SS[:, 0:512], SA[:, 0:512], SCALE, nb3[:, ty, 0:512], ALU.mult, ALU.add)
                    c.stt(SS[:, 512:768], SB[:, 0:256], SCALE, nb3[:, ty, 512:768], ALU.mult, ALU.add)
                    c.act(PT[:, 0:768], SS[:, 0:768], AF.Exp)
                    c.act(PT[:, 768:1024], SB[:, 256:512], AF.Exp, scale=SCALE)
                else:
                    c.act(PT[:, 0:256], SA[:, 0:256], AF.Exp, scale=SCALE)
                return PT

            def PV_of(job, PT, O_dst, L_dst):
                q0, tiles, ty = job
                nt = len(tiles)
                for m, kt in enumerate(tiles):
                    c.mm(O_dst, VV3[:, kt, :], PT[:, m * 128:(m + 1) * 128], start=(m == 0), stop=(m == nt - 1))
                for m, kt in enumerate(tiles):
                    c.mm(L_dst, self.ones1.v(), PT[:, m * 128:(m + 1) * 128], start=(m == 0), stop=(m == nt - 1))

            def finish_group(gjobs):
                n = 128 * len(gjobs)
                col0 = gjobs[0][0]
                r = 0 if gjobs[0][2] is not None else 1
                kk = self.rr("narl", 2)
                rl = rl_tmp[kk][:, 0:n]
                c.act(rl, self.bank(5)[:, 0:n], AF.Ln)
                c.act(rl, rl, AF.Exp, scale=-1.0)
                og = og_tmp[kk][:, 0:n]
                c.tt("dve", og, self.bank(4)[:, 0:n], rl, ALU.mult)
                self.outproj(og, wo.v(), col0, n, r)

            S_next = S_of(jobs[0])
            gjobs = []
            for ji, job in enumerate(jobs):
                S_cur = S_next
                if ji + 1 < len(jobs):
                    S_next = S_of(jobs[ji + 1])
                PT = P_of(job, S_cur)
                slot = len(gjobs)
                PV_of(job, PT, self.bank(4)[:, slot * 128:(slot + 1) * 128], self.bank(5)[:, slot * 128:(slot + 1) * 128])
                gjobs.append(job)
                if len(gjobs) == 4 or ji == len(jobs) - 1 or jobs[ji + 1][2] is None:
                    finish_group(gjobs)
                    gjobs = []
            c.free(wq, wk, wv, wo)
        c.free(HOt, *KTs, *VVs, *QTs, *nbs, *ss_tmp, *pt_tmp, *rl_tmp, *og_tmp)

    def emit_ln(self, l, which):
        c = self.c
        gi, bi_ = (0, 1) if which == 1 else (2, 3)
        eps = LN_EPS / (ALPHA * ALPHA)
        zb = [c.alloc("zb", KC * 512, BF16) for _ in range(2)]
        zq = [c.alloc("zq", KC * 512, BF16) for _ in range(2)]
        tmp = [dict(M=c.alloc("M", 512, F32), m2=c.alloc("m2", 512, F32), rstd=c.alloc("rstdl", 512, F32)) for _ in range(2)]
        t1s = [c.alloc("t1", 512, F32) for _ in range(3)]
        if which == 1:
            h2f = [c.alloc("h2f", KC * 512, F32) for _ in range(2)]
            wr = c.alloc("wr", KC * 64, F32)
            wr3 = wr.r("p (k e) -> p k e", k=KC)
            c.dma("sp", wr.v(), self.inp(f"wr{l}", [P, KC * 64]))
            mb = c.alloc("mb", 64, F32)
            c.dma("sp", mb.v(), self.inp(f"mb{l}", [P, 64]))
            gt = [c.alloc("gt", 64 * 3 + 16, F32) for _ in range(2)]
            GTS = c.alloc("GTS", NT, F32)
            c.memset("dve", self.G3[:, :, 64:65], 1.0)
            self.GTD = c.dram(f"gtd{l}", [NEXP, NT], F32)
        groups = [(i * 512, 512, 0) for i in range(4)] + [(NLAT, 128, 1)]
        for gidx, (c0, n, r) in enumerate(groups):
            k = gidx % 2
            zb3 = zb[k].r("p (c t) -> p c t", c=KC)[:, :, 0:n]
            zq3 = zq[k].r("p (c t) -> p c t", c=KC)[:, :, 0:n]
            mean = self.bank(0)[:, 0:n]
            msq = self.bank(1)[:, 0:n]
            for cc in range(KC):
                xs = self.XT3[:, cc, c0:c0 + n]
                c.copy("dve", zb3[:, cc, :], xs)
                c.act(zq3[:, cc, :], xs, AF.Square)
            for cc in range(KC):
                c.mm(mean, self.ones_mean.v(), zb3[:, cc, :], start=(cc == 0), stop=(cc == KC - 1))
            for cc in range(KC):
                c.mm(msq, self.ones_mean.v(), zq3[:, cc, :], start=(cc == 0), stop=(cc == KC - 1))
            M = tmp[k]["M"][:, 0:n]
            m2 = tmp[k]["m2"][:, 0:n]
            rstd = tmp[k]["rstd"][:, 0:n]
            c.copy("act", M, mean)
            c.act(m2, mean, AF.Square)
            c.tt("dve", m2, msq, m2, ALU.subtract)
            c.act(rstd, m2, AF.Ln, bias=eps)
            c.act(rstd, rstd, AF.Exp, scale=-0.5)
            if which == 1:
                h2f3 = h2f[k].r("p (c t) -> p c t", c=KC)[:, :, 0:n]
            for cc in range(KC):
                xs = self.XT3[:, cc, c0:c0 + n]
                t1 = t1s[self.rr("t1", 3)][:, 0:n]
                c.tt("dve", t1, xs, M, ALU.subtract)
                c.tt("pool", t1, t1, rstd, ALU.mult)
                c.act(xs, t1, AF.Identity, scale=self.LNV3[:, gi, cc:cc + 1], bias=self.LNV3[:, bi_, cc:cc + 1])
                if which == 1:
                    c.act(h2f3[:, cc, :], t1, AF.Identity, scale=self.A23[:, cc, r:r + 1], bias=self.B23[:, cc, r:r + 1])
                    c.copy("dve", self.HB3[:, cc, c0:c0 + n], h2f3[:, cc, :])
            if which == 1:
                for tt in range(n // 128):
                    tile_i = c0 // 128 + tt
                    lg = self.bank(2 + self.rr("lg", 2))[:, 0:64]
                    for cc in range(KC):
                        c.mm(lg, h2f3[:, cc, tt * 128:(tt + 1) * 128], wr3[:, cc, :], start=(cc == 0), stop=(cc == KC - 1))
                    g_ = gt[self.rr("gt", 2)]
                    sc, sel, wsel, m8, den = g_[:, 0:64], g_[:, 64:128], g_[:, 128:192], g_[:, 192:200], g_[:, 200:201]
                    rden = g_[:, 201:202]
                    c.act(sc, lg, AF.Sigmoid)
                    c.tt("dve", sel, sc, mb.v(), ALU.add)
                    c.op("dve", lambda e: e.max(out=m8.ap, in_=sel.ap), reads=[sel], writes=[m8])
                    c.ts("dve", sel, sel, m8[:, 7:8], op0=ALU.is_ge)
                    c.tt("dve", wsel, sel, sc, ALU.mult)
                    c.op("dve", lambda e: e.reduce_sum(out=den.ap, in_=wsel.ap, axis=AX.X), reads=[wsel], writes=[den])
                    c.op("dve", lambda e: e.reciprocal(out=rden.ap, in_=den.ap), reads=[den], writes=[rden])
                    c.ts("dve", self.G3[:, tile_i, 0:64], wsel, rden, ROUTE_SCALE, ALU.mult, ALU.mult)
                    gtp = self.bank(4 + self.rr("gtp", 2))[0:NEXP, 0:128]
                    c.transpose(gtp, self.G3[:, tile_i, :], self.identf.v())
                    c.copy("act", GTS[0:NEXP, tile_i * 128:(tile_i + 1) * 128], gtp)
        if which == 1:
            c.dma("sp", self.GTD.v(), GTS[0:NEXP, :])
            c.free(*h2f, wr, mb, *gt, GTS)
        c.free(*zb, *zq, *t1s)
        for t_ in tmp:
            c.free(*t_.values())

    def emit_moe(self, l):
        c = self.c
        moew = self.inp(f"moew{l}", [NEXP, P, 6144])
        NWB = 4
        WB = [c.alloc("WB", 6144, BF16) for _ in range(NWB)]
        GB = [c.alloc("GB", NT, F32) for _ in range(NWB)]
        sT = [c.alloc("sT", 512, BF16) for _ in range(2)]
        tT = [c.alloc("tT", 512, BF16) for _ in range(2)]
        aT = [c.alloc("aT", 512, BF16) for _ in range(4)]
        groups = [(i * 256, 256, 0) for i in range(8)] + [(NLAT, 128, 1)]
        pairs = [(2 * i, 2 * i + 1) for i in range(32)] + [(64,)]

        def prefetch(e):
            w = WB[e % NWB]
            for q in range(3):
                c.dma("pool", w[:, q * 2048:(q + 1) * 2048], moew[e][:, q * 2048:(q + 1) * 2048])
            c.dma("sp", GB[e % NWB].r("p (o t) -> p o t", o=1), V(self.GTD, self.GTD.h[e:e + 1, :].partition_broadcast(P)))

        def down_mm(sts):
            e0, c0, n, r, _ = sts[0]
            Y = [self.bank(4 + cc // 2)[:, (cc % 2) * 256:(cc % 2) * 256 + n] for cc in range(KC)]
            nmm = 2 * len(sts)
            for cc in range(KC):
                i = 0
                for (e, _, _, _, a3) in sts:
                    w = WB[e % NWB].v()
                    for jj in range(2):
                        c.mm(Y[cc], w[:, 4096 + jj * 1024 + cc * 128:4096 + jj * 1024 + (cc + 1) * 128], a3[:, jj, :],
                             start=(i == 0), stop=(i == nmm - 1))
                        i += 1

        def down_acc(sts):
            e0, c0, n, r, _ = sts[0]
            Y = [self.bank(4 + cc // 2)[:, (cc % 2) * 256:(cc % 2) * 256 + n] for cc in range(KC)]
            for cc in range(KC):
                xs = self.XT3[:, cc, c0:c0 + n]
                c.stt(xs, Y[cc], self.MODV3[:, 40 + cc, r:r + 1], xs, ALU.mult, ALU.add)

        for e in pairs[0]:
            prefetch(e)
        pend = []
        ready = None
        it = 0
        for pi, pr in enumerate(pairs):
            for gidx, (c0, n, r) in enumerate(groups):
                for ei, e in enumerate(pr):
                    w = WB[e % NWB].v()
                    gb = GB[e % NWB].v()
                    k = it % 2
                    gps = self.bank(2 * k)
                    ups = self.bank(2 * k + 1)
                    g3 = gps[:, 0:2 * n].r("p (j t) -> p j t", j=2)
                    u3 = ups[:, 0:2 * n].r("p (j t) -> p j t", j=2)
                    for jj in range(2):
                        for kk in range(KC):
                            c.mm(g3[:, jj, :], w[:, kk * 256 + jj * 128:kk * 256 + (jj + 1) * 128], self.HB3[:, kk, c0:c0 + n],
                                 start=(kk == 0), stop=(kk == KC - 1))
                    for jj in range(2):
                        for kk in range(KC):
                            c.mm(u3[:, jj, :], w[:, 2048 + kk * 256 + jj * 128:2048 + kk * 256 + (jj + 1) * 128],
                                 self.HB3[:, kk, c0:c0 + n], start=(kk == 0), stop=(kk == KC - 1))
                    did = None
                    if ei == 0 and ready is not None:
                        down_mm(ready)
                        did = ready
                        ready = None
                        if gidx == 0 and pi + 1 < len(pairs):
                            for e2 in pairs[pi + 1]:
                                prefetch(e2)
                    elif ei == 0 and gidx == 0 and pi == 0:
                        for e2 in pairs[1]:
                            prefetch(e2)
                    s3 = sT[k][:, 0:2 * n].r("p (j t) -> p j t", j=2)
                    t3 = tT[k][:, 0:2 * n].r("p (j t) -> p j t", j=2)
                    a3 = aT[it % 4][:, 0:2 * n].r("p (j t) -> p j t", j=2)
                    c.act(s3, g3, AF.Silu)
                    for jj in range(2):
                        c.tt("dve", t3[:, jj, :], u3[:, jj, :], gb[:, c0:c0 + n], ALU.mult)
                    c.tt("pool", a3, s3, t3, ALU.mult)
                    if did is not None:
                        down_acc(did)
                    pend.append((e, c0, n, r, a3))
                    if ei == len(pr) - 1:
                        ready = pend
                        pend = []
                    it += 1
        down_mm(ready)
        down_acc(ready)
        c.free(*WB, *GB, *sT, *tT, *aT)


BF = ml_dtypes.bfloat16


def _fm(tok):
    T_ = tok.shape[0]
    return np.ascontiguousarray(tok.T.reshape(KC, P, T_).transpose(1, 0, 2)).reshape(P, KC * T_)


def _unfm(a, T_):
    return np.ascontiguousarray(a.reshape(P, KC, T_).transpose(1, 0, 2).reshape(D, T_).T)


_CONST_CACHE = {}


def _consts(s):
    if s in _CONST_CACHE:
        return _CONST_CACHE[s]
    out = {}
    out["ident"] = np.eye(P, dtype=np.float32)
    d = np.arange(P)
    i = d % 64
    partner = np.where(i < 32, d + 32, d - 32)
    perm = np.zeros((P, P), np.float32)
    perm[partner, d] = 1.0
    out["perm"] = perm
    pos = np.arange(4096)
    inv = 10000.0 ** (-(2.0 / 64) * np.arange(32, dtype=np.float64))
    pv = np.where((d // 64)[:, None] == 0, (pos // 64)[None, :], (pos % 64)[None, :]).astype(np.float64)
    ang = pv * inv[i % 32][:, None]
    cos = np.cos(ang)
    sin = np.sin(ang) * np.where(i < 32, -1.0, 1.0)[:, None]
    rope = np.stack([cos, sin], axis=1)
    out["rope"] = rope.reshape(P, 2 * 4096).astype(BF)
    out["ropeq"] = np.ascontiguousarray(rope[:, :, s * NLAT:(s + 1) * NLAT]).reshape(P, 2 * NLAT).astype(BF)
    cc = np.arange(64)
    c64 = np.cos(2 * np.pi * np.outer(cc, cc) / 64) / 8.0
    s64 = np.sin(2 * np.pi * np.outer(cc, cc) / 64) / 8.0
    cs2 = np.zeros((P, 256), np.float32)
    for g2 in range(2):
        cs2[g2 * 64:(g2 + 1) * 64, g2 * 64:(g2 + 1) * 64] = c64
        cs2[g2 * 64:(g2 + 1) * 64, 128 + g2 * 64:128 + (g2 + 1) * 64] = s64
    out["cs2"] = cs2
    kk = s * NLAT + np.arange(NLAT)
    ph = (np.outer(pos, kk) % 4096).astype(np.float64) * (2 * np.pi / 4096)
    tab = np.stack([np.cos(ph) / 64.0, -np.sin(ph) / 64.0], axis=0).astype(np.float32)
    tab = tab.reshape(2, 8, 4, P, 4, 512)
    out["dft"] = np.ascontiguousarray(tab.transpose(4, 1, 3, 0, 2, 5)).reshape(4, 8, P, 4096).astype(BF)
    cpos = np.stack([np.arange(128), 128 + np.arange(128)], axis=0)
    ck = s * 128 + np.arange(128)
    cph = (cpos[:, :, None] * ck[None, None, :] % 256) * (2 * np.pi / 256)
    ctab = np.stack([np.cos(cph) / 16.0, -np.sin(cph) / 16.0], axis=0)
    out["cdft"] = np.ascontiguousarray(ctab.transpose(2, 0, 1, 3)).reshape(P, 512).astype(np.float32)
    _CONST_CACHE[s] = out
    return out


def _na_bias(rpb, s):
    out = np.full((8, P, 5, 768), NEG, np.float32)
    q = np.arange(P)
    w = np.arange(768)
    qcol = q % 64
    kcol = w % 64
    cstart = np.clip(qcol - 8, 0, 48)
    okc = (kcol[None, :] >= cstart[:, None]) & (kcol[None, :] < cstart[:, None] + 16)
    dc = kcol[None, :] - qcol[:, None] + 15
    rep = {0: 0, 1: 1, 2: 2, 3: 14, 4: 15}
    for ty, bi in rep.items():
        i = 16 * s + bi
        wb = 2 * bi if bi <= 13 else 28
        qrow = 2 * i + q // 64
        rstart = np.clip(qrow - 4, 0, 56)
        krow = wb + (32 * s - 4) + w // 64
        okr = (krow[None, :] >= rstart[:, None]) & (krow[None, :] < rstart[:, None] + 8)
        dr = krow[None, :] - qrow[:, None] + 7
        ok = okr & okc
        drc = np.clip(dr, 0, 14)
        dcc = np.clip(dc, 0, 30)
        vals = rpb[:, drc, dcc]
        out[:, :, ty, :] = np.where(ok[None], vals, NEG)
    outT = out.reshape(8, P, 5, 6, P).transpose(0, 4, 2, 3, 1)
    return np.ascontiguousarray(outT).reshape(8, P, 5 * 768)


def _layer_weights(l, inp, tag):
    j = l // 2
    w = {}
    w[f"wmod{tag}"] = inp["w_mod"][l]
    w[f"bmod{tag}"] = np.ascontiguousarray(inp["b_mod"][l].reshape(48, P).T)
    w[f"lnv{tag}"] = np.ascontiguousarray(
        np.stack([inp[k][l].reshape(KC, P).T for k in ("ln1_g", "ln1_b", "ln2_g", "ln2_b")], axis=1)).reshape(P, 32)
    if l % 2 == 0:
        w[f"win{tag}"] = inp["ab_w_in"][j]
        w[f"wout{tag}"] = inp["ab_w_out"][j]
        wf = inp["ab_w_fnet"][j]
        wfn = np.zeros((P, 2, P), np.float32)
        for jj in range(2):
            wfn[0:64, jj, 0:64] = wf[2 * jj]
            wfn[64:128, jj, 64:128] = wf[2 * jj + 1]
        w[f"wfn{tag}"] = wfn.reshape(P, 256)
        w[f"qkn{tag}"] = np.ascontiguousarray(np.stack([inp["ab_q_norm"][j], inp["ab_k_norm"][j]], axis=1))
    else:
        w[f"win{tag}"] = inp["na_w_in"][j]
        w[f"wout{tag}"] = inp["na_w_out"][j]
    w[f"wr{tag}"] = np.ascontiguousarray(inp["moe_w_router"][l].reshape(KC, P, 64).transpose(1, 0, 2)).reshape(P, KC * 64)
    w[f"mb{tag}"] = np.ascontiguousarray(np.broadcast_to(inp["moe_bias"][l][None, :], (P, 64)))
    wg = np.concatenate([inp["moe_w_gate"][l], inp["sh_w_gate"][l][None]], axis=0)
    wu = np.concatenate([inp["moe_w_up"][l], inp["sh_w_up"][l][None]], axis=0)
    wd = np.concatenate([inp["moe_w_down"][l], inp["sh_w_down"][l][None]], axis=0)
    moew = np.empty((NEXP, P, 6144), np.float32)
    moew[:, :, 0:2048] = wg.reshape(NEXP, KC, P, 256).transpose(0, 2, 1, 3).reshape(NEXP, P, 2048)
    moew[:, :, 2048:4096] = wu.reshape(NEXP, KC, P, 256).transpose(0, 2, 1, 3).reshape(NEXP, P, 2048)
    moew[:, :, 4096:6144] = wd.reshape(NEXP, 2, P, D).transpose(0, 2, 1, 3).reshape(NEXP, P, 2048)
    w[f"moew{tag}"] = moew
    return w


_PROG_CACHE = {}


def _get_prog(layers, debug=None):
    key = (tuple(layers), tuple(sorted((debug or {}).items())))
    if key not in _PROG_CACHE:
        pr = Prog(list(layers), debug)
        nc = pr.build()
        _PROG_CACHE[key] = (nc, set(pr.inputs.keys()))
    return _PROG_CACHE[key]


def _init_toks(inp):
    toks = []
    for r in range(8):
        b, s = r // 2, r % 2
        toks.append(np.concatenate([inp["x"][b, s * NLAT:(s + 1) * NLAT], inp["ctx"][b, s * NCTX:(s + 1) * NCTX]], axis=0))
    return toks


def _launch(inp, layers, toks, debug=None):
    nc, names = _get_prog(layers, debug)
    lw = {}
    for l in layers:
        lw.update(_layer_weights(l, inp, l))
    fm = [_fm(t) for t in toks]
    in_maps = []
    for r in range(8):
        b, s = r // 2, r % 2
        m = dict(lw)
        cs = _consts(s)
        for k in ("ident", "perm", "rope", "ropeq", "cs2", "dft", "cdft"):
            m[k] = cs[k]
        for l in layers:
            if l % 2 == 1:
                m[f"nab{l}"] = _na_bias(inp["na_rpb"][l // 2], s)
        m["cT"] = np.ascontiguousarray(
            np.stack([inp["c"][b].reshape(KC, P).T, inp["c_ctx"].reshape(KC, P).T], axis=2)).reshape(P, 16)
        m["xT"] = fm[r]
        m[f"xg{layers[0]}"] = np.ascontiguousarray(
            np.stack([fm[r - s].reshape(P, KC, NT), fm[r - s + 1].reshape(P, KC, NT)], axis=0).transpose(2, 0, 1, 3)
        ).reshape(KC, 2 * P, NT)
        in_maps.append({k: v for k, v in m.items() if k in names})
    res = run_bass_kernel_spmd(nc, in_maps, core_ids=list(range(8)))
    return [_unfm(res.results[r]["xout"], NT) for r in range(8)]


def run_layers(inp, layers=range(DEPTH), toks=None, debug=None, fused=False):
    inp = {k: np.asarray(v) for k, v in inp.items()}
    if toks is None:
        toks = _init_toks(inp)
    if fused:
        return _launch(inp, list(layers), toks, debug)
    for l in layers:
        toks = _launch(inp, [l], toks, debug)
    return toks


def kernel(**inputs):
    toks = run_layers(inputs, fused=True)
    out = np.empty((4, 4096, D), np.float32)
    for r in range(8):
        b, s = r // 2, r % 2
        out[b, s * NLAT:(s + 1) * NLAT] = toks[r][0:NLAT]
    return out
```

```python
import math
import numpy as np
import ml_dtypes
import concourse.bass as bass
import concourse.mybir as mybir
from concourse.bass_utils import run_bass_kernel_spmd
from contextlib import ExitStack

F32 = mybir.dt.float32
BF16 = mybir.dt.bfloat16
AF = mybir.ActivationFunctionType
ALU = mybir.AluOpType
AX = mybir.AxisListType

P = 128
D = 1024
KC = 8
NLAT = 2048
NCTX = 128
NT = NLAT + NCTX
DEPTH = 4
ALPHA = (2 * DEPTH) ** 0.25
LN_EPS = 1e-6
RMS_EPS = 1e-6
HD = 128
SCALE = HD ** -0.5
NEG = -1e30
NEXP = 65
ROUTE_SCALE = 2.5

ENGS = ("pe", "act", "dve", "pool", "sp")
DTSIZE = {F32: 4, BF16: 2}


class V:
    __slots__ = ("t", "ap")

    def __init__(self, t, ap):
        self.t = t
        self.ap = ap

    def __getitem__(self, idx):
        return V(self.t, self.ap[idx])

    def r(self, pat, **kw):
        return V(self.t, self.ap.rearrange(pat, **kw))

    def bc(self, shape):
        return V(self.t, self.ap.broadcast_to(shape))

    def bitcast(self, dt):
        return V(self.t, self.ap.bitcast(dt))


class T:
    def __init__(self, h, name, space, rng=None):
        self.h = h
        self.name = name
        self.space = space
        self.w = None
        self.r_ = []
        self.dsem = None
        self.dcnt = 0
        self.rng = rng

    def __getitem__(self, idx):
        return V(self, self.h[idx])

    def r(self, pat, **kw):
        return V(self, self.h.rearrange(pat, **kw))

    def v(self):
        return V(self, self.h)


class Ctx:
    def __init__(self, nc, arena_bytes=211968):
        self.nc = nc
        self.es = ExitStack()
        self.E = {"pe": nc.tensor, "act": nc.scalar, "dve": nc.vector, "pool": nc.gpsimd, "sp": nc.sync}
        self.sem = {}
        self.cnt = {}
        for e in ENGS:
            self.sem[e] = self.es.enter_context(nc.semaphore("s_" + e))
            self.cnt[e] = 0
        self.seen = {e: {} for e in ENGS}
        self.nbuf = 0
        self.sem_pool = []
        self.arena = self.es.enter_context(nc.sbuf_tensor("arena", [P, arena_bytes // 2], BF16))
        self.arena_bytes = arena_bytes
        self.free_list = [(0, arena_bytes)]
        self.grave = []
        self.live = {}
        self.banks = []
        for i in range(8):
            h = self.es.enter_context(nc.psum_tensor(f"bank{i}", [P, 512], F32))
            self.banks.append(T(h[:], f"bank{i}", "ps"))

    def alloc(self, name, nelem, dt, parts=P):
        nbytes = (nelem * DTSIZE[dt] + 63) // 64 * 64
        for i, (s, e) in enumerate(self.free_list):
            if e - s >= nbytes:
                self.free_list[i] = (s + nbytes, e)
                if self.free_list[i][0] == self.free_list[i][1]:
                    del self.free_list[i]
                break
        else:
            raise RuntimeError(f"arena OOM allocating {name} {nbytes}B; free={self.free_list}")
        ap = self.arena[0:parts, s // 2:(s + nbytes) // 2]
        if dt != BF16:
            ap = ap.bitcast(dt)
        ap = ap[:, 0:nelem]
        self.nbuf += 1
        t = T(ap, f"{name}_{self.nbuf}", "sb", (s, s + nbytes))
        keep = []
        for (gs, ge, toks) in self.grave:
            if gs < s + nbytes and s < ge:
                t.r_.extend(toks)
                if gs >= s and ge <= s + nbytes:
                    continue
            keep.append((gs, ge, toks))
        self.grave = keep
        return t

    def free(self, *ts):
        for t in ts:
            s, e = t.rng
            toks = [x for x in ([t.w] + t.r_) if x is not None]
            self.grave.append((s, e, self._compress(toks)))
            self.free_list.append((s, e))
            self.free_list.sort()
            merged = []
            for a, b in self.free_list:
                if merged and merged[-1][1] == a:
                    merged[-1] = (merged[-1][0], b)
                else:
                    merged.append((a, b))
            self.free_list = merged
            t.rng = None
            if t.dsem is not None:
                self.sem_pool.append((t.dsem, t.dcnt, t.dkey))
                t.dsem = None

    @staticmethod
    def _compress(toks):
        best = {}
        for tok in toks:
            key = tok[1] if tok[0] == "eng" else tok[3]
            v = tok[2]
            if key not in best or best[key][2] < v:
                best[key] = tok
        return list(best.values())

    def dram(self, name, shape, dt, kind="Internal"):
        h = self.nc.dram_tensor(name, list(shape), dt, kind=kind)
        return T(h.ap(), name, "dram")

    def _wait(self, e, tok):
        if tok is None:
            return
        if tok[0] == "eng":
            _, f, v = tok
            key = "e:" + f
            sem = self.sem[f]
        else:
            _, sem, v, key = tok
        if e == "pe" and tok[0] == "eng" and tok[1] == "pe":
            return
        if self.seen[e].get(key, 0) >= v:
            return
        self.E[e].wait_ge(sem, v)
        self.seen[e][key] = v

    def _deps(self, e, reads, writes, acc=False):
        for t in reads:
            self._wait(e, t.w)
        if not acc:
            for t in writes:
                self._wait(e, t.w)
                for tok in t.r_:
                    self._wait(e, tok)

    def op(self, e, fn, reads=(), writes=(), acc=False, inc=True):
        reads = [v.t for v in reads if v is not None]
        writes = [v.t for v in writes if v is not None]
        self._deps(e, reads, writes, acc)
        ins = fn(self.E[e])
        if inc:
            self.cnt[e] += 1
            ins.then_inc(self.sem[e], 1)
            tok = ("eng", e, self.cnt[e])
        else:
            tok = ("eng", e, self.cnt[e] + 1)
        for t in reads:
            t.r_.append(tok)
            if len(t.r_) > 24:
                t.r_ = self._compress(t.r_)
        for t in writes:
            t.w = tok
            t.r_ = []
        return ins

    def dma(self, q, out, in_, **kw):
        ot = out.t if isinstance(out, V) else None
        it = in_.t if isinstance(in_, V) else None
        oap = out.ap if isinstance(out, V) else out
        iap = in_.ap if isinstance(in_, V) else in_
        reads = [it] if it is not None else []
        writes = [ot] if ot is not None else []
        self._deps(q, reads, writes)
        owner = None
        for t in (ot, it):
            if t is not None and t.space != "dram":
                owner = t
                break
        if owner is None:
            owner = ot if ot is not None else it
        if owner.dsem is None:
            if self.sem_pool:
                sem, cnt, key = self.sem_pool.pop()
                if cnt > 0:
                    self._wait(q, ("dma", sem, cnt, key))
            else:
                self.nsem = getattr(self, "nsem", 0) + 1
                key = f"d:{self.nsem}"
                sem, cnt = self.es.enter_context(self.nc.semaphore(f"dsem{self.nsem}")), 0
            owner.dsem, owner.dcnt, owner.dkey = sem, cnt, key
        owner.dcnt += 16
        ins = self.E[q].dma_start(out=oap, in_=iap, **kw)
        ins.then_inc(owner.dsem, 16)
        tok = ("dma", owner.dsem, owner.dcnt, owner.dkey)
        for t in reads:
            t.r_.append(tok)
        for t in writes:
            t.w = tok
            t.r_ = []
        return tok

    def wait_all(self, e, ts):
        for t in ts:
            self._wait(e, t.w)
            for tok in t.r_:
                self._wait(e, tok)

    def mm(self, out, lhsT, rhs, start=True, stop=True, inc=None):
        self.op("pe", lambda e: e.matmul(out.ap, lhsT=lhsT.ap, rhs=rhs.ap, start=start, stop=stop),
                reads=[lhsT, rhs], writes=[out], acc=not start, inc=(stop if inc is None else inc))

    def transpose(self, out, in_, ident):
        self.op("pe", lambda e: e.transpose(out.ap, in_.ap, ident.ap), reads=[in_, ident], writes=[out])

    def act(self, out, in_, func, scale=None, bias=None, accum=None):
        kw = {}
        rd = [in_]
        wr = [out]
        if scale is not None:
            if isinstance(scale, V):
                kw["scale"] = scale.ap
                rd.append(scale)
            else:
                kw["scale"] = float(scale)
        if bias is not None:
            if isinstance(bias, V):
                kw["bias"] = bias.ap
                rd.append(bias)
            else:
                kw["bias"] = float(bias)
        if accum is not None:
            kw["accum_out"] = accum.ap
            wr.append(accum)
        self.op("act", lambda e: e.activation(out=out.ap, in_=in_.ap, func=func, **kw), reads=rd, writes=wr)

    def copy(self, eng, out, in_):
        if eng == "act":
            self.op("act", lambda e: e.copy(out=out.ap, in_=in_.ap), reads=[in_], writes=[out])
        else:
            self.op(eng, lambda e: e.tensor_copy(out=out.ap, in_=in_.ap), reads=[in_], writes=[out])

    def tt(self, eng, out, in0, in1, op):
        self.op(eng, lambda e: e.tensor_tensor(out=out.ap, in0=in0.ap, in1=in1.ap, op=op),
                reads=[in0, in1], writes=[out])

    def ts(self, eng, out, in0, s1, s2=None, op0=ALU.mult, op1=None):
        rd = [in0]
        a1 = s1.ap if isinstance(s1, V) else float(s1)
        if isinstance(s1, V):
            rd.append(s1)
        a2 = None
        if s2 is not None:
            a2 = s2.ap if isinstance(s2, V) else float(s2)
            if isinstance(s2, V):
                rd.append(s2)
        if op1 is None:
            self.op(eng, lambda e: e.tensor_scalar(out=out.ap, in0=in0.ap, scalar1=a1, scalar2=None, op0=op0),
                    reads=rd, writes=[out])
        else:
            self.op(eng, lambda e: e.tensor_scalar(out=out.ap, in0=in0.ap, scalar1=a1, scalar2=a2, op0=op0, op1=op1),
                    reads=rd, writes=[out])

    def stt(self, out, in0, scalar, in1, op0, op1, eng="dve"):
        rd = [in0, in1]
        a = scalar.ap if isinstance(scalar, V) else float(scalar)
        if isinstance(scalar, V):
            rd.append(scalar)
        self.op(eng, lambda e: e.scalar_tensor_tensor(out=out.ap, in0=in0.ap, scalar=a, in1=in1.ap, op0=op0, op1=op1),
                reads=rd, writes=[out])

    def memset(self, eng, out, val):
        self.op(eng, lambda e: e.memset(out.ap, val), writes=[out])

    def close(self):
        self.es.close()


class Prog:
    def __init__(self, layers, debug=None):
        self.layers = list(layers)
        self.debug = debug or {}
        self.nc = bass.Bass("TRN2", target_bir_lowering=False)
        self.c = Ctx(self.nc)
        self.inputs = {}
        self.rot = {}
        self.xg = {}

    def inp(self, name, shape, dt=F32):
        if name not in self.inputs:
            self.inputs[name] = self.nc.dram_tensor(name, list(shape), dt, kind="ExternalInput").ap()
        return self.inputs[name]

    def bank(self, i):
        return self.c.banks[i]

    def rr(self, key, n):
        v = self.rot.get(key, 0)
        self.rot[key] = v + 1
        return v % n

    def build(self):
        c = self.c
        nc = self.nc
        first = self.layers[0]
        self.XT = c.alloc("XT", KC * NT, F32)
        self.XT3 = self.XT.r("p (c t) -> p c t", c=KC)
        self.HB = c.alloc("HB", KC * NT, BF16)
        self.HB3 = self.HB.r("p (c t) -> p c t", c=KC)
        self.identb = c.alloc("identb", P, BF16)
        self.identf = c.alloc("identf", P, F32)
        c.dma("pool", self.identb.v(), self.inp("ident", [P, P]))
        c.dma("sp", self.identf.v(), self.inp("ident", [P, P]))
        self.ones_mean = c.alloc("ones_mean", P, BF16)
        self.ones_rms = c.alloc("ones_rms", P, BF16)
        self.ones1 = c.alloc("ones1", P, BF16)
        c.memset("dve", self.ones_mean.v(), 1.0 / D)
        self.ones_mean_f = c.alloc("ones_mean_f", P, F32)
        c.memset("dve", self.ones_mean_f.v(), 1.0 / D)
        c.memset("dve", self.ones_rms.v(), 1.0 / HD)
        c.memset("dve", self.ones1.v(), 1.0)
        self.MODV = c.alloc("MODV", 96, F32)
        self.MODV3 = self.MODV.r("p (m r) -> p m r", r=2)
        self.LNV = c.alloc("LNV", 32, F32)
        self.LNV3 = self.LNV.r("p (m c) -> p m c", c=KC)
        self.A2 = c.alloc("A2", 16, F32)
        self.A23 = self.A2.r("p (c r) -> p c r", r=2)
        self.B2 = c.alloc("B2", 16, F32)
        self.B23 = self.B2.r("p (c r) -> p c r", r=2)
        self.G = c.alloc("G", 17 * NEXP, F32)
        self.G3 = self.G.r("p (t e) -> p t e", e=NEXP)
        self.scT = c.alloc("scT", 16, F32)
        self.scT3 = self.scT.r("p (k r) -> p k r", r=2)
        c.dma("sp", self.scT.v(), self.inp("cT", [P, 16]))
        c.act(self.scT.v(), self.scT.v(), AF.Silu)
        c.dma("sp", self.XT.v(), self.inp("xT", [P, KC * NT]))
        for li, l in enumerate(self.layers):
            self.layer(l)
            if li + 1 < len(self.layers):
                self.exchange(l, self.layers[li + 1])
        xout = nc.dram_tensor("xout", [P, KC * NT], F32, kind="ExternalOutput").ap()
        tok = c.dma("sp", xout, self.XT.v())
        c._wait("sp", tok)
        c.close()
        return nc

    def exchange(self, l, lnext):
        c = self.c
        nc = self.nc
        sem = c.es.enter_context(nc.semaphore(f"ccsem{l}"))
        tok = ("dma", sem, KC, f"cc{l}")
        xgh = nc.dram_tensor(f"xgi{lnext}", [KC, 2 * P, NT], F32)
        xg = T(xgh.ap(), f"xgi{lnext}", "dram")
        for cc in range(KC):
            xsh = nc.dram_tensor(f"xs{l}_{cc}", [P, NT], F32)
            xs = T(xsh.ap(), f"xs{l}_{cc}", "dram")
            c.dma("sp", xs.v(), self.XT3[:, cc, :])
            c._deps("pool", [xs], [xg] if cc == 0 else [])
            ins = nc.gpsimd.collective_compute("AllGather", ALU.bypass, replica_groups=[[0, 1], [2, 3], [4, 5], [6, 7]],
                                               ins=[xsh.ap().opt()], outs=[xgh.ap()[cc]])
            ins.then_inc(sem)
            xs.r_.append(tok)
        xg.w = tok
        xg.r_ = []
        self.xg[lnext] = xg

    def layer(self, l):
        self.emit_mod(l)
        if self.debug.get("stop") == "mod":
            return
        self.emit_h1(l)
        if self.debug.get("stop") == "h1":
            return
        if l % 2 == 0:
            self.emit_gqa(l)
        else:
            self.emit_na(l)
        stop = self.debug.get("stop")
        if stop == "mixer":
            return
        self.emit_ln(l, 1)
        if stop == "ln1":
            return
        self.emit_moe(l)
        if stop == "moe":
            return
        self.emit_ln(l, 2)

    def emit_mod(self, l):
        c = self.c
        wmod = self.inp(f"wmod{l}", [D, 6 * D]).rearrange("(k p) n -> p k n", p=P)
        bmod = c.alloc("bmod", 48, F32)
        c.dma("sp", bmod.v(), self.inp(f"bmod{l}", [P, 48]))
        c.dma("sp", self.LNV.v(), self.inp(f"lnv{l}", [P, 32]))
        st = [c.alloc("wmst", KC * 512, F32) for _ in range(2)]
        scb3 = self.scT3
        ps = self.bank(0)
        ps3 = ps[:, 0:96].r("p (m r) -> p m r", r=2)
        for blk in range(12):
            s = st[blk % 2]
            s3 = s.r("p (k n) -> p k n", k=KC)
            c.dma("sp", s3, wmod[:, :, blk * 512:(blk + 1) * 512])
            for fc in range(4):
                for k in range(KC):
                    c.mm(ps3[:, blk * 4 + fc, :], s3[:, k, fc * 128:(fc + 1) * 128], scb3[:, k, :],
                         start=(k == 0), stop=(k == KC - 1))
        M3 = self.MODV3
        for r in range(2):
            c.tt("dve", M3[:, :, r], ps3[:, :, r], bmod.v(), ALU.add)
        c.ts("dve", M3[:, 8:16, :], M3[:, 8:16, :], 1.0, op0=ALU.add)
        c.ts("dve", M3[:, 32:40, :], M3[:, 32:40, :], 1.0, op0=ALU.add)
        c.ts("dve", M3[:, 16:24, :], M3[:, 16:24, :], 1.0 / ALPHA, op0=ALU.mult)
        c.ts("dve", M3[:, 40:48, :], M3[:, 40:48, :], 1.0 / ALPHA, op0=ALU.mult)
        for r in range(2):
            c.tt("dve", self.A23[:, :, r], self.LNV3[:, 0, :], M3[:, 32:40, r], ALU.mult)
            c.tt("dve", self.B23[:, :, r], self.LNV3[:, 1, :], M3[:, 32:40, r], ALU.mult)
            c.tt("dve", self.B23[:, :, r], self.B23[:, :, r], M3[:, 24:32, r], ALU.add)
        c.free(bmod, *st)

    def mod_h(self, eng, out, in_, cc, r):
        if eng == "act":
            self.c.act(out, in_, AF.Identity, scale=self.MODV3[:, 8 + cc, r:r + 1], bias=self.MODV3[:, cc, r:r + 1])
        else:
            self.c.ts(eng, out, in_, self.MODV3[:, 8 + cc, r:r + 1], self.MODV3[:, cc, r:r + 1], ALU.mult, ALU.add)

    def emit_h1(self, l):
        for cc in range(KC):
            eng = ("dve", "act")[cc % 2]
            self.mod_h(eng, self.HB3[:, cc, 0:NLAT], self.XT3[:, cc, 0:NLAT], cc, 0)
            self.mod_h(eng, self.HB3[:, cc, NLAT:NT], self.XT3[:, cc, NLAT:NT], cc, 1)

    def xg_ap(self, l):
        if l in self.xg:
            return self.xg[l]
        ap = self.inp(f"xg{l}", [KC, 2 * P, NT])
        self.xg[l] = T(ap, f"xg{l}", "dram")
        return self.xg[l]

    def gstream(self, l, groups):
        c = self.c
        xg = self.xg_ap(l)
        st = [c.alloc("xost", KC * 256, F32) for _ in range(3)]
        ho = [c.alloc("xoh", KC * 256, BF16) for _ in range(2)]
        for i, (slot, c0, n, r) in enumerate(groups):
            s3 = st[i % 3].r("p (c t) -> p c t", c=KC)[:, :, 0:n]
            h3 = ho[i % 2].r("p (c t) -> p c t", c=KC)[:, :, 0:n]
            c.dma("sp", s3, V(xg, xg.h[:, slot * P:(slot + 1) * P, c0:c0 + n].rearrange("c p t -> p c t")))
            for cc in range(KC):
                self.mod_h(("dve", "act")[cc % 2], h3[:, cc, :], s3[:, cc, :], cc, r)
            yield (slot, c0, n, r, h3)
        c.free(*st, *ho)

    def load_w(self, name, dram_ap, ncols):
        c = self.c
        t = c.alloc(name, KC * ncols, BF16)
        t3 = t.r("p (k n) -> p k n", k=KC)
        c.dma("pool", t3, dram_ap.rearrange("(k p) n -> p k n", p=P))
        return t, t3

    def outproj(self, oT, wo, col0, n, r):
        c = self.c
        oTs = oT if isinstance(oT, list) else [oT]
        wos = wo if isinstance(wo, list) else [wo]
        for cc in range(KC):
            y = self.bank(6 + self.rr("yb", 2))[:, 0:n]
            for i, (o, w) in enumerate(zip(oTs, wos)):
                c.mm(y, w[:, cc * 128:(cc + 1) * 128], o, start=(i == 0), stop=(i == len(oTs) - 1))
            xs = self.XT3[:, cc, col0:col0 + n]
            c.stt(xs, y, self.MODV3[:, 16 + cc, r:r + 1], xs, ALU.mult, ALU.add)

    def rmsnorm_rope(self, ps, n, gain, out, rope_col0=None, table=None):
        c = self.c
        k = self.rr("rn", 2)
        tm = self.rn_tmp[k]
        sq = tm["sq"][:, 0:n]
        rstd = tm["rstd"][:, 0:n]
        c.act(sq, ps, AF.Square)
        ss = self.bank(2 + k)[:, 0:n]
        c.mm(ss, self.ones_rms.v(), sq)
        c.act(rstd, ss, AF.Ln, bias=RMS_EPS)
        c.act(rstd, rstd, AF.Exp, scale=-0.5)
        if rope_col0 is None:
            c.stt(out, ps, gain, rstd, ALU.mult, ALU.mult)
            return
        qn = tm["qn"][:, 0:n]
        c.stt(qn, ps, gain, rstd, ALU.mult, ALU.mult)
        rp = tm["rope"].r("p (a n) -> p a n", a=2)[:, :, 0:n]
        c.dma("sp", rp, (table if table is not None else self.rope_dram)[:, :, rope_col0:rope_col0 + n])
        pp = self.bank(4 + k)[:, 0:n]
        c.mm(pp, self.perm.v(), qn)
        t1 = tm["t1"][:, 0:n]
        t2 = tm["t2"][:, 0:n]
        c.tt("pool", t1, qn, rp[:, 0, :], ALU.mult)
        c.tt("dve", t2, pp, rp[:, 1, :], ALU.mult)
        c.tt("dve", out, t1, t2, ALU.add)

    def attn_T(self, KT, VV3, qv, n, tiles):
        c = self.c
        O = self.bank(2)[:, 0:n]
        L = self.bank(3)[:, 0:n]
        nt = len(tiles)
        sbanks = (0, 1, 4, 5)
        LA = 3

        def S(i):
            s = self.bank(sbanks[self.rr("sb", 4)])[:, 0:n]
            kt = tiles[i]
            c.mm(s, KT[:, kt * 128:(kt + 1) * 128], qv)
            return s
        q = [S(i) for i in range(min(LA, nt))]
        for i in range(nt):
            s_cur = q.pop(0)
            if i + LA < nt:
                q.append(S(i + LA))
            pt = self.pt_tmp[self.rr("pt", len(self.pt_tmp))][:, 0:n]
            c.act(pt, s_cur, AF.Exp, scale=SCALE)
            c.mm(O, VV3[:, tiles[i], :], pt, start=(i == 0), stop=(i == nt - 1))
            c.mm(L, self.ones1.v(), pt, start=(i == 0), stop=(i == nt - 1))
        kk = self.rr("ot", 2)
        rl = self.rl_tmp[kk][:, 0:n]
        c.act(rl, L, AF.Ln)
        c.act(rl, rl, AF.Exp, scale=-1.0)
        oT = self.ot_tmp[kk][:, 0:n]
        c.tt("dve", oT, O, rl, ALU.mult)
        return oT

    def emit_gqa(self, l):
        c = self.c
        j = l // 2
        win = self.inp(f"win{l}", [D, 1536])
        wout = self.inp(f"wout{l}", [D, D])
        g_own = [(i * 256, 256, 0) for i in range(8)] + [(NLAT, 128, 1)]
        wf, wf3 = self.load_w("wf", win[:, 0:256], 256)
        cs2 = c.alloc("cs2", 256, BF16)
        c.dma("pool", cs2.v(), self.inp("cs2", [P, 256]))
        wfn = c.alloc("wfn", 256, BF16)
        wfn3 = wfn.r("p (j m) -> p j m", j=2)
        c.dma("pool", wfn.v(), self.inp(f"wfn{l}", [P, 256]))
        wo_f = c.alloc("wo_f", 2 * D, BF16)
        wo_f3 = wo_f.r("p (j n) -> p j n", j=2)
        c.dma("pool", wo_f3, wout[0:256, :].rearrange("(j p) n -> p j n", p=P))
        cdft = c.alloc("cdft", 512, BF16)
        cdft4 = cdft.r("p (a t k) -> p a t k", a=2, t=2)
        c.dma("pool", cdft.v(), self.inp("cdft", [P, 512]))
        FCS = c.alloc("FCS", 34 * 512, BF16)
        FCS3 = FCS.r("p (t f) -> p t f", f=512)
        fts = [c.alloc("fts", 512, BF16) for _ in range(2)]

        def stage1(buf0, n, h3):
            fps = self.bank(self.rr("fps", 2))
            fps3 = fps[:, 0:2 * n].r("p (j t) -> p j t", j=2)
            for jj in range(2):
                for k in range(KC):
                    c.mm(fps3[:, jj, :], wf3[:, k, jj * 128:(jj + 1) * 128], h3[:, k, :], start=(k == 0), stop=(k == KC - 1))
            ft = fts[self.rr("fts", 2)]
            ft3 = ft[:, 0:2 * n].r("p (j t) -> p j t", j=2)
            c.copy("act", ft3, fps3)
            for tt in range(n // 128):
                cps = self.bank(2 + self.rr("cps", 2))
                for jj in range(2):
                    c.mm(cps[:, jj * 256:(jj + 1) * 256], ft3[:, jj, tt * 128:(tt + 1) * 128], cs2.v())
                c.copy("dve", FCS3[:, (buf0 + tt * 128) // 128, :], cps.v())

        g_all = [(slot, c0, n, r) for slot in range(2) for (c0, n, r) in g_own]
        for (slot, c0, n, r, h3) in self.gstream(l, g_all):
            stage1(slot * NT + c0, n, h3)
        c.free(wf, cs2, *fts)

        dft = self.inp("dft", [4, 8, P, 4096], BF16)
        dst = [c.alloc("dftst", 4096, BF16) for _ in range(2)]
        frs = [c.alloc("frs", 512, BF16) for _ in range(2)]
        ots = [c.alloc("ofs", 512, BF16) for _ in range(2)]

        def finish(acc, n, col0, r):
            oTs = []
            for jj in range(2):
                fr = frs[jj][:, 0:n]
                c.copy("act", fr, acc[jj])
                wps = self.bank(6 + self.rr("yb", 2))[:, 0:n]
                c.mm(wps, wfn3[:, jj, :], fr)
                o = ots[jj][:, 0:n]
                c.copy("act", o, wps)
                oTs.append(o)
            self.outproj(oTs, [wo_f3[:, 0, :], wo_f3[:, 1, :]], col0, n, r)

        for kc in range(4):
            acc = [self.bank(4)[:, 0:512], self.bank(5)[:, 0:512]]
            for ng in range(8):
                dt_ = dst[self.rr("dst", 2)]
                c.dma("sp", dt_.v(), dft[kc, ng])
                dt4 = dt_.r("p (a i k) -> p a i k", a=2, i=4)
                for ni in range(4):
                    nn = ng * 4 + ni
                    ft_i = nn if nn < 16 else nn + 1
                    for jj in range(2):
                        c.mm(acc[jj], FCS3[:, ft_i, jj * 256:jj * 256 + 128], dt4[:, 0, ni, :], start=(nn == 0), stop=False)
                        c.mm(acc[jj], FCS3[:, ft_i, jj * 256 + 128:(jj + 1) * 256], dt4[:, 1, ni, :], start=False, stop=(nn == 31),
                             inc=(nn == 31 or (ni == 3 and jj == 1)))
            finish(acc, 512, kc * 512, 0)
        acc = [self.bank(4)[:, 0:128], self.bank(5)[:, 0:128]]
        for ti, ft_i in enumerate((16, 33)):
            for jj in range(2):
                c.mm(acc[jj], FCS3[:, ft_i, jj * 256:jj * 256 + 128], cdft4[:, 0, ti, :], start=(ti == 0), stop=False)
                c.mm(acc[jj], FCS3[:, ft_i, jj * 256 + 128:(jj + 1) * 256], cdft4[:, 1, ti, :], start=False, stop=(ti == 1))
        finish(acc, 128, NLAT, 1)
        c.free(FCS, wfn, wo_f, cdft, *dst, *frs, *ots)
        if self.debug.get("stop") == "fnet":
            return

        self.rope_dram = self.inp("rope", [P, 2 * 4096], BF16).rearrange("p (a n) -> p a n", a=2)
        ropeq = self.inp("ropeq", [P, 2 * NLAT], BF16).rearrange("p (a n) -> p a n", a=2)
        self.perm = c.alloc("perm", P, BF16)
        c.dma("pool", self.perm.v(), self.inp("perm", [P, P]))
        qkn = c.alloc("qkn", 2, F32)
        c.dma("sp", qkn.v(), self.inp(f"qkn{l}", [P, 2]))
        self.rn_tmp = [dict(sq=c.alloc("sq", 256, BF16), rstd=c.alloc("rstd", 256, F32), qn=c.alloc("qn", 256, BF16),
                            rope=c.alloc("rope", 512, BF16), t1=c.alloc("t1", 256, F32), t2=c.alloc("t2", 256, F32))
                       for _ in range(2)]
        self.pt_tmp = [c.alloc("pt", 512, BF16) for _ in range(5)]
        self.rl_tmp = [c.alloc("rl", 512, F32) for _ in range(2)]
        self.ot_tmp = [c.alloc("ot", 512, BF16) for _ in range(2)]
        KTt = c.alloc("KT", 2 * NT, BF16)
        KT = KTt.v()
        VVt = c.alloc("VV", 34 * 128, BF16)
        VV3 = VVt.r("p (t d) -> p t d", d=128)
        QTt = [c.alloc("QT", NT, BF16) for _ in range(2)]
        for g in range(2):
            wk, wk3 = self.load_w("wk", win[:, 1024 + g * 128:1024 + (g + 1) * 128], 128)
            wv, wv3 = self.load_w("wv", win[:, 1280 + g * 128:1280 + (g + 1) * 128], 128)

            def kv(buf0, n, r, h3, rope0):
                kps = self.bank(self.rr("kps", 2))[:, 0:n]
                for k in range(KC):
                    c.mm(kps, wk3[:, k, :], h3[:, k, :], start=(k == 0), stop=(k == KC - 1))
                self.rmsnorm_rope(kps, n, qkn[:, 1:2], KT[:, buf0:buf0 + n], rope0)
                for tt in range(n // 128):
                    vps = self.bank(6 + self.rr("yb", 2))[:, 0:128]
                    for k in range(KC):
                        c.mm(vps, h3[:, k, tt * 128:(tt + 1) * 128], wv3[:, k, :], start=(k == 0), stop=(k == KC - 1))
                    c.copy("act", VV3[:, buf0 // 128 + tt, :], vps)

            for (slot, c0, n, r, h3) in self.gstream(l, g_all):
                kv(slot * NT + c0, n, r, h3, (slot * NLAT + c0) if r == 0 else None)
            c.free(wk, wv)
            for hh in range(3):
                hg = g * 3 + hh
                wq, wq3 = self.load_w("wq", win[:, 256 + hg * 128:256 + (hg + 1) * 128], 128)
                wo = c.alloc("wo", D, BF16)
                c.dma("pool", wo.v(), wout[(2 + hg) * 128:(3 + hg) * 128, :])
                QT = QTt[self.rr("qt", 2)].v()
                for (c0, n, r) in g_own:
                    qps = self.bank(self.rr("kps", 2))[:, 0:n]
                    for k in range(KC):
                        c.mm(qps, wq3[:, k, :], self.HB3[:, k, c0:c0 + n], start=(k == 0), stop=(k == KC - 1))
                    self.rmsnorm_rope(qps, n, qkn[:, 0:1], QT[:, c0:c0 + n], c0 if r == 0 else None, table=ropeq)
                for qc in range(4):
                    oT = self.attn_T(KT, VV3, QT[:, qc * 512:(qc + 1) * 512], 512, list(range(34)))
                    self.outproj(oT, wo.v(), qc * 512, 512, 0)
                oT = self.attn_T(KT, VV3, QT[:, NLAT:NT], 128, [16, 33])
                self.outproj(oT, wo.v(), NLAT, 128, 1)
                c.free(wq, wo)
        c.free(KTt, VVt, *QTt, self.perm, qkn, *self.pt_tmp, *self.rl_tmp, *self.ot_tmp)
        for tm in self.rn_tmp:
            c.free(*tm.values())

    def emit_na(self, l):
        c = self.c
        win = self.inp(f"win{l}", [D, 3 * D])
        wout = self.inp(f"wout{l}", [D, D])
        nab = self.inp(f"nab{l}", [8, P, 5 * 768])
        HOt = c.alloc("HO", KC * 768, BF16)
        HO3 = HOt.r("p (c t) -> p c t", c=KC)
        hgroups = [(0, NLAT - 256, 256, 0), (1, 0, 256, 0), (0, NLAT, 128, 1), (1, NLAT, 128, 1)]
        hdst = [0, 256, 512, 640]
        for gi_, (slot, c0, n, r, h3) in enumerate(self.gstream(l, hgroups)):
            for cc in range(KC):
                c.copy(("dve", "act")[cc % 2], HO3[:, cc, hdst[gi_]:hdst[gi_] + n], h3[:, cc, :])
        NK = 2816
        srcs = [(0, 256, HO3[:, :, 0:256])] + \
               [(256 + i * 512, 512, self.HB3[:, :, i * 512:(i + 1) * 512]) for i in range(4)] + \
               [(2304, 256, HO3[:, :, 256:512]), (2560, 256, HO3[:, :, 512:768])]
        KTs = [c.alloc("KT", NK, BF16) for _ in range(2)]
        VVs = [c.alloc("VV", NK, BF16) for _ in range(2)]
        QTs = [c.alloc("QT", NT, BF16) for _ in range(2)]
        nbs = [c.alloc("nb", 5 * 768, F32) for _ in range(1)]
        ss_tmp = [c.alloc("ss", 768, F32) for _ in range(2)]
        pt_tmp = [c.alloc("pt", 1024, BF16) for _ in range(3)]
        rl_tmp = [c.alloc("rl", 512, F32) for _ in range(2)]
        og_tmp = [c.alloc("og", 512, BF16) for _ in range(2)]

        for h in range(8):
            wq, wq3 = self.load_w("wq", win[:, h * 128:(h + 1) * 128], 128)
            wk, wk3 = self.load_w("wk", win[:, D + h * 128:D + (h + 1) * 128], 128)
            wv, wv3 = self.load_w("wv", win[:, 2 * D + h * 128:2 * D + (h + 1) * 128], 128)
            wo = c.alloc("wo", D, BF16)
            c.dma("pool", wo.v(), wout[h * 128:(h + 1) * 128, :])
            nb = nbs[0]
            c.dma("sp", nb.v(), nab[h])
            nb3 = nb.r("p (t w) -> p t w", w=768)
            KT = KTs[h % 2].v()
            VV3 = VVs[h % 2].r("p (t d) -> p t d", d=128)
            QT = QTs[h % 2].v()
            for (b0, n, h3) in srcs:
                kps = self.bank(6 + self.rr("yb", 2))[:, 0:n]
                for k in range(KC):
                    c.mm(kps, wk3[:, k, :], h3[:, k, :], start=(k == 0), stop=(k == KC - 1))
                c.copy("act", KT[:, b0:b0 + n], kps)
                for tt in range(n // 128):
                    vps = self.bank(6 + self.rr("yb", 2))[:, 0:128]
                    for k in range(KC):
                        c.mm(vps, h3[:, k, tt * 128:(tt + 1) * 128], wv3[:, k, :], start=(k == 0), stop=(k == KC - 1))
                    c.copy("dve", VV3[:, b0 // 128 + tt, :], vps)
            for (c0, n) in [(i * 512, 512) for i in range(4)] + [(NLAT, 128)]:
                qps = self.bank(6 + self.rr("yb", 2))[:, 0:n]
                for k in range(KC):
                    c.mm(qps, wq3[:, k, :], self.HB3[:, k, c0:c0 + n], start=(k == 0), stop=(k == KC - 1))
                c.copy("act", QT[:, c0:c0 + n], qps)
            jobs = []
            for bi in range(16):
                ty = {0: 0, 1: 1, 14: 3, 15: 4}.get(bi, 2)
                wb = 2 * bi if bi <= 13 else 28
                t0_ = wb * 64 // 128
                jobs.append((bi * 128, [t0_ + m for m in range(6)] + [20, 21], ty))
            jobs.append((NLAT, [20, 21], None))

            def S_of(job):
                q0, tiles, ty = job
                k = self.rr("nas", 2)
                SA = self.bank(2 * k)
                SB = self.bank(2 * k + 1)
                for m, kt in enumerate(tiles):
                    dst = (SA if m < 4 else SB)[:, (m % 4) * 128:(m % 4 + 1) * 128]
                    c.mm(dst, KT[:, kt * 128:(kt + 1) * 128], QT[:, q0:q0 + 128])
                return SA, SB

            def P_of(job, S):
                q0, tiles, ty = job
                SA, SB = S
                PT = pt_tmp[self.rr("napt", 3)]
                if ty is not None:
                    SS = ss_tmp[self.rr("nass", 2)]
                    c.stt(SS[:, 0:512], SA[:, 0:512], SCALE, nb3[:, ty, 0:512], ALU.mult, ALU.add)
                    c.stt(SS[:, 512:768], SB[:, 0:256], SCALE, nb3[:, ty, 512:768], ALU.mult, ALU.add)
                    c.act(PT[:, 0:768], SS[:, 0:768], AF.Exp)
                    c.act(PT[:, 768:1024], SB[:, 256:512], AF.Exp, scale=SCALE)
                else:
                    c.act(PT[:, 0:256], SA[:, 0:256], AF.Exp, scale=SCALE)
                return PT

            def PV_of(job, PT, O_dst, L_dst):
                q0, tiles, ty = job
                nt = len(tiles)
                for m, kt in enumerate(tiles):
                    c.mm(O_dst, VV3[:, kt, :], PT[:, m * 128:(m + 1) * 128], start=(m == 0), stop=(m == nt - 1))
                for m, kt in enumerate(tiles):
                    c.mm(L_dst, self.ones1.v(), PT[:, m * 128:(m + 1) * 128], start=(m == 0), stop=(m == nt - 1))

            def finish_group(gjobs):
                n = 128 * len(gjobs)
                col0 = gjobs[0][0]
                r = 0 if gjobs[0][2] is not None else 1
                kk = self.rr("narl", 2)
                rl = rl_tmp[kk][:, 0:n]
                c.act(rl, self.bank(5)[:, 0:n], AF.Ln)
                c.act(rl, rl, AF.Exp, scale=-1.0)
                og = og_tmp[kk][:, 0:n]
                c.tt("dve", og, self.bank(4)[:, 0:n], rl, ALU.mult)
                self.outproj(og, wo.v(), col0, n, r)

            S_next = S_of(jobs[0])
            gjobs = []
            for ji, job in enumerate(jobs):
                S_cur = S_next
                if ji + 1 < len(jobs):
                    S_next = S_of(jobs[ji + 1])
                PT = P_of(job, S_cur)
                slot = len(gjobs)
                PV_of(job, PT, self.bank(4)[:, slot * 128:(slot + 1) * 128], self.bank(5)[:, slot * 128:(slot + 1) * 128])
                gjobs.append(job)
                if len(gjobs) == 4 or ji == len(jobs) - 1 or jobs[ji + 1][2] is None:
                    finish_group(gjobs)
                    gjobs = []
            c.free(wq, wk, wv, wo)
        c.free(HOt, *KTs, *VVs, *QTs, *nbs, *ss_tmp, *pt_tmp, *rl_tmp, *og_tmp)

    def emit_ln(self, l, which):
        c = self.c
        gi, bi_ = (0, 1) if which == 1 else (2, 3)
        eps = LN_EPS / (ALPHA * ALPHA)
        zq = [c.alloc("zq", KC * 512, BF16) for _ in range(2)]
        tmp = [dict(M=c.alloc("M", 512, F32), m2=c.alloc("m2", 512, F32), rstd=c.alloc("rstdl", 512, F32)) for _ in range(2)]
        t1s = [c.alloc("t1", 512, F32) for _ in range(3)]
        if which == 1:
            h2f = [c.alloc("h2f", KC * 512, F32) for _ in range(2)]
            wr = c.alloc("wr", KC * 64, F32)
            wr3 = wr.r("p (k e) -> p k e", k=KC)
            c.dma("sp", wr.v(), self.inp(f"wr{l}", [P, KC * 64]))
            mb = c.alloc("mb", 64, F32)
            c.dma("sp", mb.v(), self.inp(f"mb{l}", [P, 64]))
            gt = [c.alloc("gt", 64 * 3 + 16, F32) for _ in range(2)]
            GTS = c.alloc("GTS", NT, F32)
            c.memset("dve", self.G3[:, :, 64:65], 1.0)
            self.GTD = c.dram(f"gtd{l}", [NEXP, NT], F32)
        groups = [(i * 512, 512, 0) for i in range(4)] + [(NLAT, 128, 1)]
        for gidx, (c0, n, r) in enumerate(groups):
            k = gidx % 2
            zq3 = zq[k].r("p (c t) -> p c t", c=KC)[:, :, 0:n]
            mean = self.bank(0)[:, 0:n]
            msq = self.bank(1)[:, 0:n]
            for cc in range(KC):
                xs = self.XT3[:, cc, c0:c0 + n]
                c.act(zq3[:, cc, :], xs, AF.Square)
            for cc in range(KC):
                c.mm(mean, self.ones_mean_f.v(), self.XT3[:, cc, c0:c0 + n], start=(cc == 0), stop=(cc == KC - 1))
            for cc in range(KC):
                c.mm(msq, self.ones_mean.v(), zq3[:, cc, :], start=(cc == 0), stop=(cc == KC - 1))
            M = tmp[k]["M"][:, 0:n]
            m2 = tmp[k]["m2"][:, 0:n]
            rstd = tmp[k]["rstd"][:, 0:n]
            c.copy("act", M, mean)
            c.act(m2, mean, AF.Square)
            c.tt("dve", m2, msq, m2, ALU.subtract)
            c.act(rstd, m2, AF.Ln, bias=eps)
            c.act(rstd, rstd, AF.Exp, scale=-0.5)
            if which == 1:
                h2f3 = h2f[k].r("p (c t) -> p c t", c=KC)[:, :, 0:n]
            for cc in range(KC):
                xs = self.XT3[:, cc, c0:c0 + n]
                t1 = t1s[self.rr("t1", 3)][:, 0:n]
                c.tt("dve", t1, xs, M, ALU.subtract)
                c.tt("dve", t1, t1, rstd, ALU.mult)
                c.act(xs, t1, AF.Identity, scale=self.LNV3[:, gi, cc:cc + 1], bias=self.LNV3[:, bi_, cc:cc + 1])
                if which == 1:
                    c.act(h2f3[:, cc, :], t1, AF.Identity, scale=self.A23[:, cc, r:r + 1], bias=self.B23[:, cc, r:r + 1])
                    c.copy("dve", self.HB3[:, cc, c0:c0 + n], h2f3[:, cc, :])
            if which == 1:
                for tt in range(n // 128):
                    tile_i = c0 // 128 + tt
                    lg = self.bank(2 + self.rr("lg", 2))[:, 0:64]
                    for cc in range(KC):
                        c.mm(lg, h2f3[:, cc, tt * 128:(tt + 1) * 128], wr3[:, cc, :], start=(cc == 0), stop=(cc == KC - 1))
                    g_ = gt[self.rr("gt", 2)]
                    sc, sel, wsel, m8, den = g_[:, 0:64], g_[:, 64:128], g_[:, 128:192], g_[:, 192:200], g_[:, 200:201]
                    rden = g_[:, 201:202]
                    c.act(sc, lg, AF.Sigmoid)
                    c.tt("dve", sel, sc, mb.v(), ALU.add)
                    c.op("dve", lambda e: e.max(out=m8.ap, in_=sel.ap), reads=[sel], writes=[m8])
                    c.ts("dve", sel, sel, m8[:, 7:8], op0=ALU.is_ge)
                    c.tt("dve", wsel, sel, sc, ALU.mult)
                    c.op("dve", lambda e: e.reduce_sum(out=den.ap, in_=wsel.ap, axis=AX.X), reads=[wsel], writes=[den])
                    c.op("dve", lambda e: e.reciprocal(out=rden.ap, in_=den.ap), reads=[den], writes=[rden])
                    c.ts("dve", self.G3[:, tile_i, 0:64], wsel, rden, ROUTE_SCALE, ALU.mult, ALU.mult)
                    gtp = self.bank(4 + self.rr("gtp", 2))[0:NEXP, 0:128]
                    c.transpose(gtp, self.G3[:, tile_i, :], self.identf.v())
                    c.copy("act", GTS[0:NEXP, tile_i * 128:(tile_i + 1) * 128], gtp)
        if which == 1:
            c.dma("sp", self.GTD.v(), GTS[0:NEXP, :])
            c.free(*h2f, wr, mb, *gt, GTS)
        c.free(*zq, *t1s)
        for t_ in tmp:
            c.free(*t_.values())

    def emit_moe(self, l):
        c = self.c
        moew = self.inp(f"moew{l}", [NEXP, P, 6144])
        NWB = 4
        WB = [c.alloc("WB", 6144, BF16) for _ in range(NWB)]
        GB = [c.alloc("GB", NT, F32) for _ in range(NWB)]
        sT = [c.alloc("sT", 512, BF16) for _ in range(2)]
        tT = [c.alloc("tT", 512, BF16) for _ in range(2)]
        aT = [c.alloc("aT", 512, BF16) for _ in range(4)]
        groups = [(i * 256, 256, 0) for i in range(8)] + ([(NLAT, 128, 1)] if l < DEPTH - 1 else [])
        pairs = [(2 * i, 2 * i + 1) for i in range(32)] + [(64,)]

        def prefetch(e):
            w = WB[e % NWB]
            for q in range(3):
                c.dma("pool", w[:, q * 2048:(q + 1) * 2048], moew[e][:, q * 2048:(q + 1) * 2048])
            c.dma("sp", GB[e % NWB].r("p (o t) -> p o t", o=1), V(self.GTD, self.GTD.h[e:e + 1, :].partition_broadcast(P)))

        def down_mm(sts):
            e0, c0, n, r, _ = sts[0]
            Y = [self.bank(4 + cc // 2)[:, (cc % 2) * 256:(cc % 2) * 256 + n] for cc in range(KC)]
            nmm = 2 * len(sts)
            for cc in range(KC):
                i = 0
                for (e, _, _, _, a3) in sts:
                    w = WB[e % NWB].v()
                    for jj in range(2):
                        c.mm(Y[cc], w[:, 4096 + jj * 1024 + cc * 128:4096 + jj * 1024 + (cc + 1) * 128], a3[:, jj, :],
                             start=(i == 0), stop=(i == nmm - 1))
                        i += 1

        def down_acc(sts):
            e0, c0, n, r, _ = sts[0]
            Y = [self.bank(4 + cc // 2)[:, (cc % 2) * 256:(cc % 2) * 256 + n] for cc in range(KC)]
            for cc in range(KC):
                xs = self.XT3[:, cc, c0:c0 + n]
                c.stt(xs, Y[cc], self.MODV3[:, 40 + cc, r:r + 1], xs, ALU.mult, ALU.add)

        for e in pairs[0]:
            prefetch(e)
        pend = []
        ready = None
        it = 0
        for pi, pr in enumerate(pairs):
            for gidx, (c0, n, r) in enumerate(groups):
                for ei, e in enumerate(pr):
                    w = WB[e % NWB].v()
                    gb = GB[e % NWB].v()
                    k = it % 2
                    gps = self.bank(2 * k)
                    ups = self.bank(2 * k + 1)
                    g3 = gps[:, 0:2 * n].r("p (j t) -> p j t", j=2)
                    u3 = ups[:, 0:2 * n].r("p (j t) -> p j t", j=2)
                    for jj in range(2):
                        for kk in range(KC):
                            c.mm(g3[:, jj, :], w[:, kk * 256 + jj * 128:kk * 256 + (jj + 1) * 128], self.HB3[:, kk, c0:c0 + n],
                                 start=(kk == 0), stop=(kk == KC - 1))
                    for jj in range(2):
                        for kk in range(KC):
                            c.mm(u3[:, jj, :], w[:, 2048 + kk * 256 + jj * 128:2048 + kk * 256 + (jj + 1) * 128],
                                 self.HB3[:, kk, c0:c0 + n], start=(kk == 0), stop=(kk == KC - 1))
                    did = None
                    if ei == 0 and ready is not None:
                        down_mm(ready)
                        did = ready
                        ready = None
                        if gidx == 0 and pi + 1 < len(pairs):
                            for e2 in pairs[pi + 1]:
                                prefetch(e2)
                    elif ei == 0 and gidx == 0 and pi == 0:
                        for e2 in pairs[1]:
                            prefetch(e2)
                    s3 = sT[k][:, 0:2 * n].r("p (j t) -> p j t", j=2)
                    t3 = tT[k][:, 0:2 * n].r("p (j t) -> p j t", j=2)
                    a3 = aT[it % 4][:, 0:2 * n].r("p (j t) -> p j t", j=2)
                    c.act(s3, g3, AF.Silu)
                    for jj in range(2):
                        c.tt("dve", t3[:, jj, :], u3[:, jj, :], gb[:, c0:c0 + n], ALU.mult)
                    c.tt("dve", a3, s3, t3, ALU.mult)
                    if did is not None:
                        down_acc(did)
                    pend.append((e, c0, n, r, a3))
                    if ei == len(pr) - 1:
                        ready = pend
                        pend = []
                    it += 1
        down_mm(ready)
        down_acc(ready)
        c.free(*WB, *GB, *sT, *tT, *aT)


BF = ml_dtypes.bfloat16


def _fm(tok):
    T_ = tok.shape[0]
    return np.ascontiguousarray(tok.T.reshape(KC, P, T_).transpose(1, 0, 2)).reshape(P, KC * T_)


def _unfm(a, T_):
    return np.ascontiguousarray(a.reshape(P, KC, T_).transpose(1, 0, 2).reshape(D, T_).T)


_CONST_CACHE = {}


def _consts(s):
    if s in _CONST_CACHE:
        return _CONST_CACHE[s]
    out = {}
    out["ident"] = np.eye(P, dtype=np.float32)
    d = np.arange(P)
    i = d % 64
    partner = np.where(i < 32, d + 32, d - 32)
    perm = np.zeros((P, P), np.float32)
    perm[partner, d] = 1.0
    out["perm"] = perm
    pos = np.arange(4096)
    inv = 10000.0 ** (-(2.0 / 64) * np.arange(32, dtype=np.float64))
    pv = np.where((d // 64)[:, None] == 0, (pos // 64)[None, :], (pos % 64)[None, :]).astype(np.float64)
    ang = pv * inv[i % 32][:, None]
    cos = np.cos(ang)
    sin = np.sin(ang) * np.where(i < 32, -1.0, 1.0)[:, None]
    rope = np.stack([cos, sin], axis=1)
    out["rope"] = rope.reshape(P, 2 * 4096).astype(BF)
    out["ropeq"] = np.ascontiguousarray(rope[:, :, s * NLAT:(s + 1) * NLAT]).reshape(P, 2 * NLAT).astype(BF)
    cc = np.arange(64)
    c64 = np.cos(2 * np.pi * np.outer(cc, cc) / 64) / 8.0
    s64 = np.sin(2 * np.pi * np.outer(cc, cc) / 64) / 8.0
    cs2 = np.zeros((P, 256), np.float32)
    for g2 in range(2):
        cs2[g2 * 64:(g2 + 1) * 64, g2 * 64:(g2 + 1) * 64] = c64
        cs2[g2 * 64:(g2 + 1) * 64, 128 + g2 * 64:128 + (g2 + 1) * 64] = s64
    out["cs2"] = cs2
    kk = s * NLAT + np.arange(NLAT)
    ph = (np.outer(pos, kk) % 4096).astype(np.float64) * (2 * np.pi / 4096)
    tab = np.stack([np.cos(ph) / 64.0, -np.sin(ph) / 64.0], axis=0).astype(np.float32)
    tab = tab.reshape(2, 8, 4, P, 4, 512)
    out["dft"] = np.ascontiguousarray(tab.transpose(4, 1, 3, 0, 2, 5)).reshape(4, 8, P, 4096).astype(BF)
    cpos = np.stack([np.arange(128), 128 + np.arange(128)], axis=0)
    ck = s * 128 + np.arange(128)
    cph = (cpos[:, :, None] * ck[None, None, :] % 256) * (2 * np.pi / 256)
    ctab = np.stack([np.cos(cph) / 16.0, -np.sin(cph) / 16.0], axis=0)
    out["cdft"] = np.ascontiguousarray(ctab.transpose(2, 0, 1, 3)).reshape(P, 512).astype(np.float32)
    _CONST_CACHE[s] = out
    return out


def _na_bias(rpb, s):
    out = np.full((8, P, 5, 768), NEG, np.float32)
    q = np.arange(P)
    w = np.arange(768)
    qcol = q % 64
    kcol = w % 64
    cstart = np.clip(qcol - 8, 0, 48)
    okc = (kcol[None, :] >= cstart[:, None]) & (kcol[None, :] < cstart[:, None] + 16)
    dc = kcol[None, :] - qcol[:, None] + 15
    rep = {0: 0, 1: 1, 2: 2, 3: 14, 4: 15}
    for ty, bi in rep.items():
        i = 16 * s + bi
        wb = 2 * bi if bi <= 13 else 28
        qrow = 2 * i + q // 64
        rstart = np.clip(qrow - 4, 0, 56)
        krow = wb + (32 * s - 4) + w // 64
        okr = (krow[None, :] >= rstart[:, None]) & (krow[None, :] < rstart[:, None] + 8)
        dr = krow[None, :] - qrow[:, None] + 7
        ok = okr & okc
        drc = np.clip(dr, 0, 14)
        dcc = np.clip(dc, 0, 30)
        vals = rpb[:, drc, dcc]
        out[:, :, ty, :] = np.where(ok[None], vals, NEG)
    outT = out.reshape(8, P, 5, 6, P).transpose(0, 4, 2, 3, 1)
    return np.ascontiguousarray(outT).reshape(8, P, 5 * 768)


def _layer_weights(l, inp, tag):
    j = l // 2
    w = {}
    w[f"wmod{tag}"] = inp["w_mod"][l]
    w[f"bmod{tag}"] = np.ascontiguousarray(inp["b_mod"][l].reshape(48, P).T)
    w[f"lnv{tag}"] = np.ascontiguousarray(
        np.stack([inp[k][l].reshape(KC, P).T for k in ("ln1_g", "ln1_b", "ln2_g", "ln2_b")], axis=1)).reshape(P, 32)
    if l % 2 == 0:
        w[f"win{tag}"] = inp["ab_w_in"][j]
        w[f"wout{tag}"] = inp["ab_w_out"][j]
        wf = inp["ab_w_fnet"][j]
        wfn = np.zeros((P, 2, P), np.float32)
        for jj in range(2):
            wfn[0:64, jj, 0:64] = wf[2 * jj]
            wfn[64:128, jj, 64:128] = wf[2 * jj + 1]
        w[f"wfn{tag}"] = wfn.reshape(P, 256)
        w[f"qkn{tag}"] = np.ascontiguousarray(np.stack([inp["ab_q_norm"][j], inp["ab_k_norm"][j]], axis=1))
    else:
        w[f"win{tag}"] = inp["na_w_in"][j]
        w[f"wout{tag}"] = inp["na_w_out"][j]
    w[f"wr{tag}"] = np.ascontiguousarray(inp["moe_w_router"][l].reshape(KC, P, 64).transpose(1, 0, 2)).reshape(P, KC * 64)
    w[f"mb{tag}"] = np.ascontiguousarray(np.broadcast_to(inp["moe_bias"][l][None, :], (P, 64)))
    wg = np.concatenate([inp["moe_w_gate"][l], inp["sh_w_gate"][l][None]], axis=0)
    wu = np.concatenate([inp["moe_w_up"][l], inp["sh_w_up"][l][None]], axis=0)
    wd = np.concatenate([inp["moe_w_down"][l], inp["sh_w_down"][l][None]], axis=0)
    moew = np.empty((NEXP, P, 6144), np.float32)
    moew[:, :, 0:2048] = wg.reshape(NEXP, KC, P, 256).transpose(0, 2, 1, 3).reshape(NEXP, P, 2048)
    moew[:, :, 2048:4096] = wu.reshape(NEXP, KC, P, 256).transpose(0, 2, 1, 3).reshape(NEXP, P, 2048)
    moew[:, :, 4096:6144] = wd.reshape(NEXP, 2, P, D).transpose(0, 2, 1, 3).reshape(NEXP, P, 2048)
    w[f"moew{tag}"] = moew
    return w


_PROG_CACHE = {}


def _get_prog(layers, debug=None):
    key = (tuple(layers), tuple(sorted((debug or {}).items())))
    if key not in _PROG_CACHE:
        pr = Prog(list(layers), debug)
        nc = pr.build()
        _PROG_CACHE[key] = (nc, set(pr.inputs.keys()))
    return _PROG_CACHE[key]


def _init_toks(inp):
    toks = []
    for r in range(8):
        b, s = r // 2, r % 2
        toks.append(np.concatenate([inp["x"][b, s * NLAT:(s + 1) * NLAT], inp["ctx"][b, s * NCTX:(s + 1) * NCTX]], axis=0))
    return toks


def _launch(inp, layers, toks, debug=None):
    nc, names = _get_prog(layers, debug)
    lw = {}
    for l in layers:
        lw.update(_layer_weights(l, inp, l))
    fm = [_fm(t) for t in toks]
    in_maps = []
    for r in range(8):
        b, s = r // 2, r % 2
        m = dict(lw)
        cs = _consts(s)
        for k in ("ident", "perm", "rope", "ropeq", "cs2", "dft", "cdft"):
            m[k] = cs[k]
        for l in layers:
            if l % 2 == 1:
                m[f"nab{l}"] = _na_bias(inp["na_rpb"][l // 2], s)
        m["cT"] = np.ascontiguousarray(
            np.stack([inp["c"][b].reshape(KC, P).T, inp["c_ctx"].reshape(KC, P).T], axis=2)).reshape(P, 16)
        m["xT"] = fm[r]
        m[f"xg{layers[0]}"] = np.ascontiguousarray(
            np.stack([fm[r - s].reshape(P, KC, NT), fm[r - s + 1].reshape(P, KC, NT)], axis=0).transpose(2, 0, 1, 3)
        ).reshape(KC, 2 * P, NT)
        in_maps.append({k: v for k, v in m.items() if k in names})
    res = run_bass_kernel_spmd(nc, in_maps, core_ids=list(range(8)))
    return [_unfm(res.results[r]["xout"], NT) for r in range(8)]


def run_layers(inp, layers=range(DEPTH), toks=None, debug=None, fused=False):
    inp = {k: np.asarray(v) for k, v in inp.items()}
    if toks is None:
        toks = _init_toks(inp)
    if fused:
        return _launch(inp, list(layers), toks, debug)
    for l in layers:
        toks = _launch(inp, [l], toks, debug)
    return toks


def kernel(**inputs):
    toks = run_layers(inputs, fused=True)
    out = np.empty((4, 4096, D), np.float32)
    for r in range(8):
        b, s = r // 2, r % 2
        out[b, s * NLAT:(s + 1) * NLAT] = toks[r][0:NLAT]
    return out
```

```python
import math
import numpy as np
import ml_dtypes
import concourse.bass as bass
import concourse.mybir as mybir
from concourse.bass_utils import run_bass_kernel_spmd
from contextlib import ExitStack

F32 = mybir.dt.float32
BF16 = mybir.dt.bfloat16
AF = mybir.ActivationFunctionType
ALU = mybir.AluOpType
AX = mybir.AxisListType

P = 128
D = 1024
KC = 8
NLAT = 2048
NCTX = 128
NT = NLAT + NCTX
DEPTH = 4
ALPHA = (2 * DEPTH) ** 0.25
LN_EPS = 1e-6
RMS_EPS = 1e-6
HD = 128
SCALE = HD ** -0.5
NEG = -1e30
NEXP = 65
ROUTE_SCALE = 2.5

ENGS = ("pe", "act", "dve", "pool", "sp")
DTSIZE = {F32: 4, BF16: 2}


class V:
    __slots__ = ("t", "ap")

    def __init__(self, t, ap):
        self.t = t
        self.ap = ap

    def __getitem__(self, idx):
        return V(self.t, self.ap[idx])

    def r(self, pat, **kw):
        return V(self.t, self.ap.rearrange(pat, **kw))

    def bc(self, shape):
        return V(self.t, self.ap.broadcast_to(shape))

    def bitcast(self, dt):
        return V(self.t, self.ap.bitcast(dt))


class T:
    def __init__(self, h, name, space, rng=None):
        self.h = h
        self.name = name
        self.space = space
        self.w = None
        self.r_ = []
        self.dsem = None
        self.dcnt = 0
        self.rng = rng

    def __getitem__(self, idx):
        return V(self, self.h[idx])

    def r(self, pat, **kw):
        return V(self, self.h.rearrange(pat, **kw))

    def v(self):
        return V(self, self.h)


class Ctx:
    def __init__(self, nc, arena_bytes=211968):
        self.nc = nc
        self.es = ExitStack()
        self.E = {"pe": nc.tensor, "act": nc.scalar, "dve": nc.vector, "pool": nc.gpsimd, "sp": nc.sync}
        self.sem = {}
        self.cnt = {}
        for e in ENGS:
            self.sem[e] = self.es.enter_context(nc.semaphore("s_" + e))
            self.cnt[e] = 0
        self.seen = {e: {} for e in ENGS}
        self.nbuf = 0
        self.sem_pool = []
        self.arena = self.es.enter_context(nc.sbuf_tensor("arena", [P, arena_bytes // 2], BF16))
        self.arena_bytes = arena_bytes
        self.free_list = [(0, arena_bytes)]
        self.grave = []
        self.live = {}
        self.banks = []
        for i in range(8):
            h = self.es.enter_context(nc.psum_tensor(f"bank{i}", [P, 512], F32))
            self.banks.append(T(h[:], f"bank{i}", "ps"))

    def alloc(self, name, nelem, dt, parts=P):
        nbytes = (nelem * DTSIZE[dt] + 63) // 64 * 64
        for i, (s, e) in enumerate(self.free_list):
            if e - s >= nbytes:
                self.free_list[i] = (s + nbytes, e)
                if self.free_list[i][0] == self.free_list[i][1]:
                    del self.free_list[i]
                break
        else:
            raise RuntimeError(f"arena OOM allocating {name} {nbytes}B; free={self.free_list}")
        ap = self.arena[0:parts, s // 2:(s + nbytes) // 2]
        if dt != BF16:
            ap = ap.bitcast(dt)
        ap = ap[:, 0:nelem]
        self.nbuf += 1
        t = T(ap, f"{name}_{self.nbuf}", "sb", (s, s + nbytes))
        keep = []
        for (gs, ge, toks) in self.grave:
            if gs < s + nbytes and s < ge:
                t.r_.extend(toks)
                if gs >= s and ge <= s + nbytes:
                    continue
            keep.append((gs, ge, toks))
        self.grave = keep
        return t

    def free(self, *ts):
        for t in ts:
            s, e = t.rng
            toks = [x for x in ([t.w] + t.r_) if x is not None]
            self.grave.append((s, e, self._compress(toks)))
            self.free_list.append((s, e))
            self.free_list.sort()
            merged = []
            for a, b in self.free_list:
                if merged and merged[-1][1] == a:
                    merged[-1] = (merged[-1][0], b)
                else:
                    merged.append((a, b))
            self.free_list = merged
            t.rng = None
            if t.dsem is not None:
                self.sem_pool.append((t.dsem, t.dcnt, t.dkey))
                t.dsem = None

    @staticmethod
    def _compress(toks):
        best = {}
        for tok in toks:
            key = tok[1] if tok[0] == "eng" else tok[3]
            v = tok[2]
            if key not in best or best[key][2] < v:
                best[key] = tok
        return list(best.values())

    def dram(self, name, shape, dt, kind="Internal"):
        h = self.nc.dram_tensor(name, list(shape), dt, kind=kind)
        return T(h.ap(), name, "dram")

    def _wait(self, e, tok):
        if tok is None:
            return
        if tok[0] == "eng":
            _, f, v = tok
            key = "e:" + f
            sem = self.sem[f]
        else:
            _, sem, v, key = tok
        if e == "pe" and tok[0] == "eng" and tok[1] == "pe":
            return
        if self.seen[e].get(key, 0) >= v:
            return
        self.E[e].wait_ge(sem, v)
        self.seen[e][key] = v

    def _deps(self, e, reads, writes, acc=False):
        for t in reads:
            self._wait(e, t.w)
        if not acc:
            for t in writes:
                self._wait(e, t.w)
                for tok in t.r_:
                    self._wait(e, tok)

    def op(self, e, fn, reads=(), writes=(), acc=False, inc=True):
        reads = [v.t for v in reads if v is not None]
        writes = [v.t for v in writes if v is not None]
        self._deps(e, reads, writes, acc)
        ins = fn(self.E[e])
        if inc:
            self.cnt[e] += 1
            ins.then_inc(self.sem[e], 1)
            tok = ("eng", e, self.cnt[e])
        else:
            tok = ("eng", e, self.cnt[e] + 1)
        for t in reads:
            t.r_.append(tok)
            if len(t.r_) > 24:
                t.r_ = self._compress(t.r_)
        for t in writes:
            t.w = tok
            t.r_ = []
        return ins

    def dma(self, q, out, in_, **kw):
        ot = out.t if isinstance(out, V) else None
        it = in_.t if isinstance(in_, V) else None
        oap = out.ap if isinstance(out, V) else out
        iap = in_.ap if isinstance(in_, V) else in_
        reads = [it] if it is not None else []
        writes = [ot] if ot is not None else []
        self._deps(q, reads, writes)
        owner = None
        for t in (ot, it):
            if t is not None and t.space != "dram":
                owner = t
                break
        if owner is None:
            owner = ot if ot is not None else it
        if owner.dsem is None:
            if self.sem_pool:
                sem, cnt, key = self.sem_pool.pop()
                if cnt > 0:
                    self._wait(q, ("dma", sem, cnt, key))
            else:
                self.nsem = getattr(self, "nsem", 0) + 1
                key = f"d:{self.nsem}"
                sem, cnt = self.es.enter_context(self.nc.semaphore(f"dsem{self.nsem}")), 0
            owner.dsem, owner.dcnt, owner.dkey = sem, cnt, key
        owner.dcnt += 16
        ins = self.E[q].dma_start(out=oap, in_=iap, **kw)
        ins.then_inc(owner.dsem, 16)
        tok = ("dma", owner.dsem, owner.dcnt, owner.dkey)
        for t in reads:
            t.r_.append(tok)
        for t in writes:
            t.w = tok
            t.r_ = []
        return tok

    def wait_all(self, e, ts):
        for t in ts:
            self._wait(e, t.w)
            for tok in t.r_:
                self._wait(e, tok)

    def mm(self, out, lhsT, rhs, start=True, stop=True, inc=None):
        self.op("pe", lambda e: e.matmul(out.ap, lhsT=lhsT.ap, rhs=rhs.ap, start=start, stop=stop),
                reads=[lhsT, rhs], writes=[out], acc=not start, inc=(stop if inc is None else inc))

    def transpose(self, out, in_, ident):
        self.op("pe", lambda e: e.transpose(out.ap, in_.ap, ident.ap), reads=[in_, ident], writes=[out])

    def act(self, out, in_, func, scale=None, bias=None, accum=None):
        kw = {}
        rd = [in_]
        wr = [out]
        if scale is not None:
            if isinstance(scale, V):
                kw["scale"] = scale.ap
                rd.append(scale)
            else:
                kw["scale"] = float(scale)
        if bias is not None:
            if isinstance(bias, V):
                kw["bias"] = bias.ap
                rd.append(bias)
            else:
                kw["bias"] = float(bias)
        if accum is not None:
            kw["accum_out"] = accum.ap
            wr.append(accum)
        self.op("act", lambda e: e.activation(out=out.ap, in_=in_.ap, func=func, **kw), reads=rd, writes=wr)

    def copy(self, eng, out, in_):
        if eng == "act":
            self.op("act", lambda e: e.copy(out=out.ap, in_=in_.ap), reads=[in_], writes=[out])
        else:
            self.op(eng, lambda e: e.tensor_copy(out=out.ap, in_=in_.ap), reads=[in_], writes=[out])

    def tt(self, eng, out, in0, in1, op):
        self.op(eng, lambda e: e.tensor_tensor(out=out.ap, in0=in0.ap, in1=in1.ap, op=op),
                reads=[in0, in1], writes=[out])

    def ts(self, eng, out, in0, s1, s2=None, op0=ALU.mult, op1=None):
        rd = [in0]
        a1 = s1.ap if isinstance(s1, V) else float(s1)
        if isinstance(s1, V):
            rd.append(s1)
        a2 = None
        if s2 is not None:
            a2 = s2.ap if isinstance(s2, V) else float(s2)
            if isinstance(s2, V):
                rd.append(s2)
        if op1 is None:
            self.op(eng, lambda e: e.tensor_scalar(out=out.ap, in0=in0.ap, scalar1=a1, scalar2=None, op0=op0),
                    reads=rd, writes=[out])
        else:
            self.op(eng, lambda e: e.tensor_scalar(out=out.ap, in0=in0.ap, scalar1=a1, scalar2=a2, op0=op0, op1=op1),
                    reads=rd, writes=[out])

    def stt(self, out, in0, scalar, in1, op0, op1, eng="dve"):
        rd = [in0, in1]
        a = scalar.ap if isinstance(scalar, V) else float(scalar)
        if isinstance(scalar, V):
            rd.append(scalar)
        self.op(eng, lambda e: e.scalar_tensor_tensor(out=out.ap, in0=in0.ap, scalar=a, in1=in1.ap, op0=op0, op1=op1),
                reads=rd, writes=[out])

    def memset(self, eng, out, val):
        self.op(eng, lambda e: e.memset(out.ap, val), writes=[out])

    def close(self):
        self.es.close()


class Prog:
    def __init__(self, layers, debug=None):
        self.layers = list(layers)
        self.debug = debug or {}
        self.nc = bass.Bass("TRN2", target_bir_lowering=False)
        self.c = Ctx(self.nc)
        self.inputs = {}
        self.rot = {}
        self.xg = {}

    def inp(self, name, shape, dt=F32):
        if name not in self.inputs:
            self.inputs[name] = self.nc.dram_tensor(name, list(shape), dt, kind="ExternalInput").ap()
        return self.inputs[name]

    def bank(self, i):
        return self.c.banks[i]

    def rr(self, key, n):
        v = self.rot.get(key, 0)
        self.rot[key] = v + 1
        return v % n

    def build(self):
        c = self.c
        nc = self.nc
        first = self.layers[0]
        self.XT = c.alloc("XT", KC * NT, F32)
        self.XT3 = self.XT.r("p (c t) -> p c t", c=KC)
        self.HB = c.alloc("HB", KC * NT, BF16)
        self.HB3 = self.HB.r("p (c t) -> p c t", c=KC)
        self.identb = c.alloc("identb", P, BF16)
        self.identf = c.alloc("identf", P, F32)
        c.dma("pool", self.identb.v(), self.inp("ident", [P, P]))
        c.dma("sp", self.identf.v(), self.inp("ident", [P, P]))
        self.ones_mean = c.alloc("ones_mean", P, BF16)
        self.ones_rms = c.alloc("ones_rms", P, BF16)
        self.ones1 = c.alloc("ones1", P, BF16)
        c.memset("dve", self.ones_mean.v(), 1.0 / D)
        self.ones_mean_f = c.alloc("ones_mean_f", P, F32)
        c.memset("dve", self.ones_mean_f.v(), 1.0 / D)
        c.memset("dve", self.ones_rms.v(), 1.0 / HD)
        c.memset("dve", self.ones1.v(), 1.0)
        self.MODV = c.alloc("MODV", 96, F32)
        self.MODV3 = self.MODV.r("p (m r) -> p m r", r=2)
        self.LNV = c.alloc("LNV", 32, F32)
        self.LNV3 = self.LNV.r("p (m c) -> p m c", c=KC)
        self.A2 = c.alloc("A2", 16, F32)
        self.A23 = self.A2.r("p (c r) -> p c r", r=2)
        self.B2 = c.alloc("B2", 16, F32)
        self.B23 = self.B2.r("p (c r) -> p c r", r=2)
        self.G = c.alloc("G", 17 * NEXP, F32)
        self.G3 = self.G.r("p (t e) -> p t e", e=NEXP)
        self.scT = c.alloc("scT", 16, F32)
        self.scT3 = self.scT.r("p (k r) -> p k r", r=2)
        c.dma("sp", self.scT.v(), self.inp("cT", [P, 16]))
        c.act(self.scT.v(), self.scT.v(), AF.Silu)
        c.dma("sp", self.XT.v(), self.inp("xT", [P, KC * NT]))
        for li, l in enumerate(self.layers):
            self.layer(l)
            if li + 1 < len(self.layers):
                self.exchange(l, self.layers[li + 1])
        xout = nc.dram_tensor("xout", [P, KC * NT], F32, kind="ExternalOutput").ap()
        tok = c.dma("sp", xout, self.XT.v())
        c._wait("sp", tok)
        c.close()
        return nc

    def exchange(self, l, lnext):
        c = self.c
        nc = self.nc
        sem = c.es.enter_context(nc.semaphore(f"ccsem{l}"))
        tok = ("dma", sem, KC, f"cc{l}")
        xgh = nc.dram_tensor(f"xgi{lnext}", [KC, 2 * P, NT], F32)
        xg = T(xgh.ap(), f"xgi{lnext}", "dram")
        for cc in range(KC):
            xsh = nc.dram_tensor(f"xs{l}_{cc}", [P, NT], F32)
            xs = T(xsh.ap(), f"xs{l}_{cc}", "dram")
            c.dma("sp", xs.v(), self.XT3[:, cc, :])
            c._deps("pool", [xs], [xg] if cc == 0 else [])
            ins = nc.gpsimd.collective_compute("AllGather", ALU.bypass, replica_groups=[[0, 1], [2, 3], [4, 5], [6, 7]],
                                               ins=[xsh.ap().opt()], outs=[xgh.ap()[cc]])
            ins.then_inc(sem)
            xs.r_.append(tok)
        xg.w = tok
        xg.r_ = []
        self.xg[lnext] = xg

    def layer(self, l):
        self.emit_mod(l)
        if self.debug.get("stop") == "mod":
            return
        self.emit_h1(l)
        if self.debug.get("stop") == "h1":
            return
        if l % 2 == 0:
            self.emit_gqa(l)
        else:
            self.emit_na(l)
        stop = self.debug.get("stop")
        if stop == "mixer":
            return
        self.emit_ln(l, 1)
        if stop == "ln1":
            return
        self.emit_moe(l)
        if stop == "moe":
            return
        self.emit_ln(l, 2)

    def emit_mod(self, l):
        c = self.c
        wmod = self.inp(f"wmod{l}", [D, 6 * D]).rearrange("(k p) n -> p k n", p=P)
        bmod = c.alloc("bmod", 48, F32)
        c.dma("sp", bmod.v(), self.inp(f"bmod{l}", [P, 48]))
        c.dma("sp", self.LNV.v(), self.inp(f"lnv{l}", [P, 32]))
        st = [c.alloc("wmst", KC * 512, F32) for _ in range(2)]
        scb3 = self.scT3
        ps = self.bank(0)
        ps3 = ps[:, 0:96].r("p (m r) -> p m r", r=2)
        for blk in range(12):
            s = st[blk % 2]
            s3 = s.r("p (k n) -> p k n", k=KC)
            c.dma("sp", s3, wmod[:, :, blk * 512:(blk + 1) * 512])
            for fc in range(4):
                for k in range(KC):
                    c.mm(ps3[:, blk * 4 + fc, :], s3[:, k, fc * 128:(fc + 1) * 128], scb3[:, k, :],
                         start=(k == 0), stop=(k == KC - 1))
        M3 = self.MODV3
        for r in range(2):
            c.tt("dve", M3[:, :, r], ps3[:, :, r], bmod.v(), ALU.add)
        c.ts("dve", M3[:, 8:16, :], M3[:, 8:16, :], 1.0, op0=ALU.add)
        c.ts("dve", M3[:, 32:40, :], M3[:, 32:40, :], 1.0, op0=ALU.add)
        c.ts("dve", M3[:, 16:24, :], M3[:, 16:24, :], 1.0 / ALPHA, op0=ALU.mult)
        c.ts("dve", M3[:, 40:48, :], M3[:, 40:48, :], 1.0 / ALPHA, op0=ALU.mult)
        for r in range(2):
            c.tt("dve", self.A23[:, :, r], self.LNV3[:, 0, :], M3[:, 32:40, r], ALU.mult)
            c.tt("dve", self.B23[:, :, r], self.LNV3[:, 1, :], M3[:, 32:40, r], ALU.mult)
            c.tt("dve", self.B23[:, :, r], self.B23[:, :, r], M3[:, 24:32, r], ALU.add)
        c.free(bmod, *st)

    def mod_h(self, eng, out, in_, cc, r):
        if eng == "act":
            self.c.act(out, in_, AF.Identity, scale=self.MODV3[:, 8 + cc, r:r + 1], bias=self.MODV3[:, cc, r:r + 1])
        else:
            self.c.ts(eng, out, in_, self.MODV3[:, 8 + cc, r:r + 1], self.MODV3[:, cc, r:r + 1], ALU.mult, ALU.add)

    def emit_h1(self, l):
        for cc in range(KC):
            eng = ("dve", "act")[cc % 2]
            self.mod_h(eng, self.HB3[:, cc, 0:NLAT], self.XT3[:, cc, 0:NLAT], cc, 0)
            self.mod_h(eng, self.HB3[:, cc, NLAT:NT], self.XT3[:, cc, NLAT:NT], cc, 1)

    def xg_ap(self, l):
        if l in self.xg:
            return self.xg[l]
        ap = self.inp(f"xg{l}", [KC, 2 * P, NT])
        self.xg[l] = T(ap, f"xg{l}", "dram")
        return self.xg[l]

    def gstream(self, l, groups):
        c = self.c
        xg = self.xg_ap(l)
        st = [c.alloc("xost", KC * 256, F32) for _ in range(3)]
        ho = [c.alloc("xoh", KC * 256, BF16) for _ in range(3)]
        views = {}

        def load(i):
            slot, c0, n, r = groups[i]
            s3 = st[i % 3].r("p (c t) -> p c t", c=KC)[:, :, 0:n]
            c.dma("sp", s3, V(xg, xg.h[:, slot * P:(slot + 1) * P, c0:c0 + n].rearrange("c p t -> p c t")))
            views[i] = s3

        def prep(i):
            slot, c0, n, r = groups[i]
            s3 = views.pop(i)
            h3 = ho[i % 3].r("p (c t) -> p c t", c=KC)[:, :, 0:n]
            for cc in range(KC):
                self.mod_h(("dve", "act")[cc % 2], h3[:, cc, :], s3[:, cc, :], cc, r)
            return h3

        ng = len(groups)
        load(0)
        if ng > 1:
            load(1)
        h_next = prep(0)
        for i in range(ng):
            h_cur = h_next
            if i + 2 < ng:
                load(i + 2)
            if i + 1 < ng:
                h_next = prep(i + 1)
            slot, c0, n, r = groups[i]
            yield (slot, c0, n, r, h_cur)
        c.free(*st, *ho)

    def load_w(self, name, dram_ap, ncols):
        c = self.c
        t = c.alloc(name, KC * ncols, BF16)
        t3 = t.r("p (k n) -> p k n", k=KC)
        c.dma("pool", t3, dram_ap.rearrange("(k p) n -> p k n", p=P))
        return t, t3

    def outproj(self, oT, wo, col0, n, r):
        c = self.c
        oTs = oT if isinstance(oT, list) else [oT]
        wos = wo if isinstance(wo, list) else [wo]
        for cc in range(KC):
            y = self.bank(6 + self.rr("yb", 2))[:, 0:n]
            for i, (o, w) in enumerate(zip(oTs, wos)):
                c.mm(y, w[:, cc * 128:(cc + 1) * 128], o, start=(i == 0), stop=(i == len(oTs) - 1))
            xs = self.XT3[:, cc, col0:col0 + n]
            c.stt(xs, y, self.MODV3[:, 16 + cc, r:r + 1], xs, ALU.mult, ALU.add)

    def rmsnorm_rope(self, ps, n, gain, out, rope_col0=None, table=None):
        c = self.c
        k = self.rr("rn", 2)
        tm = self.rn_tmp[k]
        sq = tm["sq"][:, 0:n]
        rstd = tm["rstd"][:, 0:n]
        c.act(sq, ps, AF.Square)
        ss = self.bank(2 + k)[:, 0:n]
        c.mm(ss, self.ones_rms.v(), sq)
        c.act(rstd, ss, AF.Ln, bias=RMS_EPS)
        c.act(rstd, rstd, AF.Exp, scale=-0.5)
        if rope_col0 is None:
            c.stt(out, ps, gain, rstd, ALU.mult, ALU.mult)
            return
        qn = tm["qn"][:, 0:n]
        c.stt(qn, ps, gain, rstd, ALU.mult, ALU.mult)
        rp = tm["rope"].r("p (a n) -> p a n", a=2)[:, :, 0:n]
        c.dma("sp", rp, (table if table is not None else self.rope_dram)[:, :, rope_col0:rope_col0 + n])
        pp = self.bank(4 + k)[:, 0:n]
        c.mm(pp, self.perm.v(), qn)
        t1 = tm["t1"][:, 0:n]
        t2 = tm["t2"][:, 0:n]
        c.tt("dve", t1, qn, rp[:, 0, :], ALU.mult)
        c.tt("dve", t2, pp, rp[:, 1, :], ALU.mult)
        c.tt("dve", out, t1, t2, ALU.add)

    def attn_T(self, KT, VV3, qv, n, tiles):
        c = self.c
        O = self.bank(2)[:, 0:n]
        L = self.bank(3)[:, 0:n]
        nt = len(tiles)
        sbanks = (0, 1, 4, 5)
        LA = 3

        def S(i):
            s = self.bank(sbanks[self.rr("sb", 4)])[:, 0:n]
            kt = tiles[i]
            c.mm(s, KT[:, kt * 128:(kt + 1) * 128], qv)
            return s
        q = [S(i) for i in range(min(LA, nt))]
        for i in range(nt):
            s_cur = q.pop(0)
            if i + LA < nt:
                q.append(S(i + LA))
            pt = self.pt_tmp[self.rr("pt", len(self.pt_tmp))][:, 0:n]
            c.act(pt, s_cur, AF.Exp, scale=SCALE)
            c.mm(O, VV3[:, tiles[i], :], pt, start=(i == 0), stop=(i == nt - 1))
            c.mm(L, self.ones1.v(), pt, start=(i == 0), stop=(i == nt - 1))
        kk = self.rr("ot", 2)
        rl = self.rl_tmp[kk][:, 0:n]
        c.act(rl, L, AF.Ln)
        c.act(rl, rl, AF.Exp, scale=-1.0)
        oT = self.ot_tmp[kk][:, 0:n]
        c.tt("dve", oT, O, rl, ALU.mult)
        return oT

    def emit_gqa(self, l):
        c = self.c
        j = l // 2
        win = self.inp(f"win{l}", [D, 1536])
        wout = self.inp(f"wout{l}", [D, D])
        g_own = [(i * 256, 256, 0) for i in range(8)] + [(NLAT, 128, 1)]
        wf, wf3 = self.load_w("wf", win[:, 0:256], 256)
        cs2 = c.alloc("cs2", 256, BF16)
        c.dma("pool", cs2.v(), self.inp("cs2", [P, 256]))
        wfn = c.alloc("wfn", 256, BF16)
        wfn3 = wfn.r("p (j m) -> p j m", j=2)
        c.dma("pool", wfn.v(), self.inp(f"wfn{l}", [P, 256]))
        wo_f = c.alloc("wo_f", 2 * D, BF16)
        wo_f3 = wo_f.r("p (j n) -> p j n", j=2)
        c.dma("pool", wo_f3, wout[0:256, :].rearrange("(j p) n -> p j n", p=P))
        cdft = c.alloc("cdft", 512, BF16)
        cdft4 = cdft.r("p (a t k) -> p a t k", a=2, t=2)
        c.dma("pool", cdft.v(), self.inp("cdft", [P, 512]))
        FCS = c.alloc("FCS", 34 * 512, BF16)
        FCS3 = FCS.r("p (t f) -> p t f", f=512)
        fts = [c.alloc("fts", 512, BF16) for _ in range(2)]

        def stage1(buf0, n, h3):
            fps = self.bank(self.rr("fps", 2))
            fps3 = fps[:, 0:2 * n].r("p (j t) -> p j t", j=2)
            for jj in range(2):
                for k in range(KC):
                    c.mm(fps3[:, jj, :], wf3[:, k, jj * 128:(jj + 1) * 128], h3[:, k, :], start=(k == 0), stop=(k == KC - 1))
            ft = fts[self.rr("fts", 2)]
            ft3 = ft[:, 0:2 * n].r("p (j t) -> p j t", j=2)
            c.copy("act", ft3, fps3)
            for tt in range(n // 128):
                cps = self.bank(2 + self.rr("cps", 2))
                for jj in range(2):
                    c.mm(cps[:, jj * 256:(jj + 1) * 256], ft3[:, jj, tt * 128:(tt + 1) * 128], cs2.v())
                c.copy("dve", FCS3[:, (buf0 + tt * 128) // 128, :], cps.v())

        g_all = [(slot, c0, n, r) for slot in range(2) for (c0, n, r) in g_own]
        for (slot, c0, n, r, h3) in self.gstream(l, g_all):
            stage1(slot * NT + c0, n, h3)
        c.free(wf, cs2, *fts)

        dft = self.inp("dft", [4, 8, P, 4096], BF16)
        dst = [c.alloc("dftst", 4096, BF16) for _ in range(2)]
        frs = [c.alloc("frs", 512, BF16) for _ in range(2)]
        ots = [c.alloc("ofs", 512, BF16) for _ in range(2)]

        def finish(acc, n, col0, r):
            oTs = []
            for jj in range(2):
                fr = frs[jj][:, 0:n]
                c.copy("act", fr, acc[jj])
                wps = self.bank(6 + self.rr("yb", 2))[:, 0:n]
                c.mm(wps, wfn3[:, jj, :], fr)
                o = ots[jj][:, 0:n]
                c.copy("act", o, wps)
                oTs.append(o)
            self.outproj(oTs, [wo_f3[:, 0, :], wo_f3[:, 1, :]], col0, n, r)

        for kc in range(4):
            acc = [self.bank(4)[:, 0:512], self.bank(5)[:, 0:512]]
            for ng in range(8):
                dt_ = dst[self.rr("dst", 2)]
                c.dma("sp", dt_.v(), dft[kc, ng])
                dt4 = dt_.r("p (a i k) -> p a i k", a=2, i=4)
                for ni in range(4):
                    nn = ng * 4 + ni
                    ft_i = nn if nn < 16 else nn + 1
                    for jj in range(2):
                        c.mm(acc[jj], FCS3[:, ft_i, jj * 256:jj * 256 + 128], dt4[:, 0, ni, :], start=(nn == 0), stop=False)
                        c.mm(acc[jj], FCS3[:, ft_i, jj * 256 + 128:(jj + 1) * 256], dt4[:, 1, ni, :], start=False, stop=(nn == 31),
                             inc=(nn == 31 or (ni == 3 and jj == 1)))
            finish(acc, 512, kc * 512, 0)
        acc = [self.bank(4)[:, 0:128], self.bank(5)[:, 0:128]]
        for ti, ft_i in enumerate((16, 33)):
            for jj in range(2):
                c.mm(acc[jj], FCS3[:, ft_i, jj * 256:jj * 256 + 128], cdft4[:, 0, ti, :], start=(ti == 0), stop=False)
                c.mm(acc[jj], FCS3[:, ft_i, jj * 256 + 128:(jj + 1) * 256], cdft4[:, 1, ti, :], start=False, stop=(ti == 1))
        finish(acc, 128, NLAT, 1)
        c.free(FCS, wfn, wo_f, cdft, *dst, *frs, *ots)
        if self.debug.get("stop") == "fnet":
            return

        self.rope_dram = self.inp("rope", [P, 2 * 4096], BF16).rearrange("p (a n) -> p a n", a=2)
        ropeq = self.inp("ropeq", [P, 2 * NLAT], BF16).rearrange("p (a n) -> p a n", a=2)
        self.perm = c.alloc("perm", P, BF16)
        c.dma("pool", self.perm.v(), self.inp("perm", [P, P]))
        qkn = c.alloc("qkn", 2, F32)
        c.dma("sp", qkn.v(), self.inp(f"qkn{l}", [P, 2]))
        self.rn_tmp = [dict(sq=c.alloc("sq", 256, BF16), rstd=c.alloc("rstd", 256, F32), qn=c.alloc("qn", 256, BF16),
                            rope=c.alloc("rope", 512, BF16), t1=c.alloc("t1", 256, F32), t2=c.alloc("t2", 256, F32))
                       for _ in range(2)]
        self.pt_tmp = [c.alloc("pt", 512, BF16) for _ in range(5)]
        self.rl_tmp = [c.alloc("rl", 512, F32) for _ in range(2)]
        self.ot_tmp = [c.alloc("ot", 512, BF16) for _ in range(2)]
        KTt = c.alloc("KT", 2 * NT, BF16)
        KT = KTt.v()
        VVt = c.alloc("VV", 34 * 128, BF16)
        VV3 = VVt.r("p (t d) -> p t d", d=128)
        QTt = [c.alloc("QT", NT, BF16) for _ in range(2)]
        for g in range(2):
            wk, wk3 = self.load_w("wk", win[:, 1024 + g * 128:1024 + (g + 1) * 128], 128)
            wv, wv3 = self.load_w("wv", win[:, 1280 + g * 128:1280 + (g + 1) * 128], 128)

            def kv(buf0, n, r, h3, rope0):
                kps = self.bank(self.rr("kps", 2))[:, 0:n]
                for k in range(KC):
                    c.mm(kps, wk3[:, k, :], h3[:, k, :], start=(k == 0), stop=(k == KC - 1))
                self.rmsnorm_rope(kps, n, qkn[:, 1:2], KT[:, buf0:buf0 + n], rope0)
                for tt in range(n // 128):
                    vps = self.bank(6 + self.rr("yb", 2))[:, 0:128]
                    for k in range(KC):
                        c.mm(vps, h3[:, k, tt * 128:(tt + 1) * 128], wv3[:, k, :], start=(k == 0), stop=(k == KC - 1))
                    c.copy("act", VV3[:, buf0 // 128 + tt, :], vps)

            for (slot, c0, n, r, h3) in self.gstream(l, g_all):
                kv(slot * NT + c0, n, r, h3, (slot * NLAT + c0) if r == 0 else None)
            c.free(wk, wv)
            for hh in range(3):
                hg = g * 3 + hh
                wq, wq3 = self.load_w("wq", win[:, 256 + hg * 128:256 + (hg + 1) * 128], 128)
                wo = c.alloc("wo", D, BF16)
                c.dma("pool", wo.v(), wout[(2 + hg) * 128:(3 + hg) * 128, :])
                QT = QTt[self.rr("qt", 2)].v()
                for (c0, n, r) in g_own:
                    qps = self.bank(self.rr("kps", 2))[:, 0:n]
                    for k in range(KC):
                        c.mm(qps, wq3[:, k, :], self.HB3[:, k, c0:c0 + n], start=(k == 0), stop=(k == KC - 1))
                    self.rmsnorm_rope(qps, n, qkn[:, 0:1], QT[:, c0:c0 + n], c0 if r == 0 else None, table=ropeq)
                for qc in range(4):
                    oT = self.attn_T(KT, VV3, QT[:, qc * 512:(qc + 1) * 512], 512, list(range(34)))
                    self.outproj(oT, wo.v(), qc * 512, 512, 0)
                oT = self.attn_T(KT, VV3, QT[:, NLAT:NT], 128, [16, 33])
                self.outproj(oT, wo.v(), NLAT, 128, 1)
                c.free(wq, wo)
        c.free(KTt, VVt, *QTt, self.perm, qkn, *self.pt_tmp, *self.rl_tmp, *self.ot_tmp)
        for tm in self.rn_tmp:
            c.free(*tm.values())

    def emit_na(self, l):
        c = self.c
        win = self.inp(f"win{l}", [D, 3 * D])
        wout = self.inp(f"wout{l}", [D, D])
        nab = self.inp(f"nab{l}", [8, P, 5 * 768])
        HOt = c.alloc("HO", KC * 768, BF16)
        HO3 = HOt.r("p (c t) -> p c t", c=KC)
        hgroups = [(0, NLAT - 256, 256, 0), (1, 0, 256, 0), (0, NLAT, 128, 1), (1, NLAT, 128, 1)]
        hdst = [0, 256, 512, 640]
        for gi_, (slot, c0, n, r, h3) in enumerate(self.gstream(l, hgroups)):
            for cc in range(KC):
                c.copy(("dve", "act")[cc % 2], HO3[:, cc, hdst[gi_]:hdst[gi_] + n], h3[:, cc, :])
        NK = 2816
        srcs = [(0, 256, HO3[:, :, 0:256])] + \
               [(256 + i * 512, 512, self.HB3[:, :, i * 512:(i + 1) * 512]) for i in range(4)] + \
               [(2304, 256, HO3[:, :, 256:512]), (2560, 256, HO3[:, :, 512:768])]
        KTs = [c.alloc("KT", NK, BF16) for _ in range(2)]
        VVs = [c.alloc("VV", NK, BF16) for _ in range(2)]
        QTs = [c.alloc("QT", NT, BF16) for _ in range(2)]
        nbs = [c.alloc("nb", 5 * 768, F32) for _ in range(1)]
        ss_tmp = [c.alloc("ss", 768, F32) for _ in range(2)]
        pt_tmp = [c.alloc("pt", 1024, BF16) for _ in range(3)]
        rl_tmp = [c.alloc("rl", 512, F32) for _ in range(2)]
        og_tmp = [c.alloc("og", 512, BF16) for _ in range(2)]

        for h in range(8):
            wq, wq3 = self.load_w("wq", win[:, h * 128:(h + 1) * 128], 128)
            wk, wk3 = self.load_w("wk", win[:, D + h * 128:D + (h + 1) * 128], 128)
            wv, wv3 = self.load_w("wv", win[:, 2 * D + h * 128:2 * D + (h + 1) * 128], 128)
            wo = c.alloc("wo", D, BF16)
            c.dma("pool", wo.v(), wout[h * 128:(h + 1) * 128, :])
            nb = nbs[0]
            c.dma("sp", nb.v(), nab[h])
            nb3 = nb.r("p (t w) -> p t w", w=768)
            KT = KTs[h % 2].v()
            VV3 = VVs[h % 2].r("p (t d) -> p t d", d=128)
            QT = QTs[h % 2].v()
            for (b0, n, h3) in srcs:
                kps = self.bank(6 + self.rr("yb", 2))[:, 0:n]
                for k in range(KC):
                    c.mm(kps, wk3[:, k, :], h3[:, k, :], start=(k == 0), stop=(k == KC - 1))
                c.copy("act", KT[:, b0:b0 + n], kps)
                for tt in range(n // 128):
                    vps = self.bank(6 + self.rr("yb", 2))[:, 0:128]
                    for k in range(KC):
                        c.mm(vps, h3[:, k, tt * 128:(tt + 1) * 128], wv3[:, k, :], start=(k == 0), stop=(k == KC - 1))
                    c.copy("dve", VV3[:, b0 // 128 + tt, :], vps)
            for (c0, n) in [(i * 512, 512) for i in range(4)] + [(NLAT, 128)]:
                qps = self.bank(6 + self.rr("yb", 2))[:, 0:n]
                for k in range(KC):
                    c.mm(qps, wq3[:, k, :], self.HB3[:, k, c0:c0 + n], start=(k == 0), stop=(k == KC - 1))
                c.copy("act", QT[:, c0:c0 + n], qps)
            jobs = []
            for bi in range(16):
                ty = {0: 0, 1: 1, 14: 3, 15: 4}.get(bi, 2)
                wb = 2 * bi if bi <= 13 else 28
                t0_ = wb * 64 // 128
                jobs.append((bi * 128, [t0_ + m for m in range(6)] + [20, 21], ty))
            jobs.append((NLAT, [20, 21], None))

            def S_of(job):
                q0, tiles, ty = job
                k = self.rr("nas", 2)
                SA = self.bank(2 * k)
                SB = self.bank(2 * k + 1)
                for m, kt in enumerate(tiles):
                    dst = (SA if m < 4 else SB)[:, (m % 4) * 128:(m % 4 + 1) * 128]
                    c.mm(dst, KT[:, kt * 128:(kt + 1) * 128], QT[:, q0:q0 + 128])
                return SA, SB

            def P_of(job, S):
                q0, tiles, ty = job
                SA, SB = S
                PT = pt_tmp[self.rr("napt", 3)]
                if ty is not None:
                    SS = ss_tmp[self.rr("nass", 2)]
                    c.stt(SS[:, 0:512], SA[:, 0:512], SCALE, nb3[:, ty, 0:512], ALU.mult, ALU.add)
                    c.stt(SS[:, 512:768], SB[:, 0:256], SCALE, nb3[:, ty, 512:768], ALU.mult, ALU.add)
                    c.act(PT[:, 0:768], SS[:, 0:768], AF.Exp)
                    c.act(PT[:, 768:1024], SB[:, 256:512], AF.Exp, scale=SCALE)
                else:
                    c.act(PT[:, 0:256], SA[:, 0:256], AF.Exp, scale=SCALE)
                return PT

            def PV_of(job, PT, O_dst, L_dst):
                q0, tiles, ty = job
                nt = len(tiles)
                for m, kt in enumerate(tiles):
                    c.mm(O_dst, VV3[:, kt, :], PT[:, m * 128:(m + 1) * 128], start=(m == 0), stop=(m == nt - 1))
                for m, kt in enumerate(tiles):
                    c.mm(L_dst, self.ones1.v(), PT[:, m * 128:(m + 1) * 128], start=(m == 0), stop=(m == nt - 1))

            def finish_group(gjobs):
                n = 128 * len(gjobs)
                col0 = gjobs[0][0]
                r = 0 if gjobs[0][2] is not None else 1
                kk = self.rr("narl", 2)
                rl = rl_tmp[kk][:, 0:n]
                c.act(rl, self.bank(5)[:, 0:n], AF.Ln)
                c.act(rl, rl, AF.Exp, scale=-1.0)
                og = og_tmp[kk][:, 0:n]
                c.tt("dve", og, self.bank(4)[:, 0:n], rl, ALU.mult)
                self.outproj(og, wo.v(), col0, n, r)

            S_next = S_of(jobs[0])
            gjobs = []
            for ji, job in enumerate(jobs):
                S_cur = S_next
                if ji + 1 < len(jobs):
                    S_next = S_of(jobs[ji + 1])
                PT = P_of(job, S_cur)
                slot = len(gjobs)
                PV_of(job, PT, self.bank(4)[:, slot * 128:(slot + 1) * 128], self.bank(5)[:, slot * 128:(slot + 1) * 128])
                gjobs.append(job)
                if len(gjobs) == 4 or ji == len(jobs) - 1 or jobs[ji + 1][2] is None:
                    finish_group(gjobs)
                    gjobs = []
            c.free(wq, wk, wv, wo)
        c.free(HOt, *KTs, *VVs, *QTs, *nbs, *ss_tmp, *pt_tmp, *rl_tmp, *og_tmp)

    def emit_ln(self, l, which):
        c = self.c
        gi, bi_ = (0, 1) if which == 1 else (2, 3)
        eps = LN_EPS / (ALPHA * ALPHA)
        zq = [c.alloc("zq", KC * 512, BF16) for _ in range(2)]
        tmp = [dict(M=c.alloc("M", 512, F32), m2=c.alloc("m2", 512, F32), rstd=c.alloc("rstdl", 512, F32)) for _ in range(2)]
        t1s = [c.alloc("t1", 512, F32) for _ in range(3)]
        if which == 1:
            h2f = [c.alloc("h2f", KC * 512, F32) for _ in range(2)]
            wr = c.alloc("wr", KC * 64, F32)
            wr3 = wr.r("p (k e) -> p k e", k=KC)
            c.dma("sp", wr.v(), self.inp(f"wr{l}", [P, KC * 64]))
            mb = c.alloc("mb", 64, F32)
            c.dma("sp", mb.v(), self.inp(f"mb{l}", [P, 64]))
            gt = [c.alloc("gt", 64 * 3 + 16, F32) for _ in range(2)]
            GTS = c.alloc("GTS", NT, F32)
            c.memset("dve", self.G3[:, :, 64:65], 1.0)
            self.GTD = c.dram(f"gtd{l}", [NEXP, NT], F32)
        groups = [(i * 512, 512, 0) for i in range(4)] + [(NLAT, 128, 1)]
        def stats(gidx):
            c0, n, r = groups[gidx]
            k = gidx % 2
            zq3 = zq[k].r("p (c t) -> p c t", c=KC)[:, :, 0:n]
            mean = self.bank(0 if k == 0 else 6)[:, 0:n]
            msq = self.bank(1 if k == 0 else 7)[:, 0:n]
            for cc in range(KC):
                xs = self.XT3[:, cc, c0:c0 + n]
                c.act(zq3[:, cc, :], xs, AF.Square)
            for cc in range(KC):
                c.mm(mean, self.ones_mean_f.v(), self.XT3[:, cc, c0:c0 + n], start=(cc == 0), stop=(cc == KC - 1))
            for cc in range(KC):
                c.mm(msq, self.ones_mean.v(), zq3[:, cc, :], start=(cc == 0), stop=(cc == KC - 1))
            M = tmp[k]["M"][:, 0:n]
            m2 = tmp[k]["m2"][:, 0:n]
            rstd = tmp[k]["rstd"][:, 0:n]
            c.copy("act", M, mean)
            c.act(m2, mean, AF.Square)
            c.tt("dve", m2, msq, m2, ALU.subtract)
            c.act(rstd, m2, AF.Ln, bias=eps)
            c.act(rstd, rstd, AF.Exp, scale=-0.5)

        stats(0)
        for gidx, (c0, n, r) in enumerate(groups):
            k = gidx % 2
            M = tmp[k]["M"][:, 0:n]
            rstd = tmp[k]["rstd"][:, 0:n]
            if gidx + 1 < len(groups):
                stats(gidx + 1)
            if which == 1:
                h2f3 = h2f[k].r("p (c t) -> p c t", c=KC)[:, :, 0:n]
            for cc in range(KC):
                xs = self.XT3[:, cc, c0:c0 + n]
                t1 = t1s[self.rr("t1", 3)][:, 0:n]
                c.tt("dve", t1, xs, M, ALU.subtract)
                c.tt("dve", t1, t1, rstd, ALU.mult)
                c.act(xs, t1, AF.Identity, scale=self.LNV3[:, gi, cc:cc + 1], bias=self.LNV3[:, bi_, cc:cc + 1])
                if which == 1:
                    c.act(h2f3[:, cc, :], t1, AF.Identity, scale=self.A23[:, cc, r:r + 1], bias=self.B23[:, cc, r:r + 1])
                    c.copy("dve", self.HB3[:, cc, c0:c0 + n], h2f3[:, cc, :])
            if which == 1:
                for tt in range(n // 128):
                    tile_i = c0 // 128 + tt
                    lg = self.bank(2 + self.rr("lg", 2))[:, 0:64]
                    for cc in range(KC):
                        c.mm(lg, h2f3[:, cc, tt * 128:(tt + 1) * 128], wr3[:, cc, :], start=(cc == 0), stop=(cc == KC - 1))
                    g_ = gt[self.rr("gt", 2)]
                    sc, sel, wsel, m8, den = g_[:, 0:64], g_[:, 64:128], g_[:, 128:192], g_[:, 192:200], g_[:, 200:201]
                    rden = g_[:, 201:202]
                    c.act(sc, lg, AF.Sigmoid)
                    c.tt("dve", sel, sc, mb.v(), ALU.add)
                    c.op("dve", lambda e: e.max(out=m8.ap, in_=sel.ap), reads=[sel], writes=[m8])
                    c.ts("dve", sel, sel, m8[:, 7:8], op0=ALU.is_ge)
                    c.tt("dve", wsel, sel, sc, ALU.mult)
                    c.op("dve", lambda e: e.reduce_sum(out=den.ap, in_=wsel.ap, axis=AX.X), reads=[wsel], writes=[den])
                    c.op("dve", lambda e: e.reciprocal(out=rden.ap, in_=den.ap), reads=[den], writes=[rden])
                    c.ts("dve", self.G3[:, tile_i, 0:64], wsel, rden, ROUTE_SCALE, ALU.mult, ALU.mult)
                    gtp = self.bank(4 + self.rr("gtp", 2))[0:NEXP, 0:128]
                    c.transpose(gtp, self.G3[:, tile_i, :], self.identf.v())
                    c.copy("act", GTS[0:NEXP, tile_i * 128:(tile_i + 1) * 128], gtp)
        if which == 1:
            c.dma("sp", self.GTD.v(), GTS[0:NEXP, :])
            c.free(*h2f, wr, mb, *gt, GTS)
        c.free(*zq, *t1s)
        for t_ in tmp:
            c.free(*t_.values())

    def emit_moe(self, l):
        c = self.c
        moew = self.inp(f"moew{l}", [NEXP, P, 6144])
        NWB = 4
        WB = [c.alloc("WB", 6144, BF16) for _ in range(NWB)]
        GB = [c.alloc("GB", NT, F32) for _ in range(NWB)]
        sT = [c.alloc("sT", 512, BF16) for _ in range(2)]
        tT = [c.alloc("tT", 512, BF16) for _ in range(2)]
        aT = [c.alloc("aT", 512, BF16) for _ in range(4)]
        groups = [(i * 256, 256, 0) for i in range(8)] + ([(NLAT, 128, 1)] if l < DEPTH - 1 else [])
        pairs = [(2 * i, 2 * i + 1) for i in range(32)] + [(64,)]

        def prefetch(e):
            w = WB[e % NWB]
            for q in range(3):
                c.dma("pool", w[:, q * 2048:(q + 1) * 2048], moew[e][:, q * 2048:(q + 1) * 2048])
            c.dma("sp", GB[e % NWB].r("p (o t) -> p o t", o=1), V(self.GTD, self.GTD.h[e:e + 1, :].partition_broadcast(P)))

        def down_mm(sts):
            e0, c0, n, r, _ = sts[0]
            Y = [self.bank(4 + cc // 2)[:, (cc % 2) * 256:(cc % 2) * 256 + n] for cc in range(KC)]
            nmm = 2 * len(sts)
            for cc in range(KC):
                i = 0
                for (e, _, _, _, a3) in sts:
                    w = WB[e % NWB].v()
                    for jj in range(2):
                        c.mm(Y[cc], w[:, 4096 + jj * 1024 + cc * 128:4096 + jj * 1024 + (cc + 1) * 128], a3[:, jj, :],
                             start=(i == 0), stop=(i == nmm - 1))
                        i += 1

        def down_acc(sts):
            e0, c0, n, r, _ = sts[0]
            Y = [self.bank(4 + cc // 2)[:, (cc % 2) * 256:(cc % 2) * 256 + n] for cc in range(KC)]
            for cc in range(KC):
                xs = self.XT3[:, cc, c0:c0 + n]
                c.stt(xs, Y[cc], self.MODV3[:, 40 + cc, r:r + 1], xs, ALU.mult, ALU.add)

        for e in pairs[0]:
            prefetch(e)
        pend = []
        ready = None
        it = 0
        for pi, pr in enumerate(pairs):
            for gidx, (c0, n, r) in enumerate(groups):
                for ei, e in enumerate(pr):
                    w = WB[e % NWB].v()
                    gb = GB[e % NWB].v()
                    k = it % 2
                    gps = self.bank(2 * k)
                    ups = self.bank(2 * k + 1)
                    g3 = gps[:, 0:2 * n].r("p (j t) -> p j t", j=2)
                    u3 = ups[:, 0:2 * n].r("p (j t) -> p j t", j=2)
                    for jj in range(2):
                        for kk in range(KC):
                            c.mm(g3[:, jj, :], w[:, kk * 256 + jj * 128:kk * 256 + (jj + 1) * 128], self.HB3[:, kk, c0:c0 + n],
                                 start=(kk == 0), stop=(kk == KC - 1))
                    for jj in range(2):
                        for kk in range(KC):
                            c.mm(u3[:, jj, :], w[:, 2048 + kk * 256 + jj * 128:2048 + kk * 256 + (jj + 1) * 128],
                                 self.HB3[:, kk, c0:c0 + n], start=(kk == 0), stop=(kk == KC - 1))
                    did = None
                    if ei == 0 and ready is not None:
                        down_mm(ready)
                        did = ready
                        ready = None
                        if gidx == 0 and pi + 1 < len(pairs):
                            for e2 in pairs[pi + 1]:
                                prefetch(e2)
                    elif ei == 0 and gidx == 0 and pi == 0:
                        for e2 in pairs[1]:
                            prefetch(e2)
                    s3 = sT[k][:, 0:2 * n].r("p (j t) -> p j t", j=2)
                    t3 = tT[k][:, 0:2 * n].r("p (j t) -> p j t", j=2)
                    a3 = aT[it % 4][:, 0:2 * n].r("p (j t) -> p j t", j=2)
                    c.act(s3, g3, AF.Silu)
                    for jj in range(2):
                        c.tt("dve", t3[:, jj, :], u3[:, jj, :], gb[:, c0:c0 + n], ALU.mult)
                    c.tt("dve", a3, s3, t3, ALU.mult)
                    if did is not None:
                        down_acc(did)
                    pend.append((e, c0, n, r, a3))
                    if ei == len(pr) - 1:
                        ready = pend
                        pend = []
                    it += 1
        down_mm(ready)
        down_acc(ready)
        c.free(*WB, *GB, *sT, *tT, *aT)


BF = ml_dtypes.bfloat16


def _fm(tok):
    T_ = tok.shape[0]
    return np.ascontiguousarray(tok.T.reshape(KC, P, T_).transpose(1, 0, 2)).reshape(P, KC * T_)


def _unfm(a, T_):
    return np.ascontiguousarray(a.reshape(P, KC, T_).transpose(1, 0, 2).reshape(D, T_).T)


_CONST_CACHE = {}


def _consts(s):
    if s in _CONST_CACHE:
        return _CONST_CACHE[s]
    out = {}
    out["ident"] = np.eye(P, dtype=np.float32)
    d = np.arange(P)
    i = d % 64
    partner = np.where(i < 32, d + 32, d - 32)
    perm = np.zeros((P, P), np.float32)
    perm[partner, d] = 1.0
    out["perm"] = perm
    pos = np.arange(4096)
    inv = 10000.0 ** (-(2.0 / 64) * np.arange(32, dtype=np.float64))
    pv = np.where((d // 64)[:, None] == 0, (pos // 64)[None, :], (pos % 64)[None, :]).astype(np.float64)
    ang = pv * inv[i % 32][:, None]
    cos = np.cos(ang)
    sin = np.sin(ang) * np.where(i < 32, -1.0, 1.0)[:, None]
    rope = np.stack([cos, sin], axis=1)
    out["rope"] = rope.reshape(P, 2 * 4096).astype(BF)
    out["ropeq"] = np.ascontiguousarray(rope[:, :, s * NLAT:(s + 1) * NLAT]).reshape(P, 2 * NLAT).astype(BF)
    cc = np.arange(64)
    c64 = np.cos(2 * np.pi * np.outer(cc, cc) / 64) / 8.0
    s64 = np.sin(2 * np.pi * np.outer(cc, cc) / 64) / 8.0
    cs2 = np.zeros((P, 256), np.float32)
    for g2 in range(2):
        cs2[g2 * 64:(g2 + 1) * 64, g2 * 64:(g2 + 1) * 64] = c64
        cs2[g2 * 64:(g2 + 1) * 64, 128 + g2 * 64:128 + (g2 + 1) * 64] = s64
    out["cs2"] = cs2
    kk = s * NLAT + np.arange(NLAT)
    ph = (np.outer(pos, kk) % 4096).astype(np.float64) * (2 * np.pi / 4096)
    tab = np.stack([np.cos(ph) / 64.0, -np.sin(ph) / 64.0], axis=0).astype(np.float32)
    tab = tab.reshape(2, 8, 4, P, 4, 512)
    out["dft"] = np.ascontiguousarray(tab.transpose(4, 1, 3, 0, 2, 5)).reshape(4, 8, P, 4096).astype(BF)
    cpos = np.stack([np.arange(128), 128 + np.arange(128)], axis=0)
    ck = s * 128 + np.arange(128)
    cph = (cpos[:, :, None] * ck[None, None, :] % 256) * (2 * np.pi / 256)
    ctab = np.stack([np.cos(cph) / 16.0, -np.sin(cph) / 16.0], axis=0)
    out["cdft"] = np.ascontiguousarray(ctab.transpose(2, 0, 1, 3)).reshape(P, 512).astype(np.float32)
    _CONST_CACHE[s] = out
    return out


def _na_bias(rpb, s):
    out = np.full((8, P, 5, 768), NEG, np.float32)
    q = np.arange(P)
    w = np.arange(768)
    qcol = q % 64
    kcol = w % 64
    cstart = np.clip(qcol - 8, 0, 48)
    okc = (kcol[None, :] >= cstart[:, None]) & (kcol[None, :] < cstart[:, None] + 16)
    dc = kcol[None, :] - qcol[:, None] + 15
    rep = {0: 0, 1: 1, 2: 2, 3: 14, 4: 15}
    for ty, bi in rep.items():
        i = 16 * s + bi
        wb = 2 * bi if bi <= 13 else 28
        qrow = 2 * i + q // 64
        rstart = np.clip(qrow - 4, 0, 56)
        krow = wb + (32 * s - 4) + w // 64
        okr = (krow[None, :] >= rstart[:, None]) & (krow[None, :] < rstart[:, None] + 8)
        dr = krow[None, :] - qrow[:, None] + 7
        ok = okr & okc
        drc = np.clip(dr, 0, 14)
        dcc = np.clip(dc, 0, 30)
        vals = rpb[:, drc, dcc]
        out[:, :, ty, :] = np.where(ok[None], vals, NEG)
    outT = out.reshape(8, P, 5, 6, P).transpose(0, 4, 2, 3, 1)
    return np.ascontiguousarray(outT).reshape(8, P, 5 * 768)


def _layer_weights(l, inp, tag):
    j = l // 2
    w = {}
    w[f"wmod{tag}"] = inp["w_mod"][l]
    w[f"bmod{tag}"] = np.ascontiguousarray(inp["b_mod"][l].reshape(48, P).T)
    w[f"lnv{tag}"] = np.ascontiguousarray(
        np.stack([inp[k][l].reshape(KC, P).T for k in ("ln1_g", "ln1_b", "ln2_g", "ln2_b")], axis=1)).reshape(P, 32)
    if l % 2 == 0:
        w[f"win{tag}"] = inp["ab_w_in"][j]
        w[f"wout{tag}"] = inp["ab_w_out"][j]
        wf = inp["ab_w_fnet"][j]
        wfn = np.zeros((P, 2, P), np.float32)
        for jj in range(2):
            wfn[0:64, jj, 0:64] = wf[2 * jj]
            wfn[64:128, jj, 64:128] = wf[2 * jj + 1]
        w[f"wfn{tag}"] = wfn.reshape(P, 256)
        w[f"qkn{tag}"] = np.ascontiguousarray(np.stack([inp["ab_q_norm"][j], inp["ab_k_norm"][j]], axis=1))
    else:
        w[f"win{tag}"] = inp["na_w_in"][j]
        w[f"wout{tag}"] = inp["na_w_out"][j]
    w[f"wr{tag}"] = np.ascontiguousarray(inp["moe_w_router"][l].reshape(KC, P, 64).transpose(1, 0, 2)).reshape(P, KC * 64)
    w[f"mb{tag}"] = np.ascontiguousarray(np.broadcast_to(inp["moe_bias"][l][None, :], (P, 64)))
    wg = np.concatenate([inp["moe_w_gate"][l], inp["sh_w_gate"][l][None]], axis=0)
    wu = np.concatenate([inp["moe_w_up"][l], inp["sh_w_up"][l][None]], axis=0)
    wd = np.concatenate([inp["moe_w_down"][l], inp["sh_w_down"][l][None]], axis=0)
    moew = np.empty((NEXP, P, 6144), np.float32)
    moew[:, :, 0:2048] = wg.reshape(NEXP, KC, P, 256).transpose(0, 2, 1, 3).reshape(NEXP, P, 2048)
    moew[:, :, 2048:4096] = wu.reshape(NEXP, KC, P, 256).transpose(0, 2, 1, 3).reshape(NEXP, P, 2048)
    moew[:, :, 4096:6144] = wd.reshape(NEXP, 2, P, D).transpose(0, 2, 1, 3).reshape(NEXP, P, 2048)
    w[f"moew{tag}"] = moew
    return w


_PROG_CACHE = {}


def _get_prog(layers, debug=None):
    key = (tuple(layers), tuple(sorted((debug or {}).items())))
    if key not in _PROG_CACHE:
        pr = Prog(list(layers), debug)
        nc = pr.build()
        _PROG_CACHE[key] = (nc, set(pr.inputs.keys()))
    return _PROG_CACHE[key]


def _init_toks(inp):
    toks = []
    for r in range(8):
        b, s = r // 2, r % 2
        toks.append(np.concatenate([inp["x"][b, s * NLAT:(s + 1) * NLAT], inp["ctx"][b, s * NCTX:(s + 1) * NCTX]], axis=0))
    return toks


def _launch(inp, layers, toks, debug=None):
    nc, names = _get_prog(layers, debug)
    lw = {}
    for l in layers:
        lw.update(_layer_weights(l, inp, l))
    fm = [_fm(t) for t in toks]
    in_maps = []
    for r in range(8):
        b, s = r // 2, r % 2
        m = dict(lw)
        cs = _consts(s)
        for k in ("ident", "perm", "rope", "ropeq", "cs2", "dft", "cdft"):
            m[k] = cs[k]
        for l in layers:
            if l % 2 == 1:
                m[f"nab{l}"] = _na_bias(inp["na_rpb"][l // 2], s)
        m["cT"] = np.ascontiguousarray(
            np.stack([inp["c"][b].reshape(KC, P).T, inp["c_ctx"].reshape(KC, P).T], axis=2)).reshape(P, 16)
        m["xT"] = fm[r]
        m[f"xg{layers[0]}"] = np.ascontiguousarray(
            np.stack([fm[r - s].reshape(P, KC, NT), fm[r - s + 1].reshape(P, KC, NT)], axis=0).transpose(2, 0, 1, 3)
        ).reshape(KC, 2 * P, NT)
        in_maps.append({k: v for k, v in m.items() if k in names})
    res = run_bass_kernel_spmd(nc, in_maps, core_ids=list(range(8)))
    return [_unfm(res.results[r]["xout"], NT) for r in range(8)]


def run_layers(inp, layers=range(DEPTH), toks=None, debug=None, fused=False):
    inp = {k: np.asarray(v) for k, v in inp.items()}
    if toks is None:
        toks = _init_toks(inp)
    if fused:
        return _launch(inp, list(layers), toks, debug)
    for l in layers:
        toks = _launch(inp, [l], toks, debug)
    return toks


def kernel(**inputs):
    toks = run_layers(inputs, fused=True)
    out = np.empty((4, 4096, D), np.float32)
    for r in range(8):
        b, s = r // 2, r % 2
        out[b, s * NLAT:(s + 1) * NLAT] = toks[r][0:NLAT]
    return out
```

```python
import math
import numpy as np
import ml_dtypes
import concourse.bass as bass
import concourse.mybir as mybir
from concourse.bass_utils import run_bass_kernel_spmd
from contextlib import ExitStack

F32 = mybir.dt.float32
BF16 = mybir.dt.bfloat16
AF = mybir.ActivationFunctionType
ALU = mybir.AluOpType
AX = mybir.AxisListType

P = 128
D = 1024
KC = 8
NLAT = 2048
NCTX = 128
NT = NLAT + NCTX
DEPTH = 4
ALPHA = (2 * DEPTH) ** 0.25
LN_EPS = 1e-6
RMS_EPS = 1e-6
HD = 128
SCALE = HD ** -0.5
NEG = -1e30
NEXP = 65
ROUTE_SCALE = 2.5

ENGS = ("pe", "act", "dve", "pool", "sp")
DTSIZE = {F32: 4, BF16: 2}


class V:
    __slots__ = ("t", "ap")

    def __init__(self, t, ap):
        self.t = t
        self.ap = ap

    def __getitem__(self, idx):
        return V(self.t, self.ap[idx])

    def r(self, pat, **kw):
        return V(self.t, self.ap.rearrange(pat, **kw))

    def bc(self, shape):
        return V(self.t, self.ap.broadcast_to(shape))

    def bitcast(self, dt):
        return V(self.t, self.ap.bitcast(dt))


class T:
    def __init__(self, h, name, space, rng=None):
        self.h = h
        self.name = name
        self.space = space
        self.w = None
        self.r_ = []
        self.dsem = None
        self.dcnt = 0
        self.rng = rng

    def __getitem__(self, idx):
        return V(self, self.h[idx])

    def r(self, pat, **kw):
        return V(self, self.h.rearrange(pat, **kw))

    def v(self):
        return V(self, self.h)


class Ctx:
    def __init__(self, nc, arena_bytes=211968):
        self.nc = nc
        self.es = ExitStack()
        self.E = {"pe": nc.tensor, "act": nc.scalar, "dve": nc.vector, "pool": nc.gpsimd, "sp": nc.sync}
        self.sem = {}
        self.cnt = {}
        for e in ENGS:
            self.sem[e] = self.es.enter_context(nc.semaphore("s_" + e))
            self.cnt[e] = 0
        self.seen = {e: {} for e in ENGS}
        self.nbuf = 0
        self.sem_pool = []
        self.arena = self.es.enter_context(nc.sbuf_tensor("arena", [P, arena_bytes // 2], BF16))
        self.arena_bytes = arena_bytes
        self.free_list = [(0, arena_bytes)]
        self.grave = []
        self.live = {}
        self.banks = []
        for i in range(8):
            h = self.es.enter_context(nc.psum_tensor(f"bank{i}", [P, 512], F32))
            self.banks.append(T(h[:], f"bank{i}", "ps"))

    def alloc(self, name, nelem, dt, parts=P):
        nbytes = (nelem * DTSIZE[dt] + 63) // 64 * 64
        for i, (s, e) in enumerate(self.free_list):
            if e - s >= nbytes:
                self.free_list[i] = (s + nbytes, e)
                if self.free_list[i][0] == self.free_list[i][1]:
                    del self.free_list[i]
                break
        else:
            raise RuntimeError(f"arena OOM allocating {name} {nbytes}B; free={self.free_list}")
        ap = self.arena[0:parts, s // 2:(s + nbytes) // 2]
        if dt != BF16:
            ap = ap.bitcast(dt)
        ap = ap[:, 0:nelem]
        self.nbuf += 1
        t = T(ap, f"{name}_{self.nbuf}", "sb", (s, s + nbytes))
        keep = []
        for (gs, ge, toks) in self.grave:
            if gs < s + nbytes and s < ge:
                t.r_.extend(toks)
                if gs >= s and ge <= s + nbytes:
                    continue
            keep.append((gs, ge, toks))
        self.grave = keep
        return t

    def free(self, *ts):
        for t in ts:
            s, e = t.rng
            toks = [x for x in ([t.w] + t.r_) if x is not None]
            self.grave.append((s, e, self._compress(toks)))
            self.free_list.append((s, e))
            self.free_list.sort()
            merged = []
            for a, b in self.free_list:
                if merged and merged[-1][1] == a:
                    merged[-1] = (merged[-1][0], b)
                else:
                    merged.append((a, b))
            self.free_list = merged
            t.rng = None
            if t.dsem is not None:
                self.sem_pool.append((t.dsem, t.dcnt, t.dkey))
                t.dsem = None

    @staticmethod
    def _compress(toks):
        best = {}
        for tok in toks:
            key = tok[1] if tok[0] == "eng" else tok[3]
            v = tok[2]
            if key not in best or best[key][2] < v:
                best[key] = tok
        return list(best.values())

    def dram(self, name, shape, dt, kind="Internal"):
        h = self.nc.dram_tensor(name, list(shape), dt, kind=kind)
        return T(h.ap(), name, "dram")

    def _wait(self, e, tok):
        if tok is None:
            return
        if tok[0] == "eng":
            _, f, v = tok
            key = "e:" + f
            sem = self.sem[f]
        else:
            _, sem, v, key = tok
        if e == "pe" and tok[0] == "eng" and tok[1] == "pe":
            return
        if self.seen[e].get(key, 0) >= v:
            return
        self.E[e].wait_ge(sem, v)
        self.seen[e][key] = v

    def _deps(self, e, reads, writes, acc=False):
        for t in reads:
            self._wait(e, t.w)
        if not acc:
            for t in writes:
                self._wait(e, t.w)
                for tok in t.r_:
                    self._wait(e, tok)

    def op(self, e, fn, reads=(), writes=(), acc=False, inc=True):
        reads = [v.t for v in reads if v is not None]
        writes = [v.t for v in writes if v is not None]
        self._deps(e, reads, writes, acc)
        ins = fn(self.E[e])
        if inc:
            self.cnt[e] += 1
            ins.then_inc(self.sem[e], 1)
            tok = ("eng", e, self.cnt[e])
        else:
            tok = ("eng", e, self.cnt[e] + 1)
        for t in reads:
            t.r_.append(tok)
            if len(t.r_) > 24:
                t.r_ = self._compress(t.r_)
        for t in writes:
            t.w = tok
            t.r_ = []
        return ins

    def dma(self, q, out, in_, **kw):
        ot = out.t if isinstance(out, V) else None
        it = in_.t if isinstance(in_, V) else None
        oap = out.ap if isinstance(out, V) else out
        iap = in_.ap if isinstance(in_, V) else in_
        reads = [it] if it is not None else []
        writes = [ot] if ot is not None else []
        self._deps(q, reads, writes)
        owner = None
        for t in (ot, it):
            if t is not None and t.space != "dram":
                owner = t
                break
        if owner is None:
            owner = ot if ot is not None else it
        if owner.dsem is None:
            if self.sem_pool:
                sem, cnt, key = self.sem_pool.pop()
                if cnt > 0:
                    self._wait(q, ("dma", sem, cnt, key))
            else:
                self.nsem = getattr(self, "nsem", 0) + 1
                key = f"d:{self.nsem}"
                sem, cnt = self.es.enter_context(self.nc.semaphore(f"dsem{self.nsem}")), 0
            owner.dsem, owner.dcnt, owner.dkey = sem, cnt, key
        owner.dcnt += 16
        ins = self.E[q].dma_start(out=oap, in_=iap, **kw)
        ins.then_inc(owner.dsem, 16)
        tok = ("dma", owner.dsem, owner.dcnt, owner.dkey)
        for t in reads:
            t.r_.append(tok)
        for t in writes:
            t.w = tok
            t.r_ = []
        return tok

    def wait_all(self, e, ts):
        for t in ts:
            self._wait(e, t.w)
            for tok in t.r_:
                self._wait(e, tok)

    def mm(self, out, lhsT, rhs, start=True, stop=True, inc=None):
        self.op("pe", lambda e: e.matmul(out.ap, lhsT=lhsT.ap, rhs=rhs.ap, start=start, stop=stop),
                reads=[lhsT, rhs], writes=[out], acc=not start, inc=(stop if inc is None else inc))

    def transpose(self, out, in_, ident):
        self.op("pe", lambda e: e.transpose(out.ap, in_.ap, ident.ap), reads=[in_, ident], writes=[out])

    def act(self, out, in_, func, scale=None, bias=None, accum=None):
        kw = {}
        rd = [in_]
        wr = [out]
        if scale is not None:
            if isinstance(scale, V):
                kw["scale"] = scale.ap
                rd.append(scale)
            else:
                kw["scale"] = float(scale)
        if bias is not None:
            if isinstance(bias, V):
                kw["bias"] = bias.ap
                rd.append(bias)
            else:
                kw["bias"] = float(bias)
        if accum is not None:
            kw["accum_out"] = accum.ap
            wr.append(accum)
        self.op("act", lambda e: e.activation(out=out.ap, in_=in_.ap, func=func, **kw), reads=rd, writes=wr)

    def copy(self, eng, out, in_):
        if eng == "act":
            self.op("act", lambda e: e.copy(out=out.ap, in_=in_.ap), reads=[in_], writes=[out])
        else:
            self.op(eng, lambda e: e.tensor_copy(out=out.ap, in_=in_.ap), reads=[in_], writes=[out])

    def tt(self, eng, out, in0, in1, op):
        self.op(eng, lambda e: e.tensor_tensor(out=out.ap, in0=in0.ap, in1=in1.ap, op=op),
                reads=[in0, in1], writes=[out])

    def ts(self, eng, out, in0, s1, s2=None, op0=ALU.mult, op1=None):
        rd = [in0]
        a1 = s1.ap if isinstance(s1, V) else float(s1)
        if isinstance(s1, V):
            rd.append(s1)
        a2 = None
        if s2 is not None:
            a2 = s2.ap if isinstance(s2, V) else float(s2)
            if isinstance(s2, V):
                rd.append(s2)
        if op1 is None:
            self.op(eng, lambda e: e.tensor_scalar(out=out.ap, in0=in0.ap, scalar1=a1, scalar2=None, op0=op0),
                    reads=rd, writes=[out])
        else:
            self.op(eng, lambda e: e.tensor_scalar(out=out.ap, in0=in0.ap, scalar1=a1, scalar2=a2, op0=op0, op1=op1),
                    reads=rd, writes=[out])

    def stt(self, out, in0, scalar, in1, op0, op1, eng="dve"):
        rd = [in0, in1]
        a = scalar.ap if isinstance(scalar, V) else float(scalar)
        if isinstance(scalar, V):
            rd.append(scalar)
        self.op(eng, lambda e: e.scalar_tensor_tensor(out=out.ap, in0=in0.ap, scalar=a, in1=in1.ap, op0=op0, op1=op1),
                reads=rd, writes=[out])

    def memset(self, eng, out, val):
        self.op(eng, lambda e: e.memset(out.ap, val), writes=[out])

    def close(self):
        self.es.close()


class Prog:
    def __init__(self, layers, debug=None):
        self.layers = list(layers)
        self.debug = debug or {}
        self.nc = bass.Bass("TRN2", target_bir_lowering=False)
        self.c = Ctx(self.nc)
        self.inputs = {}
        self.pending_mod = None
        self.rot = {}
        self.xg = {}

    def inp(self, name, shape, dt=F32):
        if name not in self.inputs:
            self.inputs[name] = self.nc.dram_tensor(name, list(shape), dt, kind="ExternalInput").ap()
        return self.inputs[name]

    def bank(self, i):
        return self.c.banks[i]

    def rr(self, key, n):
        v = self.rot.get(key, 0)
        self.rot[key] = v + 1
        return v % n

    def build(self):
        c = self.c
        nc = self.nc
        first = self.layers[0]
        self.XT = c.alloc("XT", KC * NT, F32)
        self.XT3 = self.XT.r("p (c t) -> p c t", c=KC)
        self.HB = c.alloc("HB", KC * NT, BF16)
        self.HB3 = self.HB.r("p (c t) -> p c t", c=KC)
        self.identb = c.alloc("identb", P, BF16)
        self.identf = c.alloc("identf", P, F32)
        c.dma("pool", self.identb.v(), self.inp("ident", [P, P]))
        c.dma("sp", self.identf.v(), self.inp("ident", [P, P]))
        self.ones_mean = c.alloc("ones_mean", P, BF16)
        self.ones_rms = c.alloc("ones_rms", P, BF16)
        self.ones1 = c.alloc("ones1", P, BF16)
        c.memset("dve", self.ones_mean.v(), 1.0 / D)
        self.ones_mean_f = c.alloc("ones_mean_f", P, F32)
        c.memset("dve", self.ones_mean_f.v(), 1.0 / D)
        c.memset("dve", self.ones_rms.v(), 1.0 / HD)
        c.memset("dve", self.ones1.v(), 1.0)
        self.sets = []
        for _ in range(2):
            d_ = {}
            d_["MODV"] = c.alloc("MODV", 96, F32)
            d_["MODV3"] = d_["MODV"].r("p (m r) -> p m r", r=2)
            d_["LNV"] = c.alloc("LNV", 32, F32)
            d_["LNV3"] = d_["LNV"].r("p (m c) -> p m c", c=KC)
            d_["A2"] = c.alloc("A2", 16, F32)
            d_["A23"] = d_["A2"].r("p (c r) -> p c r", r=2)
            d_["B2"] = c.alloc("B2", 16, F32)
            d_["B23"] = d_["B2"].r("p (c r) -> p c r", r=2)
            self.sets.append(d_)
        self.use_set(self.layers[0])
        self.G = c.alloc("G", 17 * NEXP, F32)
        self.G3 = self.G.r("p (t e) -> p t e", e=NEXP)
        self.scT = c.alloc("scT", 16, F32)
        self.scT3 = self.scT.r("p (k r) -> p k r", r=2)
        c.dma("sp", self.scT.v(), self.inp("cT", [P, 16]))
        c.act(self.scT.v(), self.scT.v(), AF.Silu)
        c.dma("sp", self.XT.v(), self.inp("xT", [P, KC * NT]))
        for li, l in enumerate(self.layers):
            self.layer(l)
            if li + 1 < len(self.layers):
                self.exchange(l, self.layers[li + 1])
        xout = nc.dram_tensor("xout", [P, KC * NT], F32, kind="ExternalOutput").ap()
        tok = c.dma("sp", xout, self.XT.v())
        c._wait("sp", tok)
        c.close()
        return nc

    def exchange(self, l, lnext):
        c = self.c
        nc = self.nc
        sem = c.es.enter_context(nc.semaphore(f"ccsem{l}"))
        tok = ("dma", sem, KC, f"cc{l}")
        xgh = nc.dram_tensor(f"xgi{lnext}", [KC, 2 * P, NT], F32)
        xg = T(xgh.ap(), f"xgi{lnext}", "dram")
        for cc in range(KC):
            xsh = nc.dram_tensor(f"xs{l}_{cc}", [P, NT], F32)
            xs = T(xsh.ap(), f"xs{l}_{cc}", "dram")
            c.dma("sp", xs.v(), self.XT3[:, cc, :])
            c._deps("pool", [xs], [xg] if cc == 0 else [])
            ins = nc.gpsimd.collective_compute("AllGather", ALU.bypass, replica_groups=[[0, 1], [2, 3], [4, 5], [6, 7]],
                                               ins=[xsh.ap().opt()], outs=[xgh.ap()[cc]])
            ins.then_inc(sem)
            xs.r_.append(tok)
        xg.w = tok
        xg.r_ = []
        self.xg[lnext] = xg

    def use_set(self, l):
        for k_, v_ in self.sets[l % 2].items():
            setattr(self, k_, v_)

    def layer(self, l):
        self.use_set(l)
        if l == self.layers[0]:
            self.emit_mod(l)
        if self.debug.get("stop") == "mod":
            return
        self.emit_h1(l)
        if self.debug.get("stop") == "h1":
            return
        if l % 2 == 0:
            self.emit_gqa(l)
        else:
            self.emit_na(l)
        stop = self.debug.get("stop")
        if stop == "mixer":
            return
        self.emit_ln(l, 1)
        if stop == "ln1":
            return
        self.emit_moe(l)
        if stop == "moe":
            return
        li = self.layers.index(l)
        if li + 1 < len(self.layers):
            self.pending_mod = self.mod_gen(self.layers[li + 1])
        self.emit_ln(l, 2)

    def emit_mod(self, l):
        for _ in self.mod_gen(l):
            pass

    def mod_gen(self, l):
        c = self.c
        tg = self.sets[l % 2]
        wmod = self.inp(f"wmod{l}", [D, 6 * D]).rearrange("(k p) n -> p k n", p=P)
        bmod = c.alloc("bmod", 48, F32)
        c.dma("sp", bmod.v(), self.inp(f"bmod{l}", [P, 48]))
        c.dma("sp", tg["LNV"].v(), self.inp(f"lnv{l}", [P, 32]))
        st = [c.alloc("wmst", KC * 512, F32) for _ in range(2)]
        scb3 = self.scT3
        ps = self.bank(3)
        ps3 = ps[:, 0:96].r("p (m r) -> p m r", r=2)
        for blk in range(12):
            s = st[blk % 2]
            s3 = s.r("p (k n) -> p k n", k=KC)
            c.dma("sp", s3, wmod[:, :, blk * 512:(blk + 1) * 512])
            for fc in range(4):
                for k in range(KC):
                    c.mm(ps3[:, blk * 4 + fc, :], s3[:, k, fc * 128:(fc + 1) * 128], scb3[:, k, :],
                         start=(k == 0), stop=(k == KC - 1))
            yield blk
        M3 = tg["MODV3"]
        for r in range(2):
            c.tt("dve", M3[:, :, r], ps3[:, :, r], bmod.v(), ALU.add)
        c.ts("dve", M3[:, 8:16, :], M3[:, 8:16, :], 1.0, op0=ALU.add)
        c.ts("dve", M3[:, 32:40, :], M3[:, 32:40, :], 1.0, op0=ALU.add)
        c.ts("dve", M3[:, 16:24, :], M3[:, 16:24, :], 1.0 / ALPHA, op0=ALU.mult)
        c.ts("dve", M3[:, 40:48, :], M3[:, 40:48, :], 1.0 / ALPHA, op0=ALU.mult)
        for r in range(2):
            c.tt("dve", tg["A23"][:, :, r], tg["LNV3"][:, 0, :], M3[:, 32:40, r], ALU.mult)
            c.tt("dve", tg["B23"][:, :, r], tg["LNV3"][:, 1, :], M3[:, 32:40, r], ALU.mult)
            c.tt("dve", tg["B23"][:, :, r], tg["B23"][:, :, r], M3[:, 24:32, r], ALU.add)
        c.free(bmod, *st)

    def mod_h(self, eng, out, in_, cc, r):
        if eng == "act":
            self.c.act(out, in_, AF.Identity, scale=self.MODV3[:, 8 + cc, r:r + 1], bias=self.MODV3[:, cc, r:r + 1])
        else:
            self.c.ts(eng, out, in_, self.MODV3[:, 8 + cc, r:r + 1], self.MODV3[:, cc, r:r + 1], ALU.mult, ALU.add)

    def emit_h1(self, l):
        for cc in range(KC):
            eng = ("dve", "act")[cc % 2]
            self.mod_h(eng, self.HB3[:, cc, 0:NLAT], self.XT3[:, cc, 0:NLAT], cc, 0)
            self.mod_h(eng, self.HB3[:, cc, NLAT:NT], self.XT3[:, cc, NLAT:NT], cc, 1)

    def xg_ap(self, l):
        if l in self.xg:
            return self.xg[l]
        ap = self.inp(f"xg{l}", [KC, 2 * P, NT])
        self.xg[l] = T(ap, f"xg{l}", "dram")
        return self.xg[l]

    def gstream(self, l, groups):
        c = self.c
        xg = self.xg_ap(l)
        st = [c.alloc("xost", KC * 256, F32) for _ in range(3)]
        ho = [c.alloc("xoh", KC * 256, BF16) for _ in range(3)]
        views = {}

        def load(i):
            slot, c0, n, r = groups[i]
            s3 = st[i % 3].r("p (c t) -> p c t", c=KC)[:, :, 0:n]
            c.dma("sp", s3, V(xg, xg.h[:, slot * P:(slot + 1) * P, c0:c0 + n].rearrange("c p t -> p c t")))
            views[i] = s3

        def prep(i):
            slot, c0, n, r = groups[i]
            s3 = views.pop(i)
            h3 = ho[i % 3].r("p (c t) -> p c t", c=KC)[:, :, 0:n]
            for cc in range(KC):
                self.mod_h(("dve", "act")[cc % 2], h3[:, cc, :], s3[:, cc, :], cc, r)
            return h3

        ng = len(groups)
        load(0)
        if ng > 1:
            load(1)
        h_next = prep(0)
        for i in range(ng):
            h_cur = h_next
            if i + 2 < ng:
                load(i + 2)
            if i + 1 < ng:
                h_next = prep(i + 1)
            slot, c0, n, r = groups[i]
            yield (slot, c0, n, r, h_cur)
        c.free(*st, *ho)

    def load_w(self, name, dram_ap, ncols):
        c = self.c
        t = c.alloc(name, KC * ncols, BF16)
        t3 = t.r("p (k n) -> p k n", k=KC)
        c.dma("pool", t3, dram_ap.rearrange("(k p) n -> p k n", p=P))
        return t, t3

    def outproj(self, oT, wo, col0, n, r):
        c = self.c
        oTs = oT if isinstance(oT, list) else [oT]
        wos = wo if isinstance(wo, list) else [wo]
        for cc in range(KC):
            y = self.bank(6 + self.rr("yb", 2))[:, 0:n]
            for i, (o, w) in enumerate(zip(oTs, wos)):
                c.mm(y, w[:, cc * 128:(cc + 1) * 128], o, start=(i == 0), stop=(i == len(oTs) - 1))
            xs = self.XT3[:, cc, col0:col0 + n]
            c.stt(xs, y, self.MODV3[:, 16 + cc, r:r + 1], xs, ALU.mult, ALU.add)

    def rmsnorm_rope(self, ps, n, gain, out, rope_col0=None, table=None):
        c = self.c
        k = self.rr("rn", 2)
        tm = self.rn_tmp[k]
        sq = tm["sq"][:, 0:n]
        rstd = tm["rstd"][:, 0:n]
        c.act(sq, ps, AF.Square)
        ss = self.bank(2 + k)[:, 0:n]
        c.mm(ss, self.ones_rms.v(), sq)
        c.act(rstd, ss, AF.Ln, bias=RMS_EPS)
        c.act(rstd, rstd, AF.Exp, scale=-0.5)
        if rope_col0 is None:
            c.stt(out, ps, gain, rstd, ALU.mult, ALU.mult)
            return
        qn = tm["qn"][:, 0:n]
        c.stt(qn, ps, gain, rstd, ALU.mult, ALU.mult)
        rp = tm["rope"].r("p (a n) -> p a n", a=2)[:, :, 0:n]
        c.dma("sp", rp, (table if table is not None else self.rope_dram)[:, :, rope_col0:rope_col0 + n])
        pp = self.bank(4 + k)[:, 0:n]
        c.mm(pp, self.perm.v(), qn)
        t1 = tm["t1"][:, 0:n]
        t2 = tm["t2"][:, 0:n]
        c.tt("dve", t1, qn, rp[:, 0, :], ALU.mult)
        c.tt("dve", t2, pp, rp[:, 1, :], ALU.mult)
        c.tt("dve", out, t1, t2, ALU.add)

    def attn_T(self, KT, VV3, qv, n, tiles):
        c = self.c
        O = self.bank(2)[:, 0:n]
        L = self.bank(3)[:, 0:n]
        nt = len(tiles)
        sbanks = (0, 1, 4, 5)
        LA = 3

        def S(i):
            s = self.bank(sbanks[self.rr("sb", 4)])[:, 0:n]
            kt = tiles[i]
            c.mm(s, KT[:, kt * 128:(kt + 1) * 128], qv)
            return s
        q = [S(i) for i in range(min(LA, nt))]
        for i in range(nt):
            s_cur = q.pop(0)
            if i + LA < nt:
                q.append(S(i + LA))
            pt = self.pt_tmp[self.rr("pt", len(self.pt_tmp))][:, 0:n]
            c.act(pt, s_cur, AF.Exp, scale=SCALE)
            c.mm(O, VV3[:, tiles[i], :], pt, start=(i == 0), stop=(i == nt - 1))
            c.mm(L, self.ones1.v(), pt, start=(i == 0), stop=(i == nt - 1))
        kk = self.rr("ot", 2)
        rl = self.rl_tmp[kk][:, 0:n]
        c.act(rl, L, AF.Ln)
        c.act(rl, rl, AF.Exp, scale=-1.0)
        oT = self.ot_tmp[kk][:, 0:n]
        c.tt("dve", oT, O, rl, ALU.mult)
        return oT

    def emit_gqa(self, l):
        c = self.c
        j = l // 2
        win = self.inp(f"win{l}", [D, 1536])
        wout = self.inp(f"wout{l}", [D, D])
        g_own = [(i * 256, 256, 0) for i in range(8)] + [(NLAT, 128, 1)]
        wf, wf3 = self.load_w("wf", win[:, 0:256], 256)
        cs2 = c.alloc("cs2", 256, BF16)
        c.dma("pool", cs2.v(), self.inp("cs2", [P, 256]))
        wfn = c.alloc("wfn", 256, BF16)
        wfn3 = wfn.r("p (j m) -> p j m", j=2)
        c.dma("pool", wfn.v(), self.inp(f"wfn{l}", [P, 256]))
        wo_f = c.alloc("wo_f", 2 * D, BF16)
        wo_f3 = wo_f.r("p (j n) -> p j n", j=2)
        c.dma("pool", wo_f3, wout[0:256, :].rearrange("(j p) n -> p j n", p=P))
        cdft = c.alloc("cdft", 512, BF16)
        cdft4 = cdft.r("p (a t k) -> p a t k", a=2, t=2)
        c.dma("pool", cdft.v(), self.inp("cdft", [P, 512]))
        FCS = c.alloc("FCS", 34 * 512, BF16)
        FCS3 = FCS.r("p (t f) -> p t f", f=512)
        fts = [c.alloc("fts", 512, BF16) for _ in range(2)]

        def stage1(buf0, n, h3):
            fps = self.bank(self.rr("fps", 2))
            fps3 = fps[:, 0:2 * n].r("p (j t) -> p j t", j=2)
            for jj in range(2):
                for k in range(KC):
                    c.mm(fps3[:, jj, :], wf3[:, k, jj * 128:(jj + 1) * 128], h3[:, k, :], start=(k == 0), stop=(k == KC - 1))
            ft = fts[self.rr("fts", 2)]
            ft3 = ft[:, 0:2 * n].r("p (j t) -> p j t", j=2)
            c.copy("act", ft3, fps3)
            for tt in range(n // 128):
                cps = self.bank(2 + self.rr("cps", 2))
                for jj in range(2):
                    c.mm(cps[:, jj * 256:(jj + 1) * 256], ft3[:, jj, tt * 128:(tt + 1) * 128], cs2.v())
                c.copy("dve", FCS3[:, (buf0 + tt * 128) // 128, :], cps.v())

        g_all = [(slot, c0, n, r) for slot in range(2) for (c0, n, r) in g_own]
        for (slot, c0, n, r, h3) in self.gstream(l, g_all):
            stage1(slot * NT + c0, n, h3)
        c.free(wf, cs2, *fts)

        dft = self.inp("dft", [4, 8, P, 4096], BF16)
        dst = [c.alloc("dftst", 4096, BF16) for _ in range(2)]
        frs = [c.alloc("frs", 512, BF16) for _ in range(2)]
        ots = [c.alloc("ofs", 512, BF16) for _ in range(2)]

        def finish(acc, n, col0, r):
            oTs = []
            for jj in range(2):
                fr = frs[jj][:, 0:n]
                c.copy("act", fr, acc[jj])
                wps = self.bank(6 + self.rr("yb", 2))[:, 0:n]
                c.mm(wps, wfn3[:, jj, :], fr)
                o = ots[jj][:, 0:n]
                c.copy("act", o, wps)
                oTs.append(o)
            self.outproj(oTs, [wo_f3[:, 0, :], wo_f3[:, 1, :]], col0, n, r)

        for kc in range(4):
            acc = [self.bank(4)[:, 0:512], self.bank(5)[:, 0:512]]
            for ng in range(8):
                dt_ = dst[self.rr("dst", 2)]
                c.dma("sp", dt_.v(), dft[kc, ng])
                dt4 = dt_.r("p (a i k) -> p a i k", a=2, i=4)
                for ni in range(4):
                    nn = ng * 4 + ni
                    ft_i = nn if nn < 16 else nn + 1
                    for jj in range(2):
                        c.mm(acc[jj], FCS3[:, ft_i, jj * 256:jj * 256 + 128], dt4[:, 0, ni, :], start=(nn == 0), stop=False)
                        c.mm(acc[jj], FCS3[:, ft_i, jj * 256 + 128:(jj + 1) * 256], dt4[:, 1, ni, :], start=False, stop=(nn == 31),
                             inc=(nn == 31 or (ni == 3 and jj == 1)))
            finish(acc, 512, kc * 512, 0)
        acc = [self.bank(4)[:, 0:128], self.bank(5)[:, 0:128]]
        for ti, ft_i in enumerate((16, 33)):
            for jj in range(2):
                c.mm(acc[jj], FCS3[:, ft_i, jj * 256:jj * 256 + 128], cdft4[:, 0, ti, :], start=(ti == 0), stop=False)
                c.mm(acc[jj], FCS3[:, ft_i, jj * 256 + 128:(jj + 1) * 256], cdft4[:, 1, ti, :], start=False, stop=(ti == 1))
        finish(acc, 128, NLAT, 1)
        c.free(FCS, wfn, wo_f, cdft, *dst, *frs, *ots)
        if self.debug.get("stop") == "fnet":
            return

        self.rope_dram = self.inp("rope", [P, 2 * 4096], BF16).rearrange("p (a n) -> p a n", a=2)
        ropeq = self.inp("ropeq", [P, 2 * NLAT], BF16).rearrange("p (a n) -> p a n", a=2)
        self.perm = c.alloc("perm", P, BF16)
        c.dma("pool", self.perm.v(), self.inp("perm", [P, P]))
        qkn = c.alloc("qkn", 2, F32)
        c.dma("sp", qkn.v(), self.inp(f"qkn{l}", [P, 2]))
        self.rn_tmp = [dict(sq=c.alloc("sq", 256, BF16), rstd=c.alloc("rstd", 256, F32), qn=c.alloc("qn", 256, BF16),
                            rope=c.alloc("rope", 512, BF16), t1=c.alloc("t1", 256, F32), t2=c.alloc("t2", 256, F32))
                       for _ in range(2)]
        self.pt_tmp = [c.alloc("pt", 512, BF16) for _ in range(5)]
        self.rl_tmp = [c.alloc("rl", 512, F32) for _ in range(2)]
        self.ot_tmp = [c.alloc("ot", 512, BF16) for _ in range(2)]
        KTt = c.alloc("KT", 2 * NT, BF16)
        KT = KTt.v()
        VVt = c.alloc("VV", 34 * 128, BF16)
        VV3 = VVt.r("p (t d) -> p t d", d=128)
        QTt = [c.alloc("QT", NT, BF16) for _ in range(2)]
        for g in range(2):
            wk, wk3 = self.load_w("wk", win[:, 1024 + g * 128:1024 + (g + 1) * 128], 128)
            wv, wv3 = self.load_w("wv", win[:, 1280 + g * 128:1280 + (g + 1) * 128], 128)

            def kv(buf0, n, r, h3, rope0):
                kps = self.bank(self.rr("kps", 2))[:, 0:n]
                for k in range(KC):
                    c.mm(kps, wk3[:, k, :], h3[:, k, :], start=(k == 0), stop=(k == KC - 1))
                self.rmsnorm_rope(kps, n, qkn[:, 1:2], KT[:, buf0:buf0 + n], rope0)
                for tt in range(n // 128):
                    vps = self.bank(6 + self.rr("yb", 2))[:, 0:128]
                    for k in range(KC):
                        c.mm(vps, h3[:, k, tt * 128:(tt + 1) * 128], wv3[:, k, :], start=(k == 0), stop=(k == KC - 1))
                    c.copy("act", VV3[:, buf0 // 128 + tt, :], vps)

            for (slot, c0, n, r, h3) in self.gstream(l, g_all):
                kv(slot * NT + c0, n, r, h3, (slot * NLAT + c0) if r == 0 else None)
            c.free(wk, wv)
            for hh in range(3):
                hg = g * 3 + hh
                wq, wq3 = self.load_w("wq", win[:, 256 + hg * 128:256 + (hg + 1) * 128], 128)
                wo = c.alloc("wo", D, BF16)
                c.dma("pool", wo.v(), wout[(2 + hg) * 128:(3 + hg) * 128, :])
                QT = QTt[self.rr("qt", 2)].v()
                for (c0, n, r) in g_own:
                    qps = self.bank(self.rr("kps", 2))[:, 0:n]
                    for k in range(KC):
                        c.mm(qps, wq3[:, k, :], self.HB3[:, k, c0:c0 + n], start=(k == 0), stop=(k == KC - 1))
                    self.rmsnorm_rope(qps, n, qkn[:, 0:1], QT[:, c0:c0 + n], c0 if r == 0 else None, table=ropeq)
                for qc in range(4):
                    oT = self.attn_T(KT, VV3, QT[:, qc * 512:(qc + 1) * 512], 512, list(range(34)))
                    self.outproj(oT, wo.v(), qc * 512, 512, 0)
                oT = self.attn_T(KT, VV3, QT[:, NLAT:NT], 128, [16, 33])
                self.outproj(oT, wo.v(), NLAT, 128, 1)
                c.free(wq, wo)
        c.free(KTt, VVt, *QTt, self.perm, qkn, *self.pt_tmp, *self.rl_tmp, *self.ot_tmp)
        for tm in self.rn_tmp:
            c.free(*tm.values())

    def emit_na(self, l):
        c = self.c
        win = self.inp(f"win{l}", [D, 3 * D])
        wout = self.inp(f"wout{l}", [D, D])
        nab = self.inp(f"nab{l}", [8, P, 5 * 768])
        HOt = c.alloc("HO", KC * 768, BF16)
        HO3 = HOt.r("p (c t) -> p c t", c=KC)
        hgroups = [(0, NLAT - 256, 256, 0), (1, 0, 256, 0), (0, NLAT, 128, 1), (1, NLAT, 128, 1)]
        hdst = [0, 256, 512, 640]
        for gi_, (slot, c0, n, r, h3) in enumerate(self.gstream(l, hgroups)):
            for cc in range(KC):
                c.copy(("dve", "act")[cc % 2], HO3[:, cc, hdst[gi_]:hdst[gi_] + n], h3[:, cc, :])
        NK = 2816
        srcs = [(0, 256, HO3[:, :, 0:256])] + \
               [(256 + i * 512, 512, self.HB3[:, :, i * 512:(i + 1) * 512]) for i in range(4)] + \
               [(2304, 256, HO3[:, :, 256:512]), (2560, 256, HO3[:, :, 512:768])]
        KTs = [c.alloc("KT", NK, BF16) for _ in range(2)]
        VVs = [c.alloc("VV", NK, BF16) for _ in range(2)]
        QTs = [c.alloc("QT", NT, BF16) for _ in range(2)]
        nbs = [c.alloc("nb", 5 * 768, F32) for _ in range(1)]
        ss_tmp = [c.alloc("ss", 768, F32) for _ in range(2)]
        pt_tmp = [c.alloc("pt", 1024, BF16) for _ in range(3)]
        rl_tmp = [c.alloc("rl", 512, F32) for _ in range(2)]
        og_tmp = [c.alloc("og", 512, BF16) for _ in range(2)]

        for h in range(8):
            wq, wq3 = self.load_w("wq", win[:, h * 128:(h + 1) * 128], 128)
            wk, wk3 = self.load_w("wk", win[:, D + h * 128:D + (h + 1) * 128], 128)
            wv, wv3 = self.load_w("wv", win[:, 2 * D + h * 128:2 * D + (h + 1) * 128], 128)
            wo = c.alloc("wo", D, BF16)
            c.dma("pool", wo.v(), wout[h * 128:(h + 1) * 128, :])
            nb = nbs[0]
            c.dma("sp", nb.v(), nab[h])
            nb3 = nb.r("p (t w) -> p t w", w=768)
            KT = KTs[h % 2].v()
            VV3 = VVs[h % 2].r("p (t d) -> p t d", d=128)
            QT = QTs[h % 2].v()
            for (b0, n, h3) in srcs:
                kps = self.bank(6 + self.rr("yb", 2))[:, 0:n]
                for k in range(KC):
                    c.mm(kps, wk3[:, k, :], h3[:, k, :], start=(k == 0), stop=(k == KC - 1))
                c.copy("act", KT[:, b0:b0 + n], kps)
                for tt in range(n // 128):
                    vps = self.bank(6 + self.rr("yb", 2))[:, 0:128]
                    for k in range(KC):
                        c.mm(vps, h3[:, k, tt * 128:(tt + 1) * 128], wv3[:, k, :], start=(k == 0), stop=(k == KC - 1))
                    c.copy("dve", VV3[:, b0 // 128 + tt, :], vps)
            for (c0, n) in [(i * 512, 512) for i in range(4)] + [(NLAT, 128)]:
                qps = self.bank(6 + self.rr("yb", 2))[:, 0:n]
                for k in range(KC):
                    c.mm(qps, wq3[:, k, :], self.HB3[:, k, c0:c0 + n], start=(k == 0), stop=(k == KC - 1))
                c.copy("act", QT[:, c0:c0 + n], qps)
            jobs = []
            for bi in range(16):
                ty = {0: 0, 1: 1, 14: 3, 15: 4}.get(bi, 2)
                wb = 2 * bi if bi <= 13 else 28
                t0_ = wb * 64 // 128
                jobs.append((bi * 128, [t0_ + m for m in range(6)] + [20, 21], ty))
            jobs.append((NLAT, [20, 21], None))

            def S_of(job):
                q0, tiles, ty = job
                k = self.rr("nas", 2)
                SA = self.bank(2 * k)
                SB = self.bank(2 * k + 1)
                for m, kt in enumerate(tiles):
                    dst = (SA if m < 4 else SB)[:, (m % 4) * 128:(m % 4 + 1) * 128]
                    c.mm(dst, KT[:, kt * 128:(kt + 1) * 128], QT[:, q0:q0 + 128])
                return SA, SB

            def P_of(job, S):
                q0, tiles, ty = job
                SA, SB = S
                PT = pt_tmp[self.rr("napt", 3)]
                if ty is not None:
                    SS = ss_tmp[self.rr("nass", 2)]
                    c.stt(SS[:, 0:512], SA[:, 0:512], SCALE, nb3[:, ty, 0:512], ALU.mult, ALU.add)
                    c.stt(SS[:, 512:768], SB[:, 0:256], SCALE, nb3[:, ty, 512:768], ALU.mult, ALU.add)
                    c.act(PT[:, 0:768], SS[:, 0:768], AF.Exp)
                    c.act(PT[:, 768:1024], SB[:, 256:512], AF.Exp, scale=SCALE)
                else:
                    c.act(PT[:, 0:256], SA[:, 0:256], AF.Exp, scale=SCALE)
                return PT

            def PV_of(job, PT, O_dst, L_dst):
                q0, tiles, ty = job
                nt = len(tiles)
                for m, kt in enumerate(tiles):
                    c.mm(O_dst, VV3[:, kt, :], PT[:, m * 128:(m + 1) * 128], start=(m == 0), stop=(m == nt - 1))
                for m, kt in enumerate(tiles):
                    c.mm(L_dst, self.ones1.v(), PT[:, m * 128:(m + 1) * 128], start=(m == 0), stop=(m == nt - 1))

            def finish_group(gjobs):
                n = 128 * len(gjobs)
                col0 = gjobs[0][0]
                r = 0 if gjobs[0][2] is not None else 1
                kk = self.rr("narl", 2)
                rl = rl_tmp[kk][:, 0:n]
                c.act(rl, self.bank(5)[:, 0:n], AF.Ln)
                c.act(rl, rl, AF.Exp, scale=-1.0)
                og = og_tmp[kk][:, 0:n]
                c.tt("dve", og, self.bank(4)[:, 0:n], rl, ALU.mult)
                self.outproj(og, wo.v(), col0, n, r)

            S_next = S_of(jobs[0])
            gjobs = []
            for ji, job in enumerate(jobs):
                S_cur = S_next
                if ji + 1 < len(jobs):
                    S_next = S_of(jobs[ji + 1])
                PT = P_of(job, S_cur)
                slot = len(gjobs)
                PV_of(job, PT, self.bank(4)[:, slot * 128:(slot + 1) * 128], self.bank(5)[:, slot * 128:(slot + 1) * 128])
                gjobs.append(job)
                if len(gjobs) == 4 or ji == len(jobs) - 1 or jobs[ji + 1][2] is None:
                    finish_group(gjobs)
                    gjobs = []
            c.free(wq, wk, wv, wo)
        c.free(HOt, *KTs, *VVs, *QTs, *nbs, *ss_tmp, *pt_tmp, *rl_tmp, *og_tmp)

    def emit_ln(self, l, which):
        c = self.c
        gi, bi_ = (0, 1) if which == 1 else (2, 3)
        eps = LN_EPS / (ALPHA * ALPHA)
        zq = [c.alloc("zq", KC * 512, BF16) for _ in range(2)]
        tmp = [dict(M=c.alloc("M", 512, F32), m2=c.alloc("m2", 512, F32), rstd=c.alloc("rstdl", 512, F32)) for _ in range(2)]
        t1s = [c.alloc("t1", 512, F32) for _ in range(3)]
        if which == 1:
            h2f = [c.alloc("h2f", KC * 512, F32) for _ in range(2)]
            wr = c.alloc("wr", KC * 64, F32)
            wr3 = wr.r("p (k e) -> p k e", k=KC)
            c.dma("sp", wr.v(), self.inp(f"wr{l}", [P, KC * 64]))
            mb = c.alloc("mb", 64, F32)
            c.dma("sp", mb.v(), self.inp(f"mb{l}", [P, 64]))
            gt = [c.alloc("gt", 64 * 3 + 16, F32) for _ in range(2)]
            GTS = c.alloc("GTS", NT, F32)
            c.memset("dve", self.G3[:, :, 64:65], 1.0)
            self.GTD = c.dram(f"gtd{l}", [NEXP, NT], F32)
        groups = [(i * 512, 512, 0) for i in range(4)] + [(NLAT, 128, 1)]
        def stats(gidx):
            c0, n, r = groups[gidx]
            k = gidx % 2
            zq3 = zq[k].r("p (c t) -> p c t", c=KC)[:, :, 0:n]
            mean = self.bank(0 if k == 0 else 6)[:, 0:n]
            msq = self.bank(1 if k == 0 else 7)[:, 0:n]
            for cc in range(KC):
                xs = self.XT3[:, cc, c0:c0 + n]
                c.act(zq3[:, cc, :], xs, AF.Square)
            for cc in range(KC):
                c.mm(mean, self.ones_mean_f.v(), self.XT3[:, cc, c0:c0 + n], start=(cc == 0), stop=(cc == KC - 1))
            for cc in range(KC):
                c.mm(msq, self.ones_mean.v(), zq3[:, cc, :], start=(cc == 0), stop=(cc == KC - 1))
            M = tmp[k]["M"][:, 0:n]
            m2 = tmp[k]["m2"][:, 0:n]
            rstd = tmp[k]["rstd"][:, 0:n]
            c.copy("act", M, mean)
            c.act(m2, mean, AF.Square)
            c.tt("dve", m2, msq, m2, ALU.subtract)
            c.act(rstd, m2, AF.Ln, bias=eps)
            c.act(rstd, rstd, AF.Exp, scale=-0.5)

        def advance(nblk):
            g_ = getattr(self, "pending_mod", None)
            if g_ is None:
                return
            for _ in range(nblk):
                try:
                    next(g_)
                except StopIteration:
                    self.pending_mod = None
                    return

        stats(0)
        advance(2)
        for gidx, (c0, n, r) in enumerate(groups):
            k = gidx % 2
            M = tmp[k]["M"][:, 0:n]
            rstd = tmp[k]["rstd"][:, 0:n]
            if gidx + 1 < len(groups):
                stats(gidx + 1)
            advance(3)
            if which == 1:
                h2f3 = h2f[k].r("p (c t) -> p c t", c=KC)[:, :, 0:n]
            for cc in range(KC):
                xs = self.XT3[:, cc, c0:c0 + n]
                t1 = t1s[self.rr("t1", 3)][:, 0:n]
                c.tt("dve", t1, xs, M, ALU.subtract)
                c.tt("dve", t1, t1, rstd, ALU.mult)
                c.act(xs, t1, AF.Identity, scale=self.LNV3[:, gi, cc:cc + 1], bias=self.LNV3[:, bi_, cc:cc + 1])
                if which == 1:
                    c.act(h2f3[:, cc, :], t1, AF.Identity, scale=self.A23[:, cc, r:r + 1], bias=self.B23[:, cc, r:r + 1])
                    c.copy("dve", self.HB3[:, cc, c0:c0 + n], h2f3[:, cc, :])
            if which == 1:
                for tt in range(n // 128):
                    tile_i = c0 // 128 + tt
                    lg = self.bank(2 + self.rr("lg", 2))[:, 0:64]
                    for cc in range(KC):
                        c.mm(lg, h2f3[:, cc, tt * 128:(tt + 1) * 128], wr3[:, cc, :], start=(cc == 0), stop=(cc == KC - 1))
                    g_ = gt[self.rr("gt", 2)]
                    sc, sel, wsel, m8, den = g_[:, 0:64], g_[:, 64:128], g_[:, 128:192], g_[:, 192:200], g_[:, 200:201]
                    rden = g_[:, 201:202]
                    c.act(sc, lg, AF.Sigmoid)
                    c.tt("dve", sel, sc, mb.v(), ALU.add)
                    c.op("dve", lambda e: e.max(out=m8.ap, in_=sel.ap), reads=[sel], writes=[m8])
                    c.ts("dve", sel, sel, m8[:, 7:8], op0=ALU.is_ge)
                    c.tt("dve", wsel, sel, sc, ALU.mult)
                    c.op("dve", lambda e: e.reduce_sum(out=den.ap, in_=wsel.ap, axis=AX.X), reads=[wsel], writes=[den])
                    c.op("dve", lambda e: e.reciprocal(out=rden.ap, in_=den.ap), reads=[den], writes=[rden])
                    c.ts("dve", self.G3[:, tile_i, 0:64], wsel, rden, ROUTE_SCALE, ALU.mult, ALU.mult)
                    gtp = self.bank(4 + self.rr("gtp", 2))[0:NEXP, 0:128]
                    c.transpose(gtp, self.G3[:, tile_i, :], self.identf.v())
                    c.copy("act", GTS[0:NEXP, tile_i * 128:(tile_i + 1) * 128], gtp)
        if which == 1:
            c.dma("sp", self.GTD.v(), GTS[0:NEXP, :])
            c.free(*h2f, wr, mb, *gt, GTS)
        advance(100)
        c.free(*zq, *t1s)
        for t_ in tmp:
            c.free(*t_.values())

    def emit_moe(self, l):
        c = self.c
        moew = self.inp(f"moew{l}", [NEXP, P, 6144])
        NWB = 4
        WB = [c.alloc("WB", 6144, BF16) for _ in range(NWB)]
        GB = [c.alloc("GB", NT, F32) for _ in range(NWB)]
        sT = [c.alloc("sT", 512, BF16) for _ in range(2)]
        tT = [c.alloc("tT", 512, BF16) for _ in range(2)]
        aT = [c.alloc("aT", 512, BF16) for _ in range(4)]
        groups = [(i * 256, 256, 0) for i in range(8)] + ([(NLAT, 128, 1)] if l < DEPTH - 1 else [])
        pairs = [(2 * i, 2 * i + 1) for i in range(32)] + [(64,)]

        def prefetch(e):
            w = WB[e % NWB]
            for q in range(3):
                c.dma("pool", w[:, q * 2048:(q + 1) * 2048], moew[e][:, q * 2048:(q + 1) * 2048])
            c.dma("sp", GB[e % NWB].r("p (o t) -> p o t", o=1), V(self.GTD, self.GTD.h[e:e + 1, :].partition_broadcast(P)))

        def down_mm(sts):
            e0, c0, n, r, _ = sts[0]
            Y = [self.bank(4 + cc // 2)[:, (cc % 2) * 256:(cc % 2) * 256 + n] for cc in range(KC)]
            nmm = 2 * len(sts)
            for cc in range(KC):
                i = 0
                for (e, _, _, _, a3) in sts:
                    w = WB[e % NWB].v()
                    for jj in range(2):
                        c.mm(Y[cc], w[:, 4096 + jj * 1024 + cc * 128:4096 + jj * 1024 + (cc + 1) * 128], a3[:, jj, :],
                             start=(i == 0), stop=(i == nmm - 1))
                        i += 1

        def down_acc(sts):
            e0, c0, n, r, _ = sts[0]
            Y = [self.bank(4 + cc // 2)[:, (cc % 2) * 256:(cc % 2) * 256 + n] for cc in range(KC)]
            for cc in range(KC):
                xs = self.XT3[:, cc, c0:c0 + n]
                c.stt(xs, Y[cc], self.MODV3[:, 40 + cc, r:r + 1], xs, ALU.mult, ALU.add)

        for e in pairs[0]:
            prefetch(e)
        pend = []
        ready = None
        it = 0
        for pi, pr in enumerate(pairs):
            for gidx, (c0, n, r) in enumerate(groups):
                for ei, e in enumerate(pr):
                    w = WB[e % NWB].v()
                    gb = GB[e % NWB].v()
                    k = it % 2
                    gps = self.bank(2 * k)
                    ups = self.bank(2 * k + 1)
                    g3 = gps[:, 0:2 * n].r("p (j t) -> p j t", j=2)
                    u3 = ups[:, 0:2 * n].r("p (j t) -> p j t", j=2)
                    for jj in range(2):
                        for kk in range(KC):
                            c.mm(g3[:, jj, :], w[:, kk * 256 + jj * 128:kk * 256 + (jj + 1) * 128], self.HB3[:, kk, c0:c0 + n],
                                 start=(kk == 0), stop=(kk == KC - 1))
                    for jj in range(2):
                        for kk in range(KC):
                            c.mm(u3[:, jj, :], w[:, 2048 + kk * 256 + jj * 128:2048 + kk * 256 + (jj + 1) * 128],
                                 self.HB3[:, kk, c0:c0 + n], start=(kk == 0), stop=(kk == KC - 1))
                    did = None
                    if ei == 0 and ready is not None:
                        down_mm(ready)
                        did = ready
                        ready = None
                        if gidx == 0 and pi + 1 < len(pairs):
                            for e2 in pairs[pi + 1]:
                                prefetch(e2)
                    elif ei == 0 and gidx == 0 and pi == 0:
                        for e2 in pairs[1]:
                            prefetch(e2)
                    s3 = sT[k][:, 0:2 * n].r("p (j t) -> p j t", j=2)
                    t3 = tT[k][:, 0:2 * n].r("p (j t) -> p j t", j=2)
                    a3 = aT[it % 4][:, 0:2 * n].r("p (j t) -> p j t", j=2)
                    c.act(s3, g3, AF.Silu)
                    for jj in range(2):
                        c.tt("dve", t3[:, jj, :], u3[:, jj, :], gb[:, c0:c0 + n], ALU.mult)
                    c.tt("dve", a3, s3, t3, ALU.mult)
                    if did is not None:
                        down_acc(did)
                    pend.append((e, c0, n, r, a3))
                    if ei == len(pr) - 1:
                        ready = pend
                        pend = []
                    it += 1
        down_mm(ready)
        down_acc(ready)
        c.free(*WB, *GB, *sT, *tT, *aT)


BF = ml_dtypes.bfloat16


def _fm(tok):
    T_ = tok.shape[0]
    return np.ascontiguousarray(tok.T.reshape(KC, P, T_).transpose(1, 0, 2)).reshape(P, KC * T_)


def _unfm(a, T_):
    return np.ascontiguousarray(a.reshape(P, KC, T_).transpose(1, 0, 2).reshape(D, T_).T)


_CONST_CACHE = {}


def _consts(s):
    if s in _CONST_CACHE:
        return _CONST_CACHE[s]
    out = {}
    out["ident"] = np.eye(P, dtype=np.float32)
    d = np.arange(P)
    i = d % 64
    partner = np.where(i < 32, d + 32, d - 32)
    perm = np.zeros((P, P), np.float32)
    perm[partner, d] = 1.0
    out["perm"] = perm
    pos = np.arange(4096)
    inv = 10000.0 ** (-(2.0 / 64) * np.arange(32, dtype=np.float64))
    pv = np.where((d // 64)[:, None] == 0, (pos // 64)[None, :], (pos % 64)[None, :]).astype(np.float64)
    ang = pv * inv[i % 32][:, None]
    cos = np.cos(ang)
    sin = np.sin(ang) * np.where(i < 32, -1.0, 1.0)[:, None]
    rope = np.stack([cos, sin], axis=1)
    out["rope"] = rope.reshape(P, 2 * 4096).astype(BF)
    out["ropeq"] = np.ascontiguousarray(rope[:, :, s * NLAT:(s + 1) * NLAT]).reshape(P, 2 * NLAT).astype(BF)
    cc = np.arange(64)
    c64 = np.cos(2 * np.pi * np.outer(cc, cc) / 64) / 8.0
    s64 = np.sin(2 * np.pi * np.outer(cc, cc) / 64) / 8.0
    cs2 = np.zeros((P, 256), np.float32)
    for g2 in range(2):
        cs2[g2 * 64:(g2 + 1) * 64, g2 * 64:(g2 + 1) * 64] = c64
        cs2[g2 * 64:(g2 + 1) * 64, 128 + g2 * 64:128 + (g2 + 1) * 64] = s64
    out["cs2"] = cs2
    kk = s * NLAT + np.arange(NLAT)
    ph = (np.outer(pos, kk) % 4096).astype(np.float64) * (2 * np.pi / 4096)
    tab = np.stack([np.cos(ph) / 64.0, -np.sin(ph) / 64.0], axis=0).astype(np.float32)
    tab = tab.reshape(2, 8, 4, P, 4, 512)
    out["dft"] = np.ascontiguousarray(tab.transpose(4, 1, 3, 0, 2, 5)).reshape(4, 8, P, 4096).astype(BF)
    cpos = np.stack([np.arange(128), 128 + np.arange(128)], axis=0)
    ck = s * 128 + np.arange(128)
    cph = (cpos[:, :, None] * ck[None, None, :] % 256) * (2 * np.pi / 256)
    ctab = np.stack([np.cos(cph) / 16.0, -np.sin(cph) / 16.0], axis=0)
    out["cdft"] = np.ascontiguousarray(ctab.transpose(2, 0, 1, 3)).reshape(P, 512).astype(np.float32)
    _CONST_CACHE[s] = out
    return out


def _na_bias(rpb, s):
    out = np.full((8, P, 5, 768), NEG, np.float32)
    q = np.arange(P)
    w = np.arange(768)
    qcol = q % 64
    kcol = w % 64
    cstart = np.clip(qcol - 8, 0, 48)
    okc = (kcol[None, :] >= cstart[:, None]) & (kcol[None, :] < cstart[:, None] + 16)
    dc = kcol[None, :] - qcol[:, None] + 15
    rep = {0: 0, 1: 1, 2: 2, 3: 14, 4: 15}
    for ty, bi in rep.items():
        i = 16 * s + bi
        wb = 2 * bi if bi <= 13 else 28
        qrow = 2 * i + q // 64
        rstart = np.clip(qrow - 4, 0, 56)
        krow = wb + (32 * s - 4) + w // 64
        okr = (krow[None, :] >= rstart[:, None]) & (krow[None, :] < rstart[:, None] + 8)
        dr = krow[None, :] - qrow[:, None] + 7
        ok = okr & okc
        drc = np.clip(dr, 0, 14)
        dcc = np.clip(dc, 0, 30)
        vals = rpb[:, drc, dcc]
        out[:, :, ty, :] = np.where(ok[None], vals, NEG)
    outT = out.reshape(8, P, 5, 6, P).transpose(0, 4, 2, 3, 1)
    return np.ascontiguousarray(outT).reshape(8, P, 5 * 768)


def _layer_weights(l, inp, tag):
    j = l // 2
    w = {}
    w[f"wmod{tag}"] = inp["w_mod"][l]
    w[f"bmod{tag}"] = np.ascontiguousarray(inp["b_mod"][l].reshape(48, P).T)
    w[f"lnv{tag}"] = np.ascontiguousarray(
        np.stack([inp[k][l].reshape(KC, P).T for k in ("ln1_g", "ln1_b", "ln2_g", "ln2_b")], axis=1)).reshape(P, 32)
    if l % 2 == 0:
        w[f"win{tag}"] = inp["ab_w_in"][j]
        w[f"wout{tag}"] = inp["ab_w_out"][j]
        wf = inp["ab_w_fnet"][j]
        wfn = np.zeros((P, 2, P), np.float32)
        for jj in range(2):
            wfn[0:64, jj, 0:64] = wf[2 * jj]
            wfn[64:128, jj, 64:128] = wf[2 * jj + 1]
        w[f"wfn{tag}"] = wfn.reshape(P, 256)
        w[f"qkn{tag}"] = np.ascontiguousarray(np.stack([inp["ab_q_norm"][j], inp["ab_k_norm"][j]], axis=1))
    else:
        w[f"win{tag}"] = inp["na_w_in"][j]
        w[f"wout{tag}"] = inp["na_w_out"][j]
    w[f"wr{tag}"] = np.ascontiguousarray(inp["moe_w_router"][l].reshape(KC, P, 64).transpose(1, 0, 2)).reshape(P, KC * 64)
    w[f"mb{tag}"] = np.ascontiguousarray(np.broadcast_to(inp["moe_bias"][l][None, :], (P, 64)))
    wg = np.concatenate([inp["moe_w_gate"][l], inp["sh_w_gate"][l][None]], axis=0)
    wu = np.concatenate([inp["moe_w_up"][l], inp["sh_w_up"][l][None]], axis=0)
    wd = np.concatenate([inp["moe_w_down"][l], inp["sh_w_down"][l][None]], axis=0)
    moew = np.empty((NEXP, P, 6144), np.float32)
    moew[:, :, 0:2048] = wg.reshape(NEXP, KC, P, 256).transpose(0, 2, 1, 3).reshape(NEXP, P, 2048)
    moew[:, :, 2048:4096] = wu.reshape(NEXP, KC, P, 256).transpose(0, 2, 1, 3).reshape(NEXP, P, 2048)
    moew[:, :, 4096:6144] = wd.reshape(NEXP, 2, P, D).transpose(0, 2, 1, 3).reshape(NEXP, P, 2048)
    w[f"moew{tag}"] = moew
    return w


_PROG_CACHE = {}


def _get_prog(layers, debug=None):
    key = (tuple(layers), tuple(sorted((debug or {}).items())))
    if key not in _PROG_CACHE:
        pr = Prog(list(layers), debug)
        nc = pr.build()
        _PROG_CACHE[key] = (nc, set(pr.inputs.keys()))
    return _PROG_CACHE[key]


def _init_toks(inp):
    toks = []
    for r in range(8):
        b, s = r // 2, r % 2
        toks.append(np.concatenate([inp["x"][b, s * NLAT:(s + 1) * NLAT], inp["ctx"][b, s * NCTX:(s + 1) * NCTX]], axis=0))
    return toks


def _launch(inp, layers, toks, debug=None):
    nc, names = _get_prog(layers, debug)
    lw = {}
    for l in layers:
        lw.update(_layer_weights(l, inp, l))
    fm = [_fm(t) for t in toks]
    in_maps = []
    for r in range(8):
        b, s = r // 2, r % 2
        m = dict(lw)
        cs = _consts(s)
        for k in ("ident", "perm", "rope", "ropeq", "cs2", "dft", "cdft"):
            m[k] = cs[k]
        for l in layers:
            if l % 2 == 1:
                m[f"nab{l}"] = _na_bias(inp["na_rpb"][l // 2], s)
        m["cT"] = np.ascontiguousarray(
            np.stack([inp["c"][b].reshape(KC, P).T, inp["c_ctx"].reshape(KC, P).T], axis=2)).reshape(P, 16)
        m["xT"] = fm[r]
        m[f"xg{layers[0]}"] = np.ascontiguousarray(
            np.stack([fm[r - s].reshape(P, KC, NT), fm[r - s + 1].reshape(P, KC, NT)], axis=0).transpose(2, 0, 1, 3)
        ).reshape(KC, 2 * P, NT)
        in_maps.append({k: v for k, v in m.items() if k in names})
    res = run_bass_kernel_spmd(nc, in_maps, core_ids=list(range(8)))
    return [_unfm(res.results[r]["xout"], NT) for r in range(8)]


def run_layers(inp, layers=range(DEPTH), toks=None, debug=None, fused=False):
    inp = {k: np.asarray(v) for k, v in inp.items()}
    if toks is None:
        toks = _init_toks(inp)
    if fused:
        return _launch(inp, list(layers), toks, debug)
    for l in layers:
        toks = _launch(inp, [l], toks, debug)
    return toks


def kernel(**inputs):
    toks = run_layers(inputs, fused=True)
    out = np.empty((4, 4096, D), np.float32)
    for r in range(8):
        b, s = r // 2, r % 2
        out[b, s * NLAT:(s + 1) * NLAT] = toks[r][0:NLAT]
    return out
```

```python
import math
import numpy as np
import ml_dtypes
import concourse.bass as bass
import concourse.mybir as mybir
from concourse.bass_utils import run_bass_kernel_spmd
from contextlib import ExitStack

F32 = mybir.dt.float32
BF16 = mybir.dt.bfloat16
AF = mybir.ActivationFunctionType
ALU = mybir.AluOpType
AX = mybir.AxisListType

P = 128
D = 1024
KC = 8
NLAT = 2048
NCTX = 128
NT = NLAT + NCTX
DEPTH = 4
ALPHA = (2 * DEPTH) ** 0.25
LN_EPS = 1e-6
RMS_EPS = 1e-6
HD = 128
SCALE = HD ** -0.5
NEG = -1e30
NEXP = 65
ROUTE_SCALE = 2.5

ENGS = ("pe", "act", "dve", "pool", "sp")
DTSIZE = {F32: 4, BF16: 2}


class V:
    __slots__ = ("t", "ap")

    def __init__(self, t, ap):
        self.t = t
        self.ap = ap

    def __getitem__(self, idx):
        return V(self.t, self.ap[idx])

    def r(self, pat, **kw):
        return V(self.t, self.ap.rearrange(pat, **kw))

    def bc(self, shape):
        return V(self.t, self.ap.broadcast_to(shape))

    def bitcast(self, dt):
        return V(self.t, self.ap.bitcast(dt))


class T:
    def __init__(self, h, name, space, rng=None):
        self.h = h
        self.name = name
        self.space = space
        self.w = None
        self.r_ = []
        self.dsem = None
        self.dcnt = 0
        self.rng = rng

    def __getitem__(self, idx):
        return V(self, self.h[idx])

    def r(self, pat, **kw):
        return V(self, self.h.rearrange(pat, **kw))

    def v(self):
        return V(self, self.h)


class Ctx:
    def __init__(self, nc, arena_bytes=211968):
        self.nc = nc
        self.es = ExitStack()
        self.E = {"pe": nc.tensor, "act": nc.scalar, "dve": nc.vector, "pool": nc.gpsimd, "sp": nc.sync}
        self.sem = {}
        self.cnt = {}
        for e in ENGS:
            self.sem[e] = self.es.enter_context(nc.semaphore("s_" + e))
            self.cnt[e] = 0
        self.seen = {e: {} for e in ENGS}
        self.nbuf = 0
        self.sem_pool = []
        self.arena = self.es.enter_context(nc.sbuf_tensor("arena", [P, arena_bytes // 2], BF16))
        self.arena_bytes = arena_bytes
        self.free_list = [(0, arena_bytes)]
        self.grave = []
        self.live = {}
        self.banks = []
        for i in range(8):
            h = self.es.enter_context(nc.psum_tensor(f"bank{i}", [P, 512], F32))
            self.banks.append(T(h[:], f"bank{i}", "ps"))

    def alloc(self, name, nelem, dt, parts=P):
        nbytes = (nelem * DTSIZE[dt] + 63) // 64 * 64
        for i, (s, e) in enumerate(self.free_list):
            if e - s >= nbytes:
                self.free_list[i] = (s + nbytes, e)
                if self.free_list[i][0] == self.free_list[i][1]:
                    del self.free_list[i]
                break
        else:
            raise RuntimeError(f"arena OOM allocating {name} {nbytes}B; free={self.free_list}")
        ap = self.arena[0:parts, s // 2:(s + nbytes) // 2]
        if dt != BF16:
            ap = ap.bitcast(dt)
        ap = ap[:, 0:nelem]
        self.nbuf += 1
        t = T(ap, f"{name}_{self.nbuf}", "sb", (s, s + nbytes))
        keep = []
        for (gs, ge, toks) in self.grave:
            if gs < s + nbytes and s < ge:
                t.r_.extend(toks)
                if gs >= s and ge <= s + nbytes:
                    continue
            keep.append((gs, ge, toks))
        self.grave = keep
        return t

    def free(self, *ts):
        for t in ts:
            s, e = t.rng
            toks = [x for x in ([t.w] + t.r_) if x is not None]
            self.grave.append((s, e, self._compress(toks)))
            self.free_list.append((s, e))
            self.free_list.sort()
            merged = []
            for a, b in self.free_list:
                if merged and merged[-1][1] == a:
                    merged[-1] = (merged[-1][0], b)
                else:
                    merged.append((a, b))
            self.free_list = merged
            t.rng = None
            if t.dsem is not None:
                self.sem_pool.append((t.dsem, t.dcnt, t.dkey))
                t.dsem = None

    @staticmethod
    def _compress(toks):
        best = {}
        for tok in toks:
            key = tok[1] if tok[0] == "eng" else tok[3]
            v = tok[2]
            if key not in best or best[key][2] < v:
                best[key] = tok
        return list(best.values())

    def dram(self, name, shape, dt, kind="Internal"):
        h = self.nc.dram_tensor(name, list(shape), dt, kind=kind)
        return T(h.ap(), name, "dram")

    def _wait(self, e, tok):
        if tok is None:
            return
        if tok[0] == "eng":
            _, f, v = tok
            key = "e:" + f
            sem = self.sem[f]
        else:
            _, sem, v, key = tok
        if e == "pe" and tok[0] == "eng" and tok[1] == "pe":
            return
        if self.seen[e].get(key, 0) >= v:
            return
        self.E[e].wait_ge(sem, v)
        self.seen[e][key] = v

    def _deps(self, e, reads, writes, acc=False):
        for t in reads:
            self._wait(e, t.w)
        if not acc:
            for t in writes:
                self._wait(e, t.w)
                for tok in t.r_:
                    self._wait(e, tok)

    def op(self, e, fn, reads=(), writes=(), acc=False, inc=True):
        reads = [v.t for v in reads if v is not None]
        writes = [v.t for v in writes if v is not None]
        self._deps(e, reads, writes, acc)
        ins = fn(self.E[e])
        if inc:
            self.cnt[e] += 1
            ins.then_inc(self.sem[e], 1)
            tok = ("eng", e, self.cnt[e])
        else:
            tok = ("eng", e, self.cnt[e] + 1)
        for t in reads:
            t.r_.append(tok)
            if len(t.r_) > 24:
                t.r_ = self._compress(t.r_)
        for t in writes:
            t.w = tok
            t.r_ = []
        return ins

    def dma(self, q, out, in_, **kw):
        ot = out.t if isinstance(out, V) else None
        it = in_.t if isinstance(in_, V) else None
        oap = out.ap if isinstance(out, V) else out
        iap = in_.ap if isinstance(in_, V) else in_
        reads = [it] if it is not None else []
        writes = [ot] if ot is not None else []
        self._deps(q, reads, writes)
        owner = None
        for t in (ot, it):
            if t is not None and t.space != "dram":
                owner = t
                break
        if owner is None:
            owner = ot if ot is not None else it
        if owner.dsem is None:
            if self.sem_pool:
                sem, cnt, key = self.sem_pool.pop()
                if cnt > 0:
                    self._wait(q, ("dma", sem, cnt, key))
            else:
                self.nsem = getattr(self, "nsem", 0) + 1
                key = f"d:{self.nsem}"
                sem, cnt = self.es.enter_context(self.nc.semaphore(f"dsem{self.nsem}")), 0
            owner.dsem, owner.dcnt, owner.dkey = sem, cnt, key
        owner.dcnt += 16
        ins = self.E[q].dma_start(out=oap, in_=iap, **kw)
        ins.then_inc(owner.dsem, 16)
        tok = ("dma", owner.dsem, owner.dcnt, owner.dkey)
        for t in reads:
            t.r_.append(tok)
        for t in writes:
            t.w = tok
            t.r_ = []
        return tok

    def wait_all(self, e, ts):
        for t in ts:
            self._wait(e, t.w)
            for tok in t.r_:
                self._wait(e, tok)

    def mm(self, out, lhsT, rhs, start=True, stop=True, inc=None):
        self.op("pe", lambda e: e.matmul(out.ap, lhsT=lhsT.ap, rhs=rhs.ap, start=start, stop=stop),
                reads=[lhsT, rhs], writes=[out], acc=not start, inc=(stop if inc is None else inc))

    def transpose(self, out, in_, ident):
        self.op("pe", lambda e: e.transpose(out.ap, in_.ap, ident.ap), reads=[in_, ident], writes=[out])

    def act(self, out, in_, func, scale=None, bias=None, accum=None):
        kw = {}
        rd = [in_]
        wr = [out]
        if scale is not None:
            if isinstance(scale, V):
                kw["scale"] = scale.ap
                rd.append(scale)
            else:
                kw["scale"] = float(scale)
        if bias is not None:
            if isinstance(bias, V):
                kw["bias"] = bias.ap
                rd.append(bias)
            else:
                kw["bias"] = float(bias)
        if accum is not None:
            kw["accum_out"] = accum.ap
            wr.append(accum)
        self.op("act", lambda e: e.activation(out=out.ap, in_=in_.ap, func=func, **kw), reads=rd, writes=wr)

    def copy(self, eng, out, in_):
        if eng == "act":
            self.op("act", lambda e: e.copy(out=out.ap, in_=in_.ap), reads=[in_], writes=[out])
        else:
            self.op(eng, lambda e: e.tensor_copy(out=out.ap, in_=in_.ap), reads=[in_], writes=[out])

    def tt(self, eng, out, in0, in1, op):
        self.op(eng, lambda e: e.tensor_tensor(out=out.ap, in0=in0.ap, in1=in1.ap, op=op),
                reads=[in0, in1], writes=[out])

    def ts(self, eng, out, in0, s1, s2=None, op0=ALU.mult, op1=None):
        rd = [in0]
        a1 = s1.ap if isinstance(s1, V) else float(s1)
        if isinstance(s1, V):
            rd.append(s1)
        a2 = None
        if s2 is not None:
            a2 = s2.ap if isinstance(s2, V) else float(s2)
            if isinstance(s2, V):
                rd.append(s2)
        if op1 is None:
            self.op(eng, lambda e: e.tensor_scalar(out=out.ap, in0=in0.ap, scalar1=a1, scalar2=None, op0=op0),
                    reads=rd, writes=[out])
        else:
            self.op(eng, lambda e: e.tensor_scalar(out=out.ap, in0=in0.ap, scalar1=a1, scalar2=a2, op0=op0, op1=op1),
                    reads=rd, writes=[out])

    def stt(self, out, in0, scalar, in1, op0, op1, eng="dve"):
        rd = [in0, in1]
        a = scalar.ap if isinstance(scalar, V) else float(scalar)
        if isinstance(scalar, V):
            rd.append(scalar)
        self.op(eng, lambda e: e.scalar_tensor_tensor(out=out.ap, in0=in0.ap, scalar=a, in1=in1.ap, op0=op0, op1=op1),
                reads=rd, writes=[out])

    def memset(self, eng, out, val):
        self.op(eng, lambda e: e.memset(out.ap, val), writes=[out])

    def close(self):
        self.es.close()


class Prog:
    def __init__(self, layers, debug=None):
        self.layers = list(layers)
        self.debug = debug or {}
        self.nc = bass.Bass("TRN2", target_bir_lowering=False)
        self.c = Ctx(self.nc)
        self.inputs = {}
        self.pending_mod = None
        self.rot = {}
        self.xg = {}

    def inp(self, name, shape, dt=F32):
        if name not in self.inputs:
            self.inputs[name] = self.nc.dram_tensor(name, list(shape), dt, kind="ExternalInput").ap()
        return self.inputs[name]

    def bank(self, i):
        return self.c.banks[i]

    def rr(self, key, n):
        v = self.rot.get(key, 0)
        self.rot[key] = v + 1
        return v % n

    def build(self):
        c = self.c
        nc = self.nc
        first = self.layers[0]
        self.XT = c.alloc("XT", KC * NT, F32)
        self.XT3 = self.XT.r("p (c t) -> p c t", c=KC)
        self.HB = c.alloc("HB", KC * NT, BF16)
        self.HB3 = self.HB.r("p (c t) -> p c t", c=KC)
        self.identb = c.alloc("identb", P, BF16)
        self.identf = c.alloc("identf", P, F32)
        c.dma("pool", self.identb.v(), self.inp("ident", [P, P]))
        c.dma("sp", self.identf.v(), self.inp("ident", [P, P]))
        self.ones_mean = c.alloc("ones_mean", P, BF16)
        self.ones_rms = c.alloc("ones_rms", P, BF16)
        self.ones1 = c.alloc("ones1", P, BF16)
        c.memset("dve", self.ones_mean.v(), 1.0 / D)
        self.ones_mean_f = c.alloc("ones_mean_f", P, F32)
        c.memset("dve", self.ones_mean_f.v(), 1.0 / D)
        c.memset("dve", self.ones_rms.v(), 1.0 / HD)
        c.memset("dve", self.ones1.v(), 1.0)
        self.sets = []
        for _ in range(2):
            d_ = {}
            d_["MODV"] = c.alloc("MODV", 96, F32)
            d_["MODV3"] = d_["MODV"].r("p (m r) -> p m r", r=2)
            d_["LNV"] = c.alloc("LNV", 32, F32)
            d_["LNV3"] = d_["LNV"].r("p (m c) -> p m c", c=KC)
            d_["A2"] = c.alloc("A2", 16, F32)
            d_["A23"] = d_["A2"].r("p (c r) -> p c r", r=2)
            d_["B2"] = c.alloc("B2", 16, F32)
            d_["B23"] = d_["B2"].r("p (c r) -> p c r", r=2)
            self.sets.append(d_)
        self.use_set(self.layers[0])
        self.G = c.alloc("G", 17 * NEXP, F32)
        self.G3 = self.G.r("p (t e) -> p t e", e=NEXP)
        self.scT = c.alloc("scT", 16, F32)
        self.scT3 = self.scT.r("p (k r) -> p k r", r=2)
        c.dma("sp", self.scT.v(), self.inp("cT", [P, 16]))
        c.act(self.scT.v(), self.scT.v(), AF.Silu)
        c.dma("sp", self.XT.v(), self.inp("xT", [P, KC * NT]))
        for li, l in enumerate(self.layers):
            self.layer(l)
            if li + 1 < len(self.layers):
                self.exchange(l, self.layers[li + 1])
        xout = nc.dram_tensor("xout", [P, KC * NT], F32, kind="ExternalOutput").ap()
        tok = c.dma("sp", xout, self.XT.v())
        c._wait("sp", tok)
        c.close()
        return nc

    def exchange(self, l, lnext):
        c = self.c
        nc = self.nc
        sem = c.es.enter_context(nc.semaphore(f"ccsem{l}"))
        tok = ("dma", sem, KC, f"cc{l}")
        xgh = nc.dram_tensor(f"xgi{lnext}", [KC, 2 * P, NT], F32)
        xg = T(xgh.ap(), f"xgi{lnext}", "dram")
        for cc in range(KC):
            xsh = nc.dram_tensor(f"xs{l}_{cc}", [P, NT], F32)
            xs = T(xsh.ap(), f"xs{l}_{cc}", "dram")
            c.dma("sp", xs.v(), self.XT3[:, cc, :])
            c._deps("pool", [xs], [xg] if cc == 0 else [])
            ins = nc.gpsimd.collective_compute("AllGather", ALU.bypass, replica_groups=[[0, 1], [2, 3], [4, 5], [6, 7]],
                                               ins=[xsh.ap().opt()], outs=[xgh.ap()[cc]])
            ins.then_inc(sem)
            xs.r_.append(tok)
        xg.w = tok
        xg.r_ = []
        self.xg[lnext] = xg

    def use_set(self, l):
        for k_, v_ in self.sets[l % 2].items():
            setattr(self, k_, v_)

    def layer(self, l):
        self.use_set(l)
        if l == self.layers[0]:
            self.emit_mod(l)
        if self.debug.get("stop") == "mod":
            return
        self.emit_h1(l)
        if self.debug.get("stop") == "h1":
            return
        if l % 2 == 0:
            self.emit_gqa(l)
        else:
            self.emit_na(l)
        stop = self.debug.get("stop")
        if stop == "mixer":
            return
        self.emit_ln(l, 1)
        if stop == "ln1":
            return
        self.emit_moe(l)
        if stop == "moe":
            return
        li = self.layers.index(l)
        if li + 1 < len(self.layers):
            self.pending_mod = self.mod_gen(self.layers[li + 1])
        self.emit_ln(l, 2)

    def emit_mod(self, l):
        for _ in self.mod_gen(l):
            pass

    def mod_gen(self, l):
        c = self.c
        tg = self.sets[l % 2]
        wmod = self.inp(f"wmod{l}", [D, 6 * D]).rearrange("(k p) n -> p k n", p=P)
        bmod = c.alloc("bmod", 48, F32)
        c.dma("sp", bmod.v(), self.inp(f"bmod{l}", [P, 48]))
        c.dma("sp", tg["LNV"].v(), self.inp(f"lnv{l}", [P, 32]))
        st = [c.alloc("wmst", KC * 512, F32) for _ in range(2)]
        scb3 = self.scT3
        ps = self.bank(3)
        ps3 = ps[:, 0:96].r("p (m r) -> p m r", r=2)
        for blk in range(12):
            s = st[blk % 2]
            s3 = s.r("p (k n) -> p k n", k=KC)
            c.dma("sp", s3, wmod[:, :, blk * 512:(blk + 1) * 512])
            for fc in range(4):
                for k in range(KC):
                    c.mm(ps3[:, blk * 4 + fc, :], s3[:, k, fc * 128:(fc + 1) * 128], scb3[:, k, :],
                         start=(k == 0), stop=(k == KC - 1))
            yield blk
        M3 = tg["MODV3"]
        for r in range(2):
            c.tt("dve", M3[:, :, r], ps3[:, :, r], bmod.v(), ALU.add)
        c.ts("dve", M3[:, 8:16, :], M3[:, 8:16, :], 1.0, op0=ALU.add)
        c.ts("dve", M3[:, 32:40, :], M3[:, 32:40, :], 1.0, op0=ALU.add)
        c.ts("dve", M3[:, 16:24, :], M3[:, 16:24, :], 1.0 / ALPHA, op0=ALU.mult)
        c.ts("dve", M3[:, 40:48, :], M3[:, 40:48, :], 1.0 / ALPHA, op0=ALU.mult)
        for r in range(2):
            c.tt("dve", tg["A23"][:, :, r], tg["LNV3"][:, 0, :], M3[:, 32:40, r], ALU.mult)
            c.tt("dve", tg["B23"][:, :, r], tg["LNV3"][:, 1, :], M3[:, 32:40, r], ALU.mult)
            c.tt("dve", tg["B23"][:, :, r], tg["B23"][:, :, r], M3[:, 24:32, r], ALU.add)
        c.free(bmod, *st)

    def mod_h(self, eng, out, in_, cc, r):
        if eng == "act":
            self.c.act(out, in_, AF.Identity, scale=self.MODV3[:, 8 + cc, r:r + 1], bias=self.MODV3[:, cc, r:r + 1])
        else:
            self.c.ts(eng, out, in_, self.MODV3[:, 8 + cc, r:r + 1], self.MODV3[:, cc, r:r + 1], ALU.mult, ALU.add)

    def emit_h1(self, l):
        for cc in range(KC):
            eng = ("dve", "act")[cc % 2]
            self.mod_h(eng, self.HB3[:, cc, 0:NLAT], self.XT3[:, cc, 0:NLAT], cc, 0)
            self.mod_h(eng, self.HB3[:, cc, NLAT:NT], self.XT3[:, cc, NLAT:NT], cc, 1)

    def xg_ap(self, l):
        if l in self.xg:
            return self.xg[l]
        ap = self.inp(f"xg{l}", [KC, 2 * P, NT])
        self.xg[l] = T(ap, f"xg{l}", "dram")
        return self.xg[l]

    def gstream(self, l, groups):
        c = self.c
        xg = self.xg_ap(l)
        st = [c.alloc("xost", KC * 256, F32) for _ in range(3)]
        ho = [c.alloc("xoh", KC * 256, BF16) for _ in range(3)]
        views = {}

        def load(i):
            slot, c0, n, r = groups[i]
            s3 = st[i % 3].r("p (c t) -> p c t", c=KC)[:, :, 0:n]
            c.dma("sp", s3, V(xg, xg.h[:, slot * P:(slot + 1) * P, c0:c0 + n].rearrange("c p t -> p c t")))
            views[i] = s3

        def prep(i):
            slot, c0, n, r = groups[i]
            s3 = views.pop(i)
            h3 = ho[i % 3].r("p (c t) -> p c t", c=KC)[:, :, 0:n]
            for cc in range(KC):
                self.mod_h(("dve", "act")[cc % 2], h3[:, cc, :], s3[:, cc, :], cc, r)
            return h3

        ng = len(groups)
        load(0)
        if ng > 1:
            load(1)
        h_next = prep(0)
        for i in range(ng):
            h_cur = h_next
            if i + 2 < ng:
                load(i + 2)
            if i + 1 < ng:
                h_next = prep(i + 1)
            slot, c0, n, r = groups[i]
            yield (slot, c0, n, r, h_cur)
        c.free(*st, *ho)

    def load_w(self, name, dram_ap, ncols):
        c = self.c
        t = c.alloc(name, KC * ncols, BF16)
        t3 = t.r("p (k n) -> p k n", k=KC)
        c.dma("pool", t3, dram_ap.rearrange("(k p) n -> p k n", p=P))
        return t, t3

    def outproj(self, oT, wo, col0, n, r):
        c = self.c
        oTs = oT if isinstance(oT, list) else [oT]
        wos = wo if isinstance(wo, list) else [wo]
        for cc in range(KC):
            y = self.bank(6 + self.rr("yb", 2))[:, 0:n]
            for i, (o, w) in enumerate(zip(oTs, wos)):
                c.mm(y, w[:, cc * 128:(cc + 1) * 128], o, start=(i == 0), stop=(i == len(oTs) - 1))
            xs = self.XT3[:, cc, col0:col0 + n]
            c.stt(xs, y, self.MODV3[:, 16 + cc, r:r + 1], xs, ALU.mult, ALU.add)

    def rmsnorm_rope(self, ps, n, gain, out, rope_col0=None, table=None):
        c = self.c
        k = self.rr("rn", 2)
        tm = self.rn_tmp[k]
        sq = tm["sq"][:, 0:n]
        rstd = tm["rstd"][:, 0:n]
        c.act(sq, ps, AF.Square)
        ss = self.bank(2 + k)[:, 0:n]
        c.mm(ss, self.ones_rms.v(), sq)
        c.act(rstd, ss, AF.Ln, bias=RMS_EPS)
        c.act(rstd, rstd, AF.Exp, scale=-0.5)
        if rope_col0 is None:
            c.stt(out, ps, gain, rstd, ALU.mult, ALU.mult)
            return
        qn = tm["qn"][:, 0:n]
        c.stt(qn, ps, gain, rstd, ALU.mult, ALU.mult)
        rp = tm["rope"].r("p (a n) -> p a n", a=2)[:, :, 0:n]
        c.dma("sp", rp, (table if table is not None else self.rope_dram)[:, :, rope_col0:rope_col0 + n])
        pp = self.bank(4 + k)[:, 0:n]
        c.mm(pp, self.perm.v(), qn)
        t1 = tm["t1"][:, 0:n]
        t2 = tm["t2"][:, 0:n]
        c.tt("dve", t1, qn, rp[:, 0, :], ALU.mult)
        c.tt("dve", t2, pp, rp[:, 1, :], ALU.mult)
        c.tt("dve", out, t1, t2, ALU.add)

    def attn_T(self, KT, VV3, qv, n, tiles):
        c = self.c
        O = self.bank(2)[:, 0:n]
        L = self.bank(3)[:, 0:n]
        nt = len(tiles)
        sbanks = (0, 1, 4, 5)
        LA = 3

        def S(i):
            s = self.bank(sbanks[self.rr("sb", 4)])[:, 0:n]
            kt = tiles[i]
            c.mm(s, KT[:, kt * 128:(kt + 1) * 128], qv)
            return s
        q = [S(i) for i in range(min(LA, nt))]
        for i in range(nt):
            s_cur = q.pop(0)
            if i + LA < nt:
                q.append(S(i + LA))
            pt = self.pt_tmp[self.rr("pt", len(self.pt_tmp))][:, 0:n]
            c.act(pt, s_cur, AF.Exp, scale=SCALE)
            c.mm(O, VV3[:, tiles[i], :], pt, start=(i == 0), stop=(i == nt - 1))
            c.mm(L, self.ones1.v(), pt, start=(i == 0), stop=(i == nt - 1))
        kk = self.rr("ot", 2)
        rl = self.rl_tmp[kk][:, 0:n]
        c.act(rl, L, AF.Ln)
        c.act(rl, rl, AF.Exp, scale=-1.0)
        oT = self.ot_tmp[kk][:, 0:n]
        c.tt("dve", oT, O, rl, ALU.mult)
        return oT

    def emit_gqa(self, l):
        c = self.c
        j = l // 2
        win = self.inp(f"win{l}", [D, 1536])
        wout = self.inp(f"wout{l}", [D, D])
        g_own = [(i * 256, 256, 0) for i in range(8)] + [(NLAT, 128, 1)]
        wf, wf3 = self.load_w("wf", win[:, 0:256], 256)
        cs2 = c.alloc("cs2", 256, BF16)
        c.dma("pool", cs2.v(), self.inp("cs2", [P, 256]))
        wfn = c.alloc("wfn", 256, BF16)
        wfn3 = wfn.r("p (j m) -> p j m", j=2)
        c.dma("pool", wfn.v(), self.inp(f"wfn{l}", [P, 256]))
        wo_f = c.alloc("wo_f", 2 * D, BF16)
        wo_f3 = wo_f.r("p (j n) -> p j n", j=2)
        c.dma("pool", wo_f3, wout[0:256, :].rearrange("(j p) n -> p j n", p=P))
        cdft = c.alloc("cdft", 512, BF16)
        cdft4 = cdft.r("p (a t k) -> p a t k", a=2, t=2)
        c.dma("pool", cdft.v(), self.inp("cdft", [P, 512]))
        FCS = c.alloc("FCS", 34 * 512, BF16)
        FCS3 = FCS.r("p (t f) -> p t f", f=512)
        fts = [c.alloc("fts", 512, BF16) for _ in range(2)]

        def stage1(buf0, n, h3):
            fps = self.bank(self.rr("fps", 2))
            fps3 = fps[:, 0:2 * n].r("p (j t) -> p j t", j=2)
            for jj in range(2):
                for k in range(KC):
                    c.mm(fps3[:, jj, :], wf3[:, k, jj * 128:(jj + 1) * 128], h3[:, k, :], start=(k == 0), stop=(k == KC - 1))
            ft = fts[self.rr("fts", 2)]
            ft3 = ft[:, 0:2 * n].r("p (j t) -> p j t", j=2)
            c.copy("act", ft3, fps3)
            for tt in range(n // 128):
                cps = self.bank(2 + self.rr("cps", 2))
                for jj in range(2):
                    c.mm(cps[:, jj * 256:(jj + 1) * 256], ft3[:, jj, tt * 128:(tt + 1) * 128], cs2.v())
                c.copy("dve", FCS3[:, (buf0 + tt * 128) // 128, :], cps.v())

        g_all = [(slot, c0, n, r) for slot in range(2) for (c0, n, r) in g_own]
        for (slot, c0, n, r, h3) in self.gstream(l, g_all):
            stage1(slot * NT + c0, n, h3)
        c.free(wf, cs2, *fts)

        dft = self.inp("dft", [4, 8, P, 4096], BF16)
        dst = [c.alloc("dftst", 4096, BF16) for _ in range(2)]
        frs = [c.alloc("frs", 512, BF16) for _ in range(2)]
        ots = [c.alloc("ofs", 512, BF16) for _ in range(2)]

        def finish(acc, n, col0, r):
            oTs = []
            for jj in range(2):
                fr = frs[jj][:, 0:n]
                c.copy("act", fr, acc[jj])
                wps = self.bank(6 + self.rr("yb", 2))[:, 0:n]
                c.mm(wps, wfn3[:, jj, :], fr)
                o = ots[jj][:, 0:n]
                c.copy("act", o, wps)
                oTs.append(o)
            self.outproj(oTs, [wo_f3[:, 0, :], wo_f3[:, 1, :]], col0, n, r)

        for kc in range(4):
            acc = [self.bank(4)[:, 0:512], self.bank(5)[:, 0:512]]
            for ng in range(8):
                dt_ = dst[self.rr("dst", 2)]
                c.dma("sp", dt_.v(), dft[kc, ng])
                dt4 = dt_.r("p (a i k) -> p a i k", a=2, i=4)
                for ni in range(4):
                    nn = ng * 4 + ni
                    ft_i = nn if nn < 16 else nn + 1
                    for jj in range(2):
                        c.mm(acc[jj], FCS3[:, ft_i, jj * 256:jj * 256 + 128], dt4[:, 0, ni, :], start=(nn == 0), stop=False)
                        c.mm(acc[jj], FCS3[:, ft_i, jj * 256 + 128:(jj + 1) * 256], dt4[:, 1, ni, :], start=False, stop=(nn == 31),
                             inc=(nn == 31 or (ni == 3 and jj == 1)))
            finish(acc, 512, kc * 512, 0)
        acc = [self.bank(4)[:, 0:128], self.bank(5)[:, 0:128]]
        for ti, ft_i in enumerate((16, 33)):
            for jj in range(2):
                c.mm(acc[jj], FCS3[:, ft_i, jj * 256:jj * 256 + 128], cdft4[:, 0, ti, :], start=(ti == 0), stop=False)
                c.mm(acc[jj], FCS3[:, ft_i, jj * 256 + 128:(jj + 1) * 256], cdft4[:, 1, ti, :], start=False, stop=(ti == 1))
        finish(acc, 128, NLAT, 1)
        c.free(FCS, wfn, wo_f, cdft, *dst, *frs, *ots)
        if self.debug.get("stop") == "fnet":
            return

        self.rope_dram = self.inp("rope", [P, 2 * 4096], BF16).rearrange("p (a n) -> p a n", a=2)
        ropeq = self.inp("ropeq", [P, 2 * NLAT], BF16).rearrange("p (a n) -> p a n", a=2)
        self.perm = c.alloc("perm", P, BF16)
        c.dma("pool", self.perm.v(), self.inp("perm", [P, P]))
        qkn = c.alloc("qkn", 2, F32)
        c.dma("sp", qkn.v(), self.inp(f"qkn{l}", [P, 2]))
        self.rn_tmp = [dict(sq=c.alloc("sq", 256, BF16), rstd=c.alloc("rstd", 256, F32), qn=c.alloc("qn", 256, BF16),
                            rope=c.alloc("rope", 512, BF16), t1=c.alloc("t1", 256, F32), t2=c.alloc("t2", 256, F32))
                       for _ in range(2)]
        self.pt_tmp = [c.alloc("pt", 512, BF16) for _ in range(5)]
        self.rl_tmp = [c.alloc("rl", 512, F32) for _ in range(2)]
        self.ot_tmp = [c.alloc("ot", 512, BF16) for _ in range(2)]
        KTt = c.alloc("KT", 2 * NT, BF16)
        KT = KTt.v()
        VVt = c.alloc("VV", 34 * 128, BF16)
        VV3 = VVt.r("p (t d) -> p t d", d=128)
        QTt = [c.alloc("QT", NT, BF16) for _ in range(2)]
        for g in range(2):
            wk, wk3 = self.load_w("wk", win[:, 1024 + g * 128:1024 + (g + 1) * 128], 128)
            wv, wv3 = self.load_w("wv", win[:, 1280 + g * 128:1280 + (g + 1) * 128], 128)

            def kv(buf0, n, r, h3, rope0):
                kps = self.bank(self.rr("kps", 2))[:, 0:n]
                for k in range(KC):
                    c.mm(kps, wk3[:, k, :], h3[:, k, :], start=(k == 0), stop=(k == KC - 1))
                self.rmsnorm_rope(kps, n, qkn[:, 1:2], KT[:, buf0:buf0 + n], rope0)
                for tt in range(n // 128):
                    vps = self.bank(6 + self.rr("yb", 2))[:, 0:128]
                    for k in range(KC):
                        c.mm(vps, h3[:, k, tt * 128:(tt + 1) * 128], wv3[:, k, :], start=(k == 0), stop=(k == KC - 1))
                    c.copy("act", VV3[:, buf0 // 128 + tt, :], vps)

            for (slot, c0, n, r, h3) in self.gstream(l, g_all):
                kv(slot * NT + c0, n, r, h3, (slot * NLAT + c0) if r == 0 else None)
            c.free(wk, wv)
            for hh in range(3):
                hg = g * 3 + hh
                wq, wq3 = self.load_w("wq", win[:, 256 + hg * 128:256 + (hg + 1) * 128], 128)
                wo = c.alloc("wo", D, BF16)
                c.dma("pool", wo.v(), wout[(2 + hg) * 128:(3 + hg) * 128, :])
                QT = QTt[self.rr("qt", 2)].v()
                for (c0, n, r) in g_own:
                    qps = self.bank(self.rr("kps", 2))[:, 0:n]
                    for k in range(KC):
                        c.mm(qps, wq3[:, k, :], self.HB3[:, k, c0:c0 + n], start=(k == 0), stop=(k == KC - 1))
                    self.rmsnorm_rope(qps, n, qkn[:, 0:1], QT[:, c0:c0 + n], c0 if r == 0 else None, table=ropeq)
                for qc in range(4):
                    oT = self.attn_T(KT, VV3, QT[:, qc * 512:(qc + 1) * 512], 512, list(range(34)))
                    self.outproj(oT, wo.v(), qc * 512, 512, 0)
                oT = self.attn_T(KT, VV3, QT[:, NLAT:NT], 128, [16, 33])
                self.outproj(oT, wo.v(), NLAT, 128, 1)
                c.free(wq, wo)
        c.free(KTt, VVt, *QTt, self.perm, qkn, *self.pt_tmp, *self.rl_tmp, *self.ot_tmp)
        for tm in self.rn_tmp:
            c.free(*tm.values())

    def emit_na(self, l):
        c = self.c
        win = self.inp(f"win{l}", [D, 3 * D])
        wout = self.inp(f"wout{l}", [D, D])
        nab = self.inp(f"nab{l}", [8, P, 5 * 768])
        HOt = c.alloc("HO", KC * 768, BF16)
        HO3 = HOt.r("p (c t) -> p c t", c=KC)
        hgroups = [(0, NLAT - 256, 256, 0), (1, 0, 256, 0), (0, NLAT, 128, 1), (1, NLAT, 128, 1)]
        hdst = [0, 256, 512, 640]
        for gi_, (slot, c0, n, r, h3) in enumerate(self.gstream(l, hgroups)):
            for cc in range(KC):
                c.copy(("dve", "act")[cc % 2], HO3[:, cc, hdst[gi_]:hdst[gi_] + n], h3[:, cc, :])
        NK = 2816
        srcs = [(0, 256, HO3[:, :, 0:256])] + \
               [(256 + i * 512, 512, self.HB3[:, :, i * 512:(i + 1) * 512]) for i in range(4)] + \
               [(2304, 256, HO3[:, :, 256:512]), (2560, 256, HO3[:, :, 512:768])]
        KTs = [c.alloc("KT", NK, BF16) for _ in range(2)]
        VVs = [c.alloc("VV", NK, BF16) for _ in range(2)]
        QTs = [c.alloc("QT", NT, BF16) for _ in range(2)]
        nbs = [c.alloc("nb", 5 * 768, F32) for _ in range(1)]
        ss_tmp = [c.alloc("ss", 768, F32) for _ in range(2)]
        pt_tmp = [c.alloc("pt", 1024, BF16) for _ in range(3)]
        rl_tmp = [c.alloc("rl", 512, F32) for _ in range(2)]
        og_tmp = [c.alloc("og", 512, BF16) for _ in range(2)]

        for h in range(8):
            wq, wq3 = self.load_w("wq", win[:, h * 128:(h + 1) * 128], 128)
            wk, wk3 = self.load_w("wk", win[:, D + h * 128:D + (h + 1) * 128], 128)
            wv, wv3 = self.load_w("wv", win[:, 2 * D + h * 128:2 * D + (h + 1) * 128], 128)
            wo = c.alloc("wo", D, BF16)
            c.dma("pool", wo.v(), wout[h * 128:(h + 1) * 128, :])
            nb = nbs[0]
            c.dma("sp", nb.v(), nab[h])
            nb3 = nb.r("p (t w) -> p t w", w=768)
            KT = KTs[h % 2].v()
            VV3 = VVs[h % 2].r("p (t d) -> p t d", d=128)
            QT = QTs[h % 2].v()
            for (b0, n, h3) in srcs:
                kps = self.bank(6 + self.rr("yb", 2))[:, 0:n]
                for k in range(KC):
                    c.mm(kps, wk3[:, k, :], h3[:, k, :], start=(k == 0), stop=(k == KC - 1))
                c.copy("act", KT[:, b0:b0 + n], kps)
                for tt in range(n // 128):
                    vps = self.bank(6 + self.rr("yb", 2))[:, 0:128]
                    for k in range(KC):
                        c.mm(vps, h3[:, k, tt * 128:(tt + 1) * 128], wv3[:, k, :], start=(k == 0), stop=(k == KC - 1))
                    c.copy("dve", VV3[:, b0 // 128 + tt, :], vps)
            for (c0, n) in [(i * 512, 512) for i in range(4)] + [(NLAT, 128)]:
                qps = self.bank(6 + self.rr("yb", 2))[:, 0:n]
                for k in range(KC):
                    c.mm(qps, wq3[:, k, :], self.HB3[:, k, c0:c0 + n], start=(k == 0), stop=(k == KC - 1))
                c.copy("act", QT[:, c0:c0 + n], qps)
            jobs = []
            for bi in range(16):
                ty = {0: 0, 1: 1, 14: 3, 15: 4}.get(bi, 2)
                wb = 2 * bi if bi <= 13 else 28
                t0_ = wb * 64 // 128
                jobs.append((bi * 128, [t0_ + m for m in range(6)] + [20, 21], ty))
            jobs.append((NLAT, [20, 21], None))

            def S_of(job):
                q0, tiles, ty = job
                k = self.rr("nas", 2)
                SA = self.bank(2 * k)
                SB = self.bank(2 * k + 1)
                for m, kt in enumerate(tiles):
                    dst = (SA if m < 4 else SB)[:, (m % 4) * 128:(m % 4 + 1) * 128]
                    c.mm(dst, KT[:, kt * 128:(kt + 1) * 128], QT[:, q0:q0 + 128])
                return SA, SB

            def P_of(job, S):
                q0, tiles, ty = job
                SA, SB = S
                PT = pt_tmp[self.rr("napt", 3)]
                if ty is not None:
                    SS = ss_tmp[self.rr("nass", 2)]
                    c.stt(SS[:, 0:512], SA[:, 0:512], SCALE, nb3[:, ty, 0:512], ALU.mult, ALU.add)
                    c.stt(SS[:, 512:768], SB[:, 0:256], SCALE, nb3[:, ty, 512:768], ALU.mult, ALU.add)
                    c.act(PT[:, 0:768], SS[:, 0:768], AF.Exp)
                    c.act(PT[:, 768:1024], SB[:, 256:512], AF.Exp, scale=SCALE)
                else:
                    c.act(PT[:, 0:256], SA[:, 0:256], AF.Exp, scale=SCALE)
                return PT

            def PV_of(job, PT, O_dst, L_dst):
                q0, tiles, ty = job
                nt = len(tiles)
                for m, kt in enumerate(tiles):
                    c.mm(O_dst, VV3[:, kt, :], PT[:, m * 128:(m + 1) * 128], start=(m == 0), stop=(m == nt - 1))
                for m, kt in enumerate(tiles):
                    c.mm(L_dst, self.ones1.v(), PT[:, m * 128:(m + 1) * 128], start=(m == 0), stop=(m == nt - 1))

            def finish_group(gjobs):
                n = 128 * len(gjobs)
                col0 = gjobs[0][0]
                r = 0 if gjobs[0][2] is not None else 1
                kk = self.rr("narl", 2)
                rl = rl_tmp[kk][:, 0:n]
                c.act(rl, self.bank(5)[:, 0:n], AF.Ln)
                c.act(rl, rl, AF.Exp, scale=-1.0)
                og = og_tmp[kk][:, 0:n]
                c.tt("dve", og, self.bank(4)[:, 0:n], rl, ALU.mult)
                self.outproj(og, wo.v(), col0, n, r)

            S_next = S_of(jobs[0])
            gjobs = []
            for ji, job in enumerate(jobs):
                S_cur = S_next
                if ji + 1 < len(jobs):
                    S_next = S_of(jobs[ji + 1])
                PT = P_of(job, S_cur)
                slot = len(gjobs)
                PV_of(job, PT, self.bank(4)[:, slot * 128:(slot + 1) * 128], self.bank(5)[:, slot * 128:(slot + 1) * 128])
                gjobs.append(job)
                if len(gjobs) == 4 or ji == len(jobs) - 1 or jobs[ji + 1][2] is None:
                    finish_group(gjobs)
                    gjobs = []
            c.free(wq, wk, wv, wo)
        c.free(HOt, *KTs, *VVs, *QTs, *nbs, *ss_tmp, *pt_tmp, *rl_tmp, *og_tmp)

    def emit_ln(self, l, which):
        c = self.c
        gi, bi_ = (0, 1) if which == 1 else (2, 3)
        eps = LN_EPS / (ALPHA * ALPHA)
        zq = [c.alloc("zq", KC * 512, BF16) for _ in range(2)]
        tmp = [dict(M=c.alloc("M", 512, F32), m2=c.alloc("m2", 512, F32), rstd=c.alloc("rstdl", 512, F32)) for _ in range(2)]
        t1s = [c.alloc("t1", 512, F32) for _ in range(3)]
        if which == 1:
            h2f = [c.alloc("h2f", KC * 512, F32) for _ in range(2)]
            wr = c.alloc("wr", KC * 64, F32)
            wr3 = wr.r("p (k e) -> p k e", k=KC)
            c.dma("sp", wr.v(), self.inp(f"wr{l}", [P, KC * 64]))
            mb = c.alloc("mb", 64, F32)
            c.dma("sp", mb.v(), self.inp(f"mb{l}", [P, 64]))
            gt = [c.alloc("gt", 816, F32) for _ in range(2)]
            mb4 = c.alloc("mb4", 256, F32)
            for q_ in range(4):
                c.copy("dve", mb4[:, q_ * 64:(q_ + 1) * 64], mb.v())
            GTS = c.alloc("GTS", NT, F32)
            c.memset("dve", self.G3[:, :, 64:65], 1.0)
            self.GTD = c.dram(f"gtd{l}", [NEXP, NT], F32)
        groups = [(i * 512, 512, 0) for i in range(4)] + [(NLAT, 128, 1)]
        def stats(gidx):
            c0, n, r = groups[gidx]
            k = gidx % 2
            zq3 = zq[k].r("p (c t) -> p c t", c=KC)[:, :, 0:n]
            mean = self.bank(0 if k == 0 else 6)[:, 0:n]
            msq = self.bank(1 if k == 0 else 7)[:, 0:n]
            for cc in range(KC):
                xs = self.XT3[:, cc, c0:c0 + n]
                c.act(zq3[:, cc, :], xs, AF.Square)
            for cc in range(KC):
                c.mm(mean, self.ones_mean_f.v(), self.XT3[:, cc, c0:c0 + n], start=(cc == 0), stop=(cc == KC - 1))
            for cc in range(KC):
                c.mm(msq, self.ones_mean.v(), zq3[:, cc, :], start=(cc == 0), stop=(cc == KC - 1))
            M = tmp[k]["M"][:, 0:n]
            m2 = tmp[k]["m2"][:, 0:n]
            rstd = tmp[k]["rstd"][:, 0:n]
            c.copy("act", M, mean)
            c.act(m2, mean, AF.Square)
            c.tt("dve", m2, msq, m2, ALU.subtract)
            c.act(rstd, m2, AF.Ln, bias=eps)
            c.act(rstd, rstd, AF.Exp, scale=-0.5)

        def advance(nblk):
            g_ = getattr(self, "pending_mod", None)
            if g_ is None:
                return
            for _ in range(nblk):
                try:
                    next(g_)
                except StopIteration:
                    self.pending_mod = None
                    return

        stats(0)
        advance(2)
        for gidx, (c0, n, r) in enumerate(groups):
            k = gidx % 2
            M = tmp[k]["M"][:, 0:n]
            rstd = tmp[k]["rstd"][:, 0:n]
            if gidx + 1 < len(groups):
                stats(gidx + 1)
            advance(3)
            if which == 1:
                h2f3 = h2f[k].r("p (c t) -> p c t", c=KC)[:, :, 0:n]
            for cc in range(KC):
                xs = self.XT3[:, cc, c0:c0 + n]
                t1 = t1s[self.rr("t1", 3)][:, 0:n]
                c.tt("dve", t1, xs, M, ALU.subtract)
                c.tt("dve", t1, t1, rstd, ALU.mult)
                c.act(xs, t1, AF.Identity, scale=self.LNV3[:, gi, cc:cc + 1], bias=self.LNV3[:, bi_, cc:cc + 1])
                if which == 1:
                    c.act(h2f3[:, cc, :], t1, AF.Identity, scale=self.A23[:, cc, r:r + 1], bias=self.B23[:, cc, r:r + 1])
                    c.copy("dve", self.HB3[:, cc, c0:c0 + n], h2f3[:, cc, :])
            if which == 1:
                ntl = n // 128
                lgb = self.bank(2 + self.rr("lg", 2))
                for tt in range(ntl):
                    for cc in range(KC):
                        c.mm(lgb[:, tt * 64:(tt + 1) * 64], h2f3[:, cc, tt * 128:(tt + 1) * 128], wr3[:, cc, :],
                             start=(cc == 0), stop=(cc == KC - 1))
                g_ = gt[self.rr("gt", 2)]
                W = ntl * 64
                sc, sel, wsel = g_[:, 0:W], g_[:, 256:256 + W], g_[:, 512:512 + W]
                m8 = g_[:, 768:768 + 8 * ntl].r("p (t e) -> p t e", e=8)
                den = g_[:, 800:800 + ntl]
                rden = g_[:, 804:804 + ntl]
                c.act(sc, lgb[:, 0:W], AF.Sigmoid)
                c.tt("dve", sel, sc, mb4[:, 0:W], ALU.add)
                for tt in range(ntl):
                    c.op("dve", lambda e, tt=tt: e.max(out=m8[:, tt, :].ap, in_=sel[:, tt * 64:(tt + 1) * 64].ap),
                         reads=[sel], writes=[m8])
                for tt in range(ntl):
                    c.ts("dve", sel[:, tt * 64:(tt + 1) * 64], sel[:, tt * 64:(tt + 1) * 64], m8[:, tt, 7:8], op0=ALU.is_ge)
                c.tt("dve", wsel, sel, sc, ALU.mult)
                wsel3 = wsel.r("p (t e) -> p t e", e=64)
                c.op("dve", lambda e: e.reduce_sum(out=den.ap, in_=wsel3.ap, axis=AX.X), reads=[wsel], writes=[den])
                c.op("dve", lambda e: e.reciprocal(out=rden.ap, in_=den.ap), reads=[den], writes=[rden])
                for tt in range(ntl):
                    tile_i = c0 // 128 + tt
                    c.ts("dve", self.G3[:, tile_i, 0:64], wsel[:, tt * 64:(tt + 1) * 64], rden[:, tt:tt + 1], ROUTE_SCALE,
                         ALU.mult, ALU.mult)
                for tt in range(ntl):
                    tile_i = c0 // 128 + tt
                    gtp = self.bank(4 + self.rr("gtp", 2))[0:NEXP, 0:128]
                    c.transpose(gtp, self.G3[:, tile_i, :], self.identf.v())
                    c.copy("act", GTS[0:NEXP, tile_i * 128:(tile_i + 1) * 128], gtp)
        if which == 1:
            c.dma("sp", self.GTD.v(), GTS[0:NEXP, :])
            c.free(*h2f, wr, mb, mb4, *gt, GTS)
        advance(100)
        c.free(*zq, *t1s)
        for t_ in tmp:
            c.free(*t_.values())

    def emit_moe(self, l):
        c = self.c
        moew = self.inp(f"moew{l}", [NEXP, P, 6144])
        NWB = 4
        WB = [c.alloc("WB", 6144, BF16) for _ in range(NWB)]
        GB = [c.alloc("GB", NT, F32) for _ in range(NWB)]
        sT = [c.alloc("sT", 512, BF16) for _ in range(2)]
        tT = [c.alloc("tT", 512, BF16) for _ in range(2)]
        aT = [c.alloc("aT", 512, BF16) for _ in range(4)]
        groups = [(i * 256, 256, 0) for i in range(8)] + ([(NLAT, 128, 1)] if l < DEPTH - 1 else [])
        pairs = [(2 * i, 2 * i + 1) for i in range(32)] + [(64,)]

        def prefetch(e):
            w = WB[e % NWB]
            for q in range(3):
                c.dma("pool", w[:, q * 2048:(q + 1) * 2048], moew[e][:, q * 2048:(q + 1) * 2048])
            c.dma("sp", GB[e % NWB].r("p (o t) -> p o t", o=1), V(self.GTD, self.GTD.h[e:e + 1, :].partition_broadcast(P)))

        def down_mm(sts):
            e0, c0, n, r, _ = sts[0]
            Y = [self.bank(4 + cc // 2)[:, (cc % 2) * 256:(cc % 2) * 256 + n] for cc in range(KC)]
            nmm = 2 * len(sts)
            for cc in range(KC):
                i = 0
                for (e, _, _, _, a3) in sts:
                    w = WB[e % NWB].v()
                    for jj in range(2):
                        c.mm(Y[cc], w[:, 4096 + jj * 1024 + cc * 128:4096 + jj * 1024 + (cc + 1) * 128], a3[:, jj, :],
                             start=(i == 0), stop=(i == nmm - 1))
                        i += 1

        def down_acc(sts):
            e0, c0, n, r, _ = sts[0]
            Y = [self.bank(4 + cc // 2)[:, (cc % 2) * 256:(cc % 2) * 256 + n] for cc in range(KC)]
            for cc in range(KC):
                xs = self.XT3[:, cc, c0:c0 + n]
                c.stt(xs, Y[cc], self.MODV3[:, 40 + cc, r:r + 1], xs, ALU.mult, ALU.add)

        for e in pairs[0]:
            prefetch(e)
        pend = []
        ready = None
        it = 0
        for pi, pr in enumerate(pairs):
            for gidx, (c0, n, r) in enumerate(groups):
                for ei, e in enumerate(pr):
                    w = WB[e % NWB].v()
                    gb = GB[e % NWB].v()
                    k = it % 2
                    gps = self.bank(2 * k)
                    ups = self.bank(2 * k + 1)
                    g3 = gps[:, 0:2 * n].r("p (j t) -> p j t", j=2)
                    u3 = ups[:, 0:2 * n].r("p (j t) -> p j t", j=2)
                    for jj in range(2):
                        for kk in range(KC):
                            c.mm(g3[:, jj, :], w[:, kk * 256 + jj * 128:kk * 256 + (jj + 1) * 128], self.HB3[:, kk, c0:c0 + n],
                                 start=(kk == 0), stop=(kk == KC - 1))
                    for jj in range(2):
                        for kk in range(KC):
                            c.mm(u3[:, jj, :], w[:, 2048 + kk * 256 + jj * 128:2048 + kk * 256 + (jj + 1) * 128],
                                 self.HB3[:, kk, c0:c0 + n], start=(kk == 0), stop=(kk == KC - 1))
                    did = None
                    if ei == 0 and ready is not None:
                        down_mm(ready)
                        did = ready
                        ready = None
                        if gidx == 0 and pi + 1 < len(pairs):
                            for e2 in pairs[pi + 1]:
                                prefetch(e2)
                    elif ei == 0 and gidx == 0 and pi == 0:
                        for e2 in pairs[1]:
                            prefetch(e2)
                    s3 = sT[k][:, 0:2 * n].r("p (j t) -> p j t", j=2)
                    t3 = tT[k][:, 0:2 * n].r("p (j t) -> p j t", j=2)
                    a3 = aT[it % 4][:, 0:2 * n].r("p (j t) -> p j t", j=2)
                    c.act(s3, g3, AF.Silu)
                    for jj in range(2):
                        c.tt("dve", t3[:, jj, :], u3[:, jj, :], gb[:, c0:c0 + n], ALU.mult)
                    c.tt("dve", a3, s3, t3, ALU.mult)
                    if did is not None:
                        down_acc(did)
                    pend.append((e, c0, n, r, a3))
                    if ei == len(pr) - 1:
                        ready = pend
                        pend = []
                    it += 1
        down_mm(ready)
        down_acc(ready)
        c.free(*WB, *GB, *sT, *tT, *aT)


BF = ml_dtypes.bfloat16


def _fm(tok):
    T_ = tok.shape[0]
    return np.ascontiguousarray(tok.T.reshape(KC, P, T_).transpose(1, 0, 2)).reshape(P, KC * T_)


def _unfm(a, T_):
    return np.ascontiguousarray(a.reshape(P, KC, T_).transpose(1, 0, 2).reshape(D, T_).T)


_CONST_CACHE = {}


def _consts(s):
    if s in _CONST_CACHE:
        return _CONST_CACHE[s]
    out = {}
    out["ident"] = np.eye(P, dtype=np.float32)
    d = np.arange(P)
    i = d % 64
    partner = np.where(i < 32, d + 32, d - 32)
    perm = np.zeros((P, P), np.float32)
    perm[partner, d] = 1.0
    out["perm"] = perm
    pos = np.arange(4096)
    inv = 10000.0 ** (-(2.0 / 64) * np.arange(32, dtype=np.float64))
    pv = np.where((d // 64)[:, None] == 0, (pos // 64)[None, :], (pos % 64)[None, :]).astype(np.float64)
    ang = pv * inv[i % 32][:, None]
    cos = np.cos(ang)
    sin = np.sin(ang) * np.where(i < 32, -1.0, 1.0)[:, None]
    rope = np.stack([cos, sin], axis=1)
    out["rope"] = rope.reshape(P, 2 * 4096).astype(BF)
    out["ropeq"] = np.ascontiguousarray(rope[:, :, s * NLAT:(s + 1) * NLAT]).reshape(P, 2 * NLAT).astype(BF)
    cc = np.arange(64)
    c64 = np.cos(2 * np.pi * np.outer(cc, cc) / 64) / 8.0
    s64 = np.sin(2 * np.pi * np.outer(cc, cc) / 64) / 8.0
    cs2 = np.zeros((P, 256), np.float32)
    for g2 in range(2):
        cs2[g2 * 64:(g2 + 1) * 64, g2 * 64:(g2 + 1) * 64] = c64
        cs2[g2 * 64:(g2 + 1) * 64, 128 + g2 * 64:128 + (g2 + 1) * 64] = s64
    out["cs2"] = cs2
    kk = s * NLAT + np.arange(NLAT)
    ph = (np.outer(pos, kk) % 4096).astype(np.float64) * (2 * np.pi / 4096)
    tab = np.stack([np.cos(ph) / 64.0, -np.sin(ph) / 64.0], axis=0).astype(np.float32)
    tab = tab.reshape(2, 8, 4, P, 4, 512)
    out["dft"] = np.ascontiguousarray(tab.transpose(4, 1, 3, 0, 2, 5)).reshape(4, 8, P, 4096).astype(BF)
    cpos = np.stack([np.arange(128), 128 + np.arange(128)], axis=0)
    ck = s * 128 + np.arange(128)
    cph = (cpos[:, :, None] * ck[None, None, :] % 256) * (2 * np.pi / 256)
    ctab = np.stack([np.cos(cph) / 16.0, -np.sin(cph) / 16.0], axis=0)
    out["cdft"] = np.ascontiguousarray(ctab.transpose(2, 0, 1, 3)).reshape(P, 512).astype(np.float32)
    _CONST_CACHE[s] = out
    return out


def _na_bias(rpb, s):
    out = np.full((8, P, 5, 768), NEG, np.float32)
    q = np.arange(P)
    w = np.arange(768)
    qcol = q % 64
    kcol = w % 64
    cstart = np.clip(qcol - 8, 0, 48)
    okc = (kcol[None, :] >= cstart[:, None]) & (kcol[None, :] < cstart[:, None] + 16)
    dc = kcol[None, :] - qcol[:, None] + 15
    rep = {0: 0, 1: 1, 2: 2, 3: 14, 4: 15}
    for ty, bi in rep.items():
        i = 16 * s + bi
        wb = 2 * bi if bi <= 13 else 28
        qrow = 2 * i + q // 64
        rstart = np.clip(qrow - 4, 0, 56)
        krow = wb + (32 * s - 4) + w // 64
        okr = (krow[None, :] >= rstart[:, None]) & (krow[None, :] < rstart[:, None] + 8)
        dr = krow[None, :] - qrow[:, None] + 7
        ok = okr & okc
        drc = np.clip(dr, 0, 14)
        dcc = np.clip(dc, 0, 30)
        vals = rpb[:, drc, dcc]
        out[:, :, ty, :] = np.where(ok[None], vals, NEG)
    outT = out.reshape(8, P, 5, 6, P).transpose(0, 4, 2, 3, 1)
    return np.ascontiguousarray(outT).reshape(8, P, 5 * 768)


def _layer_weights(l, inp, tag):
    j = l // 2
    w = {}
    w[f"wmod{tag}"] = inp["w_mod"][l]
    w[f"bmod{tag}"] = np.ascontiguousarray(inp["b_mod"][l].reshape(48, P).T)
    w[f"lnv{tag}"] = np.ascontiguousarray(
        np.stack([inp[k][l].reshape(KC, P).T for k in ("ln1_g", "ln1_b", "ln2_g", "ln2_b")], axis=1)).reshape(P, 32)
    if l % 2 == 0:
        w[f"win{tag}"] = inp["ab_w_in"][j]
        w[f"wout{tag}"] = inp["ab_w_out"][j]
        wf = inp["ab_w_fnet"][j]
        wfn = np.zeros((P, 2, P), np.float32)
        for jj in range(2):
            wfn[0:64, jj, 0:64] = wf[2 * jj]
            wfn[64:128, jj, 64:128] = wf[2 * jj + 1]
        w[f"wfn{tag}"] = wfn.reshape(P, 256)
        w[f"qkn{tag}"] = np.ascontiguousarray(np.stack([inp["ab_q_norm"][j], inp["ab_k_norm"][j]], axis=1))
    else:
        w[f"win{tag}"] = inp["na_w_in"][j]
        w[f"wout{tag}"] = inp["na_w_out"][j]
    w[f"wr{tag}"] = np.ascontiguousarray(inp["moe_w_router"][l].reshape(KC, P, 64).transpose(1, 0, 2)).reshape(P, KC * 64)
    w[f"mb{tag}"] = np.ascontiguousarray(np.broadcast_to(inp["moe_bias"][l][None, :], (P, 64)))
    wg = np.concatenate([inp["moe_w_gate"][l], inp["sh_w_gate"][l][None]], axis=0)
    wu = np.concatenate([inp["moe_w_up"][l], inp["sh_w_up"][l][None]], axis=0)
    wd = np.concatenate([inp["moe_w_down"][l], inp["sh_w_down"][l][None]], axis=0)
    moew = np.empty((NEXP, P, 6144), np.float32)
    moew[:, :, 0:2048] = wg.reshape(NEXP, KC, P, 256).transpose(0, 2, 1, 3).reshape(NEXP, P, 2048)
    moew[:, :, 2048:4096] = wu.reshape(NEXP, KC, P, 256).transpose(0, 2, 1, 3).reshape(NEXP, P, 2048)
    moew[:, :, 4096:6144] = wd.reshape(NEXP, 2, P, D).transpose(0, 2, 1, 3).reshape(NEXP, P, 2048)
    w[f"moew{tag}"] = moew
    return w


_PROG_CACHE = {}


def _get_prog(layers, debug=None):
    key = (tuple(layers), tuple(sorted((debug or {}).items())))
    if key not in _PROG_CACHE:
        pr = Prog(list(layers), debug)
        nc = pr.build()
        _PROG_CACHE[key] = (nc, set(pr.inputs.keys()))
    return _PROG_CACHE[key]


def _init_toks(inp):
    toks = []
    for r in range(8):
        b, s = r // 2, r % 2
        toks.append(np.concatenate([inp["x"][b, s * NLAT:(s + 1) * NLAT], inp["ctx"][b, s * NCTX:(s + 1) * NCTX]], axis=0))
    return toks


def _launch(inp, layers, toks, debug=None):
    nc, names = _get_prog(layers, debug)
    lw = {}
    for l in layers:
        lw.update(_layer_weights(l, inp, l))
    fm = [_fm(t) for t in toks]
    in_maps = []
    for r in range(8):
        b, s = r // 2, r % 2
        m = dict(lw)
        cs = _consts(s)
        for k in ("ident", "perm", "rope", "ropeq", "cs2", "dft", "cdft"):
            m[k] = cs[k]
        for l in layers:
            if l % 2 == 1:
                m[f"nab{l}"] = _na_bias(inp["na_rpb"][l // 2], s)
        m["cT"] = np.ascontiguousarray(
            np.stack([inp["c"][b].reshape(KC, P).T, inp["c_ctx"].reshape(KC, P).T], axis=2)).reshape(P, 16)
        m["xT"] = fm[r]
        m[f"xg{layers[0]}"] = np.ascontiguousarray(
            np.stack([fm[r - s].reshape(P, KC, NT), fm[r - s + 1].reshape(P, KC, NT)], axis=0).transpose(2, 0, 1, 3)
        ).reshape(KC, 2 * P, NT)
        in_maps.append({k: v for k, v in m.items() if k in names})
    res = run_bass_kernel_spmd(nc, in_maps, core_ids=list(range(8)))
    return [_unfm(res.results[r]["xout"], NT) for r in range(8)]


def run_layers(inp, layers=range(DEPTH), toks=None, debug=None, fused=False):
    inp = {k: np.asarray(v) for k, v in inp.items()}
    if toks is None:
        toks = _init_toks(inp)
    if fused:
        return _launch(inp, list(layers), toks, debug)
    for l in layers:
        toks = _launch(inp, [l], toks, debug)
    return toks


def kernel(**inputs):
    toks = run_layers(inputs, fused=True)
    out = np.empty((4, 4096, D), np.float32)
    for r in range(8):
        b, s = r // 2, r % 2
        out[b, s * NLAT:(s + 1) * NLAT] = toks[r][0:NLAT]
    return out
```

```python
import math
import numpy as np
import ml_dtypes
import concourse.bass as bass
import concourse.mybir as mybir
from concourse.bass_utils import run_bass_kernel_spmd
from contextlib import ExitStack

F32 = mybir.dt.float32
BF16 = mybir.dt.bfloat16
AF = mybir.ActivationFunctionType
ALU = mybir.AluOpType
AX = mybir.AxisListType

P = 128
D = 1024
KC = 8
NLAT = 2048
NCTX = 128
NT = NLAT + NCTX
DEPTH = 4
ALPHA = (2 * DEPTH) ** 0.25
LN_EPS = 1e-6
RMS_EPS = 1e-6
HD = 128
SCALE = HD ** -0.5
NEG = -1e30
NEXP = 65
ROUTE_SCALE = 2.5

ENGS = ("pe", "act", "dve", "pool", "sp")
DTSIZE = {F32: 4, BF16: 2}


class V:
    __slots__ = ("t", "ap")

    def __init__(self, t, ap):
        self.t = t
        self.ap = ap

    def __getitem__(self, idx):
        return V(self.t, self.ap[idx])

    def r(self, pat, **kw):
        return V(self.t, self.ap.rearrange(pat, **kw))

    def bc(self, shape):
        return V(self.t, self.ap.broadcast_to(shape))

    def bitcast(self, dt):
        return V(self.t, self.ap.bitcast(dt))


class T:
    def __init__(self, h, name, space, rng=None):
        self.h = h
        self.name = name
        self.space = space
        self.w = None
        self.r_ = []
        self.dsem = None
        self.dcnt = 0
        self.rng = rng

    def __getitem__(self, idx):
        return V(self, self.h[idx])

    def r(self, pat, **kw):
        return V(self, self.h.rearrange(pat, **kw))

    def v(self):
        return V(self, self.h)


class Ctx:
    def __init__(self, nc, arena_bytes=211968):
        self.nc = nc
        self.es = ExitStack()
        self.E = {"pe": nc.tensor, "act": nc.scalar, "dve": nc.vector, "pool": nc.gpsimd, "sp": nc.sync}
        self.sem = {}
        self.cnt = {}
        for e in ENGS:
            self.sem[e] = self.es.enter_context(nc.semaphore("s_" + e))
            self.cnt[e] = 0
        self.seen = {e: {} for e in ENGS}
        self.nbuf = 0
        self.sem_pool = []
        self.arena = self.es.enter_context(nc.sbuf_tensor("arena", [P, arena_bytes // 2], BF16))
        self.arena_bytes = arena_bytes
        self.free_list = [(0, arena_bytes)]
        self.grave = []
        self.live = {}
        self.banks = []
        for i in range(8):
            h = self.es.enter_context(nc.psum_tensor(f"bank{i}", [P, 512], F32))
            self.banks.append(T(h[:], f"bank{i}", "ps"))

    def alloc(self, name, nelem, dt, parts=P):
        nbytes = (nelem * DTSIZE[dt] + 63) // 64 * 64
        for i, (s, e) in enumerate(self.free_list):
            if e - s >= nbytes:
                self.free_list[i] = (s + nbytes, e)
                if self.free_list[i][0] == self.free_list[i][1]:
                    del self.free_list[i]
                break
        else:
            raise RuntimeError(f"arena OOM allocating {name} {nbytes}B; free={self.free_list}")
        ap = self.arena[0:parts, s // 2:(s + nbytes) // 2]
        if dt != BF16:
            ap = ap.bitcast(dt)
        ap = ap[:, 0:nelem]
        self.nbuf += 1
        t = T(ap, f"{name}_{self.nbuf}", "sb", (s, s + nbytes))
        keep = []
        for (gs, ge, toks) in self.grave:
            if gs < s + nbytes and s < ge:
                t.r_.extend(toks)
                if gs >= s and ge <= s + nbytes:
                    continue
            keep.append((gs, ge, toks))
        self.grave = keep
        return t

    def free(self, *ts):
        for t in ts:
            s, e = t.rng
            toks = [x for x in ([t.w] + t.r_) if x is not None]
            self.grave.append((s, e, self._compress(toks)))
            self.free_list.append((s, e))
            self.free_list.sort()
            merged = []
            for a, b in self.free_list:
                if merged and merged[-1][1] == a:
                    merged[-1] = (merged[-1][0], b)
                else:
                    merged.append((a, b))
            self.free_list = merged
            t.rng = None
            if t.dsem is not None:
                self.sem_pool.append((t.dsem, t.dcnt, t.dkey))
                t.dsem = None

    @staticmethod
    def _compress(toks):
        best = {}
        for tok in toks:
            key = tok[1] if tok[0] == "eng" else tok[3]
            v = tok[2]
            if key not in best or best[key][2] < v:
                best[key] = tok
        return list(best.values())

    def dram(self, name, shape, dt, kind="Internal"):
        h = self.nc.dram_tensor(name, list(shape), dt, kind=kind)
        return T(h.ap(), name, "dram")

    def _wait(self, e, tok):
        if tok is None:
            return
        if tok[0] == "eng":
            _, f, v = tok
            key = "e:" + f
            sem = self.sem[f]
        else:
            _, sem, v, key = tok
        if e == "pe" and tok[0] == "eng" and tok[1] == "pe":
            return
        if self.seen[e].get(key, 0) >= v:
            return
        self.E[e].wait_ge(sem, v)
        self.seen[e][key] = v

    def _deps(self, e, reads, writes, acc=False):
        for t in reads:
            self._wait(e, t.w)
        if not acc:
            for t in writes:
                self._wait(e, t.w)
                for tok in t.r_:
                    self._wait(e, tok)

    def op(self, e, fn, reads=(), writes=(), acc=False, inc=True):
        reads = [v.t for v in reads if v is not None]
        writes = [v.t for v in writes if v is not None]
        self._deps(e, reads, writes, acc)
        ins = fn(self.E[e])
        if inc:
            self.cnt[e] += 1
            ins.then_inc(self.sem[e], 1)
            tok = ("eng", e, self.cnt[e])
        else:
            tok = ("eng", e, self.cnt[e] + 1)
        for t in reads:
            t.r_.append(tok)
            if len(t.r_) > 24:
                t.r_ = self._compress(t.r_)
        for t in writes:
            t.w = tok
            t.r_ = []
        return ins

    def dma(self, q, out, in_, **kw):
        ot = out.t if isinstance(out, V) else None
        it = in_.t if isinstance(in_, V) else None
        oap = out.ap if isinstance(out, V) else out
        iap = in_.ap if isinstance(in_, V) else in_
        reads = [it] if it is not None else []
        writes = [ot] if ot is not None else []
        self._deps(q, reads, writes)
        owner = None
        for t in (ot, it):
            if t is not None and t.space != "dram":
                owner = t
                break
        if owner is None:
            owner = ot if ot is not None else it
        if owner.dsem is None:
            if self.sem_pool:
                sem, cnt, key = self.sem_pool.pop()
                if cnt > 0:
                    self._wait(q, ("dma", sem, cnt, key))
            else:
                self.nsem = getattr(self, "nsem", 0) + 1
                key = f"d:{self.nsem}"
                sem, cnt = self.es.enter_context(self.nc.semaphore(f"dsem{self.nsem}")), 0
            owner.dsem, owner.dcnt, owner.dkey = sem, cnt, key
        owner.dcnt += 16
        ins = self.E[q].dma_start(out=oap, in_=iap, **kw)
        ins.then_inc(owner.dsem, 16)
        tok = ("dma", owner.dsem, owner.dcnt, owner.dkey)
        for t in reads:
            t.r_.append(tok)
        for t in writes:
            t.w = tok
            t.r_ = []
        return tok

    def wait_all(self, e, ts):
        for t in ts:
            self._wait(e, t.w)
            for tok in t.r_:
                self._wait(e, tok)

    def mm(self, out, lhsT, rhs, start=True, stop=True, inc=None):
        self.op("pe", lambda e: e.matmul(out.ap, lhsT=lhsT.ap, rhs=rhs.ap, start=start, stop=stop),
                reads=[lhsT, rhs], writes=[out], acc=not start, inc=(stop if inc is None else inc))

    def transpose(self, out, in_, ident):
        self.op("pe", lambda e: e.transpose(out.ap, in_.ap, ident.ap), reads=[in_, ident], writes=[out])

    def act(self, out, in_, func, scale=None, bias=None, accum=None):
        kw = {}
        rd = [in_]
        wr = [out]
        if scale is not None:
            if isinstance(scale, V):
                kw["scale"] = scale.ap
                rd.append(scale)
            else:
                kw["scale"] = float(scale)
        if bias is not None:
            if isinstance(bias, V):
                kw["bias"] = bias.ap
                rd.append(bias)
            else:
                kw["bias"] = float(bias)
        if accum is not None:
            kw["accum_out"] = accum.ap
            wr.append(accum)
        self.op("act", lambda e: e.activation(out=out.ap, in_=in_.ap, func=func, **kw), reads=rd, writes=wr)

    def copy(self, eng, out, in_):
        if eng == "act":
            self.op("act", lambda e: e.copy(out=out.ap, in_=in_.ap), reads=[in_], writes=[out])
        else:
            self.op(eng, lambda e: e.tensor_copy(out=out.ap, in_=in_.ap), reads=[in_], writes=[out])

    def tt(self, eng, out, in0, in1, op):
        self.op(eng, lambda e: e.tensor_tensor(out=out.ap, in0=in0.ap, in1=in1.ap, op=op),
                reads=[in0, in1], writes=[out])

    def ts(self, eng, out, in0, s1, s2=None, op0=ALU.mult, op1=None):
        rd = [in0]
        a1 = s1.ap if isinstance(s1, V) else float(s1)
        if isinstance(s1, V):
            rd.append(s1)
        a2 = None
        if s2 is not None:
            a2 = s2.ap if isinstance(s2, V) else float(s2)
            if isinstance(s2, V):
                rd.append(s2)
        if op1 is None:
            self.op(eng, lambda e: e.tensor_scalar(out=out.ap, in0=in0.ap, scalar1=a1, scalar2=None, op0=op0),
                    reads=rd, writes=[out])
        else:
            self.op(eng, lambda e: e.tensor_scalar(out=out.ap, in0=in0.ap, scalar1=a1, scalar2=a2, op0=op0, op1=op1),
                    reads=rd, writes=[out])

    def stt(self, out, in0, scalar, in1, op0, op1, eng="dve"):
        rd = [in0, in1]
        a = scalar.ap if isinstance(scalar, V) else float(scalar)
        if isinstance(scalar, V):
            rd.append(scalar)
        self.op(eng, lambda e: e.scalar_tensor_tensor(out=out.ap, in0=in0.ap, scalar=a, in1=in1.ap, op0=op0, op1=op1),
                reads=rd, writes=[out])

    def memset(self, eng, out, val):
        self.op(eng, lambda e: e.memset(out.ap, val), writes=[out])

    def close(self):
        self.es.close()


class Prog:
    def __init__(self, layers, debug=None):
        self.layers = list(layers)
        self.debug = debug or {}
        self.nc = bass.Bass("TRN2", target_bir_lowering=False)
        self.c = Ctx(self.nc)
        self.inputs = {}
        self.pending_mod = None
        self.rot = {}
        self.xg = {}

    def inp(self, name, shape, dt=F32):
        if name not in self.inputs:
            self.inputs[name] = self.nc.dram_tensor(name, list(shape), dt, kind="ExternalInput").ap()
        return self.inputs[name]

    def bank(self, i):
        return self.c.banks[i]

    def rr(self, key, n):
        v = self.rot.get(key, 0)
        self.rot[key] = v + 1
        return v % n

    def build(self):
        c = self.c
        nc = self.nc
        first = self.layers[0]
        self.XT = c.alloc("XT", KC * NT, F32)
        self.XT3 = self.XT.r("p (c t) -> p c t", c=KC)
        self.HB = c.alloc("HB", KC * NT, BF16)
        self.HB3 = self.HB.r("p (c t) -> p c t", c=KC)
        self.identb = c.alloc("identb", P, BF16)
        self.identf = c.alloc("identf", P, F32)
        c.dma("pool", self.identb.v(), self.inp("ident", [P, P]))
        c.dma("sp", self.identf.v(), self.inp("ident", [P, P]))
        self.ones_mean = c.alloc("ones_mean", P, BF16)
        self.ones_rms = c.alloc("ones_rms", P, BF16)
        self.ones1 = c.alloc("ones1", P, BF16)
        c.memset("dve", self.ones_mean.v(), 1.0 / D)
        self.ones_mean_f = c.alloc("ones_mean_f", P, F32)
        c.memset("dve", self.ones_mean_f.v(), 1.0 / D)
        c.memset("dve", self.ones_rms.v(), 1.0 / HD)
        c.memset("dve", self.ones1.v(), 1.0)
        self.sets = []
        for _ in range(2):
            d_ = {}
            d_["MODV"] = c.alloc("MODV", 96, F32)
            d_["MODV3"] = d_["MODV"].r("p (m r) -> p m r", r=2)
            d_["LNV"] = c.alloc("LNV", 32, F32)
            d_["LNV3"] = d_["LNV"].r("p (m c) -> p m c", c=KC)
            d_["A2"] = c.alloc("A2", 16, F32)
            d_["A23"] = d_["A2"].r("p (c r) -> p c r", r=2)
            d_["B2"] = c.alloc("B2", 16, F32)
            d_["B23"] = d_["B2"].r("p (c r) -> p c r", r=2)
            self.sets.append(d_)
        self.use_set(self.layers[0])
        self.G = c.alloc("G", 17 * NEXP, F32)
        self.G3 = self.G.r("p (t e) -> p t e", e=NEXP)
        self.scT = c.alloc("scT", 16, F32)
        self.scT3 = self.scT.r("p (k r) -> p k r", r=2)
        c.dma("sp", self.scT.v(), self.inp("cT", [P, 16]))
        c.act(self.scT.v(), self.scT.v(), AF.Silu)
        c.dma("sp", self.XT.v(), self.inp("xT", [P, KC * NT]))
        for li, l in enumerate(self.layers):
            self.layer(l)
            if li + 1 < len(self.layers):
                self.exchange(l, self.layers[li + 1])
        xout = nc.dram_tensor("xout", [P, KC * NT], F32, kind="ExternalOutput").ap()
        tok = c.dma("sp", xout, self.XT.v())
        c._wait("sp", tok)
        c.close()
        return nc

    def exchange(self, l, lnext):
        c = self.c
        nc = self.nc
        sem = c.es.enter_context(nc.semaphore(f"ccsem{l}"))
        tok = ("dma", sem, KC, f"cc{l}")
        xgh = nc.dram_tensor(f"xgi{lnext}", [KC, 2 * P, NT], F32)
        xg = T(xgh.ap(), f"xgi{lnext}", "dram")
        for cc in range(KC):
            xsh = nc.dram_tensor(f"xs{l}_{cc}", [P, NT], F32)
            xs = T(xsh.ap(), f"xs{l}_{cc}", "dram")
            c.dma("sp", xs.v(), self.XT3[:, cc, :])
            c._deps("pool", [xs], [xg] if cc == 0 else [])
            ins = nc.gpsimd.collective_compute("AllGather", ALU.bypass, replica_groups=[[0, 1], [2, 3], [4, 5], [6, 7]],
                                               ins=[xsh.ap().opt()], outs=[xgh.ap()[cc]])
            ins.then_inc(sem)
            xs.r_.append(tok)
        xg.w = tok
        xg.r_ = []
        self.xg[lnext] = xg

    def use_set(self, l):
        for k_, v_ in self.sets[l % 2].items():
            setattr(self, k_, v_)

    def layer(self, l):
        self.use_set(l)
        if l == self.layers[0]:
            self.emit_mod(l)
        if self.debug.get("stop") == "mod":
            return
        self.emit_h1(l)
        if self.debug.get("stop") == "h1":
            return
        if l % 2 == 0:
            self.emit_gqa(l)
        else:
            self.emit_na(l)
        stop = self.debug.get("stop")
        if stop == "mixer":
            return
        self.emit_ln(l, 1)
        if stop == "ln1":
            return
        self.emit_moe(l)
        if stop == "moe":
            return
        li = self.layers.index(l)
        if li + 1 < len(self.layers):
            self.pending_mod = self.mod_gen(self.layers[li + 1])
        self.emit_ln(l, 2)

    def emit_mod(self, l):
        for _ in self.mod_gen(l):
            pass

    def mod_gen(self, l):
        c = self.c
        tg = self.sets[l % 2]
        wmod = self.inp(f"wmod{l}", [D, 6 * D]).rearrange("(k p) n -> p k n", p=P)
        bmod = c.alloc("bmod", 48, F32)
        c.dma("sp", bmod.v(), self.inp(f"bmod{l}", [P, 48]))
        c.dma("sp", tg["LNV"].v(), self.inp(f"lnv{l}", [P, 32]))
        st = [c.alloc("wmst", KC * 512, F32) for _ in range(2)]
        scb3 = self.scT3
        ps = self.bank(3)
        ps3 = ps[:, 0:96].r("p (m r) -> p m r", r=2)
        for blk in range(12):
            s = st[blk % 2]
            s3 = s.r("p (k n) -> p k n", k=KC)
            c.dma("sp", s3, wmod[:, :, blk * 512:(blk + 1) * 512])
            for fc in range(4):
                for k in range(KC):
                    c.mm(ps3[:, blk * 4 + fc, :], s3[:, k, fc * 128:(fc + 1) * 128], scb3[:, k, :],
                         start=(k == 0), stop=(k == KC - 1))
            yield blk
        M3 = tg["MODV3"]
        for r in range(2):
            c.tt("dve", M3[:, :, r], ps3[:, :, r], bmod.v(), ALU.add)
        c.ts("dve", M3[:, 8:16, :], M3[:, 8:16, :], 1.0, op0=ALU.add)
        c.ts("dve", M3[:, 32:40, :], M3[:, 32:40, :], 1.0, op0=ALU.add)
        c.ts("dve", M3[:, 16:24, :], M3[:, 16:24, :], 1.0 / ALPHA, op0=ALU.mult)
        c.ts("dve", M3[:, 40:48, :], M3[:, 40:48, :], 1.0 / ALPHA, op0=ALU.mult)
        for r in range(2):
            c.tt("dve", tg["A23"][:, :, r], tg["LNV3"][:, 0, :], M3[:, 32:40, r], ALU.mult)
            c.tt("dve", tg["B23"][:, :, r], tg["LNV3"][:, 1, :], M3[:, 32:40, r], ALU.mult)
            c.tt("dve", tg["B23"][:, :, r], tg["B23"][:, :, r], M3[:, 24:32, r], ALU.add)
        c.free(bmod, *st)

    def mod_h(self, eng, out, in_, cc, r):
        if eng == "act":
            self.c.act(out, in_, AF.Identity, scale=self.MODV3[:, 8 + cc, r:r + 1], bias=self.MODV3[:, cc, r:r + 1])
        else:
            self.c.ts(eng, out, in_, self.MODV3[:, 8 + cc, r:r + 1], self.MODV3[:, cc, r:r + 1], ALU.mult, ALU.add)

    def emit_h1(self, l):
        for cc in range(KC):
            eng = ("dve", "act")[cc % 2]
            self.mod_h(eng, self.HB3[:, cc, 0:NLAT], self.XT3[:, cc, 0:NLAT], cc, 0)
            self.mod_h(eng, self.HB3[:, cc, NLAT:NT], self.XT3[:, cc, NLAT:NT], cc, 1)

    def xg_ap(self, l):
        if l in self.xg:
            return self.xg[l]
        ap = self.inp(f"xg{l}", [KC, 2 * P, NT])
        self.xg[l] = T(ap, f"xg{l}", "dram")
        return self.xg[l]

    def gstream(self, l, groups):
        c = self.c
        xg = self.xg_ap(l)
        st = [c.alloc("xost", KC * 256, F32) for _ in range(3)]
        ho = [c.alloc("xoh", KC * 256, BF16) for _ in range(3)]
        views = {}

        def load(i):
            slot, c0, n, r = groups[i]
            s3 = st[i % 3].r("p (c t) -> p c t", c=KC)[:, :, 0:n]
            c.dma("sp", s3, V(xg, xg.h[:, slot * P:(slot + 1) * P, c0:c0 + n].rearrange("c p t -> p c t")))
            views[i] = s3

        def prep(i):
            slot, c0, n, r = groups[i]
            s3 = views.pop(i)
            h3 = ho[i % 3].r("p (c t) -> p c t", c=KC)[:, :, 0:n]
            for cc in range(KC):
                self.mod_h(("dve", "act")[cc % 2], h3[:, cc, :], s3[:, cc, :], cc, r)
            return h3

        ng = len(groups)
        load(0)
        if ng > 1:
            load(1)
        h_next = prep(0)
        for i in range(ng):
            h_cur = h_next
            if i + 2 < ng:
                load(i + 2)
            if i + 1 < ng:
                h_next = prep(i + 1)
            slot, c0, n, r = groups[i]
            yield (slot, c0, n, r, h_cur)
        c.free(*st, *ho)

    def load_w(self, name, dram_ap, ncols):
        c = self.c
        t = c.alloc(name, KC * ncols, BF16)
        t3 = t.r("p (k n) -> p k n", k=KC)
        c.dma("pool", t3, dram_ap.rearrange("(k p) n -> p k n", p=P))
        return t, t3

    def outproj(self, oT, wo, col0, n, r):
        c = self.c
        oTs = oT if isinstance(oT, list) else [oT]
        wos = wo if isinstance(wo, list) else [wo]
        for cc in range(KC):
            y = self.bank(6 + self.rr("yb", 2))[:, 0:n]
            for i, (o, w) in enumerate(zip(oTs, wos)):
                c.mm(y, w[:, cc * 128:(cc + 1) * 128], o, start=(i == 0), stop=(i == len(oTs) - 1))
            xs = self.XT3[:, cc, col0:col0 + n]
            c.stt(xs, y, self.MODV3[:, 16 + cc, r:r + 1], xs, ALU.mult, ALU.add)

    def rmsnorm_rope(self, ps, n, gain, out, rope_col0=None, table=None):
        c = self.c
        k = self.rr("rn", 2)
        tm = self.rn_tmp[k]
        sq = tm["sq"][:, 0:n]
        rstd = tm["rstd"][:, 0:n]
        c.act(sq, ps, AF.Square)
        ss = self.bank(2 + k)[:, 0:n]
        c.mm(ss, self.ones_rms.v(), sq)
        c.act(rstd, ss, AF.Ln, bias=RMS_EPS)
        c.act(rstd, rstd, AF.Exp, scale=-0.5)
        if rope_col0 is None:
            c.stt(out, ps, gain, rstd, ALU.mult, ALU.mult)
            return
        qn = tm["qn"][:, 0:n]
        c.stt(qn, ps, gain, rstd, ALU.mult, ALU.mult)
        rp = tm["rope"].r("p (a n) -> p a n", a=2)[:, :, 0:n]
        c.dma("sp", rp, (table if table is not None else self.rope_dram)[:, :, rope_col0:rope_col0 + n])
        pp = self.bank(4 + k)[:, 0:n]
        c.mm(pp, self.perm.v(), qn)
        t1 = tm["t1"][:, 0:n]
        t2 = tm["t2"][:, 0:n]
        c.tt("dve", t1, qn, rp[:, 0, :], ALU.mult)
        c.tt("dve", t2, pp, rp[:, 1, :], ALU.mult)
        c.tt("dve", out, t1, t2, ALU.add)

    def attn_T(self, KT, VV3, qv, n, tiles):
        c = self.c
        O = self.bank(2)[:, 0:n]
        L = self.bank(3)[:, 0:n]
        nt = len(tiles)
        sbanks = (0, 1, 4, 5)
        LA = 3

        def S(i):
            s = self.bank(sbanks[self.rr("sb", 4)])[:, 0:n]
            kt = tiles[i]
            c.mm(s, KT[:, kt * 128:(kt + 1) * 128], qv)
            return s
        q = [S(i) for i in range(min(LA, nt))]
        for i in range(nt):
            s_cur = q.pop(0)
            if i + LA < nt:
                q.append(S(i + LA))
            pt = self.pt_tmp[self.rr("pt", len(self.pt_tmp))][:, 0:n]
            c.act(pt, s_cur, AF.Exp, scale=SCALE)
            c.mm(O, VV3[:, tiles[i], :], pt, start=(i == 0), stop=(i == nt - 1))
            c.mm(L, self.ones1.v(), pt, start=(i == 0), stop=(i == nt - 1))
        kk = self.rr("ot", 2)
        rl = self.rl_tmp[kk][:, 0:n]
        c.act(rl, L, AF.Ln)
        c.act(rl, rl, AF.Exp, scale=-1.0)
        oT = self.ot_tmp[kk][:, 0:n]
        c.tt("dve", oT, O, rl, ALU.mult)
        return oT

    def emit_gqa(self, l):
        c = self.c
        j = l // 2
        win = self.inp(f"win{l}", [D, 1536])
        wout = self.inp(f"wout{l}", [D, D])
        g_own = [(i * 256, 256, 0) for i in range(8)] + [(NLAT, 128, 1)]
        wf, wf3 = self.load_w("wf", win[:, 0:256], 256)
        cs2 = c.alloc("cs2", 256, BF16)
        c.dma("pool", cs2.v(), self.inp("cs2", [P, 256]))
        wfn = c.alloc("wfn", 256, BF16)
        wfn3 = wfn.r("p (j m) -> p j m", j=2)
        c.dma("pool", wfn.v(), self.inp(f"wfn{l}", [P, 256]))
        wo_f = c.alloc("wo_f", 2 * D, BF16)
        wo_f3 = wo_f.r("p (j n) -> p j n", j=2)
        c.dma("pool", wo_f3, wout[0:256, :].rearrange("(j p) n -> p j n", p=P))
        cdft = c.alloc("cdft", 512, BF16)
        cdft4 = cdft.r("p (a t k) -> p a t k", a=2, t=2)
        c.dma("pool", cdft.v(), self.inp("cdft", [P, 512]))
        FCS = c.alloc("FCS", 34 * 512, BF16)
        FCS3 = FCS.r("p (t f) -> p t f", f=512)
        fts = [c.alloc("fts", 512, BF16) for _ in range(2)]

        def stage1(buf0, n, h3):
            fps = self.bank(self.rr("fps", 2))
            fps3 = fps[:, 0:2 * n].r("p (j t) -> p j t", j=2)
            for jj in range(2):
                for k in range(KC):
                    c.mm(fps3[:, jj, :], wf3[:, k, jj * 128:(jj + 1) * 128], h3[:, k, :], start=(k == 0), stop=(k == KC - 1))
            ft = fts[self.rr("fts", 2)]
            ft3 = ft[:, 0:2 * n].r("p (j t) -> p j t", j=2)
            c.copy("act", ft3, fps3)
            for tt in range(n // 128):
                cps = self.bank(2 + self.rr("cps", 2))
                for jj in range(2):
                    c.mm(cps[:, jj * 256:(jj + 1) * 256], ft3[:, jj, tt * 128:(tt + 1) * 128], cs2.v())
                c.copy("dve", FCS3[:, (buf0 + tt * 128) // 128, :], cps.v())

        g_all = [(slot, c0, n, r) for slot in range(2) for (c0, n, r) in g_own]
        for (slot, c0, n, r, h3) in self.gstream(l, g_all):
            stage1(slot * NT + c0, n, h3)
        c.free(wf, cs2, *fts)

        dft = self.inp("dft", [4, 8, P, 4096], BF16)
        dst = [c.alloc("dftst", 4096, BF16) for _ in range(2)]
        frs = [c.alloc("frs", 512, BF16) for _ in range(2)]
        ots = [c.alloc("ofs", 512, BF16) for _ in range(2)]

        def finish(acc, n, col0, r):
            oTs = []
            for jj in range(2):
                fr = frs[jj][:, 0:n]
                c.copy("act", fr, acc[jj])
                wps = self.bank(6 + self.rr("yb", 2))[:, 0:n]
                c.mm(wps, wfn3[:, jj, :], fr)
                o = ots[jj][:, 0:n]
                c.copy("act", o, wps)
                oTs.append(o)
            self.outproj(oTs, [wo_f3[:, 0, :], wo_f3[:, 1, :]], col0, n, r)

        for kc in range(4):
            acc = [self.bank(4)[:, 0:512], self.bank(5)[:, 0:512]]
            for ng in range(8):
                dt_ = dst[self.rr("dst", 2)]
                c.dma("sp", dt_.v(), dft[kc, ng])
                dt4 = dt_.r("p (a i k) -> p a i k", a=2, i=4)
                for ni in range(4):
                    nn = ng * 4 + ni
                    ft_i = nn if nn < 16 else nn + 1
                    for jj in range(2):
                        c.mm(acc[jj], FCS3[:, ft_i, jj * 256:jj * 256 + 128], dt4[:, 0, ni, :], start=(nn == 0), stop=False)
                        c.mm(acc[jj], FCS3[:, ft_i, jj * 256 + 128:(jj + 1) * 256], dt4[:, 1, ni, :], start=False, stop=(nn == 31),
                             inc=(nn == 31 or (ni == 3 and jj == 1)))
            finish(acc, 512, kc * 512, 0)
        acc = [self.bank(4)[:, 0:128], self.bank(5)[:, 0:128]]
        for ti, ft_i in enumerate((16, 33)):
            for jj in range(2):
                c.mm(acc[jj], FCS3[:, ft_i, jj * 256:jj * 256 + 128], cdft4[:, 0, ti, :], start=(ti == 0), stop=False)
                c.mm(acc[jj], FCS3[:, ft_i, jj * 256 + 128:(jj + 1) * 256], cdft4[:, 1, ti, :], start=False, stop=(ti == 1))
        finish(acc, 128, NLAT, 1)
        c.free(FCS, wfn, wo_f, cdft, *dst, *frs, *ots)
        if self.debug.get("stop") == "fnet":
            return

        self.rope_dram = self.inp("rope", [P, 2 * 4096], BF16).rearrange("p (a n) -> p a n", a=2)
        ropeq = self.inp("ropeq", [P, 2 * NLAT], BF16).rearrange("p (a n) -> p a n", a=2)
        self.perm = c.alloc("perm", P, BF16)
        c.dma("pool", self.perm.v(), self.inp("perm", [P, P]))
        qkn = c.alloc("qkn", 2, F32)
        c.dma("sp", qkn.v(), self.inp(f"qkn{l}", [P, 2]))
        self.rn_tmp = [dict(sq=c.alloc("sq", 256, BF16), rstd=c.alloc("rstd", 256, F32), qn=c.alloc("qn", 256, BF16),
                            rope=c.alloc("rope", 512, BF16), t1=c.alloc("t1", 256, F32), t2=c.alloc("t2", 256, F32))
                       for _ in range(2)]
        self.pt_tmp = [c.alloc("pt", 512, BF16) for _ in range(5)]
        self.rl_tmp = [c.alloc("rl", 512, F32) for _ in range(2)]
        self.ot_tmp = [c.alloc("ot", 512, BF16) for _ in range(2)]
        KTt = c.alloc("KT", 2 * NT, BF16)
        KT = KTt.v()
        VVt = c.alloc("VV", 34 * 128, BF16)
        VV3 = VVt.r("p (t d) -> p t d", d=128)
        QTt = [c.alloc("QT", NT, BF16) for _ in range(2)]
        for g in range(2):
            wk, wk3 = self.load_w("wk", win[:, 1024 + g * 128:1024 + (g + 1) * 128], 128)
            wv, wv3 = self.load_w("wv", win[:, 1280 + g * 128:1280 + (g + 1) * 128], 128)

            def kv(buf0, n, r, h3, rope0):
                kps = self.bank(self.rr("kps", 2))[:, 0:n]
                for k in range(KC):
                    c.mm(kps, wk3[:, k, :], h3[:, k, :], start=(k == 0), stop=(k == KC - 1))
                self.rmsnorm_rope(kps, n, qkn[:, 1:2], KT[:, buf0:buf0 + n], rope0)
                for tt in range(n // 128):
                    vps = self.bank(6 + self.rr("yb", 2))[:, 0:128]
                    for k in range(KC):
                        c.mm(vps, h3[:, k, tt * 128:(tt + 1) * 128], wv3[:, k, :], start=(k == 0), stop=(k == KC - 1))
                    c.copy("act", VV3[:, buf0 // 128 + tt, :], vps)

            for (slot, c0, n, r, h3) in self.gstream(l, g_all):
                kv(slot * NT + c0, n, r, h3, (slot * NLAT + c0) if r == 0 else None)
            c.free(wk, wv)
            for hh in range(3):
                hg = g * 3 + hh
                wq, wq3 = self.load_w("wq", win[:, 256 + hg * 128:256 + (hg + 1) * 128], 128)
                wo = c.alloc("wo", D, BF16)
                c.dma("pool", wo.v(), wout[(2 + hg) * 128:(3 + hg) * 128, :])
                QT = QTt[self.rr("qt", 2)].v()
                for (c0, n, r) in g_own:
                    qps = self.bank(self.rr("kps", 2))[:, 0:n]
                    for k in range(KC):
                        c.mm(qps, wq3[:, k, :], self.HB3[:, k, c0:c0 + n], start=(k == 0), stop=(k == KC - 1))
                    self.rmsnorm_rope(qps, n, qkn[:, 0:1], QT[:, c0:c0 + n], c0 if r == 0 else None, table=ropeq)
                for qc in range(4):
                    oT = self.attn_T(KT, VV3, QT[:, qc * 512:(qc + 1) * 512], 512, list(range(34)))
                    self.outproj(oT, wo.v(), qc * 512, 512, 0)
                oT = self.attn_T(KT, VV3, QT[:, NLAT:NT], 128, [16, 33])
                self.outproj(oT, wo.v(), NLAT, 128, 1)
                c.free(wq, wo)
        c.free(KTt, VVt, *QTt, self.perm, qkn, *self.pt_tmp, *self.rl_tmp, *self.ot_tmp)
        for tm in self.rn_tmp:
            c.free(*tm.values())

    def emit_na(self, l):
        c = self.c
        win = self.inp(f"win{l}", [D, 3 * D])
        wout = self.inp(f"wout{l}", [D, D])
        nab = self.inp(f"nab{l}", [8, P, 5 * 768])
        HOt = c.alloc("HO", KC * 768, BF16)
        HO3 = HOt.r("p (c t) -> p c t", c=KC)
        hgroups = [(0, NLAT - 256, 256, 0), (1, 0, 256, 0), (0, NLAT, 128, 1), (1, NLAT, 128, 1)]
        hdst = [0, 256, 512, 640]
        for gi_, (slot, c0, n, r, h3) in enumerate(self.gstream(l, hgroups)):
            for cc in range(KC):
                c.copy(("dve", "act")[cc % 2], HO3[:, cc, hdst[gi_]:hdst[gi_] + n], h3[:, cc, :])
        NK = 2816
        srcs = [(0, 256, HO3[:, :, 0:256])] + \
               [(256 + i * 512, 512, self.HB3[:, :, i * 512:(i + 1) * 512]) for i in range(4)] + \
               [(2304, 256, HO3[:, :, 256:512]), (2560, 256, HO3[:, :, 512:768])]
        KTs = [c.alloc("KT", NK, BF16) for _ in range(2)]
        VVs = [c.alloc("VV", NK, BF16) for _ in range(2)]
        QTs = [c.alloc("QT", NT, BF16) for _ in range(2)]
        nbs = [c.alloc("nb", 5 * 768, F32) for _ in range(1)]
        ss_tmp = [c.alloc("ss", 768, F32) for _ in range(2)]
        pt_tmp = [c.alloc("pt", 1024, BF16) for _ in range(3)]
        rl_tmp = [c.alloc("rl", 512, F32) for _ in range(2)]
        og_tmp = [c.alloc("og", 512, BF16) for _ in range(2)]

        for h in range(8):
            wq, wq3 = self.load_w("wq", win[:, h * 128:(h + 1) * 128], 128)
            wk, wk3 = self.load_w("wk", win[:, D + h * 128:D + (h + 1) * 128], 128)
            wv, wv3 = self.load_w("wv", win[:, 2 * D + h * 128:2 * D + (h + 1) * 128], 128)
            wo = c.alloc("wo", D, BF16)
            c.dma("pool", wo.v(), wout[h * 128:(h + 1) * 128, :])
            nb = nbs[0]
            c.dma("sp", nb.v(), nab[h])
            nb3 = nb.r("p (t w) -> p t w", w=768)
            KT = KTs[h % 2].v()
            VV3 = VVs[h % 2].r("p (t d) -> p t d", d=128)
            QT = QTs[h % 2].v()
            for (b0, n, h3) in srcs:
                kps = self.bank(6 + self.rr("yb", 2))[:, 0:n]
                for k in range(KC):
                    c.mm(kps, wk3[:, k, :], h3[:, k, :], start=(k == 0), stop=(k == KC - 1))
                c.copy("act", KT[:, b0:b0 + n], kps)
                for tt in range(n // 128):
                    vps = self.bank(6 + self.rr("yb", 2))[:, 0:128]
                    for k in range(KC):
                        c.mm(vps, h3[:, k, tt * 128:(tt + 1) * 128], wv3[:, k, :], start=(k == 0), stop=(k == KC - 1))
                    c.copy("dve", VV3[:, b0 // 128 + tt, :], vps)
            for (c0, n) in [(i * 512, 512) for i in range(4)] + ([(NLAT, 128)] if l < DEPTH - 1 else []):
                qps = self.bank(6 + self.rr("yb", 2))[:, 0:n]
                for k in range(KC):
                    c.mm(qps, wq3[:, k, :], self.HB3[:, k, c0:c0 + n], start=(k == 0), stop=(k == KC - 1))
                c.copy("act", QT[:, c0:c0 + n], qps)
            jobs = []
            for bi in range(16):
                ty = {0: 0, 1: 1, 14: 3, 15: 4}.get(bi, 2)
                wb = 2 * bi if bi <= 13 else 28
                t0_ = wb * 64 // 128
                jobs.append((bi * 128, [t0_ + m for m in range(6)] + [20, 21], ty))
            if l < DEPTH - 1:
                jobs.append((NLAT, [20, 21], None))

            def S_of(job):
                q0, tiles, ty = job
                k = self.rr("nas", 2)
                SA = self.bank(2 * k)
                SB = self.bank(2 * k + 1)
                for m, kt in enumerate(tiles):
                    dst = (SA if m < 4 else SB)[:, (m % 4) * 128:(m % 4 + 1) * 128]
                    c.mm(dst, KT[:, kt * 128:(kt + 1) * 128], QT[:, q0:q0 + 128])
                return SA, SB

            def P_of(job, S):
                q0, tiles, ty = job
                SA, SB = S
                PT = pt_tmp[self.rr("napt", 3)]
                if ty is not None:
                    SS = ss_tmp[self.rr("nass", 2)]
                    c.stt(SS[:, 0:512], SA[:, 0:512], SCALE, nb3[:, ty, 0:512], ALU.mult, ALU.add)
                    c.stt(SS[:, 512:768], SB[:, 0:256], SCALE, nb3[:, ty, 512:768], ALU.mult, ALU.add)
                    c.act(PT[:, 0:768], SS[:, 0:768], AF.Exp)
                    c.act(PT[:, 768:1024], SB[:, 256:512], AF.Exp, scale=SCALE)
                else:
                    c.act(PT[:, 0:256], SA[:, 0:256], AF.Exp, scale=SCALE)
                return PT

            def PV_of(job, PT, O_dst, L_dst):
                q0, tiles, ty = job
                nt = len(tiles)
                for m, kt in enumerate(tiles):
                    c.mm(O_dst, VV3[:, kt, :], PT[:, m * 128:(m + 1) * 128], start=(m == 0), stop=(m == nt - 1))
                for m, kt in enumerate(tiles):
                    c.mm(L_dst, self.ones1.v(), PT[:, m * 128:(m + 1) * 128], start=(m == 0), stop=(m == nt - 1))

            def finish_group(gjobs):
                n = 128 * len(gjobs)
                col0 = gjobs[0][0]
                r = 0 if gjobs[0][2] is not None else 1
                kk = self.rr("narl", 2)
                rl = rl_tmp[kk][:, 0:n]
                c.act(rl, self.bank(5)[:, 0:n], AF.Ln)
                c.act(rl, rl, AF.Exp, scale=-1.0)
                og = og_tmp[kk][:, 0:n]
                c.tt("dve", og, self.bank(4)[:, 0:n], rl, ALU.mult)
                self.outproj(og, wo.v(), col0, n, r)

            S_next = S_of(jobs[0])
            gjobs = []
            for ji, job in enumerate(jobs):
                S_cur = S_next
                if ji + 1 < len(jobs):
                    S_next = S_of(jobs[ji + 1])
                PT = P_of(job, S_cur)
                slot = len(gjobs)
                PV_of(job, PT, self.bank(4)[:, slot * 128:(slot + 1) * 128], self.bank(5)[:, slot * 128:(slot + 1) * 128])
                gjobs.append(job)
                if len(gjobs) == 4 or ji == len(jobs) - 1 or jobs[ji + 1][2] is None:
                    finish_group(gjobs)
                    gjobs = []
            c.free(wq, wk, wv, wo)
        c.free(HOt, *KTs, *VVs, *QTs, *nbs, *ss_tmp, *pt_tmp, *rl_tmp, *og_tmp)

    def emit_ln(self, l, which):
        c = self.c
        gi, bi_ = (0, 1) if which == 1 else (2, 3)
        eps = LN_EPS / (ALPHA * ALPHA)
        zq = [c.alloc("zq", KC * 512, BF16) for _ in range(2)]
        tmp = [dict(M=c.alloc("M", 512, F32), m2=c.alloc("m2", 512, F32), rstd=c.alloc("rstdl", 512, F32)) for _ in range(2)]
        t1s = [c.alloc("t1", 512, F32) for _ in range(3)]
        if which == 1:
            h2f = [c.alloc("h2f", KC * 512, F32) for _ in range(2)]
            wr = c.alloc("wr", KC * 64, F32)
            wr3 = wr.r("p (k e) -> p k e", k=KC)
            c.dma("sp", wr.v(), self.inp(f"wr{l}", [P, KC * 64]))
            mb = c.alloc("mb", 64, F32)
            c.dma("sp", mb.v(), self.inp(f"mb{l}", [P, 64]))
            gt = [c.alloc("gt", 64 * 3 + 16, F32) for _ in range(2)]
            GTS = c.alloc("GTS", NT, F32)
            c.memset("dve", self.G3[:, :, 64:65], 1.0)
            self.GTD = c.dram(f"gtd{l}", [NEXP, NT], F32)
        groups = [(i * 512, 512, 0) for i in range(4)] + ([(NLAT, 128, 1)] if l < DEPTH - 1 else [])
        def stats(gidx):
            c0, n, r = groups[gidx]
            k = gidx % 2
            zq3 = zq[k].r("p (c t) -> p c t", c=KC)[:, :, 0:n]
            mean = self.bank(0 if k == 0 else 6)[:, 0:n]
            msq = self.bank(1 if k == 0 else 7)[:, 0:n]
            for cc in range(KC):
                xs = self.XT3[:, cc, c0:c0 + n]
                c.act(zq3[:, cc, :], xs, AF.Square)
            for cc in range(KC):
                c.mm(mean, self.ones_mean_f.v(), self.XT3[:, cc, c0:c0 + n], start=(cc == 0), stop=(cc == KC - 1))
            for cc in range(KC):
                c.mm(msq, self.ones_mean.v(), zq3[:, cc, :], start=(cc == 0), stop=(cc == KC - 1))
            M = tmp[k]["M"][:, 0:n]
            m2 = tmp[k]["m2"][:, 0:n]
            rstd = tmp[k]["rstd"][:, 0:n]
            c.copy("act", M, mean)
            c.act(m2, mean, AF.Square)
            c.tt("dve", m2, msq, m2, ALU.subtract)
            c.act(rstd, m2, AF.Ln, bias=eps)
            c.act(rstd, rstd, AF.Exp, scale=-0.5)

        def advance(nblk):
            g_ = getattr(self, "pending_mod", None)
            if g_ is None:
                return
            for _ in range(nblk):
                try:
                    next(g_)
                except StopIteration:
                    self.pending_mod = None
                    return

        stats(0)
        advance(2)
        for gidx, (c0, n, r) in enumerate(groups):
            k = gidx % 2
            M = tmp[k]["M"][:, 0:n]
            rstd = tmp[k]["rstd"][:, 0:n]
            if gidx + 1 < len(groups):
                stats(gidx + 1)
            advance(3)
            if which == 1:
                h2f3 = h2f[k].r("p (c t) -> p c t", c=KC)[:, :, 0:n]
            for cc in range(KC):
                xs = self.XT3[:, cc, c0:c0 + n]
                t1 = t1s[self.rr("t1", 3)][:, 0:n]
                c.tt("dve", t1, xs, M, ALU.subtract)
                c.tt("dve", t1, t1, rstd, ALU.mult)
                c.act(xs, t1, AF.Identity, scale=self.LNV3[:, gi, cc:cc + 1], bias=self.LNV3[:, bi_, cc:cc + 1])
                if which == 1:
                    c.act(h2f3[:, cc, :], t1, AF.Identity, scale=self.A23[:, cc, r:r + 1], bias=self.B23[:, cc, r:r + 1])
                    c.copy("dve", self.HB3[:, cc, c0:c0 + n], h2f3[:, cc, :])
            if which == 1:
                for tt in range(n // 128):
                    tile_i = c0 // 128 + tt
                    lg = self.bank(2 + self.rr("lg", 2))[:, 0:64]
                    for cc in range(KC):
                        c.mm(lg, h2f3[:, cc, tt * 128:(tt + 1) * 128], wr3[:, cc, :], start=(cc == 0), stop=(cc == KC - 1))
                    g_ = gt[self.rr("gt", 2)]
                    sc, sel, wsel, m8, den = g_[:, 0:64], g_[:, 64:128], g_[:, 128:192], g_[:, 192:200], g_[:, 200:201]
                    rden = g_[:, 201:202]
                    c.act(sc, lg, AF.Sigmoid)
                    c.tt("dve", sel, sc, mb.v(), ALU.add)
                    c.op("dve", lambda e: e.max(out=m8.ap, in_=sel.ap), reads=[sel], writes=[m8])
                    c.ts("dve", sel, sel, m8[:, 7:8], op0=ALU.is_ge)
                    c.tt("dve", wsel, sel, sc, ALU.mult)
                    c.op("dve", lambda e: e.reduce_sum(out=den.ap, in_=wsel.ap, axis=AX.X), reads=[wsel], writes=[den])
                    c.op("dve", lambda e: e.reciprocal(out=rden.ap, in_=den.ap), reads=[den], writes=[rden])
                    c.ts("dve", self.G3[:, tile_i, 0:64], wsel, rden, ROUTE_SCALE, ALU.mult, ALU.mult)
                    gtp = self.bank(4 + self.rr("gtp", 2))[0:NEXP, 0:128]
                    c.transpose(gtp, self.G3[:, tile_i, :], self.identf.v())
                    c.copy("act", GTS[0:NEXP, tile_i * 128:(tile_i + 1) * 128], gtp)
        if which == 1:
            c.dma("sp", self.GTD.v(), GTS[0:NEXP, :])
            c.free(*h2f, wr, mb, *gt, GTS)
        advance(100)
        c.free(*zq, *t1s)
        for t_ in tmp:
            c.free(*t_.values())

    def emit_moe(self, l):
        c = self.c
        moew = self.inp(f"moew{l}", [NEXP, P, 6144])
        NWB = 4
        WB = [c.alloc("WB", 6144, BF16) for _ in range(NWB)]
        GB = [c.alloc("GB", NT, F32) for _ in range(NWB)]
        sT = [c.alloc("sT", 512, BF16) for _ in range(2)]
        tT = [c.alloc("tT", 512, BF16) for _ in range(2)]
        aT = [c.alloc("aT", 512, BF16) for _ in range(4)]
        groups = [(i * 256, 256, 0) for i in range(8)] + ([(NLAT, 128, 1)] if l < DEPTH - 1 else [])
        pairs = [(2 * i, 2 * i + 1) for i in range(32)] + [(64,)]

        def prefetch(e):
            w = WB[e % NWB]
            for q in range(3):
                c.dma("pool", w[:, q * 2048:(q + 1) * 2048], moew[e][:, q * 2048:(q + 1) * 2048])
            c.dma("sp", GB[e % NWB].r("p (o t) -> p o t", o=1), V(self.GTD, self.GTD.h[e:e + 1, :].partition_broadcast(P)))

        def down_mm(sts):
            e0, c0, n, r, _ = sts[0]
            Y = [self.bank(4 + cc // 2)[:, (cc % 2) * 256:(cc % 2) * 256 + n] for cc in range(KC)]
            nmm = 2 * len(sts)
            for cc in range(KC):
                i = 0
                for (e, _, _, _, a3) in sts:
                    w = WB[e % NWB].v()
                    for jj in range(2):
                        c.mm(Y[cc], w[:, 4096 + jj * 1024 + cc * 128:4096 + jj * 1024 + (cc + 1) * 128], a3[:, jj, :],
                             start=(i == 0), stop=(i == nmm - 1))
                        i += 1

        def down_acc(sts):
            e0, c0, n, r, _ = sts[0]
            Y = [self.bank(4 + cc // 2)[:, (cc % 2) * 256:(cc % 2) * 256 + n] for cc in range(KC)]
            for cc in range(KC):
                xs = self.XT3[:, cc, c0:c0 + n]
                c.stt(xs, Y[cc], self.MODV3[:, 40 + cc, r:r + 1], xs, ALU.mult, ALU.add)

        for e in pairs[0]:
            prefetch(e)
        pend = []
        ready = None
        it = 0
        for pi, pr in enumerate(pairs):
            for gidx, (c0, n, r) in enumerate(groups):
                for ei, e in enumerate(pr):
                    w = WB[e % NWB].v()
                    gb = GB[e % NWB].v()
                    k = it % 2
                    gps = self.bank(2 * k)
                    ups = self.bank(2 * k + 1)
                    g3 = gps[:, 0:2 * n].r("p (j t) -> p j t", j=2)
                    u3 = ups[:, 0:2 * n].r("p (j t) -> p j t", j=2)
                    for jj in range(2):
                        for kk in range(KC):
                            c.mm(g3[:, jj, :], w[:, kk * 256 + jj * 128:kk * 256 + (jj + 1) * 128], self.HB3[:, kk, c0:c0 + n],
                                 start=(kk == 0), stop=(kk == KC - 1))
                    for jj in range(2):
                        for kk in range(KC):
                            c.mm(u3[:, jj, :], w[:, 2048 + kk * 256 + jj * 128:2048 + kk * 256 + (jj + 1) * 128],
                                 self.HB3[:, kk, c0:c0 + n], start=(kk == 0), stop=(kk == KC - 1))
                    did = None
                    if ei == 0 and ready is not None:
                        down_mm(ready)
                        did = ready
                        ready = None
                        if gidx == 0 and pi + 1 < len(pairs):
                            for e2 in pairs[pi + 1]:
                                prefetch(e2)
                    elif ei == 0 and gidx == 0 and pi == 0:
                        for e2 in pairs[1]:
                            prefetch(e2)
                    s3 = sT[k][:, 0:2 * n].r("p (j t) -> p j t", j=2)
                    t3 = tT[k][:, 0:2 * n].r("p (j t) -> p j t", j=2)
                    a3 = aT[it % 4][:, 0:2 * n].r("p (j t) -> p j t", j=2)
                    c.act(s3, g3, AF.Silu)
                    for jj in range(2):
                        c.tt("dve", t3[:, jj, :], u3[:, jj, :], gb[:, c0:c0 + n], ALU.mult)
                    c.tt("dve", a3, s3, t3, ALU.mult)
                    if did is not None:
                        down_acc(did)
                    pend.append((e, c0, n, r, a3))
                    if ei == len(pr) - 1:
                        ready = pend
                        pend = []
                    it += 1
        down_mm(ready)
        down_acc(ready)
        c.free(*WB, *GB, *sT, *tT, *aT)


BF = ml_dtypes.bfloat16


def _fm(tok):
    T_ = tok.shape[0]
    return np.ascontiguousarray(tok.T.reshape(KC, P, T_).transpose(1, 0, 2)).reshape(P, KC * T_)


def _unfm(a, T_):
    return np.ascontiguousarray(a.reshape(P, KC, T_).transpose(1, 0, 2).reshape(D, T_).T)


_CONST_CACHE = {}


def _consts(s):
    if s in _CONST_CACHE:
        return _CONST_CACHE[s]
    out = {}
    out["ident"] = np.eye(P, dtype=np.float32)
    d = np.arange(P)
    i = d % 64
    partner = np.where(i < 32, d + 32, d - 32)
    perm = np.zeros((P, P), np.float32)
    perm[partner, d] = 1.0
    out["perm"] = perm
    pos = np.arange(4096)
    inv = 10000.0 ** (-(2.0 / 64) * np.arange(32, dtype=np.float64))
    pv = np.where((d // 64)[:, None] == 0, (pos // 64)[None, :], (pos % 64)[None, :]).astype(np.float64)
    ang = pv * inv[i % 32][:, None]
    cos = np.cos(ang)
    sin = np.sin(ang) * np.where(i < 32, -1.0, 1.0)[:, None]
    rope = np.stack([cos, sin], axis=1)
    out["rope"] = rope.reshape(P, 2 * 4096).astype(BF)
    out["ropeq"] = np.ascontiguousarray(rope[:, :, s * NLAT:(s + 1) * NLAT]).reshape(P, 2 * NLAT).astype(BF)
    cc = np.arange(64)
    c64 = np.cos(2 * np.pi * np.outer(cc, cc) / 64) / 8.0
    s64 = np.sin(2 * np.pi * np.outer(cc, cc) / 64) / 8.0
    cs2 = np.zeros((P, 256), np.float32)
    for g2 in range(2):
        cs2[g2 * 64:(g2 + 1) * 64, g2 * 64:(g2 + 1) * 64] = c64
        cs2[g2 * 64:(g2 + 1) * 64, 128 + g2 * 64:128 + (g2 + 1) * 64] = s64
    out["cs2"] = cs2
    kk = s * NLAT + np.arange(NLAT)
    ph = (np.outer(pos, kk) % 4096).astype(np.float64) * (2 * np.pi / 4096)
    tab = np.stack([np.cos(ph) / 64.0, -np.sin(ph) / 64.0], axis=0).astype(np.float32)
    tab = tab.reshape(2, 8, 4, P, 4, 512)
    out["dft"] = np.ascontiguousarray(tab.transpose(4, 1, 3, 0, 2, 5)).reshape(4, 8, P, 4096).astype(BF)
    cpos = np.stack([np.arange(128), 128 + np.arange(128)], axis=0)
    ck = s * 128 + np.arange(128)
    cph = (cpos[:, :, None] * ck[None, None, :] % 256) * (2 * np.pi / 256)
    ctab = np.stack([np.cos(cph) / 16.0, -np.sin(cph) / 16.0], axis=0)
    out["cdft"] = np.ascontiguousarray(ctab.transpose(2, 0, 1, 3)).reshape(P, 512).astype(np.float32)
    _CONST_CACHE[s] = out
    return out


def _na_bias(rpb, s):
    out = np.full((8, P, 5, 768), NEG, np.float32)
    q = np.arange(P)
    w = np.arange(768)
    qcol = q % 64
    kcol = w % 64
    cstart = np.clip(qcol - 8, 0, 48)
    okc = (kcol[None, :] >= cstart[:, None]) & (kcol[None, :] < cstart[:, None] + 16)
    dc = kcol[None, :] - qcol[:, None] + 15
    rep = {0: 0, 1: 1, 2: 2, 3: 14, 4: 15}
    for ty, bi in rep.items():
        i = 16 * s + bi
        wb = 2 * bi if bi <= 13 else 28
        qrow = 2 * i + q // 64
        rstart = np.clip(qrow - 4, 0, 56)
        krow = wb + (32 * s - 4) + w // 64
        okr = (krow[None, :] >= rstart[:, None]) & (krow[None, :] < rstart[:, None] + 8)
        dr = krow[None, :] - qrow[:, None] + 7
        ok = okr & okc
        drc = np.clip(dr, 0, 14)
        dcc = np.clip(dc, 0, 30)
        vals = rpb[:, drc, dcc]
        out[:, :, ty, :] = np.where(ok[None], vals, NEG)
    outT = out.reshape(8, P, 5, 6, P).transpose(0, 4, 2, 3, 1)
    return np.ascontiguousarray(outT).reshape(8, P, 5 * 768)


def _layer_weights(l, inp, tag):
    j = l // 2
    w = {}
    w[f"wmod{tag}"] = inp["w_mod"][l]
    w[f"bmod{tag}"] = np.ascontiguousarray(inp["b_mod"][l].reshape(48, P).T)
    w[f"lnv{tag}"] = np.ascontiguousarray(
        np.stack([inp[k][l].reshape(KC, P).T for k in ("ln1_g", "ln1_b", "ln2_g", "ln2_b")], axis=1)).reshape(P, 32)
    if l % 2 == 0:
        w[f"win{tag}"] = inp["ab_w_in"][j]
        w[f"wout{tag}"] = inp["ab_w_out"][j]
        wf = inp["ab_w_fnet"][j]
        wfn = np.zeros((P, 2, P), np.float32)
        for jj in range(2):
            wfn[0:64, jj, 0:64] = wf[2 * jj]
            wfn[64:128, jj, 64:128] = wf[2 * jj + 1]
        w[f"wfn{tag}"] = wfn.reshape(P, 256)
        w[f"qkn{tag}"] = np.ascontiguousarray(np.stack([inp["ab_q_norm"][j], inp["ab_k_norm"][j]], axis=1))
    else:
        w[f"win{tag}"] = inp["na_w_in"][j]
        w[f"wout{tag}"] = inp["na_w_out"][j]
    w[f"wr{tag}"] = np.ascontiguousarray(inp["moe_w_router"][l].reshape(KC, P, 64).transpose(1, 0, 2)).reshape(P, KC * 64)
    w[f"mb{tag}"] = np.ascontiguousarray(np.broadcast_to(inp["moe_bias"][l][None, :], (P, 64)))
    wg = np.concatenate([inp["moe_w_gate"][l], inp["sh_w_gate"][l][None]], axis=0)
    wu = np.concatenate([inp["moe_w_up"][l], inp["sh_w_up"][l][None]], axis=0)
    wd = np.concatenate([inp["moe_w_down"][l], inp["sh_w_down"][l][None]], axis=0)
    moew = np.empty((NEXP, P, 6144), np.float32)
    moew[:, :, 0:2048] = wg.reshape(NEXP, KC, P, 256).transpose(0, 2, 1, 3).reshape(NEXP, P, 2048)
    moew[:, :, 2048:4096] = wu.reshape(NEXP, KC, P, 256).transpose(0, 2, 1, 3).reshape(NEXP, P, 2048)
    moew[:, :, 4096:6144] = wd.reshape(NEXP, 2, P, D).transpose(0, 2, 1, 3).reshape(NEXP, P, 2048)
    w[f"moew{tag}"] = moew
    return w


_PROG_CACHE = {}


def _get_prog(layers, debug=None):
    key = (tuple(layers), tuple(sorted((debug or {}).items())))
    if key not in _PROG_CACHE:
        pr = Prog(list(layers), debug)
        nc = pr.build()
        _PROG_CACHE[key] = (nc, set(pr.inputs.keys()))
    return _PROG_CACHE[key]


def _init_toks(inp):
    toks = []
    for r in range(8):
        b, s = r // 2, r % 2
        toks.append(np.concatenate([inp["x"][b, s * NLAT:(s + 1) * NLAT], inp["ctx"][b, s * NCTX:(s + 1) * NCTX]], axis=0))
    return toks


def _launch(inp, layers, toks, debug=None):
    nc, names = _get_prog(layers, debug)
    lw = {}
    for l in layers:
        lw.update(_layer_weights(l, inp, l))
    fm = [_fm(t) for t in toks]
    in_maps = []
    for r in range(8):
        b, s = r // 2, r % 2
        m = dict(lw)
        cs = _consts(s)
        for k in ("ident", "perm", "rope", "ropeq", "cs2", "dft", "cdft"):
            m[k] = cs[k]
        for l in layers:
            if l % 2 == 1:
                m[f"nab{l}"] = _na_bias(inp["na_rpb"][l // 2], s)
        m["cT"] = np.ascontiguousarray(
            np.stack([inp["c"][b].reshape(KC, P).T, inp["c_ctx"].reshape(KC, P).T], axis=2)).reshape(P, 16)
        m["xT"] = fm[r]
        m[f"xg{layers[0]}"] = np.ascontiguousarray(
            np.stack([fm[r - s].reshape(P, KC, NT), fm[r - s + 1].reshape(P, KC, NT)], axis=0).transpose(2, 0, 1, 3)
        ).reshape(KC, 2 * P, NT)
        in_maps.append({k: v for k, v in m.items() if k in names})
    res = run_bass_kernel_spmd(nc, in_maps, core_ids=list(range(8)))
    return [_unfm(res.results[r]["xout"], NT) for r in range(8)]


def run_layers(inp, layers=range(DEPTH), toks=None, debug=None, fused=False):
    inp = {k: np.asarray(v) for k, v in inp.items()}
    if toks is None:
        toks = _init_toks(inp)
    if fused:
        return _launch(inp, list(layers), toks, debug)
    for l in layers:
        toks = _launch(inp, [l], toks, debug)
    return toks


def kernel(**inputs):
    toks = run_layers(inputs, fused=True)
    out = np.empty((4, 4096, D), np.float32)
    for r in range(8):
        b, s = r // 2, r % 2
        out[b, s * NLAT:(s + 1) * NLAT] = toks[r][0:NLAT]
    return out
```
